# Optimizing a Trainium2 kernel written in Bass

```python
import math
import jax, jax.numpy as jnp
from jax import lax
import numpy as np

D_MODEL = 1024
BATCH = 8
SEQ = 2048
DEPTH = 2

CHUNK = 64
Q_BLOCK = 128
MEM_LEN = 256

GLA_HEADS = 4
GLA_DK = 64
GLA_DV = 64
GLA_GATE_RANK = 16
GLA_TAU = 16.0
MLSTM_HEADS = 4
MLSTM_DK = 64
MLSTM_DV = 64
MLSTM_CONV = 4
MLA_HEADS = 8
MLA_NOPE = 64
MLA_ROPE = 32
MLA_V = 64
MLA_Q_RANK = 256
MLA_KV_RANK = 128
ROPE_BASE = 10000.0
XA_HEADS = 4
XA_DH = D_MODEL // XA_HEADS
N_GROUPS = 4
EXPERTS_PER_GROUP = 8
N_EXPERTS = N_GROUPS * EXPERTS_PER_GROUP
EXPERT_TOP_K = 2
D_EXPERT = 256

ALPHA = (2 * DEPTH) ** 0.25
BETA = (8 * DEPTH) ** -0.25
LN_EPS = 1e-5

GLA_QK = GLA_HEADS * GLA_DK
GLA_VW = GLA_HEADS * GLA_DV
ML_QK = MLSTM_HEADS * MLSTM_DK
ML_VW = MLSTM_HEADS * MLSTM_DV
MLA_OUT = MLA_HEADS * MLA_V
MIX_WIDTH = GLA_VW + ML_VW + MLA_OUT
IN_SIZES = (GLA_QK, GLA_QK, GLA_VW, GLA_VW, GLA_GATE_RANK,
            2 * ML_QK, ML_VW, ML_VW, MLSTM_HEADS, MLSTM_HEADS,
            MLA_Q_RANK, MLA_KV_RANK, MLA_ROPE)
IN_WIDTH = sum(IN_SIZES)

kernel_name = "hybrid_gla_mlstm_mla_hmoe_block"


def layer_norm(x, g, b):
    xf = x.astype(jnp.float32)
    mu = jnp.mean(xf, -1, keepdims=True)
    var = jnp.mean(jnp.square(xf - mu), -1, keepdims=True)
    return ((xf - mu) * lax.rsqrt(var + LN_EPS) * g + b).astype(x.dtype)


def rms_norm(x, g):
    xf = x.astype(jnp.float32)
    return (xf * lax.rsqrt(jnp.mean(jnp.square(xf), -1, keepdims=True) + LN_EPS) * g).astype(x.dtype)


def head_norm(x, g):
    mu = jnp.mean(x, -1, keepdims=True)
    var = jnp.mean(jnp.square(x - mu), -1, keepdims=True)
    return (x - mu) * lax.rsqrt(var + LN_EPS) * g.reshape(x.shape[-2:])


def rope(x, pos):
    half = x.shape[-1] // 2
    inv = ROPE_BASE ** (-jnp.arange(half, dtype=jnp.float32) / half)
    ang = pos.astype(jnp.float32)[..., None] * inv
    cos = jnp.cos(ang)[:, :, None, :]
    sin = jnp.sin(ang)[:, :, None, :]
    xf = x.astype(jnp.float32)
    x1, x2 = xf[..., :half], xf[..., half:]
    return jnp.concatenate([x1 * cos - x2 * sin, x2 * cos + x1 * sin], -1).astype(x.dtype)


def causal_depthwise_conv(x, w):
    k_w, c = w.shape
    return lax.conv_general_dilated(x, w[:, None, :], window_strides=(1,), padding=((k_w - 1, 0),),
                                    dimension_numbers=('NWC', 'WIO', 'NWC'), feature_group_count=c)


def gla_mixer(q, k, v, r, a_lr, w_a2, b_a, norm_g):
    B, S, _ = q.shape
    nc = S // CHUNK
    f32 = jnp.float32
    log_a = jax.nn.log_sigmoid((a_lr @ w_a2 + b_a).astype(f32)) / GLA_TAU

    def chunks(t, d):
        return t.astype(f32).reshape(B, nc, CHUNK, GLA_HEADS, d).transpose(1, 0, 3, 2, 4)

    causal = jnp.tril(jnp.ones((CHUNK, CHUNK), dtype=bool))[:, :, None]

    def step(state, inp):
        qb, kb, vb, la = inp
        b = jnp.cumsum(la, axis=2)
        b_end = b[:, :, -1:, :]
        o_inter = jnp.einsum('bhtd,bhde->bhte', qb * jnp.exp(b), state)
        decay = jnp.exp(jnp.where(causal, b[:, :, :, None, :] - b[:, :, None, :, :], -jnp.inf))
        scores = jnp.einsum('bhtd,bhtsd,bhsd->bhts', qb, decay, kb)
        o = o_inter + jnp.einsum('bhts,bhse->bhte', scores, vb)
        state = (jnp.exp(b_end[:, :, 0, :, None]) * state
                 + jnp.einsum('bhsd,bhse->bhde', kb * jnp.exp(b_end - b), vb))
        return state, o

    state0 = jnp.zeros((B, GLA_HEADS, GLA_DK, GLA_DV), f32)
    _, o = lax.scan(step, state0, (chunks(q, GLA_DK) * GLA_DK ** -0.5, chunks(k, GLA_DK),
                                   chunks(v, GLA_DV), chunks(log_a, GLA_DK)))
    o = o.transpose(1, 0, 3, 2, 4).reshape(B, S, GLA_HEADS, GLA_DV)
    o = head_norm(o, norm_g).reshape(B, S, GLA_VW) * jax.nn.silu(r.astype(f32))
    return o.astype(q.dtype)


def mlstm_mixer(q, k, v, o_pre, i_pre, f_pre, norm_g):
    B, S, _ = q.shape
    nc = S // CHUNK
    f32 = jnp.float32

    def chunks(t, d):
        return t.astype(f32).reshape(B, nc, CHUNK, MLSTM_HEADS, d).transpose(1, 0, 3, 2, 4)

    def gate_chunks(t):
        return t.reshape(B, nc, CHUNK, MLSTM_HEADS).transpose(1, 0, 3, 2)

    log_i = i_pre.astype(f32)
    log_f = jax.nn.log_sigmoid(f_pre.astype(f32))
    causal = jnp.tril(jnp.ones((CHUNK, CHUNK), dtype=bool))

    def step(carry, inp):
        c_st, n_st, m_st = carry
        qb, kb, vb, ig, lf = inp
        fcum = jnp.cumsum(lf, axis=-1)
        d_log = jnp.where(causal, fcum[..., :, None] - fcum[..., None, :] + ig[..., None, :], -jnp.inf)
        inter_log = fcum + m_st[..., None]
        m_t = jnp.maximum(inter_log, jnp.max(d_log, -1))
        w_intra = jnp.exp(d_log - m_t[..., None]) * jnp.einsum('bhtd,bhsd->bhts', qb, kb)
        w_inter = jnp.exp(inter_log - m_t)
        num = (w_inter[..., None] * jnp.einsum('bhtd,bhde->bhte', qb, c_st)
               + jnp.einsum('bhts,bhse->bhte', w_intra, vb))
        den = w_inter * jnp.einsum('bhtd,bhd->bht', qb, n_st) + jnp.sum(w_intra, -1)
        h = num / jnp.maximum(jnp.abs(den), jnp.exp(-m_t))[..., None]
        end_log = fcum[..., -1:] - fcum + ig
        m_new = jnp.maximum(fcum[..., -1] + m_st, jnp.max(end_log, -1))
        w_s = jnp.exp(end_log - m_new[..., None])
        decay = jnp.exp(fcum[..., -1] + m_st - m_new)
        c_st = decay[..., None, None] * c_st + jnp.einsum('bhs,bhsd,bhse->bhde', w_s, kb, vb)
        n_st = decay[..., None] * n_st + jnp.einsum('bhs,bhsd->bhd', w_s, kb)
        return (c_st, n_st, m_new), h

    carry0 = (jnp.zeros((B, MLSTM_HEADS, MLSTM_DK, MLSTM_DV), f32),
              jnp.zeros((B, MLSTM_HEADS, MLSTM_DK), f32),
              jnp.zeros((B, MLSTM_HEADS), f32))
    _, h = lax.scan(step, carry0, (chunks(q, MLSTM_DK) * MLSTM_DK ** -0.5, chunks(k, MLSTM_DK),
                                   chunks(v, MLSTM_DV), gate_chunks(log_i), gate_chunks(log_f)))
    h = h.transpose(1, 0, 3, 2, 4).reshape(B, S, MLSTM_HEADS, MLSTM_DV)
    h = head_norm(h, norm_g).reshape(B, S, ML_VW) * jax.nn.sigmoid(o_pre.astype(f32))
    return h.astype(q.dtype)


def mla_mixer(c_q, c_kv, k_r, pos, q_norm_g, w_uq, kv_norm_g, w_ukv):
    B, S, _ = c_q.shape
    q = (rms_norm(c_q, q_norm_g) @ w_uq).reshape(B, S, MLA_HEADS, MLA_NOPE + MLA_ROPE)
    q_nope = q[..., :MLA_NOPE]
    q_rope = rope(q[..., MLA_NOPE:], pos)
    kv = (rms_norm(c_kv, kv_norm_g) @ w_ukv).reshape(B, S, MLA_HEADS, MLA_NOPE + MLA_V)
    k_nope, v = kv[..., :MLA_NOPE], kv[..., MLA_NOPE:]
    k_rope = rope(k_r[:, :, None, :], pos)[:, :, 0, :]
    scale = (MLA_NOPE + MLA_ROPE) ** -0.5
    nb = S // Q_BLOCK
    qn_b = q_nope.reshape(B, nb, Q_BLOCK, MLA_HEADS, MLA_NOPE).swapaxes(0, 1)
    qr_b = q_rope.reshape(B, nb, Q_BLOCK, MLA_HEADS, MLA_ROPE).swapaxes(0, 1)
    key_chunk = jnp.arange(S) // CHUNK

    def attend(args):
        qn, qr, blk = args
        s = (jnp.einsum('bqhd,bkhd->bhqk', qn, k_nope)
             + jnp.einsum('bqhr,bkr->bhqk', qr, k_rope)).astype(jnp.float32) * scale
        q_chunk = (blk * Q_BLOCK + jnp.arange(Q_BLOCK)) // CHUNK
        mask = key_chunk[None, :] <= q_chunk[:, None]
        p = jax.nn.softmax(jnp.where(mask, s, -jnp.inf), axis=-1).astype(v.dtype)
        return jnp.einsum('bhqk,bkhd->bqhd', p, v)

    o = lax.map(attend, (qn_b, qr_b, jnp.arange(nb)))
    return o.swapaxes(0, 1).reshape(B, S, MLA_OUT)


def memory_cross_attention(x, mem, w_q, w_kv, w_o):
    B, S, D = x.shape
    q = (x @ w_q).reshape(B, S, XA_HEADS, XA_DH)
    kv = (mem @ w_kv).reshape(B, mem.shape[1], 2, XA_HEADS, XA_DH)
    k, v = kv[:, :, 0], kv[:, :, 1]
    s = jnp.einsum('bqhd,bkhd->bhqk', q, k).astype(jnp.float32) * XA_DH ** -0.5
    p = jax.nn.softmax(s, axis=-1).astype(x.dtype)
    o = jnp.einsum('bhqk,bkhd->bqhd', p, v).reshape(B, S, D)
    return o @ w_o


def hierarchical_moe(x, w_group, b_group, w_router, b_router, w_gate, w_up, w_down):
    B, S, D = x.shape
    t = x.reshape(-1, D)
    n_tok = t.shape[0]
    g_prob = jax.nn.softmax((t @ w_group + b_group).astype(jnp.float32), axis=-1)
    g_p, g_idx = lax.top_k(g_prob, 1)
    e_logits = (t @ w_router + b_router).astype(jnp.float32).reshape(n_tok, N_GROUPS, EXPERTS_PER_GROUP)
    e_sel = e_logits[jnp.arange(n_tok), g_idx[:, 0]]
    e_p, e_idx = lax.top_k(jax.nn.softmax(e_sel, axis=-1), EXPERT_TOP_K)
    weights = g_p * (e_p / jnp.sum(e_p, -1, keepdims=True))
    expert_id = g_idx * EXPERTS_PER_GROUP + e_idx
    gate = jnp.sum(jax.nn.one_hot(expert_id, N_EXPERTS, dtype=jnp.float32) * weights[..., None], axis=1)
    gate = gate.astype(t.dtype)
    out = jnp.zeros_like(t)
    for e in range(N_EXPERTS):
        h = jax.nn.silu(t @ w_gate[e]) * (t @ w_up[e])
        out = out + (h @ w_down[e]) * gate[:, e:e + 1]
    return out.reshape(B, S, D)


def hybrid_layer(x, mem, positions, w_in, w_out, gla_w_a2, gla_b_a, gla_norm_g, ml_conv_w, ml_b_i, ml_b_f,
                 ml_norm_g, mla_q_norm_g, mla_w_uq, mla_kv_norm_g, mla_w_ukv, xa_w_q, xa_w_kv, xa_w_o,
                 moe_w_group, moe_b_group, moe_w_router, moe_b_router, moe_w_gate, moe_w_up, moe_w_down,
                 ln1_g, ln1_b, ln2_g, ln2_b, ln3_g, ln3_b):
    y = x @ w_in
    split_at = np.cumsum(IN_SIZES)[:-1].tolist()
    (g_q, g_k, g_v, g_r, g_a, m_qk, m_v, m_o, m_i, m_f, c_q, c_kv, k_r) = jnp.split(y, split_at, axis=-1)
    o_gla = gla_mixer(g_q, g_k, g_v, g_r, g_a, gla_w_a2, gla_b_a, gla_norm_g)
    m_qk = jax.nn.silu(causal_depthwise_conv(m_qk, ml_conv_w))
    o_ml = mlstm_mixer(m_qk[..., :ML_QK], m_qk[..., ML_QK:], m_v, m_o, m_i + ml_b_i, m_f + ml_b_f, ml_norm_g)
    o_mla = mla_mixer(c_q, c_kv, k_r, positions, mla_q_norm_g, mla_w_uq, mla_kv_norm_g, mla_w_ukv)
    mix = jnp.concatenate([o_gla, o_ml, o_mla], axis=-1) @ w_out
    x = layer_norm(ALPHA * x + mix, ln1_g, ln1_b)
    x = layer_norm(ALPHA * x + memory_cross_attention(x, mem, xa_w_q, xa_w_kv, xa_w_o), ln2_g, ln2_b)
    moe = hierarchical_moe(x, moe_w_group, moe_b_group, moe_w_router, moe_b_router, moe_w_gate, moe_w_up, moe_w_down)
    return layer_norm(ALPHA * x + moe, ln3_g, ln3_b)


def setup_inputs(seed: int = 0) -> dict:
    key = jax.random.key(seed)
    ks = iter(jax.random.split(key, 40))
    f32 = jnp.float32

    def nrm(shape, scale):
        return jax.random.normal(next(ks), shape, f32) * scale

    def gain(shape):
        return 1.0 + 0.02 * jax.random.normal(next(ks), shape, f32)

    L = DEPTH
    x = jax.random.normal(next(ks), (BATCH, SEQ, D_MODEL), f32)
    mem = jax.random.normal(next(ks), (BATCH, MEM_LEN, D_MODEL), f32)
    offset = jax.random.randint(next(ks), (BATCH, 1), 0, 64, dtype=jnp.int32) * CHUNK
    positions = (offset + jnp.arange(SEQ, dtype=jnp.int32)[None, :]).astype(jnp.int32)
    f_bias = jnp.linspace(3.0, 6.0, MLSTM_HEADS, dtype=f32)[None, :] + 0.1 * jax.random.normal(next(ks), (L, MLSTM_HEADS), f32)
    return {
        "x": x,
        "mem": mem,
        "positions": positions,
        "w_in": nrm((L, D_MODEL, IN_WIDTH), D_MODEL ** -0.5),
        "w_out": nrm((L, MIX_WIDTH, D_MODEL), MIX_WIDTH ** -0.5 * BETA),
        "gla_w_a2": nrm((L, GLA_GATE_RANK, GLA_QK), GLA_GATE_RANK ** -0.5),
        "gla_b_a": nrm((L, GLA_QK), 0.1),
        "gla_norm_g": gain((L, GLA_VW)),
        "ml_conv_w": nrm((L, MLSTM_CONV, 2 * ML_QK), MLSTM_CONV ** -0.5),
        "ml_b_i": nrm((L, MLSTM_HEADS), 0.1),
        "ml_b_f": f_bias,
        "ml_norm_g": gain((L, ML_VW)),
        "mla_q_norm_g": gain((L, MLA_Q_RANK)),
        "mla_w_uq": nrm((L, MLA_Q_RANK, MLA_HEADS * (MLA_NOPE + MLA_ROPE)), MLA_Q_RANK ** -0.5),
        "mla_kv_norm_g": gain((L, MLA_KV_RANK)),
        "mla_w_ukv": nrm((L, MLA_KV_RANK, MLA_HEADS * (MLA_NOPE + MLA_V)), MLA_KV_RANK ** -0.5),
        "xa_w_q": nrm((L, D_MODEL, D_MODEL), D_MODEL ** -0.5),
        "xa_w_kv": nrm((L, D_MODEL, 2 * D_MODEL), D_MODEL ** -0.5),
        "xa_w_o": nrm((L, D_MODEL, D_MODEL), D_MODEL ** -0.5 * BETA),
        "moe_w_group": nrm((L, D_MODEL, N_GROUPS), D_MODEL ** -0.5),
        "moe_b_group": nrm((L, N_GROUPS), 0.01),
        "moe_w_router": nrm((L, D_MODEL, N_EXPERTS), D_MODEL ** -0.5),
        "moe_b_router": nrm((L, N_EXPERTS), 0.01),
        "moe_w_gate": nrm((L, N_EXPERTS, D_MODEL, D_EXPERT), D_MODEL ** -0.5),
        "moe_w_up": nrm((L, N_EXPERTS, D_MODEL, D_EXPERT), D_MODEL ** -0.5),
        "moe_w_down": nrm((L, N_EXPERTS, D_EXPERT, D_MODEL), D_EXPERT ** -0.5 * BETA),
        "ln1_g": gain((L, D_MODEL)),
        "ln1_b": nrm((L, D_MODEL), 0.01),
        "ln2_g": gain((L, D_MODEL)),
        "ln2_b": nrm((L, D_MODEL), 0.01),
        "ln3_g": gain((L, D_MODEL)),
        "ln3_b": nrm((L, D_MODEL), 0.01),
    }


def reference(x, mem, positions, w_in, w_out, gla_w_a2, gla_b_a, gla_norm_g, ml_conv_w, ml_b_i, ml_b_f,
              ml_norm_g, mla_q_norm_g, mla_w_uq, mla_kv_norm_g, mla_w_ukv, xa_w_q, xa_w_kv, xa_w_o,
              moe_w_group, moe_b_group, moe_w_router, moe_b_router, moe_w_gate, moe_w_up, moe_w_down,
              ln1_g, ln1_b, ln2_g, ln2_b, ln3_g, ln3_b):
    for l in range(DEPTH):
        x = hybrid_layer(x, mem, positions, w_in[l], w_out[l], gla_w_a2[l], gla_b_a[l], gla_norm_g[l],
                         ml_conv_w[l], ml_b_i[l], ml_b_f[l], ml_norm_g[l], mla_q_norm_g[l], mla_w_uq[l],
                         mla_kv_norm_g[l], mla_w_ukv[l], xa_w_q[l], xa_w_kv[l], xa_w_o[l],
                         moe_w_group[l], moe_b_group[l], moe_w_router[l], moe_b_router[l],
                         moe_w_gate[l], moe_w_up[l], moe_w_down[l],
                         ln1_g[l], ln1_b[l], ln2_g[l], ln2_b[l], ln3_g[l], ln3_b[l])
    return x
```

```python
import numpy as np
import ml_dtypes
import concourse.bass as bass
import concourse.mybir as mybir
from concourse.bass_utils import run_bass_kernel_spmd

F32 = mybir.dt.float32
BF16 = mybir.dt.bfloat16
I32 = mybir.dt.int32
AF = mybir.ActivationFunctionType
ALU = mybir.AluOpType
AX = mybir.AxisListType

S = 2048
D = 1024
NT = 16
DEPTH = 2
ALPHA = (2 * DEPTH) ** 0.25
LN_EPS = 1e-5
IN_W = 2488


class Tile:
    def __init__(self, name, h):
        self.name = name
        self.h = h
        self.st = {}
        self.dsem = None
        self.dcount = 0
        self.psum = False

    def __getitem__(self, k):
        return self.h[k]


class Op:
    __slots__ = ("fn", "deps", "inc", "dma")

    def __init__(self, fn, deps, dma=None):
        self.fn = fn
        self.deps = deps
        self.inc = False
        self.dma = dma


class Prog:
    ENG = ("sync", "scalar", "vector", "gpsimd", "tensor")

    def __init__(self, nc):
        self.nc = nc
        self.ops = {e: [] for e in self.ENG}
        self.dsems = {}
        self.dtotal = {}
        self.tiles = []

    def tile(self, name, h):
        t = Tile(name, h)
        self.tiles.append(t)
        return t

    @staticmethod
    def _norm(lst):
        out = []
        for x in lst:
            if not isinstance(x, tuple):
                x = (x, None)
            if x[0].psum:
                x = (x[0], None)
            out.append(x)
        return out

    @staticmethod
    def _states(t, s):
        if s is None:
            return list(t.st.values())
        r = []
        if None in t.st:
            r.append(t.st[None])
        if s in t.st:
            r.append(t.st[s])
        return r

    def _collect(self, reads, writes):
        deps = {}

        def add(ev):
            if ev is None:
                return
            k, v = ev
            if not isinstance(k, str):
                v = self.dtotal[k]
            if deps.get(k, -1) < v:
                deps[k] = v

        for (t, s) in reads:
            for st in self._states(t, s):
                add(st[0])
        for (t, s) in writes:
            for st in self._states(t, s):
                add(st[0])
                for k, v in st[1].items():
                    add((k, v))
        return deps

    def _update(self, ev, reads, writes):
        k, v = ev
        for (t, s) in reads:
            st = t.st.setdefault(s, [None, {}])
            if st[1].get(k, -1) < v:
                st[1][k] = v
        for (t, s) in writes:
            if s is None:
                t.st = {None: [ev, {}]}
            else:
                t.st[s] = [ev, {}]

    def op(self, eng, fn, reads=(), writes=()):
        reads = self._norm(reads)
        writes = self._norm(writes)
        writes = writes + [r for r in reads if r[0].psum]
        deps = self._collect(reads, writes)
        if eng == "tensor":
            deps.pop("tensor", None)
        idx = len(self.ops[eng])
        for k, v in deps.items():
            if isinstance(k, str) and k in self.ops:
                self.ops[k][v].inc = True
        self.ops[eng].append(Op(fn, deps))
        self._update((eng, idx), reads, writes)

    def dma(self, eng, out, in_, reads=(), writes=(), semtile=None):
        reads = self._norm(reads)
        writes = self._norm(writes)
        deps = self._collect(reads, writes)
        for k, v in deps.items():
            if isinstance(k, str) and k in self.ops:
                self.ops[k][v].inc = True
        if semtile.dsem is None:
            semtile.dsem = ("D", semtile.name)
            self.dsems[semtile.dsem] = None
        semtile.dcount = self.dtotal.get(semtile.dsem, 0) + 16
        self.dtotal[semtile.dsem] = semtile.dcount
        ev = (semtile.dsem, semtile.dcount)
        self.ops[eng].append(Op(None, deps, dma=(out, in_, semtile.dsem)))
        self._update(ev, reads, writes)

    def barrier(self):
        deps = {}
        for e in self.ENG:
            i = self._last_real(e)
            if i is not None:
                deps[e] = i
                self.ops[e][i].inc = True
        for k, v in self.dtotal.items():
            deps[k] = v
        for e in self.ENG:
            d = dict(deps)
            self.ops[e].append(Op("nop", d))
        for t in self.tiles:
            t.st = {}

    def finish(self, eng="sync"):
        deps = dict(self.dtotal)
        for e in self.ENG:
            i = self._last_real(e)
            if i is not None and e != eng:
                deps[e] = i
                self.ops[e][i].inc = True
        self.ops[eng].append(Op("nop", deps))

    def _last_real(self, e):
        for i in range(len(self.ops[e]) - 1, -1, -1):
            if self.ops[e][i].dma is None:
                return i
        return None

    def emit(self, stack):
        nc = self.nc
        esem = {e: stack.enter_context(nc.semaphore("es_" + e)) for e in self.ENG}
        for k in self.dsems:
            self.dsems[k] = stack.enter_context(nc.semaphore("ds_" + k[1]))
        cnt = {}
        for e in self.ENG:
            c = 0
            lst = []
            for o in self.ops[e]:
                if o.inc and o.dma is None:
                    c += 1
                lst.append(c)
            cnt[e] = lst
        prog = self

        def run(ename, eng):
            waited = {}
            for o in prog.ops[ename]:
                for k, v in o.deps.items():
                    if isinstance(k, str):
                        sem = esem[k]
                        val = cnt[k][v]
                    else:
                        sem = prog.dsems[k]
                        val = v
                    if waited.get(k, 0) >= val:
                        continue
                    waited[k] = val
                    eng.wait_ge(sem, val)
                if o.dma is not None:
                    out, in_, dk = o.dma
                    eng.dma_start(out=out, in_=in_).then_inc(prog.dsems[dk], 16)
                    continue
                if o.fn == "nop":
                    if o.inc:
                        eng.nop().then_inc(esem[ename], 1)
                    continue
                ins = o.fn(eng)
                if o.inc:
                    ins.then_inc(esem[ename], 1)

        stack.enter_context(nc.allow_non_contiguous_dma("tiny strided parameter loads"))
        block = stack.enter_context(nc.Block())

        @block.sync
        def _(e):
            run("sync", e)

        @block.scalar
        def _(e):
            run("scalar", e)

        @block.vector
        def _(e):
            run("vector", e)

        @block.gpsimd
        def _(e):
            run("gpsimd", e)

        @block.tensor
        def _(e):
            run("tensor", e)


def bcast(ap, shape, axis):
    return ap.unsqueeze(axis).to_broadcast(list(shape))


class K:
    def __init__(self, layers=(0, 1), phases="ABC", dbg=()):
        self.layers = layers
        self.phases = phases
        self.dbg = dbg


def build(layers=(0, 1), phases="ABC", dbg=(), sub="GML"):
    from contextlib import ExitStack
    nc = bass.Bass("TRN2", target_bir_lowering=False)
    P = Prog(nc)
    stack = ExitStack()

    def din(name, shape, dt=F32):
        return nc.dram_tensor(name, list(shape), dt, kind="ExternalInput").ap()

    x_d = din("x", [S, D])
    mem_d = din("mem", [256, D])
    pos_d = din("positions", [1, S], I32)
    w = {}
    for name, shape in [
        ("w_in", [2, D, IN_W]), ("w_out", [2, D, D]), ("gla_w_a2", [2, 16, 256]), ("gla_b_a", [2, 256]),
        ("gla_norm_g", [2, 256]), ("ml_conv_w", [2, 4, 512]), ("ml_b_i", [2, 4]), ("ml_b_f", [2, 4]),
        ("ml_norm_g", [2, 256]), ("mla_q_norm_g", [2, 256]), ("mla_w_uq", [2, 256, 768]),
        ("mla_kv_norm_g", [2, 128]), ("mla_w_ukv", [2, 128, 1024]), ("xa_w_q", [2, D, D]),
        ("xa_w_kv", [2, D, 2 * D]), ("xa_w_o", [2, D, D]), ("moe_w_group", [2, D, 4]), ("moe_b_group", [2, 4]),
        ("moe_w_router", [2, D, 32]), ("moe_b_router", [2, 32]), ("moe_w_gate", [2, 32, D, 256]),
        ("moe_w_up", [2, 32, D, 256]), ("moe_w_down", [2, 32, 256, D]),
        ("ln1_g", [2, D]), ("ln1_b", [2, D]), ("ln2_g", [2, D]), ("ln2_b", [2, D]), ("ln3_g", [2, D]), ("ln3_b", [2, D]),
    ]:
        w[name] = din(name, shape)
    cmat_d = din("cmat", [128, 5, 128])
    sel_d = din("sel", [32, 32, 128])
    ropeinv_d = din("ropeinv", [96, 1])
    out_d = nc.dram_tensor("out", [S, D], F32, kind="ExternalOutput").ap()
    dbg_d = {}
    for name, shape in dbg:
        dbg_d[name] = nc.dram_tensor(name, list(shape), F32, kind="ExternalOutput").ap()

    def sb(name, shape, dt=F32):
        return P.tile(name, stack.enter_context(nc.sbuf_tensor(name, list(shape), dt)))

    x_tm = sb("x_tm", [128, NT, D])
    xT = sb("xT", [128, 8, S], BF16)
    cmat = sb("cmat_sb", [128, 5, 128])
    cmat_bf = sb("cmat_bf", [128, 5, 128], BF16)
    lnp = sb("lnp", [128, 2, D])
    ps = [P.tile("ps%d" % i, stack.enter_context(nc.psum_tensor("ps%d" % i, [128, 512], F32))) for i in range(8)]
    for p_ in ps:
        p_.psum = True
    IDENT, TRII, TRIS, MMLA, ONES = range(5)

    def mm(out, lhsT, rhs, start, stop, reads, writes):
        P.op("tensor", lambda e: e.matmul(out, lhsT, rhs, start=start, stop=stop), reads, writes)

    def tr(out, in_, ident, reads, writes):
        P.op("tensor", lambda e: e.transpose(out, in_, ident), reads, writes)

    def act(out, in_, func, reads, writes, bias=None, scale=None, accum_out=None):
        kw = {}
        if bias is not None:
            kw["bias"] = bias
        if scale is not None:
            kw["scale"] = scale
        if accum_out is not None:
            kw["accum_out"] = accum_out
        P.op("scalar", lambda e: e.activation(out, in_, func, **kw), reads, writes)

    def tt(out, a, b, op, reads, writes, eng="vector"):
        P.op(eng, lambda e: e.tensor_tensor(out, a, b, op), reads, writes)

    def ts(out, a, s1, op0, reads, writes, s2=None, op1=None, eng="vector"):
        if op1 is None:
            P.op(eng, lambda e: e.tensor_scalar(out, a, s1, None, op0), reads, writes)
        else:
            P.op(eng, lambda e: e.tensor_scalar(out, a, s1, s2, op0, op1), reads, writes)

    def stt(out, a, s, b, op0, op1, reads, writes):
        P.op("vector", lambda e: e.scalar_tensor_tensor(out, a, s, b, op0, op1), reads, writes)

    def cp(out, in_, reads, writes, eng="vector"):
        if eng == "scalar":
            P.op("scalar", lambda e: e.copy(out, in_), reads, writes)
        else:
            P.op(eng, lambda e: e.tensor_copy(out, in_), reads, writes)

    def red(out, in_, op, reads, writes, axis=AX.X):
        P.op("vector", lambda e: e.tensor_reduce(out, in_, axis, op), reads, writes)

    def load_cast(dst_tile, dst_ap, src_ap, sub=None):
        P.dma("gpsimd", dst_ap, src_ap, writes=[(dst_tile, sub)], semtile=dst_tile)

    def load(dst_tile, dst_ap, src_ap, sub=None, eng="sync"):
        P.dma(eng, dst_ap, src_ap, writes=[(dst_tile, sub)], semtile=dst_tile)

    load(cmat, cmat[:], cmat_d)
    cp(cmat_bf[:], cmat[:], [cmat], [cmat_bf])
    for t in range(NT):
        load(x_tm, x_tm[:, t, :], x_d[t * 128:(t + 1) * 128, :], sub=t, eng="sync" if t % 2 == 0 else "scalar")

    memT = sb("memT", [128, 8, 256], BF16)
    if "B" in phases:
        from contextlib import ExitStack as _ES
        pre = _ES()
        mem_f = P.tile("mem_f", pre.enter_context(nc.sbuf_tensor("mem_f", [128, 2, D], F32)))
        mem_b = P.tile("mem_b", pre.enter_context(nc.sbuf_tensor("mem_b", [128, 2, D], BF16)))
        load(mem_f, mem_f[:], mem_d.rearrange("(t p) d -> p t d", p=128))
        cp(mem_b[:], mem_f[:], [mem_f], [mem_b])
        pbm = ps[7].h.bitcast(BF16)
        for mt in range(2):
            for c8 in range(8):
                tr(pbm[:, c8 * 128:(c8 + 1) * 128], mem_b[:, mt, c8 * 128:(c8 + 1) * 128], cmat_bf[:, 0, :],
                   [mem_b, cmat_bf], [(ps[7], c8)])
            cp(memT[:, :, mt * 128:(mt + 1) * 128], pbm[:, :].rearrange("p (c n) -> p c n", c=8), [ps[7]], [(memT, mt)])
        P.barrier()
        pre.close()

    cs = None
    if "A" in phases and "L" in sub:
        import math
        from contextlib import ExitStack as _ES2
        cs = sb("rope_cs", [96, 2, S], BF16)
        pre2 = _ES2()

        def tmp(name, dt=F32):
            return P.tile(name, pre2.enter_context(nc.sbuf_tensor(name, [96, S], dt)))
        posi, ang, rr, kf, ki, mk = tmp("rp_posi", I32), tmp("rp_ang"), tmp("rp_r"), tmp("rp_kf"), tmp("rp_ki", I32), tmp("rp_m")
        rinv = P.tile("rp_inv", pre2.enter_context(nc.sbuf_tensor("rp_inv", [96, 1], F32)))
        R_ = slice(64, 96)
        load(rinv, rinv[:], ropeinv_d)
        P.dma("sync", posi[R_, :].unsqueeze(1), pos_d[0:1, :].partition_broadcast(32), writes=[posi], semtile=posi)
        cp(ang[R_, :], posi[R_, :], [posi], [ang])
        ts(ang[R_, :], ang[R_, :], rinv[R_, 0:1], ALU.mult, [ang, rinv], [ang])
        TWO_PI = 2.0 * math.pi
        C1 = 6.28125
        C2 = TWO_PI - C1
        for which, shift in ((1, 0.0), (0, math.pi / 2)):
            ts(rr[R_, :], ang[R_, :], shift, ALU.add, [ang], [rr])
            ts(kf[R_, :], rr[R_, :], 1.0 / TWO_PI, ALU.mult, [rr], [kf])
            cp(ki[R_, :], kf[R_, :], [kf], [ki])
            cp(kf[R_, :], ki[R_, :], [ki], [kf])
            stt(rr[R_, :], kf[R_, :], -C1, rr[R_, :], ALU.mult, ALU.add, [kf, rr], [rr])
            stt(rr[R_, :], kf[R_, :], -C2, rr[R_, :], ALU.mult, ALU.add, [kf, rr], [rr])
            ts(mk[R_, :], rr[R_, :], math.pi, ALU.is_gt, [rr], [mk])
            stt(rr[R_, :], mk[R_, :], -TWO_PI, rr[R_, :], ALU.mult, ALU.add, [mk, rr], [rr])
            ts(mk[R_, :], rr[R_, :], -math.pi, ALU.is_lt, [rr], [mk])
            stt(rr[R_, :], mk[R_, :], TWO_PI, rr[R_, :], ALU.mult, ALU.add, [mk, rr], [rr])
            ts(rr[R_, :], rr[R_, :], 3.141592, ALU.min, [rr], [rr], s2=-3.141592, op1=ALU.max)
            act(cs[R_, which, :], rr[R_, :], AF.Sin, [rr], [(cs, which)])
        P.barrier()
        pre2.close()

    lnw = sb("ln_work", [128, 16])
    xbf = sb("ln_xbf", [128, 2, D], BF16)
    ps_bf = [ps[i].h.bitcast(BF16) for i in range(8)]

    def load_ln(gname, bname, l):
        load(lnp, lnp[:, 0, :].unsqueeze(1), w[gname][l:l + 1, :].partition_broadcast(128), sub=0)
        load(lnp, lnp[:, 1, :].unsqueeze(1), w[bname][l:l + 1, :].partition_broadcast(128), sub=1, eng="scalar")

    def layer_norm_tile(t, pbank):
        xt = x_tm[:, t, :]
        st = lnw[:, 0:12].rearrange("p (a b) -> p a b", a=2)
        for hh in range(2):
            P.op("vector", lambda e, hh=hh: e.bn_stats(st[:, hh, :], x_tm[:, t, hh * 512:(hh + 1) * 512]),
                 [(x_tm, t)], [(lnw, "st%d" % hh)])
        P.op("vector", lambda e: e.bn_aggr(lnw[:, 12:14], lnw[:, 0:12]), [(lnw, "st0"), (lnw, "st1")], [(lnw, "mv")])
        ts(lnw[:, 14:15], lnw[:, 13:14], LN_EPS, ALU.add, [(lnw, "mv")], [(lnw, "sd")])
        act(lnw[:, 14:15], lnw[:, 14:15], AF.Sqrt, [(lnw, "sd")], [(lnw, "sd")])
        P.op("vector", lambda e: e.reciprocal(lnw[:, 15:16], lnw[:, 14:15]), [(lnw, "sd")], [(lnw, "rs")])
        ts(xt, xt, lnw[:, 12:13], ALU.subtract, [(x_tm, t), (lnw, "mv"), (lnw, "rs")], [(x_tm, t)],
           s2=lnw[:, 15:16], op1=ALU.mult)
        tt(xt, xt, lnp[:, 0, :], ALU.mult, [(x_tm, t), (lnp, 0)], [(x_tm, t)])
        tt(xt, xt, lnp[:, 1, :], ALU.add, [(x_tm, t), (lnp, 1)], [(x_tm, t)])
        refresh_xT(t, pbank)

    def refresh_xT(t, pbank):
        xt = x_tm[:, t, :]
        b = t % 2
        cp(xbf[:, b, :], xt, [(x_tm, t)], [(xbf, b)], eng="scalar")
        pb = ps_bf[pbank]
        for c in range(8):
            tr(pb[:, c * 128:(c + 1) * 128], xbf[:, b, c * 128:(c + 1) * 128], cmat_bf[:, IDENT, :],
               [(xbf, b), cmat_bf], [(ps[pbank], c)])
        cp(xT[:, :, t * 128:(t + 1) * 128], pb[:, :].rearrange("p (c n) -> p c n", c=8),
           [ps[pbank]], [(xT, t)])

    def store_out():
        for t in range(NT):
            P.dma("sync" if t % 2 == 0 else "scalar", out_d[t * 128:(t + 1) * 128, :], x_tm[:, t, :],
                  reads=[(x_tm, t)], semtile=x_tm)

    ctx = dict(nc=nc, P=P, stack=stack, w=w, x_tm=x_tm, xT=xT, cmat=cmat, cmat_bf=cmat_bf, lnp=lnp, ps=ps,
               ps_bf=ps_bf, sb=sb, mm=mm, tr=tr, act=act, tt=tt, ts=ts, stt=stt, cp=cp, red=red,
               load=load, load_cast=load_cast, load_ln=load_ln, layer_norm_tile=layer_norm_tile,
               sel_d=sel_d, ropeinv_d=ropeinv_d, memT=memT, sub=sub, cs=cs, mem_d=mem_d, pos_d=pos_d, dbg_d=dbg_d)

    first = True
    for l in layers:
        if first:
            for t in range(NT):
                refresh_xT(t, 5 + t % 3)
        if "A" in phases:
            phase_A(ctx, l)
        if "B" in phases:
            phase_B(ctx, l)
        if "C" in phases:
            phase_C(ctx, l)
        first = False
    store_out()
    P.finish("sync")
    P.emit(stack)
    stack.close()
    return nc


def phase_C(c, l):
    from contextlib import ExitStack
    nc, P, w = c["nc"], c["P"], c["w"]
    x_tm, xT, ps, cmat, cmat_bf = c["x_tm"], c["xT"], c["ps"], c["cmat"], c["cmat_bf"]
    mm, tr, act, tt, ts, stt, cp, red = c["mm"], c["tr"], c["act"], c["tt"], c["ts"], c["stt"], c["cp"], c["red"]
    load, load_cast = c["load"], c["load_cast"]
    IDENT = 0
    ph = ExitStack()

    def sb(name, shape, dt=F32):
        return P.tile(name, ph.enter_context(nc.sbuf_tensor("%s_%d" % (name, l), list(shape), dt)))

    c["load_ln"]("ln3_g", "ln3_b", l)
    gateT = sb("c_gateT", [32, S], BF16)
    sel = sb("c_sel", [32, 32, 128], BF16)
    load_cast(sel, sel[:], c["sel_d"])
    ph_r = ExitStack()
    _sb_outer = sb

    def sb(name, shape, dt=F32):
        return P.tile(name, ph_r.enter_context(nc.sbuf_tensor("%s_%d" % (name, l), list(shape), dt)))
    wr = sb("c_wr", [128, 8, 36], BF16)
    load_cast(wr, wr[:, :, 0:4], w["moe_w_group"][l].rearrange("(kc p) n -> p kc n", p=128), sub="g")
    load_cast(wr, wr[:, :, 4:36], w["moe_w_router"][l].rearrange("(kc p) n -> p kc n", p=128), sub="r")
    rb = sb("c_rb", [128, 36])
    load(rb, rb[:, 0:4].unsqueeze(1), w["moe_b_group"][l:l + 1, :].partition_broadcast(128), sub="g")
    load(rb, rb[:, 4:36].unsqueeze(1), w["moe_b_router"][l:l + 1, :].partition_broadcast(128), sub="r")
    lg = sb("c_lg", [128, NT, 36])
    for half in range(2):
        pr = ps[half]
        for tl in range(8):
            t = half * 8 + tl
            for kc in range(8):
                mm(pr[:, tl * 36:(tl + 1) * 36], xT[:, kc, t * 128:(t + 1) * 128], wr[:, kc, :], kc == 0, kc == 7,
                   [(xT, t), wr], [(pr, tl)])
        tt(lg[:, half * 8:(half + 1) * 8, :], pr[:, 0:288].rearrange("p (t n) -> p t n", t=8),
           bcast(rb[:, :], [128, 8, 36], 1), ALU.add, [pr, rb], [(lg, half)])
    r1 = sb("c_r1", [128, NT, 64])
    lgg = lg[:, :, 0:4]
    lge = lg[:, :, 4:36].rearrange("p t (g e) -> p t g e", g=4)
    gmax, gsum, ohg, eg = r1[:, :, 0], r1[:, :, 1], r1[:, :, 4:8], r1[:, :, 8:12]
    red(gmax, lgg, ALU.max, [lg], [(r1, "gmax")])
    tt(eg, lgg, bcast(gmax, [128, NT, 4], 2), ALU.subtract, [lg, (r1, "gmax")], [(r1, "eg")])
    tt(ohg, lgg, bcast(gmax, [128, NT, 4], 2), ALU.is_equal, [lg, (r1, "gmax")], [(r1, "ohg")])
    act(eg, eg, AF.Exp, [(r1, "eg")], [(r1, "eg")])
    red(gsum, eg, ALU.add, [(r1, "eg")], [(r1, "gsum")])
    gp = r1[:, :, 2]
    P.op("vector", lambda e: e.reciprocal(gp, gsum), [(r1, "gsum")], [(r1, "gp")])
    tmp = sb("c_tmp", [128, NT, 4, 8])
    tt(tmp[:], lge, bcast(ohg, [128, NT, 4, 8], 3), ALU.mult, [lg, (r1, "ohg")], [tmp])
    esel = r1[:, :, 16:24]
    red(esel, tmp[:].rearrange("p t g e -> p t e g"), ALU.add, [tmp], [(r1, "esel")])
    m1, m2, dd = r1[:, :, 3], r1[:, :, 12], r1[:, :, 13]
    mk1, mk2, e2 = r1[:, :, 24:32], r1[:, :, 32:40], r1[:, :, 40:48]
    red(m1, esel, ALU.max, [(r1, "esel")], [(r1, "m1")])
    tt(mk1, esel, bcast(m1, [128, NT, 8], 2), ALU.is_equal, [(r1, "esel"), (r1, "m1")], [(r1, "mk1")])
    stt(e2, mk1, -1e30, esel, ALU.mult, ALU.add, [(r1, "mk1"), (r1, "esel")], [(r1, "e2")])
    red(m2, e2, ALU.max, [(r1, "e2")], [(r1, "m2")])
    tt(mk2, e2, bcast(m2, [128, NT, 8], 2), ALU.is_equal, [(r1, "e2"), (r1, "m2")], [(r1, "mk2")])
    tt(dd, m2, m1, ALU.subtract, [(r1, "m1"), (r1, "m2")], [(r1, "dd")])
    act(dd, dd, AF.Exp, [(r1, "dd")], [(r1, "dd")])
    w1, w2 = r1[:, :, 14], r1[:, :, 15]
    ts(w1, dd, 1.0, ALU.add, [(r1, "dd")], [(r1, "w1")])
    P.op("vector", lambda e: e.reciprocal(w1, w1), [(r1, "w1")], [(r1, "w1")])
    tt(w1, w1, gp, ALU.mult, [(r1, "w1"), (r1, "gp")], [(r1, "w1")])
    tt(w2, w1, dd, ALU.mult, [(r1, "w1"), (r1, "dd")], [(r1, "w2")])
    comb = r1[:, :, 48:56]
    tt(comb, mk1, bcast(w1, [128, NT, 8], 2), ALU.mult, [(r1, "mk1"), (r1, "w1")], [(r1, "comb")])
    tt(mk2, mk2, bcast(w2, [128, NT, 8], 2), ALU.mult, [(r1, "mk2"), (r1, "w2")], [(r1, "mk2")])
    tt(comb, comb, mk2, ALU.add, [(r1, "comb"), (r1, "mk2")], [(r1, "comb")])
    gate = sb("c_gate", [128, NT, 4, 8])
    tt(gate[:], bcast(ohg, [128, NT, 4, 8], 3), bcast(comb, [128, NT, 4, 8], 2), ALU.mult,
       [(r1, "ohg"), (r1, "comb")], [gate])
    for g in range(4):
        pg = ps[2 + g % 2]
        for tl in range(4):
            t = g * 4 + tl
            tr(pg[0:32, tl * 128:(tl + 1) * 128], gate[:, t, :, :].rearrange("p g e -> p (g e)"), cmat[:, IDENT, :],
               [gate, cmat], [(pg, tl)])
        cp(gateT[:, g * 512:(g + 1) * 512], pg[0:32, :], [pg], [(gateT, g)], eng="scalar")
    if "c_gate" in c["dbg_d"]:
        P.dma("sync", c["dbg_d"]["c_gate"].rearrange("(t p) n -> p t n", p=128),
              gate[:].rearrange("p t g e -> p t (g e)"), reads=[gate], semtile=gate)

    P.barrier()
    ph_r.close()
    sb = _sb_outer
    NSLOT = 4
    wg = sb("c_wg", [128, NSLOT, 8, 256], BF16)
    wu = sb("c_wu", [128, NSLOT, 8, 256], BF16)
    wd = sb("c_wd", [128, NSLOT, 2, D], BF16)
    wsem = [sb("c_wsem%d" % i, [1, 1]) for i in range(NSLOT)]
    hT = sb("c_hT", [128, 2, 2, S], BF16)
    sg = sb("c_sg", [128, 2, 512], BF16)
    gb = sb("c_gb", [128, 2, 512], BF16)

    def load_expert(e):
        s = e % NSLOT
        P.dma("gpsimd", wg[:, s, :, :], w["moe_w_gate"][l, e].rearrange("(kc p) n -> p kc n", p=128),
              writes=[(wg, s)], semtile=wsem[s])
        P.dma("gpsimd", wu[:, s, :, :], w["moe_w_up"][l, e].rearrange("(kc p) n -> p kc n", p=128),
              writes=[(wu, s)], semtile=wsem[s])
        P.dma("gpsimd", wd[:, s, :, :], w["moe_w_down"][l, e].rearrange("(kc p) n -> p kc n", p=128),
              writes=[(wd, s)], semtile=wsem[s])

    for e in range(2):
        load_expert(e)
    unit = 0
    for blk in range(16):
        for ei in range(2):
            e = blk * 2 + ei
            s = e % NSLOT
            if e + 2 < 32:
                load_expert(e + 2)
            for g in range(4):
                tok = slice(g * 512, (g + 1) * 512)
                pgb = ps[4]
                mm(pgb[:, :], sel[:, e, :], gateT[:, tok], True, True, [sel, (gateT, g)], [pgb])
                ub = (e * 4 + g) % 2
                cp(gb[:, ub, :], pgb[:, :], [pgb], [(gb, ub)], eng="scalar")
                for fc in range(2):
                    pgt, put = ps[(unit % 2) * 2], ps[(unit % 2) * 2 + 1]
                    for kc in range(8):
                        mm(pgt[:, :], wg[:, s, kc, fc * 128:(fc + 1) * 128], xT[:, kc, tok], kc == 0, kc == 7,
                           [(wg, s), xT], [pgt])
                    for kc in range(8):
                        mm(put[:, :], wu[:, s, kc, fc * 128:(fc + 1) * 128], xT[:, kc, tok], kc == 0, kc == 7,
                           [(wu, s), xT], [put])
                    u2 = unit % 2
                    act(sg[:, u2, :], pgt[:, :], AF.Silu, [pgt], [(sg, u2)])
                    tt(sg[:, u2, :], put[:, :], sg[:, u2, :], ALU.mult, [put, (sg, u2)], [(sg, u2)])
                    tt(hT[:, ei, fc, tok], sg[:, u2, :], gb[:, ub, :], ALU.mult, [(sg, u2), (gb, ub)],
                       [(hT, (ei, g))])
                    unit += 1
        for t in range(NT):
            g = t // 4
            for hf in range(2):
                po = ps[5 + (t * 2 + hf) % 3]
                k = 0
                for ei in range(2):
                    s = (blk * 2 + ei) % NSLOT
                    for fc in range(2):
                        mm(po[:, :], hT[:, ei, fc, t * 128:(t + 1) * 128], wd[:, s, fc, hf * 512:(hf + 1) * 512],
                           k == 0, k == 3, [(hT, (ei, g)), (wd, s)], [po])
                        k += 1
                xs = x_tm[:, t, hf * 512:(hf + 1) * 512]
                if blk == 0:
                    stt(xs, xs, ALPHA, po[:, :], ALU.mult, ALU.add, [(x_tm, t), po], [(x_tm, t)])
                else:
                    tt(xs, xs, po[:, :], ALU.add, [(x_tm, t), po], [(x_tm, t)])
    for t in range(NT):
        c["layer_norm_tile"](t, 5 + t % 3)
    P.barrier()
    ph.close()


def host_consts():
    cm = np.zeros((128, 5, 128), np.float32)
    i = np.arange(128)
    cm[:, 0, :] = np.eye(128, dtype=np.float32)
    cm[:, 1, :] = (i[:, None] <= i[None, :]).astype(np.float32)
    cm[:, 2, :] = (i[:, None] > i[None, :]).astype(np.float32)
    cm[:, 3, :] = ((i[:, None] // 64) <= (i[None, :] // 64)).astype(np.float32)
    cm[:, 4, :] = 1.0
    sel = np.zeros((32, 32, 128), np.float32)
    for e in range(32):
        sel[e, e, :] = 1.0
    inv = (10000.0 ** (-np.arange(16, dtype=np.float32) / 16)).astype(np.float32)
    ri = np.zeros((96, 1), np.float32)
    ri[64:80, 0] = inv
    ri[80:96, 0] = inv
    return {"cmat": cm, "sel": sel, "ropeinv": ri}


_NC_CACHE = {}


def run_cores(inputs, n_cores=8, layers=(0, 1), phases="ABC", dbg=(), sub="GML"):
    key = (tuple(layers), phases, tuple(dbg), sub)
    if key not in _NC_CACHE:
        _NC_CACHE[key] = build(layers, phases, dbg, sub)
    nc = _NC_CACHE[key]
    consts = host_consts()
    shared = {k: np.ascontiguousarray(v) for k, v in inputs.items() if k not in ("x", "mem", "positions")}
    shared.update(consts)
    in_maps = []
    for b in range(n_cores):
        m = dict(shared)
        m["x"] = np.ascontiguousarray(inputs["x"][b])
        m["mem"] = np.ascontiguousarray(inputs["mem"][b])
        m["positions"] = np.ascontiguousarray(inputs["positions"][b:b + 1]).astype(np.int32)
        in_maps.append(m)
    res = run_bass_kernel_spmd(nc, in_maps, core_ids=list(range(n_cores)))
    return res.results


def kernel(**inputs):
    inputs = {k: np.asarray(v) for k, v in inputs.items()}
    res = run_cores(inputs, 8)
    return np.stack([r["out"] for r in res], axis=0).astype(np.float32)


def phase_B(c, l):
    from contextlib import ExitStack
    nc, P, w = c["nc"], c["P"], c["w"]
    x_tm, xT, ps, cmat_bf, memT = c["x_tm"], c["xT"], c["ps"], c["cmat_bf"], c["memT"]
    mm, act, tt, stt, cp = c["mm"], c["act"], c["tt"], c["stt"], c["cp"]
    load_cast = c["load_cast"]
    ONES = 4
    ph = ExitStack()

    def sb(name, shape, dt=F32):
        return P.tile(name, ph.enter_context(nc.sbuf_tensor("%s_%d" % (name, l), list(shape), dt)))

    c["load_ln"]("ln2_g", "ln2_b", l)
    kT = sb("b_kT", [128, 8, 256], BF16)
    vx = sb("b_v", [128, 2, D], BF16)
    ph2 = ExitStack()
    wkv = P.tile("b_wkv", ph2.enter_context(nc.sbuf_tensor("b_wkv_%d" % l, [128, 8, 2 * D], BF16)))
    for kc in range(8):
        load_cast(wkv, wkv[:, kc, :], w["xa_w_kv"][l, kc * 128:(kc + 1) * 128, :], sub=kc)
    for cc in range(8):
        pk = ps[cc % 2]
        for kc in range(8):
            mm(pk[:, 0:256], wkv[:, kc, cc * 128:(cc + 1) * 128], memT[:, kc, :], kc == 0, kc == 7, [wkv, memT], [pk])
        cp(kT[:, cc, :], pk[:, 0:256], [pk], [(kT, cc)], eng="scalar" if cc % 2 else "vector")
    for mt in range(2):
        for hf in range(2):
            pv = ps[2 + hf]
            for kc in range(8):
                mm(pv[:, :], memT[:, kc, mt * 128:(mt + 1) * 128], wkv[:, kc, D + hf * 512:D + (hf + 1) * 512],
                   kc == 0, kc == 7, [wkv, memT], [pv])
            cp(vx[:, mt, hf * 512:(hf + 1) * 512], pv[:, :], [pv], [(vx, (mt, hf))], eng="scalar" if hf else "vector")
    P.barrier()
    ph2.close()
    wq = sb("b_wq", [128, 8, D], BF16)
    wo = sb("b_wo", [128, 8, D], BF16)
    for kc in range(0, 8, 2):
        load_cast(wq, wq[:, kc:kc + 2, :], w["xa_w_q"][l, kc * 128:(kc + 2) * 128, :].rearrange("(k p) n -> p k n", p=128), sub=kc)
    for kc in range(0, 8, 2):
        load_cast(wo, wo[:, kc:kc + 2, :], w["xa_w_o"][l, kc * 128:(kc + 2) * 128, :].rearrange("(k p) n -> p k n", p=128), sub=kc)
    qT = sb("b_qT", [128, 8, 512], BF16)
    xaT = sb("b_xaT", [128, 8, 512], BF16)
    PT = sb("b_PT", [128, 2, 512], BF16)
    rden = sb("b_rden", [128, 2, 512])
    scale = 256 ** -0.5
    for g in range(4):
        tok = slice(g * 512, (g + 1) * 512)
        for cc in range(8):
            pq = ps[cc % 2]
            for kc in range(8):
                mm(pq[:, :], wq[:, kc, cc * 128:(cc + 1) * 128], xT[:, kc, tok], kc == 0, kc == 7, [wq, xT], [pq])
            cp(qT[:, cc, :], pq[:, :], [pq], [(qT, cc)], eng="scalar" if cc % 2 else "vector")
        for h in range(4):
            for mt in range(2):
                pst = ps[2 + mt]
                for j in range(2):
                    mm(pst[:, :], kT[:, h * 2 + j, mt * 128:(mt + 1) * 128], qT[:, h * 2 + j, :], j == 0, j == 1,
                       [(kT, h * 2 + j), (qT, h * 2 + j)], [pst])
                act(PT[:, mt, :], pst[:, :], AF.Exp, [pst], [(PT, mt)], scale=scale)
            pden = ps[4]
            for mt in range(2):
                mm(pden[:, :], cmat_bf[:, ONES, :], PT[:, mt, :], mt == 0, mt == 1, [cmat_bf, (PT, mt)], [pden])
            rb = h % 2
            P.op("vector", lambda e, rb=rb, pden=pden: e.reciprocal(rden[:, rb, :], pden[:, :]), [pden], [(rden, rb)])
            for j in range(2):
                po = ps[5 + j]
                for mt in range(2):
                    mm(po[:, :], vx[:, mt, h * 256 + j * 128:h * 256 + (j + 1) * 128], PT[:, mt, :], mt == 0, mt == 1,
                       [vx, (PT, mt)], [po])
                tt(xaT[:, h * 2 + j, :], po[:, :], rden[:, rb, :], ALU.mult, [po, (rden, rb)], [(xaT, h * 2 + j)])
        for tl in range(4):
            t = g * 4 + tl
            for hf in range(2):
                pp = ps[hf]
                for cc in range(8):
                    mm(pp[:, :], xaT[:, cc, tl * 128:(tl + 1) * 128], wo[:, cc, hf * 512:(hf + 1) * 512], cc == 0, cc == 7,
                       [xaT, wo], [pp])
                xs = x_tm[:, t, hf * 512:(hf + 1) * 512]
                stt(xs, xs, ALPHA, pp[:, :], ALU.mult, ALU.add, [(x_tm, t), pp], [(x_tm, t)])
            c["layer_norm_tile"](t, 7)
    P.barrier()
    ph.close()


def head_norm_gate(c, sbf, name, src, gs, out_bf, b, keyp):
    P, tt, ts, red, act = c["P"], c["tt"], c["ts"], c["red"], c["act"]
    st = sbf["hn_st"]
    cen = sbf["hn_cen"]
    sq = sbf["hn_sq"]
    s4 = src.rearrange("p (h e) -> p h e", h=4)
    mean = st[:, b, 0:4]
    var = st[:, b, 4:8]
    red(mean, s4, ALU.add, [keyp], [(st, (b, "m"))])
    ts(mean, mean, -1.0 / 64, ALU.mult, [(st, (b, "m"))], [(st, (b, "m"))])
    c4 = cen[:, b, :].rearrange("p (h e) -> p h e", h=4)
    tt(c4, s4, bcast(mean, [128, 4, 64], 2), ALU.add, [keyp, (st, (b, "m"))], [(cen, b)])
    tt(sq[:, b, :], cen[:, b, :], cen[:, b, :], ALU.mult, [(cen, b)], [(sq, b)])
    red(var, sq[:, b, :].rearrange("p (h e) -> p h e", h=4), ALU.add, [(sq, b)], [(st, (b, "v"))])
    ts(var, var, 1.0 / 64, ALU.mult, [(st, (b, "v"))], [(st, (b, "v"))], s2=LN_EPS, op1=ALU.add)
    act(var, var, AF.Ln, [(st, (b, "v"))], [(st, (b, "v"))])
    act(var, var, AF.Exp, [(st, (b, "v"))], [(st, (b, "v"))], scale=-0.5)
    tt(c4, c4, bcast(var, [128, 4, 64], 2), ALU.mult, [(cen, b), (st, (b, "v"))], [(cen, b)])
    tt(out_bf, cen[:, b, :], gs, ALU.mult, [(cen, b), (sbf["gs"], b)], [(sbf["obf"], b)])


def phase_A(c, l):
    from contextlib import ExitStack
    nc, P, w = c["nc"], c["P"], c["w"]
    sub = c.get("sub", "GML")
    c["first_mixer"] = True
    if "G" in sub:
        mixer_gla(c, l)
        c["first_mixer"] = False
    if "M" in sub:
        mixer_mlstm(c, l)
        c["first_mixer"] = False
    if "L" in sub:
        mixer_mla(c, l)
    c["load_ln"]("ln1_g", "ln1_b", l)
    for t in range(NT):
        c["layer_norm_tile"](t, 5 + t % 3)
    P.barrier()


def mixer_gla(c, l):
    from contextlib import ExitStack
    nc, P, w = c["nc"], c["P"], c["w"]
    x_tm, xT, ps, ps_bf, cmat, cmat_bf = c["x_tm"], c["xT"], c["ps"], c["ps_bf"], c["cmat"], c["cmat_bf"]
    mm, tr, act, tt, ts, stt, cp, red = c["mm"], c["tr"], c["act"], c["tt"], c["ts"], c["stt"], c["cp"], c["red"]
    load, load_cast = c["load"], c["load_cast"]
    IDENT, TRII, TRIS = 0, 1, 2
    ph = ExitStack()

    def sb(name, shape, dt=F32):
        return P.tile(name, ph.enter_context(nc.sbuf_tensor("%s_%d" % (name, l), list(shape), dt)))

    win = sb("g_win", [128, 8, 1040], BF16)
    for kc in range(8):
        load_cast(win, win[:, kc, :], w["w_in"][l, kc * 128:(kc + 1) * 128, 0:1040], sub=kc)
    wa2 = sb("g_wa2", [16, 256], BF16)
    load_cast(wa2, wa2[0:16, :], w["gla_w_a2"][l])
    babc = sb("g_babc", [128, 256])
    load(babc, babc[:].unsqueeze(1), w["gla_b_a"][l:l + 1, :].partition_broadcast(128))
    wout = sb("g_wout", [128, 2, D], BF16)
    load_cast(wout, wout[:], w["w_out"][l, 0:256, :].rearrange("(k p) n -> p k n", p=128))
    gng = sb("g_gng", [128, 256])
    load(gng, gng[:].unsqueeze(1), w["gla_norm_g"][l:l + 1, :].partition_broadcast(128))
    gaT = sb("g_gaT", [16, 2, 128], BF16)
    Lsb = sb("g_L", [128, 2, 256])
    E1 = sb("g_E1", [128, 2, 256])
    E2 = sb("g_E2", [128, 2, 256])
    E3 = sb("g_E3", [128, 2, 256])
    qs = [sb("g_qs0", [128, 2, 256], BF16), sb("g_qs1", [128, 2, 256], BF16)]
    for i in range(2):
        P.op("vector", lambda e, i=i: e.memset(qs[i][:], 0.0), [], [qs[i]])
    ksT = sb("g_ksT", [128, 2, 256], BF16)
    k2 = sb("g_k2", [128, 2, 256], BF16)
    vsb = sb("g_v", [128, 2, 256], BF16)
    gs = sb("g_gs", [128, 2, 256])
    PT = sb("g_PT", [128, 2, 4, 128], BF16)
    osb = sb("g_osb", [128, 2, 256])
    obf = sb("g_obf", [128, 2, 256], BF16)
    ogT = sb("g_ogT", [128, 2, 2, 128], BF16)
    Dend = sb("g_Dend", [128, NT, 2])
    Sst = sb("g_S", [128, 2, 64])
    Sbf = sb("g_Sbf", [128, 2, 2, 64], BF16)
    P.op("vector", lambda e: e.memset(Sst[:], 0.0), [], [Sst])
    P.op("vector", lambda e: e.memset(Sbf[:], 0.0), [], [Sbf])
    sbf = dict(hn_st=sb("g_hn_st", [128, 2, 8]), hn_cen=sb("g_hn_cen", [128, 2, 256]),
               hn_sq=sb("g_hn_sq", [128, 2, 256]), gs=gs, obf=obf)

    import os
    DBGS = int(os.environ.get("DBG_STEPS", 99))
    for t in range(int(os.environ.get("DBG_TILES", NT))):
        b = t % 2
        tok = slice(t * 128, (t + 1) * 128)
        P1, P2, P3, P4, P5, P6, P7, P8 = ps
        for cc in range(4):
            for kc in range(8):
                mm(P1[:, cc * 128:(cc + 1) * 128], win[:, kc, cc * 128:(cc + 1) * 128], xT[:, kc, tok], kc == 0, kc == 7,
                   [win, xT], [(P1, cc)])
        if DBGS <= 1:
            continue
        for kc in range(8):
            mm(P2[0:16, 0:128], win[:, kc, 1024:1040], xT[:, kc, tok], kc == 0, kc == 7, [win, xT], [(P2, 0)])
        cp(gaT[0:16, b, :], P2[0:16, 0:128], [(P2, 0)], [(gaT, b)])
        if DBGS <= 2:
            continue
        mm(P3[:, 0:256], gaT[0:16, b, :], wa2[0:16, :], True, True, [(gaT, b), wa2], [(P3, 0)])
        tt(Lsb[:, b, :], P3[:, 0:256], babc[:, :], ALU.add, [(P3, 0), babc], [(Lsb, b)])
        act(Lsb[:, b, :], Lsb[:, b, :], AF.Exp, [(Lsb, b)], [(Lsb, b)], scale=-1.0)
        ts(Lsb[:, b, :], Lsb[:, b, :], 1.0, ALU.add, [(Lsb, b)], [(Lsb, b)])
        act(Lsb[:, b, :], Lsb[:, b, :], AF.Ln, [(Lsb, b)], [(Lsb, b)])
        if DBGS <= 3:
            continue
        for ch in range(2):
            mm(P4[:, ch * 128:(ch + 1) * 128], Lsb[:, b, ch * 128:(ch + 1) * 128], cmat[:, TRII, :], True, True,
               [(Lsb, b), cmat], [(P4, ch)])
        mm(P5[:, 0:256], cmat[:, TRIS, :], Lsb[:, b, :], True, True, [(Lsb, b), cmat], [(P5, 0)])
        act(E1[:, b, :], P4[:, 0:256], AF.Exp, [P4], [(E1, b)], scale=-1.0 / 16)
        act(E2[:, b, :], P4[:, 0:256], AF.Exp, [P4], [(E2, b)], scale=1.0 / 16)
        act(E3[:, b, :], P5[:, 0:256], AF.Exp, [(P5, 0)], [(E3, b)], scale=-1.0 / 16)
        cp(Dend[:, t, :], E1[:, b, :].rearrange("p (c n) -> p c n", c=2)[:, :, 127], [(E1, b)], [(Dend, t)])
        for hp in range(2):
            rws = slice(hp * 64, (hp + 1) * 64)
            stt(qs[hp][rws, b, :], P1[rws, 0:256], 0.125, E1[rws, b, :], ALU.mult, ALU.mult, [P1, (E1, b)], [(qs[hp], b)])
        tt(ksT[:, b, :], P1[:, 256:512], E2[:, b, :], ALU.mult, [P1, (E2, b)], [(ksT, b)])
        if DBGS <= 4:
            continue
        for kc in range(8):
            mm(P6[:, :], xT[:, kc, tok], win[:, kc, 256:768], kc == 0, kc == 7, [win, xT], [P6])
        for kc in range(8):
            mm(P7[:, 0:256], xT[:, kc, tok], win[:, kc, 768:1024], kc == 0, kc == 7, [win, xT], [(P7, 0)])
        SUBS = int(os.environ.get("DBG_SUB", 99))
        if SUBS >= 1:
            tt(k2[:, b, :], P6[:, 0:256], E3[:, b, :], ALU.mult, [P6, (E3, b)], [(k2, b)])
        if SUBS >= 2:
            cp(vsb[:, b, :], P6[:, 256:512], [P6], [(vsb, b)], eng="scalar")
        if SUBS >= 3:
            act(gs[:, b, :], P7[:, 0:256], AF.Silu, [(P7, 0)], [(gs, b)])
        if SUBS >= 4:
            tt(gs[:, b, :], gs[:, b, :], gng[:, :], ALU.mult, [(gs, b), gng], [(gs, b)])
        if DBGS <= 5:
            continue
        for h in range(4):
            hp, hc = h % 2, h // 2
            mm(P8[:, h * 128:(h + 1) * 128], ksT[:, b, hc * 128:(hc + 1) * 128],
               qs[hp][:, b, hc * 128:(hc + 1) * 128], True, True, [(ksT, b), (qs[hp], b)], [(P8, h)])
        tt(PT[:, b, :, :], P8[:, :].rearrange("p (h n) -> p h n", h=4), bcast(cmat[:, TRII, :], [128, 4, 128], 1),
           ALU.mult, [P8, cmat], [(PT, b)])
        if DBGS <= 6:
            continue
        for h in range(4):
            hc = h // 2
            mm(P2[:, 128 + h * 64:128 + (h + 1) * 64], k2[:, b, hc * 128:(hc + 1) * 128], vsb[:, b, h * 64:(h + 1) * 64],
               True, True, [(k2, b), (vsb, b)], [(P2, 1 + h)])
        if DBGS <= 7:
            continue
        for h in range(4):
            hp, hc = h % 2, h // 2
            mm(P3[:, 256 + h * 64:256 + (h + 1) * 64], PT[:, b, h, :], vsb[:, b, h * 64:(h + 1) * 64], True, False,
               [(PT, b), (vsb, b)], [(P3, 1)])
            mm(P3[:, 256 + h * 64:256 + (h + 1) * 64], qs[hp][:, b, hc * 128:(hc + 1) * 128],
               Sbf[:, b, hc, :], False, True, [(qs[hp], b), (Sbf, b)], [(P3, 1)])
        if DBGS <= 8:
            continue
        for h in range(4):
            hp, hc = h % 2, h // 2
            rows = slice(hp * 64, (hp + 1) * 64)
            stt(Sst[rows, hc, :], Sst[rows, hc, :], Dend[rows, t, hc:hc + 1], P2[rows, 128 + h * 64:128 + (h + 1) * 64],
                ALU.mult, ALU.add, [(Sst, h), (Dend, t), (P2, 1 + h)], [(Sst, h)])
        cp(Sbf[:, 1 - b, :, :], Sst[:, :, :], [Sst], [(Sbf, 1 - b)])
        if DBGS <= 9:
            continue
        cp(osb[:, b, :], P3[:, 256:512], [(P3, 1)], [(osb, b)], eng="scalar")
        head_norm_gate(c, sbf, "g", osb[:, b, :], gs[:, b, :], obf[:, b, :], b, (osb, b))
        if DBGS <= 10:
            continue
        pb = ps_bf[4]
        for ch in range(2):
            tr(pb[:, 512 + ch * 128:512 + (ch + 1) * 128], obf[:, b, ch * 128:(ch + 1) * 128], cmat_bf[:, IDENT, :],
               [(obf, b), cmat_bf], [(P5, 1)])
        cp(ogT[:, b, :, :], pb[:, 512:768].rearrange("p (c n) -> p c n", c=2), [(P5, 1)], [(ogT, b)])
        for q4 in range(4):
            pq, key = (P7[:, 256:512], (P7, 1)) if q4 % 2 == 0 else (P4[:, 256:512], (P4, 2))
            for ch in range(2):
                mm(pq, ogT[:, b, ch, :], wout[:, ch, q4 * 256:(q4 + 1) * 256], ch == 0, ch == 1, [(ogT, b), wout], [key])
            xs = x_tm[:, t, q4 * 256:(q4 + 1) * 256]
            if c["first_mixer"]:
                stt(xs, xs, ALPHA, pq, ALU.mult, ALU.add, [(x_tm, t), key], [(x_tm, t)])
            else:
                tt(xs, xs, pq, ALU.add, [(x_tm, t), key], [(x_tm, t)])
    P.barrier()
    ph.close()


def mixer_mlstm(c, l):
    from contextlib import ExitStack
    import os
    nc, P, w = c["nc"], c["P"], c["w"]
    x_tm, xT, ps, ps_bf, cmat, cmat_bf = c["x_tm"], c["xT"], c["ps"], c["ps_bf"], c["cmat"], c["cmat_bf"]
    mm, tr, act, tt, ts, stt, cp, red = c["mm"], c["tr"], c["act"], c["tt"], c["ts"], c["stt"], c["cp"], c["red"]
    load, load_cast = c["load"], c["load_cast"]
    IDENT, TRII, ONES = 0, 1, 4
    ph = ExitStack()

    def sb(name, shape, dt=F32):
        return P.tile(name, ph.enter_context(nc.sbuf_tensor("%s_%d" % (name, l), list(shape), dt)))

    win = sb("m_win", [128, 8, 1032], BF16)
    for kc in range(8):
        load_cast(win, win[:, kc, :], w["w_in"][l, kc * 128:(kc + 1) * 128, 1040:2072], sub=kc)
    cw = sb("m_cw", [128, 4, 4])
    for j in range(4):
        load(cw, cw[:, :, j], w["ml_conv_w"][l, j, :].rearrange("(c p) -> p c", p=128), sub=j)
    bif = sb("m_bif", [128, 8])
    load(bif, bif[:, 0:4].unsqueeze(1), w["ml_b_i"][l:l + 1, :].partition_broadcast(128), sub=0)
    load(bif, bif[:, 4:8].unsqueeze(1), w["ml_b_f"][l:l + 1, :].partition_broadcast(128), sub=1)
    mng = sb("m_mng", [128, 256])
    load(mng, mng[:].unsqueeze(1), w["ml_norm_g"][l:l + 1, :].partition_broadcast(128))
    wout = sb("m_wout", [128, 2, D], BF16)
    load_cast(wout, wout[:], w["w_out"][l, 256:512, :].rearrange("(k p) n -> p k n", p=128))

    q = [sb("m_q0", [128, 2, S], BF16), sb("m_q1", [128, 2, S], BF16)]
    kT = sb("m_kT", [128, 2, S], BF16)
    ph1 = ExitStack()
    mqk = P.tile("m_mqk", ph1.enter_context(nc.sbuf_tensor("m_mqk_%d" % l, [128, 4, S + 3], BF16)))
    acc = P.tile("m_acc", ph1.enter_context(nc.sbuf_tensor("m_acc_%d" % l, [128, 2, 1024], F32)))
    P.op("vector", lambda e: e.memset(mqk[:, :, 0:3], 0.0), [], [mqk])
    for g in range(4):
        tok = slice(g * 512, (g + 1) * 512)
        for ch in range(4):
            pp = ps[(g * 4 + ch) % 2]
            for kc in range(8):
                mm(pp[:, :], win[:, kc, ch * 128:(ch + 1) * 128], xT[:, kc, tok], kc == 0, kc == 7, [win, xT], [pp])
            cp(mqk[:, ch, 3 + g * 512:3 + (g + 1) * 512], pp[:, :], [pp], [(mqk, ch)], eng="scalar" if ch % 2 else "vector")
    for i in range(2):
        P.op("vector", lambda e, i=i: e.memset(q[i][:], 0.0), [], [q[i]])
    for ch in range(4):
        for half in range(2):
            ai = half
            a = acc[:, ai, :]
            off = half * 1024
            ts(a, mqk[:, ch, off:off + 1024], cw[:, ch, 0:1], ALU.mult, [(mqk, ch), cw], [(acc, ai)])
            for j in range(1, 4):
                stt(a, mqk[:, ch, off + j:off + j + 1024], cw[:, ch, j:j + 1], a, ALU.mult, ALU.add,
                    [(mqk, ch), cw, (acc, ai)], [(acc, ai)])
            tokh = slice(off, off + 1024)
            if ch < 2:
                for hp in range(2):
                    rws = slice(hp * 64, (hp + 1) * 64)
                    act(q[hp][rws, ch, tokh], acc[rws, ai, :], AF.Silu, [(acc, ai)], [(q[hp], (ch, half))])
            else:
                act(kT[:, ch - 2, tokh], a, AF.Silu, [(acc, ai)], [(kT, (ch, half))])
    for hp in range(2):
        ts(q[hp][:], q[hp][:], 0.125, ALU.mult, [q[hp]], [q[hp]])
    P.barrier()
    ph1.close()

    gates = sb("m_gates", [128, NT, 8])
    pg = ps[2]
    for t in range(NT):
        for kc in range(8):
            mm(pg[:, t * 8:(t + 1) * 8], xT[:, kc, t * 128:(t + 1) * 128], win[:, kc, 1024:1032], kc == 0, kc == 7,
               [win, xT], [pg])
    tt(gates[:], pg[:, 0:128].rearrange("p (t n) -> p t n", t=NT), bcast(bif[:, :], [128, NT, 8], 1), ALU.add,
       [pg, bif], [gates])
    Lf = sb("m_Lf", [128, NT, 4])
    act(Lf[:], gates[:, :, 4:8], AF.Exp, [gates], [Lf], scale=-1.0)
    ts(Lf[:], Lf[:], 1.0, ALU.add, [Lf], [Lf])
    act(Lf[:], Lf[:], AF.Ln, [Lf], [Lf])
    Lf2 = Lf[:].rearrange("p t n -> p (t n)")
    p3 = ps[3]
    mm(p3[:, 0:64], cmat[:, TRII, :], Lf2, True, True, [cmat, Lf], [p3])
    asb = sb("m_a", [128, NT, 4])
    tt(asb[:], p3[:, 0:64].rearrange("p (t n) -> p t n", t=NT), gates[:, :, 0:4], ALU.add, [p3, gates], [asb])
    cumL = sb("m_cumL", [128, 64])
    cp(cumL[:], p3[:, 0:64], [p3], [cumL])
    a2 = asb[:].rearrange("p t n -> p (t n)")
    p4 = ps[4]
    tr(p4[0:64, 0:128], a2, cmat[:, IDENT, :], [asb, cmat], [p4])
    Acol = sb("m_Acol", [64, 1])
    red(Acol[:, 0:1], p4[0:64, 0:128], ALU.max, [p4], [Acol])
    rows = sb("m_rows", [1, 5, 64])
    p5 = ps[5]
    mm(p5[0:1, 0:64], Acol[0:64, 0:1], cmat[0:64, IDENT, 0:64], True, True, [Acol, cmat], [p5])
    mm(p5[0:1, 64:128], cmat[:, ONES, 0:1], Lf2, True, True, [cmat, Lf], [p5])
    cp(rows[0:1, 0:2, :], p5[0:1, 0:128].rearrange("p (a n) -> p a n", a=2), [p5], [rows])
    P.op("vector", lambda e: e.memset(rows[0:1, 2, 0:4], 0.0), [rows], [rows])
    for cc in range(NT):
        sl = slice(cc * 4, cc * 4 + 4)
        tt(rows[0:1, 3, sl], rows[0:1, 2, sl], rows[0:1, 0, sl], ALU.max, [rows], [rows])
        if cc < NT - 1:
            tt(rows[0:1, 2, (cc + 1) * 4:(cc + 1) * 4 + 4], rows[0:1, 3, sl], rows[0:1, 1, sl], ALU.subtract, [rows], [rows])
    tt(rows[0:1, 4, :], rows[0:1, 2, :], rows[0:1, 3, :], ALU.subtract, [rows], [rows])
    act(rows[0:1, 4, :], rows[0:1, 4, :], AF.Exp, [rows], [rows])
    p6 = ps[6]
    mm(p6[:, 0:128], cmat[0:1, ONES, :], rows[0:1, 3:5, :].rearrange("p a n -> p (a n)"), True, True, [cmat, rows], [p6])
    bcs = sb("m_bcs", [128, 128])
    cp(bcs[:], p6[:, 0:128], [p6], [bcs])
    wtok = sb("m_wtok", [128, 64])
    tt(wtok[:], a2, bcs[:, 0:64], ALU.subtract, [asb, bcs], [wtok])
    act(wtok[:], wtok[:], AF.Exp, [wtok], [wtok])
    clamp = sb("m_clamp", [128, 64])
    tt(clamp[:], cumL[:], bcs[:, 0:64], ALU.subtract, [cumL, bcs], [clamp])
    act(clamp[:], clamp[:], AF.Exp, [clamp], [clamp])

    vext = sb("m_vext", [128, 2, 4, 65], BF16)
    P.op("vector", lambda e: e.memset(vext[:], 1.0), [], [vext])
    vw = sb("m_vw", [128, 2, 4, 65], BF16)
    gso = sb("m_gso", [128, 2, 256])
    ktm = sb("m_ktm", [128, 2, 256], BF16)
    WM = sb("m_WM", [128, 2, 4, 128])
    PT = sb("m_PT", [128, 2, 4, 128], BF16)
    Cn = sb("m_Cn", [128, 2, 65])
    P.op("vector", lambda e: e.memset(Cn[:], 0.0), [], [Cn])
    Cd = sb("m_Cd", [128, 2, 65])
    Cdbf = sb("m_Cdbf", [128, 2, 2, 65], BF16)
    nd = sb("m_nd", [128, 2, 4, 65])
    hsb = sb("m_h", [128, 2, 256])
    small = sb("m_small", [128, 2, 8])
    obf = sb("m_obf", [128, 2, 256], BF16)
    ohT = sb("m_ohT", [128, 2, 2, 128], BF16)
    sbf = dict(hn_st=sb("m_hn_st", [128, 2, 8]), hn_cen=sb("m_hn_cen", [128, 2, 256]),
               hn_sq=sb("m_hn_sq", [128, 2, 256]), gs=gso, obf=obf)
    PA, PB, PC, PD, PE_, PO0, PO1, PX = ps
    for t in range(int(os.environ.get("DBG_TILES", NT))):
        b = t % 2
        tok = slice(t * 128, (t + 1) * 128)
        g4 = slice(t * 4, t * 4 + 4)
        for kc in range(8):
            mm(PA[:, :], xT[:, kc, tok], win[:, kc, 512:1024], kc == 0, kc == 7, [win, xT], [PA])
        v4 = PA[:, 0:256].rearrange("p (h e) -> p h e", h=4)
        cp(vext[:, b, :, 0:64], v4, [PA], [(vext, b)], eng="scalar")
        tt(vw[:, b, :, 0:64], v4, bcast(wtok[:, g4], [128, 4, 64], 2), ALU.mult, [PA, wtok], [(vw, b)])
        cp(vw[:, b, :, 64], wtok[:, g4], [wtok], [(vw, b)])
        act(gso[:, b, :], PA[:, 256:512], AF.Sigmoid, [PA], [(gso, b)])
        tt(gso[:, b, :], gso[:, b, :], mng[:, :], ALU.mult, [(gso, b), mng], [(gso, b)])
        pb = ps_bf[1]
        for hc in range(2):
            tr(pb[:, hc * 128:(hc + 1) * 128], kT[:, hc, tok], cmat_bf[:, IDENT, :], [kT, cmat_bf], [PB])
        cp(ktm[:, b, :], pb[:, 0:256], [PB], [(ktm, b)])
        for h in range(4):
            hp, hc = h % 2, h // 2
            mm(PC[:, h * 128:(h + 1) * 128], kT[:, hc, tok], q[hp][:, hc, tok], True, True, [kT, q[hp]], [PC])
        tt(WM[:, b, :, :], bcast(wtok[:, g4], [128, 4, 128], 2), bcast(cmat[:, TRII, :], [128, 4, 128], 1), ALU.mult,
           [wtok, cmat], [(WM, b)])
        tt(PT[:, b, :, :], PC[:, :].rearrange("p (h n) -> p h n", h=4), WM[:, b, :, :], ALU.mult, [PC, (WM, b)], [(PT, b)])
        for h in range(4):
            hc = h // 2
            mm(PD[:, h * 65:(h + 1) * 65], ktm[:, b, hc * 128:(hc + 1) * 128], vw[:, b, h, :], True, True,
               [(ktm, b), (vw, b)], [PD])
        for h in range(4):
            hp, hc = h % 2, h // 2
            rws = slice(hp * 64, (hp + 1) * 64)
            ts(Cd[rws, hc, :], Cn[rws, hc, :], bcs[rws, 64 + t * 4 + h:64 + t * 4 + h + 1], ALU.mult,
               [(Cn, h), bcs], [(Cd, h)])
        cp(Cdbf[:, b, :, :], Cd[:], [Cd], [(Cdbf, b)])
        for h in range(4):
            hp, hc = h % 2, h // 2
            mm(PE_[:, h * 65:(h + 1) * 65], PT[:, b, h, :], vext[:, b, h, :], True, False, [(PT, b), (vext, b)], [PE_])
            mm(PE_[:, h * 65:(h + 1) * 65], q[hp][:, hc, tok], Cdbf[:, b, hc, :], False, True, [q[hp], (Cdbf, b)], [PE_])
        for h in range(4):
            hp, hc = h % 2, h // 2
            rws = slice(hp * 64, (hp + 1) * 64)
            tt(Cn[rws, hc, :], Cd[rws, hc, :], PD[rws, h * 65:(h + 1) * 65], ALU.add, [(Cd, h), PD], [(Cn, h)])
        cp(nd[:, b, :, :], PE_[:, 0:260].rearrange("p (h e) -> p h e", h=4), [PE_], [(nd, b)], eng="scalar")
        stt(small[:, b, 0:4], nd[:, b, :, 64], -1.0, nd[:, b, :, 64], ALU.mult, ALU.max, [(nd, b)], [(small, b)])
        tt(small[:, b, 0:4], small[:, b, 0:4], clamp[:, g4], ALU.max, [(small, b), clamp], [(small, b)])
        P.op("vector", lambda e, b=b: e.reciprocal(small[:, b, 4:8], small[:, b, 0:4]), [(small, b)], [(small, b)])
        tt(hsb[:, b, :].rearrange("p (h e) -> p h e", h=4), nd[:, b, :, 0:64], bcast(small[:, b, 4:8], [128, 4, 64], 2),
           ALU.mult, [(nd, b), (small, b)], [(hsb, b)])
        head_norm_gate(c, sbf, "m", hsb[:, b, :], gso[:, b, :], obf[:, b, :], b, (hsb, b))
        pbx = ps_bf[7]
        for ch in range(2):
            tr(pbx[:, ch * 128:(ch + 1) * 128], obf[:, b, ch * 128:(ch + 1) * 128], cmat_bf[:, IDENT, :],
               [(obf, b), cmat_bf], [PX])
        cp(ohT[:, b, :, :], pbx[:, 0:256].rearrange("p (c n) -> p c n", c=2), [PX], [(ohT, b)])
        for hf in range(2):
            po = PO0 if hf == 0 else PO1
            for ch in range(2):
                mm(po[:, :], ohT[:, b, ch, :], wout[:, ch, hf * 512:(hf + 1) * 512], ch == 0, ch == 1, [(ohT, b), wout], [po])
            xs = x_tm[:, t, hf * 512:(hf + 1) * 512]
            if c["first_mixer"]:
                stt(xs, xs, ALPHA, po[:, :], ALU.mult, ALU.add, [(x_tm, t), po], [(x_tm, t)])
            else:
                tt(xs, xs, po[:, :], ALU.add, [(x_tm, t), po], [(x_tm, t)])
    P.barrier()
    ph.close()


def mixer_mla(c, l):
    from contextlib import ExitStack
    import os
    nc, P, w = c["nc"], c["P"], c["w"]
    x_tm, xT, ps, cmat, cmat_bf, cs = c["x_tm"], c["xT"], c["ps"], c["cmat"], c["cmat_bf"], c["cs"]
    mm, act, tt, ts, stt, cp = c["mm"], c["act"], c["tt"], c["ts"], c["stt"], c["cp"]
    load, load_cast = c["load"], c["load_cast"]
    MMLA, ONES = 3, 4
    R_ = slice(64, 96)
    ph = ExitStack()

    def sb(name, shape, dt=F32, st=None):
        return P.tile(name, (st or ph).enter_context(nc.sbuf_tensor("%s_%d" % (name, l), list(shape), dt)))

    cqnT = sb("a_cqnT", [128, 2, S], BF16)
    ckvnT = sb("a_ckvnT", [128, S], BF16)
    krope = sb("a_krope", [96, S], BF16)
    v_all = sb("a_vall", [128, NT, 512], BF16)
    gq = sb("a_gq", [128, 2])
    gkv = sb("a_gkv", [128, 1])
    load(gq, gq[:], w["mla_q_norm_g"][l].rearrange("(rc p) -> p rc", p=128))
    load(gkv, gkv[:], w["mla_kv_norm_g"][l].rearrange("(o p) -> p o", o=1))

    p1 = ExitStack()
    win = sb("a_win", [128, 8, 416], BF16, p1)
    for kc in range(8):
        load_cast(win, win[:, kc, :], w["w_in"][l, kc * 128:(kc + 1) * 128, 2072:2488], sub=kc)
    wkr = sb("a_wkr", [128, 8, 2, 96], BF16, p1)
    P.op("vector", lambda e: e.memset(wkr[:], 0.0), [], [wkr])
    cp(wkr[:, :, 0, 64:96], win[:, :, 384:416], [win], [wkr])
    ts(wkr[:, :, 1, 64:80], win[:, :, 400:416], -1.0, ALU.mult, [win], [wkr])
    cp(wkr[:, :, 1, 80:96], win[:, :, 384:400], [win], [wkr])
    sq = sb("a_sq", [128, 2, 512], BF16, p1)
    rstd = sb("a_rstd", [128, 2, 512], F32, p1)
    tA = sb("a_tA", [96, 512], F32, p1)
    tB = sb("a_tB", [96, 512], F32, p1)
    for g in range(4):
        tok = slice(g * 512, (g + 1) * 512)
        for rc in range(2):
            for kc in range(8):
                mm(ps[rc][:, :], win[:, kc, rc * 128:(rc + 1) * 128], xT[:, kc, tok], kc == 0, kc == 7, [win, xT], [ps[rc]])
        for rc in range(2):
            act(sq[:, rc, :], ps[rc][:, :], AF.Square, [ps[rc]], [(sq, rc)])
        for rc in range(2):
            mm(ps[2][:, :], cmat_bf[:, ONES, :], sq[:, rc, :], rc == 0, rc == 1, [cmat_bf, (sq, rc)], [ps[2]])
        ts(rstd[:, 0, :], ps[2][:, :], 1.0 / 256, ALU.mult, [ps[2]], [(rstd, 0)], s2=LN_EPS, op1=ALU.add)
        act(rstd[:, 0, :], rstd[:, 0, :], AF.Ln, [(rstd, 0)], [(rstd, 0)])
        act(rstd[:, 0, :], rstd[:, 0, :], AF.Exp, [(rstd, 0)], [(rstd, 0)], scale=-0.5)
        for rc in range(2):
            stt(cqnT[:, rc, tok], ps[rc][:, :], gq[:, rc:rc + 1], rstd[:, 0, :], ALU.mult, ALU.mult,
                [ps[rc], gq, (rstd, 0)], [(cqnT, (rc, g))])
        for kc in range(8):
            mm(ps[3][:, :], win[:, kc, 256:384], xT[:, kc, tok], kc == 0, kc == 7, [win, xT], [ps[3]])
        act(sq[:, 0, :], ps[3][:, :], AF.Square, [ps[3]], [(sq, 0)])
        mm(ps[4][:, :], cmat_bf[:, ONES, :], sq[:, 0, :], True, True, [cmat_bf, (sq, 0)], [ps[4]])
        ts(rstd[:, 1, :], ps[4][:, :], 1.0 / 128, ALU.mult, [ps[4]], [(rstd, 1)], s2=LN_EPS, op1=ALU.add)
        act(rstd[:, 1, :], rstd[:, 1, :], AF.Ln, [(rstd, 1)], [(rstd, 1)])
        act(rstd[:, 1, :], rstd[:, 1, :], AF.Exp, [(rstd, 1)], [(rstd, 1)], scale=-0.5)
        stt(ckvnT[:, tok], ps[3][:, :], gkv[:, 0:1], rstd[:, 1, :], ALU.mult, ALU.mult, [ps[3], gkv, (rstd, 1)], [(ckvnT, g)])
        for r2 in range(2):
            for kc in range(8):
                mm(ps[5 + r2][0:96, :], wkr[:, kc, r2, :], xT[:, kc, tok], kc == 0, kc == 7, [wkr, xT], [ps[5 + r2]])
        tt(tA[R_, :], ps[5][R_, :], cs[R_, 0, tok], ALU.mult, [ps[5], cs], [tA])
        tt(tB[R_, :], ps[6][R_, :], cs[R_, 1, tok], ALU.mult, [ps[6], cs], [tB])
        tt(krope[R_, tok], tA[R_, :], tB[R_, :], ALU.add, [tA, tB], [(krope, g)])
    P.barrier()
    p1.close()

    wuk = sb("a_wuk", [128, 8, 64], BF16)
    wuv = sb("a_wuv", [128, 8, 64], BF16)
    ukv = w["mla_w_ukv"][l].rearrange("p (h two d) -> p h two d", h=8, two=2)
    load_cast(wuk, wuk[:], ukv[:, :, 0, :])
    load_cast(wuv, wuv[:], ukv[:, :, 1, :])
    wuq = sb("a_wuq", [128, 2, 768], BF16)
    load_cast(wuq, wuq[:], w["mla_w_uq"][l].rearrange("(rc p) n -> p rc n", p=128))
    wuqr = sb("a_wuqr", [128, 2, 8, 96], BF16)
    P.op("vector", lambda e: e.memset(wuqr[:], 0.0), [], [wuqr])
    wq4 = wuq[:].rearrange("p r (h c) -> p r h c", h=8)
    ts(wuqr[:, :, :, 64:80], wq4[:, :, :, 80:96], -1.0, ALU.mult, [wuq], [wuqr])
    cp(wuqr[:, :, :, 80:96], wq4[:, :, :, 64:80], [wuq], [wuqr])
    wout = sb("a_wout", [128, 4, D], BF16)
    load_cast(wout, wout[:], w["w_out"][l, 512:1024, :].rearrange("(k p) n -> p k n", p=128))
    qTh = sb("a_qTh", [96, 2, 512], BF16)
    PTb = sb("a_PT", [128, 3, 512], BF16)
    mlaT = sb("a_mlaT", [128, 4, 512], BF16)
    rden = sb("a_rden", [128, 2, 512])
    tA2 = sb("a_tA2", [96, 2, 512])
    tB2 = sb("a_tB2", [96, 2, 512])
    KH = [("k", h) for h in range(8)]
    for g in range(4):
        tok = slice(g * 512, (g + 1) * 512)
        for h in range(8):
            pk = ps[h % 2]
            mm(pk[0:64, :], wuk[:, h, :], ckvnT[:, tok], True, True, [wuk, ckvnT], [pk])
            cp(xT[0:64, h, tok], pk[0:64, :], [pk], [(xT, KH[h])], eng="scalar" if h % 2 else "vector")
        cp(xT[R_, :, tok], bcast(krope[R_, tok], [32, 8, 512], 1), [krope], [(xT, k) for k in KH])
    for t in range(NT):
        pv = ps[2 + t % 2]
        mm(pv[:, :], ckvnT[:, t * 128:(t + 1) * 128], wuv[:].rearrange("p h d -> p (h d)"), True, True, [ckvnT, wuv], [pv])
        cp(v_all[:, t, :], pv[:, :], [pv], [(v_all, t)], eng="scalar" if t % 2 else "vector")
    scale = 96.0 ** -0.5
    for g in range(int(os.environ.get("DBG_GROUPS", 4))):
        tok = slice(g * 512, (g + 1) * 512)
        nkt = 4 * g + 4
        for h in range(8):
            hp, pair, qb = h % 2, h // 2, h % 2
            pq, pr = ps[0], ps[1]
            for rc in range(2):
                mm(pq[0:96, :], wuq[:, rc, h * 96:(h + 1) * 96], cqnT[:, rc, tok], rc == 0, rc == 1, [wuq, cqnT], [pq])
            for rc in range(2):
                mm(pr[0:96, :], wuqr[:, rc, h, :], cqnT[:, rc, tok], rc == 0, rc == 1, [wuqr, cqnT], [pr])
            cp(qTh[0:64, qb, :], pq[0:64, :], [pq], [(qTh, qb)], eng="scalar")
            tt(tA2[R_, qb, :], pq[R_, :], cs[R_, 0, tok], ALU.mult, [pq, cs], [(tA2, qb)])
            tt(tB2[R_, qb, :], pr[R_, :], cs[R_, 1, tok], ALU.mult, [pr, cs], [(tB2, qb)])
            tt(qTh[R_, qb, :], tA2[R_, qb, :], tB2[R_, qb, :], ALU.add, [(tA2, qb), (tB2, qb)], [(qTh, qb)])
            po, pd = ps[4 + h % 2], ps[6 + h % 2]
            for kt in range(nkt):
                qlo = max(kt - 4 * g, 0) * 128
                N = 512 - qlo
                pst = ps[2 + kt % 2]
                pb = kt % 3
                mm(pst[:, 0:N], xT[0:96, h, kt * 128:(kt + 1) * 128], qTh[0:96, qb, qlo:512], True, True,
                   [(xT, KH[h]), (qTh, qb)], [pst])
                act(PTb[:, pb, 0:N], pst[:, 0:N], AF.Exp, [pst], [(PTb, pb)], scale=scale)
                if kt >= 4 * g:
                    tt(PTb[:, pb, 0:128], PTb[:, pb, 0:128], cmat_bf[:, MMLA, :], ALU.mult, [(PTb, pb), cmat_bf], [(PTb, pb)])
                mm(po[:, qlo:512], v_all[:, kt, pair * 128:(pair + 1) * 128], PTb[:, pb, 0:N], kt == 0, kt == nkt - 1,
                   [(v_all, kt), (PTb, pb)], [po])
                mm(pd[:, qlo:512], cmat_bf[:, ONES, :], PTb[:, pb, 0:N], kt == 0, kt == nkt - 1, [cmat_bf, (PTb, pb)], [pd])
            rws = slice(hp * 64, (hp + 1) * 64)
            P.op("vector", lambda e, rws=rws, qb=qb, pd=pd: e.reciprocal(rden[rws, qb, :], pd[rws, :]), [pd], [(rden, qb)])
            tt(mlaT[rws, pair, :], po[rws, :], rden[rws, qb, :], ALU.mult, [po, (rden, qb)], [(mlaT, h)])
        for tl in range(4):
            t = g * 4 + tl
            for hf in range(2):
                pp = ps[hf]
                for pr_ in range(4):
                    mm(pp[:, :], mlaT[:, pr_, tl * 128:(tl + 1) * 128], wout[:, pr_, hf * 512:(hf + 1) * 512], pr_ == 0, pr_ == 3,
                       [mlaT, wout], [pp])
                xs = x_tm[:, t, hf * 512:(hf + 1) * 512]
                if c["first_mixer"]:
                    stt(xs, xs, ALPHA, pp[:, :], ALU.mult, ALU.add, [(x_tm, t), pp], [(x_tm, t)])
                else:
                    tt(xs, xs, pp[:, :], ALU.add, [(x_tm, t), pp], [(x_tm, t)])
    P.barrier()
    ph.close()
```

```python
import numpy as np
import ml_dtypes
import concourse.bass as bass
import concourse.mybir as mybir
from concourse.bass_utils import run_bass_kernel_spmd

F32 = mybir.dt.float32
BF16 = mybir.dt.bfloat16
I32 = mybir.dt.int32
AF = mybir.ActivationFunctionType
ALU = mybir.AluOpType
AX = mybir.AxisListType

S = 2048
D = 1024
NT = 16
DEPTH = 2
ALPHA = (2 * DEPTH) ** 0.25
LN_EPS = 1e-5
IN_W = 2488


class Tile:
    def __init__(self, name, h):
        self.name = name
        self.h = h
        self.st = {}
        self.dsem = None
        self.dcount = 0
        self.psum = False

    def __getitem__(self, k):
        return self.h[k]


class Op:
    __slots__ = ("fn", "deps", "inc", "dma")

    def __init__(self, fn, deps, dma=None):
        self.fn = fn
        self.deps = deps
        self.inc = False
        self.dma = dma


class Prog:
    ENG = ("sync", "scalar", "vector", "gpsimd", "tensor")

    def __init__(self, nc):
        self.nc = nc
        self.ops = {e: [] for e in self.ENG}
        self.dsems = {}
        self.dtotal = {}
        self.tiles = []

    def tile(self, name, h):
        t = Tile(name, h)
        self.tiles.append(t)
        return t

    @staticmethod
    def _norm(lst):
        out = []
        for x in lst:
            if not isinstance(x, tuple):
                x = (x, None)
            if x[0].psum:
                x = (x[0], None)
            out.append(x)
        return out

    @staticmethod
    def _states(t, s):
        if s is None:
            return list(t.st.values())
        r = []
        if None in t.st:
            r.append(t.st[None])
        if s in t.st:
            r.append(t.st[s])
        return r

    def _collect(self, reads, writes):
        deps = {}

        def add(ev):
            if ev is None:
                return
            k, v = ev
            if not isinstance(k, str):
                v = self.dtotal[k]
            if deps.get(k, -1) < v:
                deps[k] = v

        for (t, s) in reads:
            for st in self._states(t, s):
                add(st[0])
        for (t, s) in writes:
            for st in self._states(t, s):
                add(st[0])
                for k, v in st[1].items():
                    add((k, v))
        return deps

    def _update(self, ev, reads, writes):
        k, v = ev
        for (t, s) in reads:
            st = t.st.setdefault(s, [None, {}])
            if st[1].get(k, -1) < v:
                st[1][k] = v
        for (t, s) in writes:
            if s is None:
                t.st = {None: [ev, {}]}
            else:
                t.st[s] = [ev, {}]

    def op(self, eng, fn, reads=(), writes=()):
        reads = self._norm(reads)
        writes = self._norm(writes)
        writes = writes + [r for r in reads if r[0].psum]
        deps = self._collect(reads, writes)
        if eng == "tensor":
            deps.pop("tensor", None)
        idx = len(self.ops[eng])
        for k, v in deps.items():
            if isinstance(k, str) and k in self.ops:
                self.ops[k][v].inc = True
        self.ops[eng].append(Op(fn, deps))
        self._update((eng, idx), reads, writes)

    def dma(self, eng, out, in_, reads=(), writes=(), semtile=None):
        reads = self._norm(reads)
        writes = self._norm(writes)
        deps = self._collect(reads, writes)
        for k, v in deps.items():
            if isinstance(k, str) and k in self.ops:
                self.ops[k][v].inc = True
        if semtile.dsem is None:
            semtile.dsem = ("D", semtile.name)
            self.dsems[semtile.dsem] = None
        semtile.dcount = self.dtotal.get(semtile.dsem, 0) + 16
        self.dtotal[semtile.dsem] = semtile.dcount
        ev = (semtile.dsem, semtile.dcount)
        self.ops[eng].append(Op(None, deps, dma=(out, in_, semtile.dsem)))
        self._update(ev, reads, writes)

    def barrier(self):
        deps = {}
        for e in self.ENG:
            i = self._last_real(e)
            if i is not None:
                deps[e] = i
                self.ops[e][i].inc = True
        for k, v in self.dtotal.items():
            deps[k] = v
        for e in self.ENG:
            d = dict(deps)
            self.ops[e].append(Op("nop", d))
        for t in self.tiles:
            t.st = {}

    def finish(self, eng="sync"):
        deps = dict(self.dtotal)
        for e in self.ENG:
            i = self._last_real(e)
            if i is not None and e != eng:
                deps[e] = i
                self.ops[e][i].inc = True
        self.ops[eng].append(Op("nop", deps))

    def _last_real(self, e):
        for i in range(len(self.ops[e]) - 1, -1, -1):
            if self.ops[e][i].dma is None:
                return i
        return None

    def emit(self, stack):
        nc = self.nc
        esem = {e: stack.enter_context(nc.semaphore("es_" + e)) for e in self.ENG}
        for k in self.dsems:
            self.dsems[k] = stack.enter_context(nc.semaphore("ds_" + k[1]))
        cnt = {}
        for e in self.ENG:
            c = 0
            lst = []
            for o in self.ops[e]:
                if o.inc and o.dma is None:
                    c += 1
                lst.append(c)
            cnt[e] = lst
        prog = self

        def run(ename, eng):
            waited = {}
            for o in prog.ops[ename]:
                for k, v in o.deps.items():
                    if isinstance(k, str):
                        sem = esem[k]
                        val = cnt[k][v]
                    else:
                        sem = prog.dsems[k]
                        val = v
                    if waited.get(k, 0) >= val:
                        continue
                    waited[k] = val
                    eng.wait_ge(sem, val)
                if o.dma is not None:
                    out, in_, dk = o.dma
                    eng.dma_start(out=out, in_=in_).then_inc(prog.dsems[dk], 16)
                    continue
                if o.fn == "nop":
                    if o.inc:
                        eng.nop().then_inc(esem[ename], 1)
                    continue
                ins = o.fn(eng)
                if o.inc:
                    ins.then_inc(esem[ename], 1)

        stack.enter_context(nc.allow_non_contiguous_dma("tiny strided parameter loads"))
        block = stack.enter_context(nc.Block())

        @block.sync
        def _(e):
            run("sync", e)

        @block.scalar
        def _(e):
            run("scalar", e)

        @block.vector
        def _(e):
            run("vector", e)

        @block.gpsimd
        def _(e):
            run("gpsimd", e)

        @block.tensor
        def _(e):
            run("tensor", e)


def bcast(ap, shape, axis):
    return ap.unsqueeze(axis).to_broadcast(list(shape))


class K:
    def __init__(self, layers=(0, 1), phases="ABC", dbg=()):
        self.layers = layers
        self.phases = phases
        self.dbg = dbg


def build(layers=(0, 1), phases="ABC", dbg=(), sub="GML"):
    from contextlib import ExitStack
    nc = bass.Bass("TRN2", target_bir_lowering=False)
    P = Prog(nc)
    stack = ExitStack()

    def din(name, shape, dt=F32):
        return nc.dram_tensor(name, list(shape), dt, kind="ExternalInput").ap()

    x_d = din("x", [S, D])
    mem_d = din("mem", [256, D])
    pos_d = din("positions", [1, S], I32)
    w = {}
    for name, shape in [
        ("w_in", [2, D, IN_W]), ("w_out", [2, D, D]), ("gla_w_a2", [2, 16, 256]), ("gla_b_a", [2, 256]),
        ("gla_norm_g", [2, 256]), ("ml_conv_w", [2, 4, 512]), ("ml_b_i", [2, 4]), ("ml_b_f", [2, 4]),
        ("ml_norm_g", [2, 256]), ("mla_q_norm_g", [2, 256]), ("mla_w_uq", [2, 256, 768]),
        ("mla_kv_norm_g", [2, 128]), ("mla_w_ukv", [2, 128, 1024]), ("xa_w_q", [2, D, D]),
        ("xa_w_kv", [2, D, 2 * D]), ("xa_w_o", [2, D, D]), ("moe_w_group", [2, D, 4]), ("moe_b_group", [2, 4]),
        ("moe_w_router", [2, D, 32]), ("moe_b_router", [2, 32]), ("moe_w_gate", [2, 32, D, 256]),
        ("moe_w_up", [2, 32, D, 256]), ("moe_w_down", [2, 32, 256, D]),
        ("ln1_g", [2, D]), ("ln1_b", [2, D]), ("ln2_g", [2, D]), ("ln2_b", [2, D]), ("ln3_g", [2, D]), ("ln3_b", [2, D]),
    ]:
        w[name] = din(name, shape)
    cmat_d = din("cmat", [128, 5, 128])
    sel_d = din("sel", [32, 32, 128])
    ropeinv_d = din("ropeinv", [96, 1])
    out_d = nc.dram_tensor("out", [S, D], F32, kind="ExternalOutput").ap()
    dbg_d = {}
    for name, shape in dbg:
        dbg_d[name] = nc.dram_tensor(name, list(shape), F32, kind="ExternalOutput").ap()

    def sb(name, shape, dt=F32):
        return P.tile(name, stack.enter_context(nc.sbuf_tensor(name, list(shape), dt)))

    x_tm = sb("x_tm", [128, NT, D])
    xT = sb("xT", [128, 8, S], BF16)
    cmat = sb("cmat_sb", [128, 5, 128])
    cmat_bf = sb("cmat_bf", [128, 5, 128], BF16)
    lnp = sb("lnp", [128, 2, D])
    ps = [P.tile("ps%d" % i, stack.enter_context(nc.psum_tensor("ps%d" % i, [128, 512], F32))) for i in range(8)]
    for p_ in ps:
        p_.psum = True
    IDENT, TRII, TRIS, MMLA, ONES = range(5)

    def mm(out, lhsT, rhs, start, stop, reads, writes):
        P.op("tensor", lambda e: e.matmul(out, lhsT, rhs, start=start, stop=stop), reads, writes)

    def tr(out, in_, ident, reads, writes):
        P.op("tensor", lambda e: e.transpose(out, in_, ident), reads, writes)

    def act(out, in_, func, reads, writes, bias=None, scale=None, accum_out=None):
        kw = {}
        if bias is not None:
            kw["bias"] = bias
        if scale is not None:
            kw["scale"] = scale
        if accum_out is not None:
            kw["accum_out"] = accum_out
        P.op("scalar", lambda e: e.activation(out, in_, func, **kw), reads, writes)

    def tt(out, a, b, op, reads, writes, eng="vector"):
        P.op(eng, lambda e: e.tensor_tensor(out, a, b, op), reads, writes)

    def ts(out, a, s1, op0, reads, writes, s2=None, op1=None, eng="vector"):
        if op1 is None:
            P.op(eng, lambda e: e.tensor_scalar(out, a, s1, None, op0), reads, writes)
        else:
            P.op(eng, lambda e: e.tensor_scalar(out, a, s1, s2, op0, op1), reads, writes)

    def stt(out, a, s, b, op0, op1, reads, writes):
        P.op("vector", lambda e: e.scalar_tensor_tensor(out, a, s, b, op0, op1), reads, writes)

    def cp(out, in_, reads, writes, eng="vector"):
        if eng == "scalar":
            P.op("scalar", lambda e: e.copy(out, in_), reads, writes)
        else:
            P.op(eng, lambda e: e.tensor_copy(out, in_), reads, writes)

    def red(out, in_, op, reads, writes, axis=AX.X):
        P.op("vector", lambda e: e.tensor_reduce(out, in_, axis, op), reads, writes)

    def load_cast(dst_tile, dst_ap, src_ap, sub=None):
        P.dma("gpsimd", dst_ap, src_ap, writes=[(dst_tile, sub)], semtile=dst_tile)

    def load(dst_tile, dst_ap, src_ap, sub=None, eng="sync"):
        P.dma(eng, dst_ap, src_ap, writes=[(dst_tile, sub)], semtile=dst_tile)

    load(cmat, cmat[:], cmat_d)
    cp(cmat_bf[:], cmat[:], [cmat], [cmat_bf])
    for t in range(NT):
        load(x_tm, x_tm[:, t, :], x_d[t * 128:(t + 1) * 128, :], sub=t, eng="sync" if t % 2 == 0 else "scalar")

    memT = sb("memT", [128, 8, 256], BF16)
    if "B" in phases:
        from contextlib import ExitStack as _ES
        pre = _ES()
        mem_f = P.tile("mem_f", pre.enter_context(nc.sbuf_tensor("mem_f", [128, 2, D], F32)))
        mem_b = P.tile("mem_b", pre.enter_context(nc.sbuf_tensor("mem_b", [128, 2, D], BF16)))
        load(mem_f, mem_f[:], mem_d.rearrange("(t p) d -> p t d", p=128))
        cp(mem_b[:], mem_f[:], [mem_f], [mem_b])
        pbm = ps[7].h.bitcast(BF16)
        for mt in range(2):
            for c8 in range(8):
                tr(pbm[:, c8 * 128:(c8 + 1) * 128], mem_b[:, mt, c8 * 128:(c8 + 1) * 128], cmat_bf[:, 0, :],
                   [mem_b, cmat_bf], [(ps[7], c8)])
            cp(memT[:, :, mt * 128:(mt + 1) * 128], pbm[:, :].rearrange("p (c n) -> p c n", c=8), [ps[7]], [(memT, mt)])
        P.barrier()
        pre.close()

    cs = None
    if "A" in phases and "L" in sub:
        import math
        from contextlib import ExitStack as _ES2
        cs = sb("rope_cs", [96, 2, S], BF16)
        pre2 = _ES2()

        def tmp(name, dt=F32):
            return P.tile(name, pre2.enter_context(nc.sbuf_tensor(name, [96, S], dt)))
        posi, ang, rr, kf, ki, mk = tmp("rp_posi", I32), tmp("rp_ang"), tmp("rp_r"), tmp("rp_kf"), tmp("rp_ki", I32), tmp("rp_m")
        rinv = P.tile("rp_inv", pre2.enter_context(nc.sbuf_tensor("rp_inv", [96, 1], F32)))
        R_ = slice(64, 96)
        load(rinv, rinv[:], ropeinv_d)
        P.dma("sync", posi[R_, :].unsqueeze(1), pos_d[0:1, :].partition_broadcast(32), writes=[posi], semtile=posi)
        cp(ang[R_, :], posi[R_, :], [posi], [ang])
        ts(ang[R_, :], ang[R_, :], rinv[R_, 0:1], ALU.mult, [ang, rinv], [ang])
        TWO_PI = 2.0 * math.pi
        C1 = 6.28125
        C2 = TWO_PI - C1
        for which, shift in ((1, 0.0), (0, math.pi / 2)):
            ts(rr[R_, :], ang[R_, :], shift, ALU.add, [ang], [rr])
            ts(kf[R_, :], rr[R_, :], 1.0 / TWO_PI, ALU.mult, [rr], [kf])
            cp(ki[R_, :], kf[R_, :], [kf], [ki])
            cp(kf[R_, :], ki[R_, :], [ki], [kf])
            stt(rr[R_, :], kf[R_, :], -C1, rr[R_, :], ALU.mult, ALU.add, [kf, rr], [rr])
            stt(rr[R_, :], kf[R_, :], -C2, rr[R_, :], ALU.mult, ALU.add, [kf, rr], [rr])
            ts(mk[R_, :], rr[R_, :], math.pi, ALU.is_gt, [rr], [mk])
            stt(rr[R_, :], mk[R_, :], -TWO_PI, rr[R_, :], ALU.mult, ALU.add, [mk, rr], [rr])
            ts(mk[R_, :], rr[R_, :], -math.pi, ALU.is_lt, [rr], [mk])
            stt(rr[R_, :], mk[R_, :], TWO_PI, rr[R_, :], ALU.mult, ALU.add, [mk, rr], [rr])
            ts(rr[R_, :], rr[R_, :], 3.141592, ALU.min, [rr], [rr], s2=-3.141592, op1=ALU.max)
            act(cs[R_, which, :], rr[R_, :], AF.Sin, [rr], [(cs, which)])
        P.barrier()
        pre2.close()

    lnw = sb("ln_work", [128, 16])
    xbf = sb("ln_xbf", [128, 2, D], BF16)
    ps_bf = [ps[i].h.bitcast(BF16) for i in range(8)]

    def load_ln(gname, bname, l):
        load(lnp, lnp[:, 0, :].unsqueeze(1), w[gname][l:l + 1, :].partition_broadcast(128), sub=0)
        load(lnp, lnp[:, 1, :].unsqueeze(1), w[bname][l:l + 1, :].partition_broadcast(128), sub=1, eng="scalar")

    def layer_norm_tile(t, pbank):
        xt = x_tm[:, t, :]
        st = lnw[:, 0:12].rearrange("p (a b) -> p a b", a=2)
        for hh in range(2):
            P.op("vector", lambda e, hh=hh: e.bn_stats(st[:, hh, :], x_tm[:, t, hh * 512:(hh + 1) * 512]),
                 [(x_tm, t)], [(lnw, "st%d" % hh)])
        P.op("vector", lambda e: e.bn_aggr(lnw[:, 12:14], lnw[:, 0:12]), [(lnw, "st0"), (lnw, "st1")], [(lnw, "mv")])
        ts(lnw[:, 14:15], lnw[:, 13:14], LN_EPS, ALU.add, [(lnw, "mv")], [(lnw, "sd")])
        act(lnw[:, 14:15], lnw[:, 14:15], AF.Sqrt, [(lnw, "sd")], [(lnw, "sd")])
        P.op("vector", lambda e: e.reciprocal(lnw[:, 15:16], lnw[:, 14:15]), [(lnw, "sd")], [(lnw, "rs")])
        ts(xt, xt, lnw[:, 12:13], ALU.subtract, [(x_tm, t), (lnw, "mv"), (lnw, "rs")], [(x_tm, t)],
           s2=lnw[:, 15:16], op1=ALU.mult)
        tt(xt, xt, lnp[:, 0, :], ALU.mult, [(x_tm, t), (lnp, 0)], [(x_tm, t)])
        tt(xt, xt, lnp[:, 1, :], ALU.add, [(x_tm, t), (lnp, 1)], [(x_tm, t)])
        refresh_xT(t, pbank)

    def refresh_xT(t, pbank):
        xt = x_tm[:, t, :]
        b = t % 2
        cp(xbf[:, b, :], xt, [(x_tm, t)], [(xbf, b)], eng="scalar")
        pb = ps_bf[pbank]
        for c in range(8):
            tr(pb[:, c * 128:(c + 1) * 128], xbf[:, b, c * 128:(c + 1) * 128], cmat_bf[:, IDENT, :],
               [(xbf, b), cmat_bf], [(ps[pbank], c)])
        cp(xT[:, :, t * 128:(t + 1) * 128], pb[:, :].rearrange("p (c n) -> p c n", c=8),
           [ps[pbank]], [(xT, t)])

    def store_out():
        for t in range(NT):
            P.dma("sync" if t % 2 == 0 else "scalar", out_d[t * 128:(t + 1) * 128, :], x_tm[:, t, :],
                  reads=[(x_tm, t)], semtile=x_tm)

    ctx = dict(nc=nc, P=P, stack=stack, w=w, x_tm=x_tm, xT=xT, cmat=cmat, cmat_bf=cmat_bf, lnp=lnp, ps=ps,
               ps_bf=ps_bf, sb=sb, mm=mm, tr=tr, act=act, tt=tt, ts=ts, stt=stt, cp=cp, red=red,
               load=load, load_cast=load_cast, load_ln=load_ln, layer_norm_tile=layer_norm_tile,
               sel_d=sel_d, ropeinv_d=ropeinv_d, memT=memT, sub=sub, cs=cs, mem_d=mem_d, pos_d=pos_d, dbg_d=dbg_d)

    first = True
    for l in layers:
        if first:
            for t in range(NT):
                refresh_xT(t, 5 + t % 3)
        if "A" in phases:
            phase_A(ctx, l)
        if "B" in phases:
            phase_B(ctx, l)
        if "C" in phases:
            phase_C(ctx, l)
        first = False
    store_out()
    P.finish("sync")
    P.emit(stack)
    stack.close()
    return nc


def phase_C(c, l):
    from contextlib import ExitStack
    nc, P, w = c["nc"], c["P"], c["w"]
    x_tm, xT, ps, cmat, cmat_bf = c["x_tm"], c["xT"], c["ps"], c["cmat"], c["cmat_bf"]
    mm, tr, act, tt, ts, stt, cp, red = c["mm"], c["tr"], c["act"], c["tt"], c["ts"], c["stt"], c["cp"], c["red"]
    load, load_cast = c["load"], c["load_cast"]
    IDENT = 0
    ph = ExitStack()

    def sb(name, shape, dt=F32):
        return P.tile(name, ph.enter_context(nc.sbuf_tensor("%s_%d" % (name, l), list(shape), dt)))

    c["load_ln"]("ln3_g", "ln3_b", l)
    gateT = sb("c_gateT", [32, S], BF16)
    sel = sb("c_sel", [32, 32, 128], BF16)
    load_cast(sel, sel[:], c["sel_d"])
    ph_r = ExitStack()
    _sb_outer = sb

    def sb(name, shape, dt=F32):
        return P.tile(name, ph_r.enter_context(nc.sbuf_tensor("%s_%d" % (name, l), list(shape), dt)))
    wr = sb("c_wr", [128, 8, 36], BF16)
    load_cast(wr, wr[:, :, 0:4], w["moe_w_group"][l].rearrange("(kc p) n -> p kc n", p=128), sub="g")
    load_cast(wr, wr[:, :, 4:36], w["moe_w_router"][l].rearrange("(kc p) n -> p kc n", p=128), sub="r")
    rb = sb("c_rb", [128, 36])
    load(rb, rb[:, 0:4].unsqueeze(1), w["moe_b_group"][l:l + 1, :].partition_broadcast(128), sub="g")
    load(rb, rb[:, 4:36].unsqueeze(1), w["moe_b_router"][l:l + 1, :].partition_broadcast(128), sub="r")
    lg = sb("c_lg", [128, NT, 36])
    for half in range(2):
        pr = ps[half]
        for tl in range(8):
            t = half * 8 + tl
            for kc in range(8):
                mm(pr[:, tl * 36:(tl + 1) * 36], xT[:, kc, t * 128:(t + 1) * 128], wr[:, kc, :], kc == 0, kc == 7,
                   [(xT, t), wr], [(pr, tl)])
        tt(lg[:, half * 8:(half + 1) * 8, :], pr[:, 0:288].rearrange("p (t n) -> p t n", t=8),
           bcast(rb[:, :], [128, 8, 36], 1), ALU.add, [pr, rb], [(lg, half)])
    r1 = sb("c_r1", [128, NT, 64])
    lgg = lg[:, :, 0:4]
    lge = lg[:, :, 4:36].rearrange("p t (g e) -> p t g e", g=4)
    gmax, gsum, ohg, eg = r1[:, :, 0], r1[:, :, 1], r1[:, :, 4:8], r1[:, :, 8:12]
    red(gmax, lgg, ALU.max, [lg], [(r1, "gmax")])
    tt(eg, lgg, bcast(gmax, [128, NT, 4], 2), ALU.subtract, [lg, (r1, "gmax")], [(r1, "eg")])
    tt(ohg, lgg, bcast(gmax, [128, NT, 4], 2), ALU.is_equal, [lg, (r1, "gmax")], [(r1, "ohg")])
    act(eg, eg, AF.Exp, [(r1, "eg")], [(r1, "eg")])
    red(gsum, eg, ALU.add, [(r1, "eg")], [(r1, "gsum")])
    gp = r1[:, :, 2]
    P.op("vector", lambda e: e.reciprocal(gp, gsum), [(r1, "gsum")], [(r1, "gp")])
    tmp = sb("c_tmp", [128, NT, 4, 8])
    tt(tmp[:], lge, bcast(ohg, [128, NT, 4, 8], 3), ALU.mult, [lg, (r1, "ohg")], [tmp])
    esel = r1[:, :, 16:24]
    red(esel, tmp[:].rearrange("p t g e -> p t e g"), ALU.add, [tmp], [(r1, "esel")])
    m1, m2, dd = r1[:, :, 3], r1[:, :, 12], r1[:, :, 13]
    mk1, mk2, e2 = r1[:, :, 24:32], r1[:, :, 32:40], r1[:, :, 40:48]
    red(m1, esel, ALU.max, [(r1, "esel")], [(r1, "m1")])
    tt(mk1, esel, bcast(m1, [128, NT, 8], 2), ALU.is_equal, [(r1, "esel"), (r1, "m1")], [(r1, "mk1")])
    stt(e2, mk1, -1e30, esel, ALU.mult, ALU.add, [(r1, "mk1"), (r1, "esel")], [(r1, "e2")])
    red(m2, e2, ALU.max, [(r1, "e2")], [(r1, "m2")])
    tt(mk2, e2, bcast(m2, [128, NT, 8], 2), ALU.is_equal, [(r1, "e2"), (r1, "m2")], [(r1, "mk2")])
    tt(dd, m2, m1, ALU.subtract, [(r1, "m1"), (r1, "m2")], [(r1, "dd")])
    act(dd, dd, AF.Exp, [(r1, "dd")], [(r1, "dd")])
    w1, w2 = r1[:, :, 14], r1[:, :, 15]
    ts(w1, dd, 1.0, ALU.add, [(r1, "dd")], [(r1, "w1")])
    P.op("vector", lambda e: e.reciprocal(w1, w1), [(r1, "w1")], [(r1, "w1")])
    tt(w1, w1, gp, ALU.mult, [(r1, "w1"), (r1, "gp")], [(r1, "w1")])
    tt(w2, w1, dd, ALU.mult, [(r1, "w1"), (r1, "dd")], [(r1, "w2")])
    comb = r1[:, :, 48:56]
    tt(comb, mk1, bcast(w1, [128, NT, 8], 2), ALU.mult, [(r1, "mk1"), (r1, "w1")], [(r1, "comb")])
    tt(mk2, mk2, bcast(w2, [128, NT, 8], 2), ALU.mult, [(r1, "mk2"), (r1, "w2")], [(r1, "mk2")])
    tt(comb, comb, mk2, ALU.add, [(r1, "comb"), (r1, "mk2")], [(r1, "comb")])
    gate = sb("c_gate", [128, NT, 4, 8])
    tt(gate[:], bcast(ohg, [128, NT, 4, 8], 3), bcast(comb, [128, NT, 4, 8], 2), ALU.mult,
       [(r1, "ohg"), (r1, "comb")], [gate])
    for g in range(4):
        pg = ps[2 + g % 2]
        for tl in range(4):
            t = g * 4 + tl
            tr(pg[0:32, tl * 128:(tl + 1) * 128], gate[:, t, :, :].rearrange("p g e -> p (g e)"), cmat[:, IDENT, :],
               [gate, cmat], [(pg, tl)])
        cp(gateT[:, g * 512:(g + 1) * 512], pg[0:32, :], [pg], [(gateT, g)], eng="scalar")
    if "c_gate" in c["dbg_d"]:
        P.dma("sync", c["dbg_d"]["c_gate"].rearrange("(t p) n -> p t n", p=128),
              gate[:].rearrange("p t g e -> p t (g e)"), reads=[gate], semtile=gate)

    P.barrier()
    ph_r.close()
    sb = _sb_outer
    NSLOT = 4
    wg = sb("c_wg", [128, NSLOT, 8, 256], BF16)
    wu = sb("c_wu", [128, NSLOT, 8, 256], BF16)
    wd = sb("c_wd", [128, NSLOT, 2, D], BF16)
    wsem = [sb("c_wsem%d" % i, [1, 1]) for i in range(NSLOT)]
    hT = sb("c_hT", [128, 2, 2, S], BF16)
    sg = sb("c_sg", [128, 2, 512], BF16)
    gb = sb("c_gb", [128, 2, 512], BF16)

    def load_expert(e):
        s = e % NSLOT
        P.dma("gpsimd", wg[:, s, :, :], w["moe_w_gate"][l, e].rearrange("(kc p) n -> p kc n", p=128),
              writes=[(wg, s)], semtile=wsem[s])
        P.dma("gpsimd", wu[:, s, :, :], w["moe_w_up"][l, e].rearrange("(kc p) n -> p kc n", p=128),
              writes=[(wu, s)], semtile=wsem[s])
        P.dma("gpsimd", wd[:, s, :, :], w["moe_w_down"][l, e].rearrange("(kc p) n -> p kc n", p=128),
              writes=[(wd, s)], semtile=wsem[s])

    for e in range(2):
        load_expert(e)
    unit = 0
    for blk in range(16):
        for ei in range(2):
            e = blk * 2 + ei
            s = e % NSLOT
            if e + 2 < 32:
                load_expert(e + 2)
            for g in range(4):
                tok = slice(g * 512, (g + 1) * 512)
                pgb = ps[4]
                mm(pgb[:, :], sel[:, e, :], gateT[:, tok], True, True, [sel, (gateT, g)], [pgb])
                ub = (e * 4 + g) % 2
                cp(gb[:, ub, :], pgb[:, :], [pgb], [(gb, ub)], eng="scalar")
                for fc in range(2):
                    pgt, put = ps[(unit % 2) * 2], ps[(unit % 2) * 2 + 1]
                    for kc in range(8):
                        mm(pgt[:, :], wg[:, s, kc, fc * 128:(fc + 1) * 128], xT[:, kc, tok], kc == 0, kc == 7,
                           [(wg, s), xT], [pgt])
                    for kc in range(8):
                        mm(put[:, :], wu[:, s, kc, fc * 128:(fc + 1) * 128], xT[:, kc, tok], kc == 0, kc == 7,
                           [(wu, s), xT], [put])
                    u2 = unit % 2
                    act(sg[:, u2, :], pgt[:, :], AF.Silu, [pgt], [(sg, u2)])
                    tt(sg[:, u2, :], put[:, :], sg[:, u2, :], ALU.mult, [put, (sg, u2)], [(sg, u2)])
                    tt(hT[:, ei, fc, tok], sg[:, u2, :], gb[:, ub, :], ALU.mult, [(sg, u2), (gb, ub)],
                       [(hT, (ei, g))])
                    unit += 1
        for t in range(NT):
            g = t // 4
            for hf in range(2):
                po = ps[5 + (t * 2 + hf) % 3]
                k = 0
                for ei in range(2):
                    s = (blk * 2 + ei) % NSLOT
                    for fc in range(2):
                        mm(po[:, :], hT[:, ei, fc, t * 128:(t + 1) * 128], wd[:, s, fc, hf * 512:(hf + 1) * 512],
                           k == 0, k == 3, [(hT, (ei, g)), (wd, s)], [po])
                        k += 1
                xs = x_tm[:, t, hf * 512:(hf + 1) * 512]
                if blk == 0:
                    stt(xs, xs, ALPHA, po[:, :], ALU.mult, ALU.add, [(x_tm, t), po], [(x_tm, t)])
                else:
                    tt(xs, xs, po[:, :], ALU.add, [(x_tm, t), po], [(x_tm, t)])
    for t in range(NT):
        c["layer_norm_tile"](t, 5 + t % 3)
    P.barrier()
    ph.close()


def host_consts():
    cm = np.zeros((128, 5, 128), np.float32)
    i = np.arange(128)
    cm[:, 0, :] = np.eye(128, dtype=np.float32)
    cm[:, 1, :] = (i[:, None] <= i[None, :]).astype(np.float32)
    cm[:, 2, :] = (i[:, None] > i[None, :]).astype(np.float32)
    cm[:, 3, :] = ((i[:, None] // 64) <= (i[None, :] // 64)).astype(np.float32)
    cm[:, 4, :] = 1.0
    sel = np.zeros((32, 32, 128), np.float32)
    for e in range(32):
        sel[e, e, :] = 1.0
    inv = (10000.0 ** (-np.arange(16, dtype=np.float32) / 16)).astype(np.float32)
    ri = np.zeros((96, 1), np.float32)
    ri[64:80, 0] = inv
    ri[80:96, 0] = inv
    return {"cmat": cm, "sel": sel, "ropeinv": ri}


_NC_CACHE = {}


def run_cores(inputs, n_cores=8, layers=(0, 1), phases="ABC", dbg=(), sub="GML"):
    key = (tuple(layers), phases, tuple(dbg), sub)
    if key not in _NC_CACHE:
        _NC_CACHE[key] = build(layers, phases, dbg, sub)
    nc = _NC_CACHE[key]
    consts = host_consts()
    shared = {k: np.ascontiguousarray(v) for k, v in inputs.items() if k not in ("x", "mem", "positions")}
    shared.update(consts)
    in_maps = []
    for b in range(n_cores):
        m = dict(shared)
        m["x"] = np.ascontiguousarray(inputs["x"][b])
        m["mem"] = np.ascontiguousarray(inputs["mem"][b])
        m["positions"] = np.ascontiguousarray(inputs["positions"][b:b + 1]).astype(np.int32)
        in_maps.append(m)
    res = run_bass_kernel_spmd(nc, in_maps, core_ids=list(range(n_cores)))
    return res.results


def kernel(**inputs):
    inputs = {k: np.asarray(v) for k, v in inputs.items()}
    res = run_cores(inputs, 8)
    return np.stack([r["out"] for r in res], axis=0).astype(np.float32)


def phase_B(c, l):
    from contextlib import ExitStack
    nc, P, w = c["nc"], c["P"], c["w"]
    x_tm, xT, ps, cmat_bf, memT = c["x_tm"], c["xT"], c["ps"], c["cmat_bf"], c["memT"]
    mm, act, tt, stt, cp = c["mm"], c["act"], c["tt"], c["stt"], c["cp"]
    load_cast = c["load_cast"]
    ONES = 4
    ph = ExitStack()

    def sb(name, shape, dt=F32):
        return P.tile(name, ph.enter_context(nc.sbuf_tensor("%s_%d" % (name, l), list(shape), dt)))

    c["load_ln"]("ln2_g", "ln2_b", l)
    kT = sb("b_kT", [128, 8, 256], BF16)
    vx = sb("b_v", [128, 2, D], BF16)
    ph2 = ExitStack()
    wkv = P.tile("b_wkv", ph2.enter_context(nc.sbuf_tensor("b_wkv_%d" % l, [128, 8, 2 * D], BF16)))
    for kc in range(8):
        load_cast(wkv, wkv[:, kc, :], w["xa_w_kv"][l, kc * 128:(kc + 1) * 128, :], sub=kc)
    for cc in range(8):
        pk = ps[cc % 2]
        for kc in range(8):
            mm(pk[:, 0:256], wkv[:, kc, cc * 128:(cc + 1) * 128], memT[:, kc, :], kc == 0, kc == 7, [wkv, memT], [pk])
        cp(kT[:, cc, :], pk[:, 0:256], [pk], [(kT, cc)], eng="scalar" if cc % 2 else "vector")
    for mt in range(2):
        for hf in range(2):
            pv = ps[2 + hf]
            for kc in range(8):
                mm(pv[:, :], memT[:, kc, mt * 128:(mt + 1) * 128], wkv[:, kc, D + hf * 512:D + (hf + 1) * 512],
                   kc == 0, kc == 7, [wkv, memT], [pv])
            cp(vx[:, mt, hf * 512:(hf + 1) * 512], pv[:, :], [pv], [(vx, (mt, hf))], eng="scalar" if hf else "vector")
    P.barrier()
    ph2.close()
    wq = sb("b_wq", [128, 8, D], BF16)
    wo = sb("b_wo", [128, 8, D], BF16)
    for kc in range(0, 8, 2):
        load_cast(wq, wq[:, kc:kc + 2, :], w["xa_w_q"][l, kc * 128:(kc + 2) * 128, :].rearrange("(k p) n -> p k n", p=128), sub=kc)
    for kc in range(0, 8, 2):
        load_cast(wo, wo[:, kc:kc + 2, :], w["xa_w_o"][l, kc * 128:(kc + 2) * 128, :].rearrange("(k p) n -> p k n", p=128), sub=kc)
    qT = sb("b_qT", [128, 8, 512], BF16)
    xaT = sb("b_xaT", [128, 8, 512], BF16)
    PT = sb("b_PT", [128, 2, 512], BF16)
    rden = sb("b_rden", [128, 2, 512])
    scale = 256 ** -0.5
    for g in range(4):
        tok = slice(g * 512, (g + 1) * 512)
        for cc in range(8):
            pq = ps[cc % 2]
            for kc in range(8):
                mm(pq[:, :], wq[:, kc, cc * 128:(cc + 1) * 128], xT[:, kc, tok], kc == 0, kc == 7, [wq, xT], [pq])
            cp(qT[:, cc, :], pq[:, :], [pq], [(qT, cc)], eng="scalar" if cc % 2 else "vector")
        for h in range(4):
            for mt in range(2):
                pst = ps[2 + mt]
                for j in range(2):
                    mm(pst[:, :], kT[:, h * 2 + j, mt * 128:(mt + 1) * 128], qT[:, h * 2 + j, :], j == 0, j == 1,
                       [(kT, h * 2 + j), (qT, h * 2 + j)], [pst])
                act(PT[:, mt, :], pst[:, :], AF.Exp, [pst], [(PT, mt)], scale=scale)
            pden = ps[4]
            for mt in range(2):
                mm(pden[:, :], cmat_bf[:, ONES, :], PT[:, mt, :], mt == 0, mt == 1, [cmat_bf, (PT, mt)], [pden])
            rb = h % 2
            P.op("vector", lambda e, rb=rb, pden=pden: e.reciprocal(rden[:, rb, :], pden[:, :]), [pden], [(rden, rb)])
            for j in range(2):
                po = ps[5 + j]
                for mt in range(2):
                    mm(po[:, :], vx[:, mt, h * 256 + j * 128:h * 256 + (j + 1) * 128], PT[:, mt, :], mt == 0, mt == 1,
                       [vx, (PT, mt)], [po])
                tt(xaT[:, h * 2 + j, :], po[:, :], rden[:, rb, :], ALU.mult, [po, (rden, rb)], [(xaT, h * 2 + j)])
        for tl in range(4):
            t = g * 4 + tl
            for hf in range(2):
                pp = ps[hf]
                for cc in range(8):
                    mm(pp[:, :], xaT[:, cc, tl * 128:(tl + 1) * 128], wo[:, cc, hf * 512:(hf + 1) * 512], cc == 0, cc == 7,
                       [xaT, wo], [pp])
                xs = x_tm[:, t, hf * 512:(hf + 1) * 512]
                stt(xs, xs, ALPHA, pp[:, :], ALU.mult, ALU.add, [(x_tm, t), pp], [(x_tm, t)])
            c["layer_norm_tile"](t, 7)
    P.barrier()
    ph.close()


def head_norm_gate(c, sbf, name, src, gs, out_bf, b, keyp):
    P, tt, ts, red, act = c["P"], c["tt"], c["ts"], c["red"], c["act"]
    st = sbf["hn_st"]
    cen = sbf["hn_cen"]
    sq = sbf["hn_sq"]
    s4 = src.rearrange("p (h e) -> p h e", h=4)
    mean = st[:, b, 0:4]
    var = st[:, b, 4:8]
    red(mean, s4, ALU.add, [keyp], [(st, (b, "m"))])
    ts(mean, mean, -1.0 / 64, ALU.mult, [(st, (b, "m"))], [(st, (b, "m"))])
    c4 = cen[:, b, :].rearrange("p (h e) -> p h e", h=4)
    tt(c4, s4, bcast(mean, [128, 4, 64], 2), ALU.add, [keyp, (st, (b, "m"))], [(cen, b)])
    tt(sq[:, b, :], cen[:, b, :], cen[:, b, :], ALU.mult, [(cen, b)], [(sq, b)])
    red(var, sq[:, b, :].rearrange("p (h e) -> p h e", h=4), ALU.add, [(sq, b)], [(st, (b, "v"))])
    ts(var, var, 1.0 / 64, ALU.mult, [(st, (b, "v"))], [(st, (b, "v"))], s2=LN_EPS, op1=ALU.add)
    act(var, var, AF.Ln, [(st, (b, "v"))], [(st, (b, "v"))])
    act(var, var, AF.Exp, [(st, (b, "v"))], [(st, (b, "v"))], scale=-0.5)
    tt(c4, c4, bcast(var, [128, 4, 64], 2), ALU.mult, [(cen, b), (st, (b, "v"))], [(cen, b)])
    tt(out_bf, cen[:, b, :], gs, ALU.mult, [(cen, b), (sbf["gs"], b)], [(sbf["obf"], b)])


def phase_A(c, l):
    from contextlib import ExitStack
    nc, P, w = c["nc"], c["P"], c["w"]
    sub = c.get("sub", "GML")
    c["first_mixer"] = True
    if "G" in sub:
        mixer_gla(c, l)
        c["first_mixer"] = False
    if "M" in sub:
        mixer_mlstm(c, l)
        c["first_mixer"] = False
    if "L" in sub:
        mixer_mla(c, l)
    c["load_ln"]("ln1_g", "ln1_b", l)
    for t in range(NT):
        c["layer_norm_tile"](t, 5 + t % 3)
    P.barrier()


def mixer_gla(c, l):
    from contextlib import ExitStack
    nc, P, w = c["nc"], c["P"], c["w"]
    x_tm, xT, ps, ps_bf, cmat, cmat_bf = c["x_tm"], c["xT"], c["ps"], c["ps_bf"], c["cmat"], c["cmat_bf"]
    mm, tr, act, tt, ts, stt, cp, red = c["mm"], c["tr"], c["act"], c["tt"], c["ts"], c["stt"], c["cp"], c["red"]
    load, load_cast = c["load"], c["load_cast"]
    IDENT, TRII, TRIS = 0, 1, 2
    ph = ExitStack()

    def sb(name, shape, dt=F32):
        return P.tile(name, ph.enter_context(nc.sbuf_tensor("%s_%d" % (name, l), list(shape), dt)))

    win = sb("g_win", [128, 8, 1040], BF16)
    for kc in range(8):
        load_cast(win, win[:, kc, :], w["w_in"][l, kc * 128:(kc + 1) * 128, 0:1040], sub=kc)
    wa2 = sb("g_wa2", [16, 256], BF16)
    load_cast(wa2, wa2[0:16, :], w["gla_w_a2"][l])
    babc = sb("g_babc", [128, 256])
    load(babc, babc[:].unsqueeze(1), w["gla_b_a"][l:l + 1, :].partition_broadcast(128))
    wout = sb("g_wout", [128, 2, D], BF16)
    load_cast(wout, wout[:], w["w_out"][l, 0:256, :].rearrange("(k p) n -> p k n", p=128))
    gng = sb("g_gng", [128, 256])
    load(gng, gng[:].unsqueeze(1), w["gla_norm_g"][l:l + 1, :].partition_broadcast(128))
    gaT = sb("g_gaT", [16, 2, 128], BF16)
    Lsb = sb("g_L", [128, 2, 256])
    E1 = sb("g_E1", [128, 2, 256])
    E2 = sb("g_E2", [128, 2, 256])
    E3 = sb("g_E3", [128, 2, 256])
    qs = [sb("g_qs0", [128, 2, 256], BF16), sb("g_qs1", [128, 2, 256], BF16)]
    for i in range(2):
        P.op("vector", lambda e, i=i: e.memset(qs[i][:], 0.0), [], [qs[i]])
    ksT = sb("g_ksT", [128, 2, 256], BF16)
    k2 = sb("g_k2", [128, 2, 256], BF16)
    vsb = sb("g_v", [128, 2, 256], BF16)
    gs = sb("g_gs", [128, 2, 256])
    PT = sb("g_PT", [128, 2, 4, 128], BF16)
    osb = sb("g_osb", [128, 2, 256])
    obf = sb("g_obf", [128, 2, 256], BF16)
    ogT = sb("g_ogT", [128, 2, 2, 128], BF16)
    Dend = sb("g_Dend", [128, NT, 2])
    Sst = sb("g_S", [128, 2, 64])
    Sbf = sb("g_Sbf", [128, 2, 2, 64], BF16)
    P.op("vector", lambda e: e.memset(Sst[:], 0.0), [], [Sst])
    P.op("vector", lambda e: e.memset(Sbf[:], 0.0), [], [Sbf])
    sbf = dict(hn_st=sb("g_hn_st", [128, 2, 8]), hn_cen=sb("g_hn_cen", [128, 2, 256]),
               hn_sq=sb("g_hn_sq", [128, 2, 256]), gs=gs, obf=obf)

    import os
    DBGS = int(os.environ.get("DBG_STEPS", 99))
    for t in range(int(os.environ.get("DBG_TILES", NT))):
        b = t % 2
        tok = slice(t * 128, (t + 1) * 128)
        P1, P2, P3, P4, P5, P6, P7, P8 = ps
        for cc in range(4):
            for kc in range(8):
                mm(P1[:, cc * 128:(cc + 1) * 128], win[:, kc, cc * 128:(cc + 1) * 128], xT[:, kc, tok], kc == 0, kc == 7,
                   [win, xT], [(P1, cc)])
        if DBGS <= 1:
            continue
        for kc in range(8):
            mm(P2[0:16, 0:128], win[:, kc, 1024:1040], xT[:, kc, tok], kc == 0, kc == 7, [win, xT], [(P2, 0)])
        cp(gaT[0:16, b, :], P2[0:16, 0:128], [(P2, 0)], [(gaT, b)])
        if DBGS <= 2:
            continue
        mm(P3[:, 0:256], gaT[0:16, b, :], wa2[0:16, :], True, True, [(gaT, b), wa2], [(P3, 0)])
        tt(Lsb[:, b, :], P3[:, 0:256], babc[:, :], ALU.add, [(P3, 0), babc], [(Lsb, b)])
        act(Lsb[:, b, :], Lsb[:, b, :], AF.Exp, [(Lsb, b)], [(Lsb, b)], scale=-1.0)
        ts(Lsb[:, b, :], Lsb[:, b, :], 1.0, ALU.add, [(Lsb, b)], [(Lsb, b)])
        act(Lsb[:, b, :], Lsb[:, b, :], AF.Ln, [(Lsb, b)], [(Lsb, b)])
        if DBGS <= 3:
            continue
        for ch in range(2):
            mm(P4[:, ch * 128:(ch + 1) * 128], Lsb[:, b, ch * 128:(ch + 1) * 128], cmat[:, TRII, :], True, True,
               [(Lsb, b), cmat], [(P4, ch)])
        mm(P5[:, 0:256], cmat[:, TRIS, :], Lsb[:, b, :], True, True, [(Lsb, b), cmat], [(P5, 0)])
        act(E1[:, b, :], P4[:, 0:256], AF.Exp, [P4], [(E1, b)], scale=-1.0 / 16)
        act(E2[:, b, :], P4[:, 0:256], AF.Exp, [P4], [(E2, b)], scale=1.0 / 16)
        act(E3[:, b, :], P5[:, 0:256], AF.Exp, [(P5, 0)], [(E3, b)], scale=-1.0 / 16)
        cp(Dend[:, t, :], E1[:, b, :].rearrange("p (c n) -> p c n", c=2)[:, :, 127], [(E1, b)], [(Dend, t)])
        for hp in range(2):
            rws = slice(hp * 64, (hp + 1) * 64)
            stt(qs[hp][rws, b, :], P1[rws, 0:256], 0.125, E1[rws, b, :], ALU.mult, ALU.mult, [P1, (E1, b)], [(qs[hp], b)])
        tt(ksT[:, b, :], P1[:, 256:512], E2[:, b, :], ALU.mult, [P1, (E2, b)], [(ksT, b)])
        if DBGS <= 4:
            continue
        for kc in range(8):
            mm(P6[:, :], xT[:, kc, tok], win[:, kc, 256:768], kc == 0, kc == 7, [win, xT], [P6])
        for kc in range(8):
            mm(P7[:, 0:256], xT[:, kc, tok], win[:, kc, 768:1024], kc == 0, kc == 7, [win, xT], [(P7, 0)])
        SUBS = int(os.environ.get("DBG_SUB", 99))
        if SUBS >= 1:
            tt(k2[:, b, :], P6[:, 0:256], E3[:, b, :], ALU.mult, [P6, (E3, b)], [(k2, b)])
        if SUBS >= 2:
            cp(vsb[:, b, :], P6[:, 256:512], [P6], [(vsb, b)], eng="scalar")
        if SUBS >= 3:
            act(gs[:, b, :], P7[:, 0:256], AF.Silu, [(P7, 0)], [(gs, b)])
        if SUBS >= 4:
            tt(gs[:, b, :], gs[:, b, :], gng[:, :], ALU.mult, [(gs, b), gng], [(gs, b)])
        if DBGS <= 5:
            continue
        for h in range(4):
            hp, hc = h % 2, h // 2
            mm(P8[:, h * 128:(h + 1) * 128], ksT[:, b, hc * 128:(hc + 1) * 128],
               qs[hp][:, b, hc * 128:(hc + 1) * 128], True, True, [(ksT, b), (qs[hp], b)], [(P8, h)])
        tt(PT[:, b, :, :], P8[:, :].rearrange("p (h n) -> p h n", h=4), bcast(cmat[:, TRII, :], [128, 4, 128], 1),
           ALU.mult, [P8, cmat], [(PT, b)])
        if DBGS <= 6:
            continue
        for h in range(4):
            hc = h // 2
            mm(P2[:, 128 + h * 64:128 + (h + 1) * 64], k2[:, b, hc * 128:(hc + 1) * 128], vsb[:, b, h * 64:(h + 1) * 64],
               True, True, [(k2, b), (vsb, b)], [(P2, 1 + h)])
        if DBGS <= 7:
            continue
        for h in range(4):
            hp, hc = h % 2, h // 2
            mm(P3[:, 256 + h * 64:256 + (h + 1) * 64], PT[:, b, h, :], vsb[:, b, h * 64:(h + 1) * 64], True, False,
               [(PT, b), (vsb, b)], [(P3, 1)])
            mm(P3[:, 256 + h * 64:256 + (h + 1) * 64], qs[hp][:, b, hc * 128:(hc + 1) * 128],
               Sbf[:, b, hc, :], False, True, [(qs[hp], b), (Sbf, b)], [(P3, 1)])
        if DBGS <= 8:
            continue
        for h in range(4):
            hp, hc = h % 2, h // 2
            rows = slice(hp * 64, (hp + 1) * 64)
            stt(Sst[rows, hc, :], Sst[rows, hc, :], Dend[rows, t, hc:hc + 1], P2[rows, 128 + h * 64:128 + (h + 1) * 64],
                ALU.mult, ALU.add, [(Sst, h), (Dend, t), (P2, 1 + h)], [(Sst, h)])
        cp(Sbf[:, 1 - b, :, :], Sst[:, :, :], [Sst], [(Sbf, 1 - b)])
        if DBGS <= 9:
            continue
        cp(osb[:, b, :], P3[:, 256:512], [(P3, 1)], [(osb, b)], eng="scalar")
        head_norm_gate(c, sbf, "g", osb[:, b, :], gs[:, b, :], obf[:, b, :], b, (osb, b))
        if DBGS <= 10:
            continue
        pb = ps_bf[4]
        for ch in range(2):
            tr(pb[:, 512 + ch * 128:512 + (ch + 1) * 128], obf[:, b, ch * 128:(ch + 1) * 128], cmat_bf[:, IDENT, :],
               [(obf, b), cmat_bf], [(P5, 1)])
        cp(ogT[:, b, :, :], pb[:, 512:768].rearrange("p (c n) -> p c n", c=2), [(P5, 1)], [(ogT, b)])
        for q4 in range(4):
            pq, key = (P7[:, 256:512], (P7, 1)) if q4 % 2 == 0 else (P4[:, 256:512], (P4, 2))
            for ch in range(2):
                mm(pq, ogT[:, b, ch, :], wout[:, ch, q4 * 256:(q4 + 1) * 256], ch == 0, ch == 1, [(ogT, b), wout], [key])
            xs = x_tm[:, t, q4 * 256:(q4 + 1) * 256]
            if c["first_mixer"]:
                stt(xs, xs, ALPHA, pq, ALU.mult, ALU.add, [(x_tm, t), key], [(x_tm, t)])
            else:
                tt(xs, xs, pq, ALU.add, [(x_tm, t), key], [(x_tm, t)])
    P.barrier()
    ph.close()


def mixer_mlstm(c, l):
    from contextlib import ExitStack
    import os
    nc, P, w = c["nc"], c["P"], c["w"]
    x_tm, xT, ps, ps_bf, cmat, cmat_bf = c["x_tm"], c["xT"], c["ps"], c["ps_bf"], c["cmat"], c["cmat_bf"]
    mm, tr, act, tt, ts, stt, cp, red = c["mm"], c["tr"], c["act"], c["tt"], c["ts"], c["stt"], c["cp"], c["red"]
    load, load_cast = c["load"], c["load_cast"]
    IDENT, TRII, ONES = 0, 1, 4
    ph = ExitStack()

    def sb(name, shape, dt=F32):
        return P.tile(name, ph.enter_context(nc.sbuf_tensor("%s_%d" % (name, l), list(shape), dt)))

    win = sb("m_win", [128, 8, 1032], BF16)
    for kc in range(8):
        load_cast(win, win[:, kc, :], w["w_in"][l, kc * 128:(kc + 1) * 128, 1040:2072], sub=kc)
    cw = sb("m_cw", [128, 4, 4])
    for j in range(4):
        load(cw, cw[:, :, j], w["ml_conv_w"][l, j, :].rearrange("(c p) -> p c", p=128), sub=j)
    bif = sb("m_bif", [128, 8])
    load(bif, bif[:, 0:4].unsqueeze(1), w["ml_b_i"][l:l + 1, :].partition_broadcast(128), sub=0)
    load(bif, bif[:, 4:8].unsqueeze(1), w["ml_b_f"][l:l + 1, :].partition_broadcast(128), sub=1)
    mng = sb("m_mng", [128, 256])
    load(mng, mng[:].unsqueeze(1), w["ml_norm_g"][l:l + 1, :].partition_broadcast(128))
    wout = sb("m_wout", [128, 2, D], BF16)
    load_cast(wout, wout[:], w["w_out"][l, 256:512, :].rearrange("(k p) n -> p k n", p=128))

    q = [sb("m_q0", [128, 2, S], BF16), sb("m_q1", [128, 2, S], BF16)]
    kT = sb("m_kT", [128, 2, S], BF16)
    ph1 = ExitStack()
    mqk = P.tile("m_mqk", ph1.enter_context(nc.sbuf_tensor("m_mqk_%d" % l, [128, 4, S + 3], BF16)))
    acc = P.tile("m_acc", ph1.enter_context(nc.sbuf_tensor("m_acc_%d" % l, [128, 2, 1024], F32)))
    P.op("vector", lambda e: e.memset(mqk[:, :, 0:3], 0.0), [], [mqk])
    for g in range(4):
        tok = slice(g * 512, (g + 1) * 512)
        for ch in range(4):
            pp = ps[(g * 4 + ch) % 2]
            for kc in range(8):
                mm(pp[:, :], win[:, kc, ch * 128:(ch + 1) * 128], xT[:, kc, tok], kc == 0, kc == 7, [win, xT], [pp])
            cp(mqk[:, ch, 3 + g * 512:3 + (g + 1) * 512], pp[:, :], [pp], [(mqk, ch)], eng="scalar" if ch % 2 else "vector")
    for i in range(2):
        P.op("vector", lambda e, i=i: e.memset(q[i][:], 0.0), [], [q[i]])
    for ch in range(4):
        for half in range(2):
            ai = half
            a = acc[:, ai, :]
            off = half * 1024
            ts(a, mqk[:, ch, off:off + 1024], cw[:, ch, 0:1], ALU.mult, [(mqk, ch), cw], [(acc, ai)])
            for j in range(1, 4):
                stt(a, mqk[:, ch, off + j:off + j + 1024], cw[:, ch, j:j + 1], a, ALU.mult, ALU.add,
                    [(mqk, ch), cw, (acc, ai)], [(acc, ai)])
            tokh = slice(off, off + 1024)
            if ch < 2:
                for hp in range(2):
                    rws = slice(hp * 64, (hp + 1) * 64)
                    act(q[hp][rws, ch, tokh], acc[rws, ai, :], AF.Silu, [(acc, ai)], [(q[hp], (ch, half))])
            else:
                act(kT[:, ch - 2, tokh], a, AF.Silu, [(acc, ai)], [(kT, (ch, half))])
    for hp in range(2):
        ts(q[hp][:], q[hp][:], 0.125, ALU.mult, [q[hp]], [q[hp]])
    P.barrier()
    ph1.close()

    gates = sb("m_gates", [128, NT, 8])
    pg = ps[2]
    for t in range(NT):
        for kc in range(8):
            mm(pg[:, t * 8:(t + 1) * 8], xT[:, kc, t * 128:(t + 1) * 128], win[:, kc, 1024:1032], kc == 0, kc == 7,
               [win, xT], [pg])
    tt(gates[:], pg[:, 0:128].rearrange("p (t n) -> p t n", t=NT), bcast(bif[:, :], [128, NT, 8], 1), ALU.add,
       [pg, bif], [gates])
    Lf = sb("m_Lf", [128, NT, 4])
    act(Lf[:], gates[:, :, 4:8], AF.Exp, [gates], [Lf], scale=-1.0)
    ts(Lf[:], Lf[:], 1.0, ALU.add, [Lf], [Lf])
    act(Lf[:], Lf[:], AF.Ln, [Lf], [Lf])
    Lf2 = Lf[:].rearrange("p t n -> p (t n)")
    p3 = ps[3]
    mm(p3[:, 0:64], cmat[:, TRII, :], Lf2, True, True, [cmat, Lf], [p3])
    asb = sb("m_a", [128, NT, 4])
    tt(asb[:], p3[:, 0:64].rearrange("p (t n) -> p t n", t=NT), gates[:, :, 0:4], ALU.add, [p3, gates], [asb])
    cumL = sb("m_cumL", [128, 64])
    cp(cumL[:], p3[:, 0:64], [p3], [cumL])
    a2 = asb[:].rearrange("p t n -> p (t n)")
    p4 = ps[4]
    tr(p4[0:64, 0:128], a2, cmat[:, IDENT, :], [asb, cmat], [p4])
    Acol = sb("m_Acol", [64, 1])
    red(Acol[:, 0:1], p4[0:64, 0:128], ALU.max, [p4], [Acol])
    rows = sb("m_rows", [1, 5, 64])
    p5 = ps[5]
    mm(p5[0:1, 0:64], Acol[0:64, 0:1], cmat[0:64, IDENT, 0:64], True, True, [Acol, cmat], [p5])
    mm(p5[0:1, 64:128], cmat[:, ONES, 0:1], Lf2, True, True, [cmat, Lf], [p5])
    cp(rows[0:1, 0:2, :], p5[0:1, 0:128].rearrange("p (a n) -> p a n", a=2), [p5], [rows])
    P.op("vector", lambda e: e.memset(rows[0:1, 2, 0:4], 0.0), [rows], [rows])
    for cc in range(NT):
        sl = slice(cc * 4, cc * 4 + 4)
        tt(rows[0:1, 3, sl], rows[0:1, 2, sl], rows[0:1, 0, sl], ALU.max, [rows], [rows])
        if cc < NT - 1:
            tt(rows[0:1, 2, (cc + 1) * 4:(cc + 1) * 4 + 4], rows[0:1, 3, sl], rows[0:1, 1, sl], ALU.subtract, [rows], [rows])
    tt(rows[0:1, 4, :], rows[0:1, 2, :], rows[0:1, 3, :], ALU.subtract, [rows], [rows])
    act(rows[0:1, 4, :], rows[0:1, 4, :], AF.Exp, [rows], [rows])
    p6 = ps[6]
    mm(p6[:, 0:128], cmat[0:1, ONES, :], rows[0:1, 3:5, :].rearrange("p a n -> p (a n)"), True, True, [cmat, rows], [p6])
    bcs = sb("m_bcs", [128, 128])
    cp(bcs[:], p6[:, 0:128], [p6], [bcs])
    wtok = sb("m_wtok", [128, 64])
    tt(wtok[:], a2, bcs[:, 0:64], ALU.subtract, [asb, bcs], [wtok])
    act(wtok[:], wtok[:], AF.Exp, [wtok], [wtok])
    clamp = sb("m_clamp", [128, 64])
    tt(clamp[:], cumL[:], bcs[:, 0:64], ALU.subtract, [cumL, bcs], [clamp])
    act(clamp[:], clamp[:], AF.Exp, [clamp], [clamp])

    vext = sb("m_vext", [128, 2, 4, 65], BF16)
    P.op("vector", lambda e: e.memset(vext[:], 1.0), [], [vext])
    vw = sb("m_vw", [128, 2, 4, 65], BF16)
    gso = sb("m_gso", [128, 2, 256])
    ktm = sb("m_ktm", [128, 2, 256], BF16)
    WM = sb("m_WM", [128, 2, 4, 128])
    PT = sb("m_PT", [128, 2, 4, 128], BF16)
    Cn = sb("m_Cn", [128, 2, 65])
    P.op("vector", lambda e: e.memset(Cn[:], 0.0), [], [Cn])
    Cd = sb("m_Cd", [128, 2, 65])
    Cdbf = sb("m_Cdbf", [128, 2, 2, 65], BF16)
    nd = sb("m_nd", [128, 2, 4, 65])
    hsb = sb("m_h", [128, 2, 256])
    small = sb("m_small", [128, 2, 8])
    obf = sb("m_obf", [128, 2, 256], BF16)
    ohT = sb("m_ohT", [128, 2, 2, 128], BF16)
    sbf = dict(hn_st=sb("m_hn_st", [128, 2, 8]), hn_cen=sb("m_hn_cen", [128, 2, 256]),
               hn_sq=sb("m_hn_sq", [128, 2, 256]), gs=gso, obf=obf)
    PA, PB, PC, PD, PE_, PO0, PO1, PX = ps
    for t in range(int(os.environ.get("DBG_TILES", NT))):
        b = t % 2
        tok = slice(t * 128, (t + 1) * 128)
        g4 = slice(t * 4, t * 4 + 4)
        for kc in range(8):
            mm(PA[:, :], xT[:, kc, tok], win[:, kc, 512:1024], kc == 0, kc == 7, [win, xT], [PA])
        v4 = PA[:, 0:256].rearrange("p (h e) -> p h e", h=4)
        cp(vext[:, b, :, 0:64], v4, [PA], [(vext, b)], eng="scalar")
        tt(vw[:, b, :, 0:64], v4, bcast(wtok[:, g4], [128, 4, 64], 2), ALU.mult, [PA, wtok], [(vw, b)])
        cp(vw[:, b, :, 64], wtok[:, g4], [wtok], [(vw, b)])
        act(gso[:, b, :], PA[:, 256:512], AF.Sigmoid, [PA], [(gso, b)])
        tt(gso[:, b, :], gso[:, b, :], mng[:, :], ALU.mult, [(gso, b), mng], [(gso, b)])
        pb = ps_bf[1]
        for hc in range(2):
            tr(pb[:, hc * 128:(hc + 1) * 128], kT[:, hc, tok], cmat_bf[:, IDENT, :], [kT, cmat_bf], [PB])
        cp(ktm[:, b, :], pb[:, 0:256], [PB], [(ktm, b)])
        for h in range(4):
            hp, hc = h % 2, h // 2
            mm(PC[:, h * 128:(h + 1) * 128], kT[:, hc, tok], q[hp][:, hc, tok], True, True, [kT, q[hp]], [PC])
        tt(WM[:, b, :, :], bcast(wtok[:, g4], [128, 4, 128], 2), bcast(cmat[:, TRII, :], [128, 4, 128], 1), ALU.mult,
           [wtok, cmat], [(WM, b)])
        tt(PT[:, b, :, :], PC[:, :].rearrange("p (h n) -> p h n", h=4), WM[:, b, :, :], ALU.mult, [PC, (WM, b)], [(PT, b)])
        for h in range(4):
            hc = h // 2
            mm(PD[:, h * 65:(h + 1) * 65], ktm[:, b, hc * 128:(hc + 1) * 128], vw[:, b, h, :], True, True,
               [(ktm, b), (vw, b)], [PD])
        for h in range(4):
            hp, hc = h % 2, h // 2
            rws = slice(hp * 64, (hp + 1) * 64)
            ts(Cd[rws, hc, :], Cn[rws, hc, :], bcs[rws, 64 + t * 4 + h:64 + t * 4 + h + 1], ALU.mult,
               [(Cn, h), bcs], [(Cd, h)])
        cp(Cdbf[:, b, :, :], Cd[:], [Cd], [(Cdbf, b)])
        for h in range(4):
            hp, hc = h % 2, h // 2
            mm(PE_[:, h * 65:(h + 1) * 65], PT[:, b, h, :], vext[:, b, h, :], True, False, [(PT, b), (vext, b)], [PE_])
            mm(PE_[:, h * 65:(h + 1) * 65], q[hp][:, hc, tok], Cdbf[:, b, hc, :], False, True, [q[hp], (Cdbf, b)], [PE_])
        for h in range(4):
            hp, hc = h % 2, h // 2
            rws = slice(hp * 64, (hp + 1) * 64)
            tt(Cn[rws, hc, :], Cd[rws, hc, :], PD[rws, h * 65:(h + 1) * 65], ALU.add, [(Cd, h), PD], [(Cn, h)])
        cp(nd[:, b, :, :], PE_[:, 0:260].rearrange("p (h e) -> p h e", h=4), [PE_], [(nd, b)], eng="scalar")
        stt(small[:, b, 0:4], nd[:, b, :, 64], -1.0, nd[:, b, :, 64], ALU.mult, ALU.max, [(nd, b)], [(small, b)])
        tt(small[:, b, 0:4], small[:, b, 0:4], clamp[:, g4], ALU.max, [(small, b), clamp], [(small, b)])
        P.op("vector", lambda e, b=b: e.reciprocal(small[:, b, 4:8], small[:, b, 0:4]), [(small, b)], [(small, b)])
        tt(hsb[:, b, :].rearrange("p (h e) -> p h e", h=4), nd[:, b, :, 0:64], bcast(small[:, b, 4:8], [128, 4, 64], 2),
           ALU.mult, [(nd, b), (small, b)], [(hsb, b)])
        head_norm_gate(c, sbf, "m", hsb[:, b, :], gso[:, b, :], obf[:, b, :], b, (hsb, b))
        pbx = ps_bf[7]
        for ch in range(2):
            tr(pbx[:, ch * 128:(ch + 1) * 128], obf[:, b, ch * 128:(ch + 1) * 128], cmat_bf[:, IDENT, :],
               [(obf, b), cmat_bf], [PX])
        cp(ohT[:, b, :, :], pbx[:, 0:256].rearrange("p (c n) -> p c n", c=2), [PX], [(ohT, b)])
        for hf in range(2):
            po = PO0 if hf == 0 else PO1
            for ch in range(2):
                mm(po[:, :], ohT[:, b, ch, :], wout[:, ch, hf * 512:(hf + 1) * 512], ch == 0, ch == 1, [(ohT, b), wout], [po])
            xs = x_tm[:, t, hf * 512:(hf + 1) * 512]
            if c["first_mixer"]:
                stt(xs, xs, ALPHA, po[:, :], ALU.mult, ALU.add, [(x_tm, t), po], [(x_tm, t)])
            else:
                tt(xs, xs, po[:, :], ALU.add, [(x_tm, t), po], [(x_tm, t)])
    P.barrier()
    ph.close()


def mixer_mla(c, l):
    from contextlib import ExitStack
    import os
    nc, P, w = c["nc"], c["P"], c["w"]
    x_tm, xT, ps, cmat, cmat_bf, cs = c["x_tm"], c["xT"], c["ps"], c["cmat"], c["cmat_bf"], c["cs"]
    mm, act, tt, ts, stt, cp = c["mm"], c["act"], c["tt"], c["ts"], c["stt"], c["cp"]
    load, load_cast = c["load"], c["load_cast"]
    MMLA, ONES = 3, 4
    R_ = slice(64, 96)
    ph = ExitStack()

    def sb(name, shape, dt=F32, st=None):
        return P.tile(name, (st or ph).enter_context(nc.sbuf_tensor("%s_%d" % (name, l), list(shape), dt)))

    cqnT = sb("a_cqnT", [128, 2, S], BF16)
    ckvnT = sb("a_ckvnT", [128, S], BF16)
    krope = sb("a_krope", [96, S], BF16)
    v_all = sb("a_vall", [128, NT, 512], BF16)
    gq = sb("a_gq", [128, 2])
    gkv = sb("a_gkv", [128, 1])
    load(gq, gq[:], w["mla_q_norm_g"][l].rearrange("(rc p) -> p rc", p=128))
    load(gkv, gkv[:], w["mla_kv_norm_g"][l].rearrange("(o p) -> p o", o=1))

    p1 = ExitStack()
    win = sb("a_win", [128, 8, 416], BF16, p1)
    for kc in range(8):
        load_cast(win, win[:, kc, :], w["w_in"][l, kc * 128:(kc + 1) * 128, 2072:2488], sub=kc)
    wkr = sb("a_wkr", [128, 8, 2, 96], BF16, p1)
    P.op("vector", lambda e: e.memset(wkr[:], 0.0), [], [wkr])
    cp(wkr[:, :, 0, 64:96], win[:, :, 384:416], [win], [wkr])
    ts(wkr[:, :, 1, 64:80], win[:, :, 400:416], -1.0, ALU.mult, [win], [wkr])
    cp(wkr[:, :, 1, 80:96], win[:, :, 384:400], [win], [wkr])
    sq = sb("a_sq", [128, 2, 512], BF16, p1)
    rstd = sb("a_rstd", [128, 2, 512], F32, p1)
    tA = sb("a_tA", [96, 512], F32, p1)
    tB = sb("a_tB", [96, 512], F32, p1)
    for g in range(4):
        tok = slice(g * 512, (g + 1) * 512)
        for rc in range(2):
            for kc in range(8):
                mm(ps[rc][:, :], win[:, kc, rc * 128:(rc + 1) * 128], xT[:, kc, tok], kc == 0, kc == 7, [win, xT], [ps[rc]])
        for rc in range(2):
            act(sq[:, rc, :], ps[rc][:, :], AF.Square, [ps[rc]], [(sq, rc)])
        for rc in range(2):
            mm(ps[2][:, :], cmat_bf[:, ONES, :], sq[:, rc, :], rc == 0, rc == 1, [cmat_bf, (sq, rc)], [ps[2]])
        ts(rstd[:, 0, :], ps[2][:, :], 1.0 / 256, ALU.mult, [ps[2]], [(rstd, 0)], s2=LN_EPS, op1=ALU.add)
        act(rstd[:, 0, :], rstd[:, 0, :], AF.Ln, [(rstd, 0)], [(rstd, 0)])
        act(rstd[:, 0, :], rstd[:, 0, :], AF.Exp, [(rstd, 0)], [(rstd, 0)], scale=-0.5)
        for rc in range(2):
            stt(cqnT[:, rc, tok], ps[rc][:, :], gq[:, rc:rc + 1], rstd[:, 0, :], ALU.mult, ALU.mult,
                [ps[rc], gq, (rstd, 0)], [(cqnT, (rc, g))])
        for kc in range(8):
            mm(ps[3][:, :], win[:, kc, 256:384], xT[:, kc, tok], kc == 0, kc == 7, [win, xT], [ps[3]])
        act(sq[:, 0, :], ps[3][:, :], AF.Square, [ps[3]], [(sq, 0)])
        mm(ps[4][:, :], cmat_bf[:, ONES, :], sq[:, 0, :], True, True, [cmat_bf, (sq, 0)], [ps[4]])
        ts(rstd[:, 1, :], ps[4][:, :], 1.0 / 128, ALU.mult, [ps[4]], [(rstd, 1)], s2=LN_EPS, op1=ALU.add)
        act(rstd[:, 1, :], rstd[:, 1, :], AF.Ln, [(rstd, 1)], [(rstd, 1)])
        act(rstd[:, 1, :], rstd[:, 1, :], AF.Exp, [(rstd, 1)], [(rstd, 1)], scale=-0.5)
        stt(ckvnT[:, tok], ps[3][:, :], gkv[:, 0:1], rstd[:, 1, :], ALU.mult, ALU.mult, [ps[3], gkv, (rstd, 1)], [(ckvnT, g)])
        for r2 in range(2):
            for kc in range(8):
                mm(ps[5 + r2][0:96, :], wkr[:, kc, r2, :], xT[:, kc, tok], kc == 0, kc == 7, [wkr, xT], [ps[5 + r2]])
        tt(tA[R_, :], ps[5][R_, :], cs[R_, 0, tok], ALU.mult, [ps[5], cs], [tA])
        tt(tB[R_, :], ps[6][R_, :], cs[R_, 1, tok], ALU.mult, [ps[6], cs], [tB])
        tt(krope[R_, tok], tA[R_, :], tB[R_, :], ALU.add, [tA, tB], [(krope, g)])
    P.barrier()
    p1.close()

    wuk = sb("a_wuk", [128, 8, 64], BF16)
    wuv = sb("a_wuv", [128, 8, 64], BF16)
    ukv = w["mla_w_ukv"][l].rearrange("p (h two d) -> p h two d", h=8, two=2)
    load_cast(wuk, wuk[:], ukv[:, :, 0, :])
    load_cast(wuv, wuv[:], ukv[:, :, 1, :])
    wuq = sb("a_wuq", [128, 2, 768], BF16)
    load_cast(wuq, wuq[:], w["mla_w_uq"][l].rearrange("(rc p) n -> p rc n", p=128))
    wuqr = sb("a_wuqr", [128, 2, 8, 96], BF16)
    P.op("vector", lambda e: e.memset(wuqr[:], 0.0), [], [wuqr])
    wq4 = wuq[:].rearrange("p r (h c) -> p r h c", h=8)
    ts(wuqr[:, :, :, 64:80], wq4[:, :, :, 80:96], -1.0, ALU.mult, [wuq], [wuqr])
    cp(wuqr[:, :, :, 80:96], wq4[:, :, :, 64:80], [wuq], [wuqr])
    wout = sb("a_wout", [128, 4, D], BF16)
    load_cast(wout, wout[:], w["w_out"][l, 512:1024, :].rearrange("(k p) n -> p k n", p=128))
    qTh = sb("a_qTh", [96, 2, 512], BF16)
    PTb = sb("a_PT", [128, 3, 512], BF16)
    mlaT = sb("a_mlaT", [128, 4, 512], BF16)
    rden = sb("a_rden", [128, 2, 512])
    tA2 = sb("a_tA2", [96, 2, 512])
    tB2 = sb("a_tB2", [96, 2, 512])
    KH = [("k", h) for h in range(8)]
    for g in range(4):
        tok = slice(g * 512, (g + 1) * 512)
        for h in range(8):
            pk = ps[h % 2]
            mm(pk[0:64, :], wuk[:, h, :], ckvnT[:, tok], True, True, [wuk, ckvnT], [pk])
            cp(xT[0:64, h, tok], pk[0:64, :], [pk], [(xT, KH[h])], eng="scalar" if h % 2 else "vector")
        cp(xT[R_, :, tok], bcast(krope[R_, tok], [32, 8, 512], 1), [krope], [(xT, k) for k in KH])
    for t in range(NT):
        pv = ps[2 + t % 2]
        mm(pv[:, :], ckvnT[:, t * 128:(t + 1) * 128], wuv[:].rearrange("p h d -> p (h d)"), True, True, [ckvnT, wuv], [pv])
        cp(v_all[:, t, :], pv[:, :], [pv], [(v_all, t)], eng="scalar" if t % 2 else "vector")
    scale = 96.0 ** -0.5
    NG = int(os.environ.get("DBG_GROUPS", 4))

    def qproj(g, h):
        tok = slice(g * 512, (g + 1) * 512)
        qb = h % 2
        pq, pr = ps[0], ps[1]
        for rc in range(2):
            mm(pq[0:96, :], wuq[:, rc, h * 96:(h + 1) * 96], cqnT[:, rc, tok], rc == 0, rc == 1, [wuq, cqnT], [pq])
        for rc in range(2):
            mm(pr[0:96, :], wuqr[:, rc, h, :], cqnT[:, rc, tok], rc == 0, rc == 1, [wuqr, cqnT], [pr])
        cp(qTh[0:64, qb, :], pq[0:64, :], [pq], [(qTh, qb)], eng="scalar")
        tt(tA2[R_, qb, :], pq[R_, :], cs[R_, 0, tok], ALU.mult, [pq, cs], [(tA2, qb)])
        tt(tB2[R_, qb, :], pr[R_, :], cs[R_, 1, tok], ALU.mult, [pr, cs], [(tB2, qb)])
        tt(qTh[R_, qb, :], tA2[R_, qb, :], tB2[R_, qb, :], ALU.add, [(tA2, qb), (tB2, qb)], [(qTh, qb)])

    def attend(g, h, nxt):
        nkt = 4 * g + 4
        hp, pair, qb = h % 2, h // 2, h % 2
        po, pd = ps[4 + h % 2], ps[6 + h % 2]

        def qk(kt):
            qlo = max(kt - 4 * g, 0) * 128
            N = 512 - qlo
            pst = ps[2 + kt % 2]
            mm(pst[:, 0:N], xT[0:96, h, kt * 128:(kt + 1) * 128], qTh[0:96, qb, qlo:512], True, True,
               [(xT, KH[h]), (qTh, qb)], [pst])

        qk(0)
        for kt in range(nkt):
            qlo = max(kt - 4 * g, 0) * 128
            N = 512 - qlo
            pst = ps[2 + kt % 2]
            pb = kt % 3
            if kt + 1 < nkt:
                qk(kt + 1)
            elif nxt is not None:
                qproj(*nxt)
            act(PTb[:, pb, 0:N], pst[:, 0:N], AF.Exp, [pst], [(PTb, pb)], scale=scale)
            if kt >= 4 * g:
                tt(PTb[:, pb, 0:128], PTb[:, pb, 0:128], cmat_bf[:, MMLA, :], ALU.mult, [(PTb, pb), cmat_bf], [(PTb, pb)])
            mm(po[:, qlo:512], v_all[:, kt, pair * 128:(pair + 1) * 128], PTb[:, pb, 0:N], kt == 0, kt == nkt - 1,
               [(v_all, kt), (PTb, pb)], [po])
            mm(pd[:, qlo:512], cmat_bf[:, ONES, :], PTb[:, pb, 0:N], kt == 0, kt == nkt - 1, [cmat_bf, (PTb, pb)], [pd])
        rws = slice(hp * 64, (hp + 1) * 64)
        P.op("vector", lambda e: e.reciprocal(rden[rws, qb, :], pd[rws, :]), [pd], [(rden, qb)])
        tt(mlaT[rws, pair, :], po[rws, :], rden[rws, qb, :], ALU.mult, [po, (rden, qb)], [(mlaT, h)])

    work = [(g, h) for g in range(NG) for h in range(8)]
    qproj(*work[0])
    for i, (g, h) in enumerate(work):
        nxt = work[i + 1] if i + 1 < len(work) else None
        if nxt is not None and nxt[0] != g:
            attend(g, h, None)
        else:
            attend(g, h, nxt)
        if h != 7:
            continue
        if nxt is not None:
            qproj(*nxt)
        for tl in range(4):
            t = g * 4 + tl
            for hf in range(2):
                pp = ps[5 + 2 * hf]
                for pr_ in range(4):
                    mm(pp[:, :], mlaT[:, pr_, tl * 128:(tl + 1) * 128], wout[:, pr_, hf * 512:(hf + 1) * 512], pr_ == 0, pr_ == 3,
                       [mlaT, wout], [pp])
                xs = x_tm[:, t, hf * 512:(hf + 1) * 512]
                if c["first_mixer"]:
                    stt(xs, xs, ALPHA, pp[:, :], ALU.mult, ALU.add, [(x_tm, t), pp], [(x_tm, t)])
                else:
                    tt(xs, xs, pp[:, :], ALU.add, [(x_tm, t), pp], [(x_tm, t)])
    P.barrier()
    ph.close()
```

```python
import numpy as np
import ml_dtypes
import concourse.bass as bass
import concourse.mybir as mybir
from concourse.bass_utils import run_bass_kernel_spmd

F32 = mybir.dt.float32
BF16 = mybir.dt.bfloat16
I32 = mybir.dt.int32
AF = mybir.ActivationFunctionType
ALU = mybir.AluOpType
AX = mybir.AxisListType

S = 2048
D = 1024
NT = 16
DEPTH = 2
ALPHA = (2 * DEPTH) ** 0.25
LN_EPS = 1e-5
IN_W = 2488


class Tile:
    def __init__(self, name, h):
        self.name = name
        self.h = h
        self.st = {}
        self.dsem = None
        self.dcount = 0
        self.psum = False

    def __getitem__(self, k):
        return self.h[k]


class Op:
    __slots__ = ("eng", "fn", "deps", "ddeps", "inc", "dma", "cost", "unit", "seg", "oi", "fin", "pos")

    def __init__(self, eng, fn, deps, ddeps, dma=None, cost=0.2):
        self.eng = eng
        self.fn = fn
        self.deps = deps
        self.ddeps = ddeps
        self.inc = False
        self.dma = dma
        self.cost = cost
        self.unit = None
        self.seg = 0
        self.oi = 0
        self.fin = 0.0
        self.pos = 0


class Prog:
    ENG = ("sync", "scalar", "vector", "gpsimd", "tensor")
    REORDER = ("scalar", "vector", "tensor")

    def __init__(self, nc):
        self.nc = nc
        self.ops = {e: [] for e in self.ENG}
        self.all = []
        self.dsems = {}
        self.dtotal = {}
        self.tiles = []
        self.seg = 0
        self.cur_unit = None
        self.nunits = 0

    def tile(self, name, h):
        t = Tile(name, h)
        self.tiles.append(t)
        return t

    @staticmethod
    def _norm(lst):
        out = []
        for x in lst:
            if not isinstance(x, tuple):
                x = (x, None)
            if x[0].psum:
                x = (x[0], None)
            out.append(x)
        return out

    @staticmethod
    def _states(t, s):
        if s is None:
            return list(t.st.values())
        r = []
        if None in t.st:
            r.append(t.st[None])
        if s in t.st:
            r.append(t.st[s])
        return r

    def _collect(self, reads, writes):
        deps, ddeps = [], {}

        def add(ev):
            if ev is None:
                return
            if isinstance(ev, Op):
                deps.append(ev)
            else:
                k = ev
                ddeps[k] = self.dtotal[k]

        for (t, s) in reads:
            for st in self._states(t, s):
                add(st[0])
        for (t, s) in writes:
            for st in self._states(t, s):
                add(st[0])
                for r in st[1]:
                    add(r)
        return deps, ddeps

    def _update(self, ev, reads, writes):
        for (t, s) in reads:
            st = t.st.setdefault(s, [None, []])
            st[1].append(ev)
        for (t, s) in writes:
            if s is None:
                t.st = {None: [ev, []]}
            else:
                t.st[s] = [ev, []]

    def _add(self, o):
        o.seg = self.seg
        o.oi = len(self.all)
        self.all.append(o)
        self.ops[o.eng].append(o)

    def op(self, eng, fn, reads=(), writes=(), cost=0.2, start=None, stop=None):
        reads = self._norm(reads)
        writes = self._norm(writes)
        writes = writes + [r for r in reads if r[0].psum]
        deps, ddeps = self._collect(reads, writes)
        o = Op(eng, fn, deps, ddeps, cost=cost)
        if eng == "tensor":
            if self.cur_unit is None or start is None or start:
                self.nunits += 1
                self.cur_unit = self.nunits
            o.unit = self.cur_unit
            if stop is None or stop:
                self.cur_unit = None
        self._add(o)
        self._update(o, reads, writes)

    def dma(self, eng, out, in_, reads=(), writes=(), semtile=None):
        assert eng in ("sync", "gpsimd")
        reads = self._norm(reads)
        writes = self._norm(writes)
        deps, ddeps = self._collect(reads, writes)
        if semtile.dsem is None:
            semtile.dsem = ("D", semtile.name)
            self.dsems[semtile.dsem] = None
        semtile.dcount = self.dtotal.get(semtile.dsem, 0) + 16
        self.dtotal[semtile.dsem] = semtile.dcount
        o = Op(eng, None, deps, ddeps, dma=(out, in_, semtile.dsem), cost=0.1)
        self._add(o)
        self._update(semtile.dsem, reads, writes)

    def barrier(self):
        for e in self.ENG:
            o = Op(e, "bar", [], {}, cost=0.05)
            self._add(o)
        self.seg += 1
        for t in self.tiles:
            t.st = {}

    def finish(self, eng="sync"):
        self.barrier()

    def schedule(self):
        LAT = 0.25
        W = 48
        order = {e: [] for e in self.ENG}
        nseg = self.seg + 1
        byseg = [{e: [] for e in self.ENG} for _ in range(nseg)]
        for o in self.all:
            byseg[o.seg][o.eng].append(o)
        for sg in range(nseg):
            pend = byseg[sg]
            free = {e: 0.0 for e in self.ENG}
            done = set()
            npend = sum(len(v) for v in pend.values())
            heads = {e: 0 for e in self.ENG}
            taken = {e: set() for e in self.ENG}
            lists = pend
            t_now = 0.0
            dmafin = {}
            while npend > 0:
                progressed = False
                best_next = None
                for e in self.ENG:
                    L = lists[e]
                    if heads[e] >= len(L):
                        continue
                    cands = []
                    i = heads[e]
                    cnt = 0
                    while i < len(L) and cnt < (W if e in self.REORDER else 1):
                        if i not in taken[e]:
                            cands.append(i)
                            cnt += 1
                        i += 1
                    pick = None
                    pick_t = None
                    for i in cands:
                        o = L[i]
                        if o.fn == "bar":
                            if i != heads[e]:
                                break
                        if o.unit is not None and i > 0 and L[i - 1].unit == o.unit:
                            continue
                        rt = 0.0
                        ok = True
                        j = i
                        while True:
                            oo = L[j]
                            for d in oo.deps:
                                if d.seg != sg:
                                    continue
                                if d.unit is not None and d.unit == oo.unit:
                                    continue
                                if id(d) not in done:
                                    ok = False
                                    break
                                lat = 0.0 if d.eng == oo.eng == "tensor" else LAT
                                rt = max(rt, d.fin + lat)
                            if not ok:
                                break
                            for k in oo.ddeps:
                                rt = max(rt, dmafin.get(k, 0.0))
                            if oo.unit is None or j + 1 >= len(L) or L[j + 1].unit != oo.unit:
                                break
                            j += 1
                        if not ok:
                            continue
                        st_t = max(rt, free[e])
                        if pick is None or st_t < pick_t - 1e-9:
                            pick, pick_t = i, st_t
                        if st_t <= free[e] + 1e-9:
                            break
                    if pick is None:
                        continue
                    j = pick
                    tcur = pick_t
                    while True:
                        oo = L[j]
                        tcur += oo.cost
                        oo.fin = tcur
                        done.add(id(oo))
                        taken[e].add(j)
                        order[e].append(oo)
                        npend -= 1
                        if oo.dma is not None:
                            dmafin[oo.dma[2]] = max(dmafin.get(oo.dma[2], 0.0), tcur + 2.5)
                        if oo.unit is None or j + 1 >= len(L) or L[j + 1].unit != oo.unit:
                            break
                        j += 1
                    free[e] = tcur
                    while heads[e] < len(L) and heads[e] in taken[e]:
                        heads[e] += 1
                    progressed = True
                if not progressed:
                    raise RuntimeError("scheduler deadlock in segment %d" % sg)
        for e in self.ENG:
            assert len(order[e]) == len(self.ops[e]), (e, len(order[e]), len(self.ops[e]))
            for i, o in enumerate(order[e]):
                o.pos = i
        self.order = order

    def emit(self, stack):
        nc = self.nc
        self.schedule()
        order = self.order
        for o in self.all:
            for d in o.deps:
                if d.eng == "tensor" and o.eng == "tensor":
                    continue
                d.inc = True
        barlast = {}
        for e in self.ENG:
            last = None
            for o in order[e]:
                if o.fn == "bar":
                    barlast[(e, o.seg)] = last
                elif o.dma is None:
                    last = o
        for v in barlast.values():
            if v is not None:
                v.inc = True
        esem = {e: stack.enter_context(nc.semaphore("es_" + e)) for e in self.ENG}
        for k in self.dsems:
            self.dsems[k] = stack.enter_context(nc.semaphore("ds_" + k[1]))
        cnt = {}
        for e in self.ENG:
            c_ = 0
            for o in order[e]:
                if o.inc and o.dma is None:
                    c_ += 1
                cnt[id(o)] = c_
        dma_at_bar = {}
        run_tot = {}
        segs = {}
        for o in self.all:
            if o.dma is not None:
                segs.setdefault(o.seg, {})
        tot = {}
        for sg in range(self.seg + 1):
            for o in self.all:
                pass
        cum = {}
        per_seg_tot = []
        cur = {}
        last_seg = 0
        for o in self.all:
            while last_seg < o.seg:
                per_seg_tot.append(dict(cur))
                last_seg += 1
            if o.dma is not None:
                cur[o.dma[2]] = cur.get(o.dma[2], 0) + 16
        while len(per_seg_tot) <= self.seg:
            per_seg_tot.append(dict(cur))
        prog = self

        def run(ename, eng):
            waited = {}

            def wait(key, sem, val):
                if val <= 0 or waited.get(key, 0) >= val:
                    return
                waited[key] = val
                eng.wait_ge(sem, val)

            for o in order[ename]:
                if o.fn == "bar":
                    for e2 in prog.ENG:
                        lo = barlast.get((e2, o.seg))
                        if lo is not None:
                            wait(e2, esem[e2], cnt[id(lo)])
                    for k, v in per_seg_tot[o.seg].items():
                        wait(k, prog.dsems[k], v)
                    continue
                need = {}
                for d in o.deps:
                    if d.eng == "tensor" and ename == "tensor":
                        continue
                    v = cnt[id(d)]
                    if need.get(d.eng, 0) < v:
                        need[d.eng] = v
                for k, v in need.items():
                    wait(k, esem[k], v)
                for k, v in o.ddeps.items():
                    wait(k, prog.dsems[k], v)
                if o.dma is not None:
                    out, in_, dk = o.dma
                    eng.dma_start(out=out, in_=in_).then_inc(prog.dsems[dk], 16)
                    continue
                ins = o.fn(eng)
                if o.inc:
                    ins.then_inc(esem[ename], 1)

        stack.enter_context(nc.allow_non_contiguous_dma("tiny strided parameter loads"))
        block = stack.enter_context(nc.Block())

        @block.sync
        def _(e):
            run("sync", e)

        @block.scalar
        def _(e):
            run("scalar", e)

        @block.vector
        def _(e):
            run("vector", e)

        @block.gpsimd
        def _(e):
            run("gpsimd", e)

        @block.tensor
        def _(e):
            run("tensor", e)


def bcast(ap, shape, axis):
    return ap.unsqueeze(axis).to_broadcast(list(shape))


class K:
    def __init__(self, layers=(0, 1), phases="ABC", dbg=()):
        self.layers = layers
        self.phases = phases
        self.dbg = dbg


def build(layers=(0, 1), phases="ABC", dbg=(), sub="GML"):
    from contextlib import ExitStack
    nc = bass.Bass("TRN2", target_bir_lowering=False)
    P = Prog(nc)
    stack = ExitStack()

    def din(name, shape, dt=F32):
        return nc.dram_tensor(name, list(shape), dt, kind="ExternalInput").ap()

    x_d = din("x", [S, D])
    mem_d = din("mem", [256, D])
    pos_d = din("positions", [1, S], I32)
    w = {}
    for name, shape in [
        ("w_in", [2, D, IN_W]), ("w_out", [2, D, D]), ("gla_w_a2", [2, 16, 256]), ("gla_b_a", [2, 256]),
        ("gla_norm_g", [2, 256]), ("ml_conv_w", [2, 4, 512]), ("ml_b_i", [2, 4]), ("ml_b_f", [2, 4]),
        ("ml_norm_g", [2, 256]), ("mla_q_norm_g", [2, 256]), ("mla_w_uq", [2, 256, 768]),
        ("mla_kv_norm_g", [2, 128]), ("mla_w_ukv", [2, 128, 1024]), ("xa_w_q", [2, D, D]),
        ("xa_w_kv", [2, D, 2 * D]), ("xa_w_o", [2, D, D]), ("moe_w_group", [2, D, 4]), ("moe_b_group", [2, 4]),
        ("moe_w_router", [2, D, 32]), ("moe_b_router", [2, 32]), ("moe_w_gate", [2, 32, D, 256]),
        ("moe_w_up", [2, 32, D, 256]), ("moe_w_down", [2, 32, 256, D]),
        ("ln1_g", [2, D]), ("ln1_b", [2, D]), ("ln2_g", [2, D]), ("ln2_b", [2, D]), ("ln3_g", [2, D]), ("ln3_b", [2, D]),
    ]:
        w[name] = din(name, shape)
    cmat_d = din("cmat", [128, 5, 128])
    sel_d = din("sel", [32, 32, 128])
    ropeinv_d = din("ropeinv", [96, 1])
    out_d = nc.dram_tensor("out", [S, D], F32, kind="ExternalOutput").ap()
    dbg_d = {}
    for name, shape in dbg:
        dbg_d[name] = nc.dram_tensor(name, list(shape), F32, kind="ExternalOutput").ap()

    def sb(name, shape, dt=F32):
        return P.tile(name, stack.enter_context(nc.sbuf_tensor(name, list(shape), dt)))

    x_tm = sb("x_tm", [128, NT, D])
    xT = sb("xT", [128, 8, S], BF16)
    cmat = sb("cmat_sb", [128, 5, 128])
    cmat_bf = sb("cmat_bf", [128, 5, 128], BF16)
    lnp = sb("lnp", [128, 2, D])
    ps = [P.tile("ps%d" % i, stack.enter_context(nc.psum_tensor("ps%d" % i, [128, 512], F32))) for i in range(8)]
    for p_ in ps:
        p_.psum = True
    IDENT, TRII, TRIS, MMLA, ONES = range(5)

    def fsz(ap):
        try:
            return int(ap.free_size())
        except Exception:
            return 256

    def mm(out, lhsT, rhs, start, stop, reads, writes):
        n = fsz(rhs)
        cst = max(64, n) / 2400.0 * (4.0 if rhs.dtype == F32 else 1.0) + 0.012
        P.op("tensor", lambda e: e.matmul(out, lhsT, rhs, start=start, stop=stop), reads, writes, cost=cst,
             start=start, stop=stop)

    def tr(out, in_, ident, reads, writes):
        P.op("tensor", lambda e: e.transpose(out, in_, ident), reads, writes, cost=0.08)

    def act(out, in_, func, reads, writes, bias=None, scale=None, accum_out=None):
        kw = {}
        if bias is not None:
            kw["bias"] = bias
        if scale is not None:
            kw["scale"] = scale
        if accum_out is not None:
            kw["accum_out"] = accum_out
        P.op("scalar", lambda e: e.activation(out, in_, func, **kw), reads, writes, cost=0.25 + fsz(out) * 0.00085)

    def vcost(out, f=1.0):
        return 0.12 + fsz(out) * 0.00105 * f

    def tt(out, a, b, op, reads, writes, eng="vector"):
        P.op(eng, lambda e: e.tensor_tensor(out, a, b, op), reads, writes, cost=vcost(out))

    def ts(out, a, s1, op0, reads, writes, s2=None, op1=None, eng="vector"):
        if op1 is None:
            P.op(eng, lambda e: e.tensor_scalar(out, a, s1, None, op0), reads, writes, cost=vcost(out, 0.6))
        else:
            P.op(eng, lambda e: e.tensor_scalar(out, a, s1, s2, op0, op1), reads, writes, cost=vcost(out, 0.6))

    def stt(out, a, s, b, op0, op1, reads, writes):
        P.op("vector", lambda e: e.scalar_tensor_tensor(out, a, s, b, op0, op1), reads, writes, cost=vcost(out))

    def cp(out, in_, reads, writes, eng="vector"):
        if eng == "scalar":
            P.op("scalar", lambda e: e.copy(out, in_), reads, writes, cost=0.25 + fsz(out) * 0.00085)
        else:
            P.op(eng, lambda e: e.tensor_copy(out, in_), reads, writes, cost=vcost(out, 0.6))

    def red(out, in_, op, reads, writes, axis=AX.X):
        P.op("vector", lambda e: e.tensor_reduce(out, in_, axis, op), reads, writes, cost=vcost(in_))

    def load_cast(dst_tile, dst_ap, src_ap, sub=None):
        P.dma("gpsimd", dst_ap, src_ap, writes=[(dst_tile, sub)], semtile=dst_tile)

    def load(dst_tile, dst_ap, src_ap, sub=None, eng="sync"):
        P.dma(eng, dst_ap, src_ap, writes=[(dst_tile, sub)], semtile=dst_tile)

    load(cmat, cmat[:], cmat_d)
    cp(cmat_bf[:], cmat[:], [cmat], [cmat_bf])
    for t in range(NT):
        load(x_tm, x_tm[:, t, :], x_d[t * 128:(t + 1) * 128, :], sub=t, eng="sync")

    memT = sb("memT", [128, 8, 256], BF16)
    if "B" in phases:
        from contextlib import ExitStack as _ES
        pre = _ES()
        mem_f = P.tile("mem_f", pre.enter_context(nc.sbuf_tensor("mem_f", [128, 2, D], F32)))
        mem_b = P.tile("mem_b", pre.enter_context(nc.sbuf_tensor("mem_b", [128, 2, D], BF16)))
        load(mem_f, mem_f[:], mem_d.rearrange("(t p) d -> p t d", p=128))
        cp(mem_b[:], mem_f[:], [mem_f], [mem_b])
        pbm = ps[7].h.bitcast(BF16)
        for mt in range(2):
            for c8 in range(8):
                tr(pbm[:, c8 * 128:(c8 + 1) * 128], mem_b[:, mt, c8 * 128:(c8 + 1) * 128], cmat_bf[:, 0, :],
                   [mem_b, cmat_bf], [(ps[7], c8)])
            cp(memT[:, :, mt * 128:(mt + 1) * 128], pbm[:, :].rearrange("p (c n) -> p c n", c=8), [ps[7]], [(memT, mt)])
        P.barrier()
        pre.close()

    cs = None
    if "A" in phases and "L" in sub:
        import math
        from contextlib import ExitStack as _ES2
        cs = sb("rope_cs", [96, 2, S], BF16)
        pre2 = _ES2()

        def tmp(name, dt=F32):
            return P.tile(name, pre2.enter_context(nc.sbuf_tensor(name, [96, S], dt)))
        posi, ang, rr, kf, ki, mk = tmp("rp_posi", I32), tmp("rp_ang"), tmp("rp_r"), tmp("rp_kf"), tmp("rp_ki", I32), tmp("rp_m")
        rinv = P.tile("rp_inv", pre2.enter_context(nc.sbuf_tensor("rp_inv", [96, 1], F32)))
        R_ = slice(64, 96)
        load(rinv, rinv[:], ropeinv_d)
        P.dma("sync", posi[R_, :].unsqueeze(1), pos_d[0:1, :].partition_broadcast(32), writes=[posi], semtile=posi)
        cp(ang[R_, :], posi[R_, :], [posi], [ang])
        ts(ang[R_, :], ang[R_, :], rinv[R_, 0:1], ALU.mult, [ang, rinv], [ang])
        TWO_PI = 2.0 * math.pi
        C1 = 6.28125
        C2 = TWO_PI - C1
        for which, shift in ((1, 0.0), (0, math.pi / 2)):
            ts(rr[R_, :], ang[R_, :], shift, ALU.add, [ang], [rr])
            ts(kf[R_, :], rr[R_, :], 1.0 / TWO_PI, ALU.mult, [rr], [kf])
            cp(ki[R_, :], kf[R_, :], [kf], [ki])
            cp(kf[R_, :], ki[R_, :], [ki], [kf])
            stt(rr[R_, :], kf[R_, :], -C1, rr[R_, :], ALU.mult, ALU.add, [kf, rr], [rr])
            stt(rr[R_, :], kf[R_, :], -C2, rr[R_, :], ALU.mult, ALU.add, [kf, rr], [rr])
            ts(mk[R_, :], rr[R_, :], math.pi, ALU.is_gt, [rr], [mk])
            stt(rr[R_, :], mk[R_, :], -TWO_PI, rr[R_, :], ALU.mult, ALU.add, [mk, rr], [rr])
            ts(mk[R_, :], rr[R_, :], -math.pi, ALU.is_lt, [rr], [mk])
            stt(rr[R_, :], mk[R_, :], TWO_PI, rr[R_, :], ALU.mult, ALU.add, [mk, rr], [rr])
            ts(rr[R_, :], rr[R_, :], 3.141592, ALU.min, [rr], [rr], s2=-3.141592, op1=ALU.max)
            act(cs[R_, which, :], rr[R_, :], AF.Sin, [rr], [(cs, which)])
        P.barrier()
        pre2.close()

    lnw = sb("ln_work", [128, 16])
    xbf = sb("ln_xbf", [128, 2, D], BF16)
    ps_bf = [ps[i].h.bitcast(BF16) for i in range(8)]

    def load_ln(gname, bname, l):
        load(lnp, lnp[:, 0, :].unsqueeze(1), w[gname][l:l + 1, :].partition_broadcast(128), sub=0)
        load(lnp, lnp[:, 1, :].unsqueeze(1), w[bname][l:l + 1, :].partition_broadcast(128), sub=1)

    def layer_norm_tile(t, pbank):
        xt = x_tm[:, t, :]
        st = lnw[:, 0:12].rearrange("p (a b) -> p a b", a=2)
        for hh in range(2):
            P.op("vector", lambda e, hh=hh: e.bn_stats(st[:, hh, :], x_tm[:, t, hh * 512:(hh + 1) * 512]),
                 [(x_tm, t)], [(lnw, "st%d" % hh)])
        P.op("vector", lambda e: e.bn_aggr(lnw[:, 12:14], lnw[:, 0:12]), [(lnw, "st0"), (lnw, "st1")], [(lnw, "mv")])
        ts(lnw[:, 14:15], lnw[:, 13:14], LN_EPS, ALU.add, [(lnw, "mv")], [(lnw, "sd")])
        act(lnw[:, 14:15], lnw[:, 14:15], AF.Sqrt, [(lnw, "sd")], [(lnw, "sd")])
        P.op("vector", lambda e: e.reciprocal(lnw[:, 15:16], lnw[:, 14:15]), [(lnw, "sd")], [(lnw, "rs")])
        ts(xt, xt, lnw[:, 12:13], ALU.subtract, [(x_tm, t), (lnw, "mv"), (lnw, "rs")], [(x_tm, t)],
           s2=lnw[:, 15:16], op1=ALU.mult)
        tt(xt, xt, lnp[:, 0, :], ALU.mult, [(x_tm, t), (lnp, 0)], [(x_tm, t)])
        tt(xt, xt, lnp[:, 1, :], ALU.add, [(x_tm, t), (lnp, 1)], [(x_tm, t)])
        refresh_xT(t, pbank)

    def refresh_xT(t, pbank):
        xt = x_tm[:, t, :]
        b = t % 2
        cp(xbf[:, b, :], xt, [(x_tm, t)], [(xbf, b)], eng="scalar")
        pb = ps_bf[pbank]
        for c in range(8):
            tr(pb[:, c * 128:(c + 1) * 128], xbf[:, b, c * 128:(c + 1) * 128], cmat_bf[:, IDENT, :],
               [(xbf, b), cmat_bf], [(ps[pbank], c)])
        cp(xT[:, :, t * 128:(t + 1) * 128], pb[:, :].rearrange("p (c n) -> p c n", c=8),
           [ps[pbank]], [(xT, t)])

    def store_out():
        for t in range(NT):
            P.dma("sync", out_d[t * 128:(t + 1) * 128, :], x_tm[:, t, :],
                  reads=[(x_tm, t)], semtile=x_tm)

    ctx = dict(nc=nc, P=P, stack=stack, w=w, x_tm=x_tm, xT=xT, cmat=cmat, cmat_bf=cmat_bf, lnp=lnp, ps=ps,
               ps_bf=ps_bf, sb=sb, mm=mm, tr=tr, act=act, tt=tt, ts=ts, stt=stt, cp=cp, red=red,
               load=load, load_cast=load_cast, load_ln=load_ln, layer_norm_tile=layer_norm_tile,
               sel_d=sel_d, ropeinv_d=ropeinv_d, memT=memT, sub=sub, cs=cs, mem_d=mem_d, pos_d=pos_d, dbg_d=dbg_d)

    first = True
    for l in layers:
        if first:
            for t in range(NT):
                refresh_xT(t, 5 + t % 3)
        if "A" in phases:
            phase_A(ctx, l)
        if "B" in phases:
            phase_B(ctx, l)
        if "C" in phases:
            phase_C(ctx, l)
        first = False
    store_out()
    P.finish("sync")
    P.emit(stack)
    stack.close()
    return nc


def phase_C(c, l):
    from contextlib import ExitStack
    nc, P, w = c["nc"], c["P"], c["w"]
    x_tm, xT, ps, cmat, cmat_bf = c["x_tm"], c["xT"], c["ps"], c["cmat"], c["cmat_bf"]
    mm, tr, act, tt, ts, stt, cp, red = c["mm"], c["tr"], c["act"], c["tt"], c["ts"], c["stt"], c["cp"], c["red"]
    load, load_cast = c["load"], c["load_cast"]
    IDENT = 0
    ph = ExitStack()

    def sb(name, shape, dt=F32):
        return P.tile(name, ph.enter_context(nc.sbuf_tensor("%s_%d" % (name, l), list(shape), dt)))

    c["load_ln"]("ln3_g", "ln3_b", l)
    gateT = sb("c_gateT", [32, S], BF16)
    sel = sb("c_sel", [32, 32, 128], BF16)
    load_cast(sel, sel[:], c["sel_d"])
    ph_r = ExitStack()
    _sb_outer = sb

    def sb(name, shape, dt=F32):
        return P.tile(name, ph_r.enter_context(nc.sbuf_tensor("%s_%d" % (name, l), list(shape), dt)))
    wr = sb("c_wr", [128, 8, 36], BF16)
    load_cast(wr, wr[:, :, 0:4], w["moe_w_group"][l].rearrange("(kc p) n -> p kc n", p=128), sub="g")
    load_cast(wr, wr[:, :, 4:36], w["moe_w_router"][l].rearrange("(kc p) n -> p kc n", p=128), sub="r")
    rb = sb("c_rb", [128, 36])
    load(rb, rb[:, 0:4].unsqueeze(1), w["moe_b_group"][l:l + 1, :].partition_broadcast(128), sub="g")
    load(rb, rb[:, 4:36].unsqueeze(1), w["moe_b_router"][l:l + 1, :].partition_broadcast(128), sub="r")
    lg = sb("c_lg", [128, NT, 36])
    for half in range(2):
        pr = ps[half]
        for tl in range(8):
            t = half * 8 + tl
            for kc in range(8):
                mm(pr[:, tl * 36:(tl + 1) * 36], xT[:, kc, t * 128:(t + 1) * 128], wr[:, kc, :], kc == 0, kc == 7,
                   [(xT, t), wr], [(pr, tl)])
        tt(lg[:, half * 8:(half + 1) * 8, :], pr[:, 0:288].rearrange("p (t n) -> p t n", t=8),
           bcast(rb[:, :], [128, 8, 36], 1), ALU.add, [pr, rb], [(lg, half)])
    r1 = sb("c_r1", [128, NT, 64])
    lgg = lg[:, :, 0:4]
    lge = lg[:, :, 4:36].rearrange("p t (g e) -> p t g e", g=4)
    gmax, gsum, ohg, eg = r1[:, :, 0], r1[:, :, 1], r1[:, :, 4:8], r1[:, :, 8:12]
    red(gmax, lgg, ALU.max, [lg], [(r1, "gmax")])
    tt(eg, lgg, bcast(gmax, [128, NT, 4], 2), ALU.subtract, [lg, (r1, "gmax")], [(r1, "eg")])
    tt(ohg, lgg, bcast(gmax, [128, NT, 4], 2), ALU.is_equal, [lg, (r1, "gmax")], [(r1, "ohg")])
    act(eg, eg, AF.Exp, [(r1, "eg")], [(r1, "eg")])
    red(gsum, eg, ALU.add, [(r1, "eg")], [(r1, "gsum")])
    gp = r1[:, :, 2]
    P.op("vector", lambda e: e.reciprocal(gp, gsum), [(r1, "gsum")], [(r1, "gp")])
    tmp = sb("c_tmp", [128, NT, 4, 8])
    tt(tmp[:], lge, bcast(ohg, [128, NT, 4, 8], 3), ALU.mult, [lg, (r1, "ohg")], [tmp])
    esel = r1[:, :, 16:24]
    red(esel, tmp[:].rearrange("p t g e -> p t e g"), ALU.add, [tmp], [(r1, "esel")])
    m1, m2, dd = r1[:, :, 3], r1[:, :, 12], r1[:, :, 13]
    mk1, mk2, e2 = r1[:, :, 24:32], r1[:, :, 32:40], r1[:, :, 40:48]
    red(m1, esel, ALU.max, [(r1, "esel")], [(r1, "m1")])
    tt(mk1, esel, bcast(m1, [128, NT, 8], 2), ALU.is_equal, [(r1, "esel"), (r1, "m1")], [(r1, "mk1")])
    stt(e2, mk1, -1e30, esel, ALU.mult, ALU.add, [(r1, "mk1"), (r1, "esel")], [(r1, "e2")])
    red(m2, e2, ALU.max, [(r1, "e2")], [(r1, "m2")])
    tt(mk2, e2, bcast(m2, [128, NT, 8], 2), ALU.is_equal, [(r1, "e2"), (r1, "m2")], [(r1, "mk2")])
    tt(dd, m2, m1, ALU.subtract, [(r1, "m1"), (r1, "m2")], [(r1, "dd")])
    act(dd, dd, AF.Exp, [(r1, "dd")], [(r1, "dd")])
    w1, w2 = r1[:, :, 14], r1[:, :, 15]
    ts(w1, dd, 1.0, ALU.add, [(r1, "dd")], [(r1, "w1")])
    P.op("vector", lambda e: e.reciprocal(w1, w1), [(r1, "w1")], [(r1, "w1")])
    tt(w1, w1, gp, ALU.mult, [(r1, "w1"), (r1, "gp")], [(r1, "w1")])
    tt(w2, w1, dd, ALU.mult, [(r1, "w1"), (r1, "dd")], [(r1, "w2")])
    comb = r1[:, :, 48:56]
    tt(comb, mk1, bcast(w1, [128, NT, 8], 2), ALU.mult, [(r1, "mk1"), (r1, "w1")], [(r1, "comb")])
    tt(mk2, mk2, bcast(w2, [128, NT, 8], 2), ALU.mult, [(r1, "mk2"), (r1, "w2")], [(r1, "mk2")])
    tt(comb, comb, mk2, ALU.add, [(r1, "comb"), (r1, "mk2")], [(r1, "comb")])
    gate = sb("c_gate", [128, NT, 4, 8])
    tt(gate[:], bcast(ohg, [128, NT, 4, 8], 3), bcast(comb, [128, NT, 4, 8], 2), ALU.mult,
       [(r1, "ohg"), (r1, "comb")], [gate])
    for g in range(4):
        pg = ps[2 + g % 2]
        for tl in range(4):
            t = g * 4 + tl
            tr(pg[0:32, tl * 128:(tl + 1) * 128], gate[:, t, :, :].rearrange("p g e -> p (g e)"), cmat[:, IDENT, :],
               [gate, cmat], [(pg, tl)])
        cp(gateT[:, g * 512:(g + 1) * 512], pg[0:32, :], [pg], [(gateT, g)], eng="scalar")
    if "c_gate" in c["dbg_d"]:
        P.dma("sync", c["dbg_d"]["c_gate"].rearrange("(t p) n -> p t n", p=128),
              gate[:].rearrange("p t g e -> p t (g e)"), reads=[gate], semtile=gate)

    P.barrier()
    ph_r.close()
    sb = _sb_outer
    NSLOT = 4
    wg = sb("c_wg", [128, NSLOT, 8, 256], BF16)
    wu = sb("c_wu", [128, NSLOT, 8, 256], BF16)
    wd = sb("c_wd", [128, NSLOT, 2, D], BF16)
    wsem = [sb("c_wsem%d" % i, [1, 1]) for i in range(NSLOT)]
    hT = sb("c_hT", [128, 2, 2, S], BF16)
    sg = sb("c_sg", [128, 2, 512], BF16)
    gb = sb("c_gb", [128, 2, 512], BF16)

    def load_expert(e):
        s = e % NSLOT
        P.dma("gpsimd", wg[:, s, :, :], w["moe_w_gate"][l, e].rearrange("(kc p) n -> p kc n", p=128),
              writes=[(wg, s)], semtile=wsem[s])
        P.dma("gpsimd", wu[:, s, :, :], w["moe_w_up"][l, e].rearrange("(kc p) n -> p kc n", p=128),
              writes=[(wu, s)], semtile=wsem[s])
        P.dma("gpsimd", wd[:, s, :, :], w["moe_w_down"][l, e].rearrange("(kc p) n -> p kc n", p=128),
              writes=[(wd, s)], semtile=wsem[s])

    for e in range(2):
        load_expert(e)
    unit = 0
    for blk in range(16):
        for ei in range(2):
            e = blk * 2 + ei
            s = e % NSLOT
            if e + 2 < 32:
                load_expert(e + 2)
            for g in range(4):
                tok = slice(g * 512, (g + 1) * 512)
                pgb = ps[4]
                mm(pgb[:, :], sel[:, e, :], gateT[:, tok], True, True, [sel, (gateT, g)], [pgb])
                ub = (e * 4 + g) % 2
                cp(gb[:, ub, :], pgb[:, :], [pgb], [(gb, ub)], eng="scalar")
                for fc in range(2):
                    pgt, put = ps[(unit % 2) * 2], ps[(unit % 2) * 2 + 1]
                    for kc in range(8):
                        mm(pgt[:, :], wg[:, s, kc, fc * 128:(fc + 1) * 128], xT[:, kc, tok], kc == 0, kc == 7,
                           [(wg, s), xT], [pgt])
                    for kc in range(8):
                        mm(put[:, :], wu[:, s, kc, fc * 128:(fc + 1) * 128], xT[:, kc, tok], kc == 0, kc == 7,
                           [(wu, s), xT], [put])
                    u2 = unit % 2
                    act(sg[:, u2, :], pgt[:, :], AF.Silu, [pgt], [(sg, u2)])
                    tt(sg[:, u2, :], put[:, :], sg[:, u2, :], ALU.mult, [put, (sg, u2)], [(sg, u2)])
                    tt(hT[:, ei, fc, tok], sg[:, u2, :], gb[:, ub, :], ALU.mult, [(sg, u2), (gb, ub)],
                       [(hT, (ei, g))])
                    unit += 1
        for t in range(NT):
            g = t // 4
            for hf in range(2):
                po = ps[5 + (t * 2 + hf) % 3]
                k = 0
                for ei in range(2):
                    s = (blk * 2 + ei) % NSLOT
                    for fc in range(2):
                        mm(po[:, :], hT[:, ei, fc, t * 128:(t + 1) * 128], wd[:, s, fc, hf * 512:(hf + 1) * 512],
                           k == 0, k == 3, [(hT, (ei, g)), (wd, s)], [po])
                        k += 1
                xs = x_tm[:, t, hf * 512:(hf + 1) * 512]
                if blk == 0:
                    stt(xs, xs, ALPHA, po[:, :], ALU.mult, ALU.add, [(x_tm, t), po], [(x_tm, t)])
                else:
                    tt(xs, xs, po[:, :], ALU.add, [(x_tm, t), po], [(x_tm, t)])
    for t in range(NT):
        c["layer_norm_tile"](t, 5 + t % 3)
    P.barrier()
    ph.close()


def host_consts():
    cm = np.zeros((128, 5, 128), np.float32)
    i = np.arange(128)
    cm[:, 0, :] = np.eye(128, dtype=np.float32)
    cm[:, 1, :] = (i[:, None] <= i[None, :]).astype(np.float32)
    cm[:, 2, :] = (i[:, None] > i[None, :]).astype(np.float32)
    cm[:, 3, :] = ((i[:, None] // 64) <= (i[None, :] // 64)).astype(np.float32)
    cm[:, 4, :] = 1.0
    sel = np.zeros((32, 32, 128), np.float32)
    for e in range(32):
        sel[e, e, :] = 1.0
    inv = (10000.0 ** (-np.arange(16, dtype=np.float32) / 16)).astype(np.float32)
    ri = np.zeros((96, 1), np.float32)
    ri[64:80, 0] = inv
    ri[80:96, 0] = inv
    return {"cmat": cm, "sel": sel, "ropeinv": ri}


_NC_CACHE = {}


def run_cores(inputs, n_cores=8, layers=(0, 1), phases="ABC", dbg=(), sub="GML"):
    key = (tuple(layers), phases, tuple(dbg), sub)
    if key not in _NC_CACHE:
        _NC_CACHE[key] = build(layers, phases, dbg, sub)
    nc = _NC_CACHE[key]
    consts = host_consts()
    shared = {k: np.ascontiguousarray(v) for k, v in inputs.items() if k not in ("x", "mem", "positions")}
    shared.update(consts)
    in_maps = []
    for b in range(n_cores):
        m = dict(shared)
        m["x"] = np.ascontiguousarray(inputs["x"][b])
        m["mem"] = np.ascontiguousarray(inputs["mem"][b])
        m["positions"] = np.ascontiguousarray(inputs["positions"][b:b + 1]).astype(np.int32)
        in_maps.append(m)
    res = run_bass_kernel_spmd(nc, in_maps, core_ids=list(range(n_cores)))
    return res.results


def kernel(**inputs):
    inputs = {k: np.asarray(v) for k, v in inputs.items()}
    res = run_cores(inputs, 8)
    return np.stack([r["out"] for r in res], axis=0).astype(np.float32)


def phase_B(c, l):
    from contextlib import ExitStack
    nc, P, w = c["nc"], c["P"], c["w"]
    x_tm, xT, ps, cmat_bf, memT = c["x_tm"], c["xT"], c["ps"], c["cmat_bf"], c["memT"]
    mm, act, tt, stt, cp = c["mm"], c["act"], c["tt"], c["stt"], c["cp"]
    load_cast = c["load_cast"]
    ONES = 4
    ph = ExitStack()

    def sb(name, shape, dt=F32):
        return P.tile(name, ph.enter_context(nc.sbuf_tensor("%s_%d" % (name, l), list(shape), dt)))

    c["load_ln"]("ln2_g", "ln2_b", l)
    kT = sb("b_kT", [128, 8, 256], BF16)
    vx = sb("b_v", [128, 2, D], BF16)
    ph2 = ExitStack()
    wkv = P.tile("b_wkv", ph2.enter_context(nc.sbuf_tensor("b_wkv_%d" % l, [128, 8, 2 * D], BF16)))
    for kc in range(8):
        load_cast(wkv, wkv[:, kc, :], w["xa_w_kv"][l, kc * 128:(kc + 1) * 128, :], sub=kc)
    for cc in range(8):
        pk = ps[cc % 2]
        for kc in range(8):
            mm(pk[:, 0:256], wkv[:, kc, cc * 128:(cc + 1) * 128], memT[:, kc, :], kc == 0, kc == 7, [wkv, memT], [pk])
        cp(kT[:, cc, :], pk[:, 0:256], [pk], [(kT, cc)], eng="scalar" if cc % 2 else "vector")
    for mt in range(2):
        for hf in range(2):
            pv = ps[2 + hf]
            for kc in range(8):
                mm(pv[:, :], memT[:, kc, mt * 128:(mt + 1) * 128], wkv[:, kc, D + hf * 512:D + (hf + 1) * 512],
                   kc == 0, kc == 7, [wkv, memT], [pv])
            cp(vx[:, mt, hf * 512:(hf + 1) * 512], pv[:, :], [pv], [(vx, (mt, hf))], eng="scalar" if hf else "vector")
    P.barrier()
    ph2.close()
    wq = sb("b_wq", [128, 8, D], BF16)
    wo = sb("b_wo", [128, 8, D], BF16)
    for kc in range(0, 8, 2):
        load_cast(wq, wq[:, kc:kc + 2, :], w["xa_w_q"][l, kc * 128:(kc + 2) * 128, :].rearrange("(k p) n -> p k n", p=128), sub=kc)
    for kc in range(0, 8, 2):
        load_cast(wo, wo[:, kc:kc + 2, :], w["xa_w_o"][l, kc * 128:(kc + 2) * 128, :].rearrange("(k p) n -> p k n", p=128), sub=kc)
    qT = sb("b_qT", [128, 8, 512], BF16)
    xaT = sb("b_xaT", [128, 8, 512], BF16)
    PT = sb("b_PT", [128, 2, 512], BF16)
    rden = sb("b_rden", [128, 2, 512])
    scale = 256 ** -0.5
    for g in range(4):
        tok = slice(g * 512, (g + 1) * 512)
        for cc in range(8):
            pq = ps[cc % 2]
            for kc in range(8):
                mm(pq[:, :], wq[:, kc, cc * 128:(cc + 1) * 128], xT[:, kc, tok], kc == 0, kc == 7, [wq, xT], [pq])
            cp(qT[:, cc, :], pq[:, :], [pq], [(qT, cc)], eng="scalar" if cc % 2 else "vector")
        for h in range(4):
            for mt in range(2):
                pst = ps[2 + mt]
                for j in range(2):
                    mm(pst[:, :], kT[:, h * 2 + j, mt * 128:(mt + 1) * 128], qT[:, h * 2 + j, :], j == 0, j == 1,
                       [(kT, h * 2 + j), (qT, h * 2 + j)], [pst])
                act(PT[:, mt, :], pst[:, :], AF.Exp, [pst], [(PT, mt)], scale=scale)
            pden = ps[4]
            for mt in range(2):
                mm(pden[:, :], cmat_bf[:, ONES, :], PT[:, mt, :], mt == 0, mt == 1, [cmat_bf, (PT, mt)], [pden])
            rb = h % 2
            act(rden[:, rb, :], pden[:, :], AF.Ln, [pden], [(rden, rb)])
            act(rden[:, rb, :], rden[:, rb, :], AF.Exp, [(rden, rb)], [(rden, rb)], scale=-1.0)
            for j in range(2):
                po = ps[5 + j]
                for mt in range(2):
                    mm(po[:, :], vx[:, mt, h * 256 + j * 128:h * 256 + (j + 1) * 128], PT[:, mt, :], mt == 0, mt == 1,
                       [vx, (PT, mt)], [po])
                tt(xaT[:, h * 2 + j, :], po[:, :], rden[:, rb, :], ALU.mult, [po, (rden, rb)], [(xaT, h * 2 + j)])
        for tl in range(4):
            t = g * 4 + tl
            for hf in range(2):
                pp = ps[hf]
                for cc in range(8):
                    mm(pp[:, :], xaT[:, cc, tl * 128:(tl + 1) * 128], wo[:, cc, hf * 512:(hf + 1) * 512], cc == 0, cc == 7,
                       [xaT, wo], [pp])
                xs = x_tm[:, t, hf * 512:(hf + 1) * 512]
                stt(xs, xs, ALPHA, pp[:, :], ALU.mult, ALU.add, [(x_tm, t), pp], [(x_tm, t)])
            c["layer_norm_tile"](t, 7)
    P.barrier()
    ph.close()


def head_norm_gate(c, sbf, name, src, gs, out_bf, b, keyp):
    P, tt, ts, red, act = c["P"], c["tt"], c["ts"], c["red"], c["act"]
    st = sbf["hn_st"]
    cen = sbf["hn_cen"]
    sq = sbf["hn_sq"]
    s4 = src.rearrange("p (h e) -> p h e", h=4)
    mean = st[:, b, 0:4]
    var = st[:, b, 4:8]
    red(mean, s4, ALU.add, [keyp], [(st, (b, "m"))])
    ts(mean, mean, -1.0 / 64, ALU.mult, [(st, (b, "m"))], [(st, (b, "m"))])
    c4 = cen[:, b, :].rearrange("p (h e) -> p h e", h=4)
    tt(c4, s4, bcast(mean, [128, 4, 64], 2), ALU.add, [keyp, (st, (b, "m"))], [(cen, b)])
    tt(sq[:, b, :], cen[:, b, :], cen[:, b, :], ALU.mult, [(cen, b)], [(sq, b)])
    red(var, sq[:, b, :].rearrange("p (h e) -> p h e", h=4), ALU.add, [(sq, b)], [(st, (b, "v"))])
    ts(var, var, 1.0 / 64, ALU.mult, [(st, (b, "v"))], [(st, (b, "v"))], s2=LN_EPS, op1=ALU.add)
    act(var, var, AF.Ln, [(st, (b, "v"))], [(st, (b, "v"))])
    act(var, var, AF.Exp, [(st, (b, "v"))], [(st, (b, "v"))], scale=-0.5)
    tt(c4, c4, bcast(var, [128, 4, 64], 2), ALU.mult, [(cen, b), (st, (b, "v"))], [(cen, b)])
    tt(out_bf, cen[:, b, :], gs, ALU.mult, [(cen, b), (sbf["gs"], b)], [(sbf["obf"], b)])


def phase_A(c, l):
    from contextlib import ExitStack
    nc, P, w = c["nc"], c["P"], c["w"]
    sub = c.get("sub", "GML")
    c["first_mixer"] = True
    if "G" in sub:
        mixer_gla(c, l)
        c["first_mixer"] = False
    if "M" in sub:
        mixer_mlstm(c, l)
        c["first_mixer"] = False
    if "L" in sub:
        mixer_mla(c, l)
    c["load_ln"]("ln1_g", "ln1_b", l)
    for t in range(NT):
        c["layer_norm_tile"](t, 5 + t % 3)
    P.barrier()


def mixer_gla(c, l):
    from contextlib import ExitStack
    nc, P, w = c["nc"], c["P"], c["w"]
    x_tm, xT, ps, ps_bf, cmat, cmat_bf = c["x_tm"], c["xT"], c["ps"], c["ps_bf"], c["cmat"], c["cmat_bf"]
    mm, tr, act, tt, ts, stt, cp, red = c["mm"], c["tr"], c["act"], c["tt"], c["ts"], c["stt"], c["cp"], c["red"]
    load, load_cast = c["load"], c["load_cast"]
    IDENT, TRII, TRIS = 0, 1, 2
    ph = ExitStack()

    def sb(name, shape, dt=F32):
        return P.tile(name, ph.enter_context(nc.sbuf_tensor("%s_%d" % (name, l), list(shape), dt)))

    win = sb("g_win", [128, 8, 1040], BF16)
    for kc in range(8):
        load_cast(win, win[:, kc, :], w["w_in"][l, kc * 128:(kc + 1) * 128, 0:1040], sub=kc)
    wa2 = sb("g_wa2", [16, 256], BF16)
    load_cast(wa2, wa2[0:16, :], w["gla_w_a2"][l])
    babc = sb("g_babc", [128, 256])
    load(babc, babc[:].unsqueeze(1), w["gla_b_a"][l:l + 1, :].partition_broadcast(128))
    wout = sb("g_wout", [128, 2, D], BF16)
    load_cast(wout, wout[:], w["w_out"][l, 0:256, :].rearrange("(k p) n -> p k n", p=128))
    gng = sb("g_gng", [128, 256])
    load(gng, gng[:].unsqueeze(1), w["gla_norm_g"][l:l + 1, :].partition_broadcast(128))
    NB = 3
    gaT = sb("g_gaT", [16, NB, 128], BF16)
    Lsb = sb("g_L", [128, NB, 256])
    E1 = sb("g_E1", [128, NB, 256])
    E2 = sb("g_E2", [128, NB, 256])
    E3 = sb("g_E3", [128, NB, 256])
    qs = [sb("g_qs0", [128, NB, 256], BF16), sb("g_qs1", [128, NB, 256], BF16)]
    for i in range(2):
        P.op("vector", lambda e, i=i: e.memset(qs[i][:], 0.0), [], [qs[i]])
    ksT = sb("g_ksT", [128, NB, 256], BF16)
    k2 = sb("g_k2", [128, NB, 256], BF16)
    vsb = sb("g_v", [128, NB, 256], BF16)
    gs = sb("g_gs", [128, NB, 256])
    PT = sb("g_PT", [128, 2, 4, 128], BF16)
    osb = sb("g_osb", [128, NB, 256])
    obf = sb("g_obf", [128, NB, 256], BF16)
    ogT = sb("g_ogT", [128, 2, 2, 128], BF16)
    Dend = sb("g_Dend", [128, NT, 2])
    Sst = sb("g_S", [128, 2, 64])
    Sbf = sb("g_Sbf", [128, 2, 2, 64], BF16)
    P.op("vector", lambda e: e.memset(Sst[:], 0.0), [], [Sst])
    P.op("vector", lambda e: e.memset(Sbf[:], 0.0), [], [Sbf])
    sbf = dict(hn_st=sb("g_hn_st", [128, NB, 8]), hn_cen=sb("g_hn_cen", [128, NB, 256]),
               hn_sq=sb("g_hn_sq", [128, NB, 256]), gs=gs, obf=obf)
    import os
    NTL = int(os.environ.get("DBG_TILES", NT))
    A_, B_, C_, D_, E_, F_, G_, H_ = ps
    pbH = ps_bf[7]

    def S0(t):
        b = t % NB
        tok = slice(t * 128, (t + 1) * 128)
        for cc in range(4):
            for kc in range(8):
                mm(A_[:, cc * 128:(cc + 1) * 128], win[:, kc, cc * 128:(cc + 1) * 128], xT[:, kc, tok], kc == 0, kc == 7,
                   [win, xT], [A_])
        for kc in range(8):
            mm(B_[0:16, 256:384], win[:, kc, 1024:1040], xT[:, kc, tok], kc == 0, kc == 7, [win, xT], [B_])
        cp(gaT[0:16, b, :], B_[0:16, 256:384], [B_], [(gaT, b)])
        mm(B_[:, 0:256], gaT[0:16, b, :], wa2[0:16, :], True, True, [(gaT, b), wa2], [B_])
        tt(Lsb[:, b, :], B_[:, 0:256], babc[:, :], ALU.add, [B_, babc], [(Lsb, b)])
        act(Lsb[:, b, :], Lsb[:, b, :], AF.Exp, [(Lsb, b)], [(Lsb, b)], scale=-1.0)
        ts(Lsb[:, b, :], Lsb[:, b, :], 1.0, ALU.add, [(Lsb, b)], [(Lsb, b)])
        act(Lsb[:, b, :], Lsb[:, b, :], AF.Ln, [(Lsb, b)], [(Lsb, b)])
        for kc in range(8):
            mm(D_[:, :], xT[:, kc, tok], win[:, kc, 256:768], kc == 0, kc == 7, [win, xT], [D_])
        for kc in range(8):
            mm(E_[:, 0:256], xT[:, kc, tok], win[:, kc, 768:1024], kc == 0, kc == 7, [win, xT], [E_])
        cp(vsb[:, b, :], D_[:, 256:512], [D_], [(vsb, b)], eng="scalar")
        act(gs[:, b, :], E_[:, 0:256], AF.Silu, [E_], [(gs, b)])
        tt(gs[:, b, :], gs[:, b, :], gng[:, :], ALU.mult, [(gs, b), gng], [(gs, b)])
        for ch in range(2):
            mm(C_[:, ch * 128:(ch + 1) * 128], Lsb[:, b, ch * 128:(ch + 1) * 128], cmat[:, TRII, :], True, True,
               [(Lsb, b), cmat], [C_])
        mm(C_[:, 256:512], cmat[:, TRIS, :], Lsb[:, b, :], True, True, [(Lsb, b), cmat], [C_])
        act(E1[:, b, :], C_[:, 0:256], AF.Exp, [C_], [(E1, b)], scale=-1.0 / 16)
        act(E2[:, b, :], C_[:, 0:256], AF.Exp, [C_], [(E2, b)], scale=1.0 / 16)
        act(E3[:, b, :], C_[:, 256:512], AF.Exp, [C_], [(E3, b)], scale=-1.0 / 16)
        cp(Dend[:, t, :], E1[:, b, :].rearrange("p (c n) -> p c n", c=2)[:, :, 127], [(E1, b)], [(Dend, t)])
        for hp in range(2):
            rws = slice(hp * 64, (hp + 1) * 64)
            stt(qs[hp][rws, b, :], A_[rws, 0:256], 0.125, E1[rws, b, :], ALU.mult, ALU.mult, [A_, (E1, b)], [(qs[hp], b)])
        tt(ksT[:, b, :], A_[:, 256:512], E2[:, b, :], ALU.mult, [A_, (E2, b)], [(ksT, b)])
        tt(k2[:, b, :], D_[:, 0:256], E3[:, b, :], ALU.mult, [D_, (E3, b)], [(k2, b)])

    def S1(t):
        b = t % NB
        b2 = t % 2
        for h in range(4):
            hp, hc = h % 2, h // 2
            mm(F_[:, h * 128:(h + 1) * 128], ksT[:, b, hc * 128:(hc + 1) * 128],
               qs[hp][:, b, hc * 128:(hc + 1) * 128], True, True, [(ksT, b), (qs[hp], b)], [F_])
        for h in range(4):
            hc = h // 2
            mm(G_[:, h * 64:(h + 1) * 64], k2[:, b, hc * 128:(hc + 1) * 128], vsb[:, b, h * 64:(h + 1) * 64],
               True, True, [(k2, b), (vsb, b)], [G_])
        tt(PT[:, b2, :, :], F_[:, :].rearrange("p (h n) -> p h n", h=4), bcast(cmat[:, TRII, :], [128, 4, 128], 1),
           ALU.mult, [F_, cmat], [(PT, b2)])
        for h in range(4):
            hp, hc = h % 2, h // 2
            mm(G_[:, 256 + h * 64:256 + (h + 1) * 64], PT[:, b2, h, :], vsb[:, b, h * 64:(h + 1) * 64], True, False,
               [(PT, b2), (vsb, b)], [G_])
            mm(G_[:, 256 + h * 64:256 + (h + 1) * 64], qs[hp][:, b, hc * 128:(hc + 1) * 128],
               Sbf[:, b2, hc, :], False, True, [(qs[hp], b), (Sbf, b2)], [G_])
        for h in range(4):
            hp, hc = h % 2, h // 2
            rows = slice(hp * 64, (hp + 1) * 64)
            stt(Sst[rows, hc, :], Sst[rows, hc, :], Dend[rows, t, hc:hc + 1], G_[rows, h * 64:(h + 1) * 64],
                ALU.mult, ALU.add, [(Sst, h), (Dend, t), G_], [(Sst, h)])
        cp(Sbf[:, 1 - b2, :, :], Sst[:, :, :], [Sst], [(Sbf, 1 - b2)])
        cp(osb[:, b, :], G_[:, 256:512], [G_], [(osb, b)], eng="scalar")

    def S2(t):
        b = t % NB
        b2 = t % 2
        head_norm_gate(c, sbf, "g", osb[:, b, :], gs[:, b, :], obf[:, b, :], b, (osb, b))
        for ch in range(2):
            tr(pbH[:, ch * 128:(ch + 1) * 128], obf[:, b, ch * 128:(ch + 1) * 128], cmat_bf[:, IDENT, :],
               [(obf, b), cmat_bf], [H_])
        cp(ogT[:, b2, :, :], pbH[:, 0:256].rearrange("p (c n) -> p c n", c=2), [H_], [(ogT, b2)])
        for q4 in range(4):
            pq = H_[:, 128:384]
            for ch in range(2):
                mm(pq, ogT[:, b2, ch, :], wout[:, ch, q4 * 256:(q4 + 1) * 256], ch == 0, ch == 1, [(ogT, b2), wout], [H_])
            xs = x_tm[:, t, q4 * 256:(q4 + 1) * 256]
            if c["first_mixer"]:
                stt(xs, xs, ALPHA, pq, ALU.mult, ALU.add, [(x_tm, t), H_], [(x_tm, t)])
            else:
                tt(xs, xs, pq, ALU.add, [(x_tm, t), H_], [(x_tm, t)])

    for step in range(NTL + 2):
        if 0 <= step - 2 < NTL:
            S2(step - 2)
        if 0 <= step - 1 < NTL:
            S1(step - 1)
        if step < NTL:
            S0(step)
    P.barrier()
    ph.close()


def mixer_mlstm(c, l):
    from contextlib import ExitStack
    import os
    nc, P, w = c["nc"], c["P"], c["w"]
    x_tm, xT, ps, ps_bf, cmat, cmat_bf = c["x_tm"], c["xT"], c["ps"], c["ps_bf"], c["cmat"], c["cmat_bf"]
    mm, tr, act, tt, ts, stt, cp, red = c["mm"], c["tr"], c["act"], c["tt"], c["ts"], c["stt"], c["cp"], c["red"]
    load, load_cast = c["load"], c["load_cast"]
    IDENT, TRII, ONES = 0, 1, 4
    ph = ExitStack()

    def sb(name, shape, dt=F32):
        return P.tile(name, ph.enter_context(nc.sbuf_tensor("%s_%d" % (name, l), list(shape), dt)))

    win = sb("m_win", [128, 8, 1032], BF16)
    for kc in range(8):
        load_cast(win, win[:, kc, :], w["w_in"][l, kc * 128:(kc + 1) * 128, 1040:2072], sub=kc)
    cw = sb("m_cw", [128, 4, 4])
    for j in range(4):
        load(cw, cw[:, :, j], w["ml_conv_w"][l, j, :].rearrange("(c p) -> p c", p=128), sub=j)
    bif = sb("m_bif", [128, 8])
    load(bif, bif[:, 0:4].unsqueeze(1), w["ml_b_i"][l:l + 1, :].partition_broadcast(128), sub=0)
    load(bif, bif[:, 4:8].unsqueeze(1), w["ml_b_f"][l:l + 1, :].partition_broadcast(128), sub=1)
    mng = sb("m_mng", [128, 256])
    load(mng, mng[:].unsqueeze(1), w["ml_norm_g"][l:l + 1, :].partition_broadcast(128))
    wout = sb("m_wout", [128, 2, D], BF16)
    load_cast(wout, wout[:], w["w_out"][l, 256:512, :].rearrange("(k p) n -> p k n", p=128))

    q = [sb("m_q0", [128, 2, S], BF16), sb("m_q1", [128, 2, S], BF16)]
    kT = sb("m_kT", [128, 2, S], BF16)
    ph1 = ExitStack()
    mqk = P.tile("m_mqk", ph1.enter_context(nc.sbuf_tensor("m_mqk_%d" % l, [128, 4, S + 3], BF16)))
    acc = P.tile("m_acc", ph1.enter_context(nc.sbuf_tensor("m_acc_%d" % l, [128, 2, 1024], F32)))
    P.op("vector", lambda e: e.memset(mqk[:, :, 0:3], 0.0), [], [mqk])
    for g in range(4):
        tok = slice(g * 512, (g + 1) * 512)
        for ch in range(4):
            pp = ps[(g * 4 + ch) % 2]
            for kc in range(8):
                mm(pp[:, :], win[:, kc, ch * 128:(ch + 1) * 128], xT[:, kc, tok], kc == 0, kc == 7, [win, xT], [pp])
            cp(mqk[:, ch, 3 + g * 512:3 + (g + 1) * 512], pp[:, :], [pp], [(mqk, ch)], eng="scalar" if ch % 2 else "vector")
    for i in range(2):
        P.op("vector", lambda e, i=i: e.memset(q[i][:], 0.0), [], [q[i]])
    for ch in range(4):
        for half in range(2):
            ai = half
            a = acc[:, ai, :]
            off = half * 1024
            ts(a, mqk[:, ch, off:off + 1024], cw[:, ch, 0:1], ALU.mult, [(mqk, ch), cw], [(acc, ai)])
            for j in range(1, 4):
                stt(a, mqk[:, ch, off + j:off + j + 1024], cw[:, ch, j:j + 1], a, ALU.mult, ALU.add,
                    [(mqk, ch), cw, (acc, ai)], [(acc, ai)])
            tokh = slice(off, off + 1024)
            if ch < 2:
                for hp in range(2):
                    rws = slice(hp * 64, (hp + 1) * 64)
                    act(q[hp][rws, ch, tokh], acc[rws, ai, :], AF.Silu, [(acc, ai)], [(q[hp], (ch, half))])
            else:
                act(kT[:, ch - 2, tokh], a, AF.Silu, [(acc, ai)], [(kT, (ch, half))])
    for hp in range(2):
        ts(q[hp][:], q[hp][:], 0.125, ALU.mult, [q[hp]], [q[hp]])
    P.barrier()
    ph1.close()

    gates = sb("m_gates", [128, NT, 8])
    pg = ps[2]
    for t in range(NT):
        for kc in range(8):
            mm(pg[:, t * 8:(t + 1) * 8], xT[:, kc, t * 128:(t + 1) * 128], win[:, kc, 1024:1032], kc == 0, kc == 7,
               [win, xT], [pg])
    tt(gates[:], pg[:, 0:128].rearrange("p (t n) -> p t n", t=NT), bcast(bif[:, :], [128, NT, 8], 1), ALU.add,
       [pg, bif], [gates])
    Lf = sb("m_Lf", [128, NT, 4])
    act(Lf[:], gates[:, :, 4:8], AF.Exp, [gates], [Lf], scale=-1.0)
    ts(Lf[:], Lf[:], 1.0, ALU.add, [Lf], [Lf])
    act(Lf[:], Lf[:], AF.Ln, [Lf], [Lf])
    Lf2 = Lf[:].rearrange("p t n -> p (t n)")
    p3 = ps[3]
    mm(p3[:, 0:64], cmat[:, TRII, :], Lf2, True, True, [cmat, Lf], [p3])
    asb = sb("m_a", [128, NT, 4])
    tt(asb[:], p3[:, 0:64].rearrange("p (t n) -> p t n", t=NT), gates[:, :, 0:4], ALU.add, [p3, gates], [asb])
    cumL = sb("m_cumL", [128, 64])
    cp(cumL[:], p3[:, 0:64], [p3], [cumL])
    a2 = asb[:].rearrange("p t n -> p (t n)")
    p4 = ps[4]
    tr(p4[0:64, 0:128], a2, cmat[:, IDENT, :], [asb, cmat], [p4])
    Acol = sb("m_Acol", [64, 1])
    red(Acol[:, 0:1], p4[0:64, 0:128], ALU.max, [p4], [Acol])
    rows = sb("m_rows", [1, 5, 64])
    p5 = ps[5]
    mm(p5[0:1, 0:64], Acol[0:64, 0:1], cmat[0:64, IDENT, 0:64], True, True, [Acol, cmat], [p5])
    mm(p5[0:1, 64:128], cmat[:, ONES, 0:1], Lf2, True, True, [cmat, Lf], [p5])
    cp(rows[0:1, 0:2, :], p5[0:1, 0:128].rearrange("p (a n) -> p a n", a=2), [p5], [rows])
    P.op("vector", lambda e: e.memset(rows[0:1, 2, 0:4], 0.0), [rows], [rows])
    for cc in range(NT):
        sl = slice(cc * 4, cc * 4 + 4)
        tt(rows[0:1, 3, sl], rows[0:1, 2, sl], rows[0:1, 0, sl], ALU.max, [rows], [rows])
        if cc < NT - 1:
            tt(rows[0:1, 2, (cc + 1) * 4:(cc + 1) * 4 + 4], rows[0:1, 3, sl], rows[0:1, 1, sl], ALU.subtract, [rows], [rows])
    tt(rows[0:1, 4, :], rows[0:1, 2, :], rows[0:1, 3, :], ALU.subtract, [rows], [rows])
    act(rows[0:1, 4, :], rows[0:1, 4, :], AF.Exp, [rows], [rows])
    p6 = ps[6]
    mm(p6[:, 0:128], cmat[0:1, ONES, :], rows[0:1, 3:5, :].rearrange("p a n -> p (a n)"), True, True, [cmat, rows], [p6])
    bcs = sb("m_bcs", [128, 128])
    cp(bcs[:], p6[:, 0:128], [p6], [bcs])
    wtok = sb("m_wtok", [128, 64])
    tt(wtok[:], a2, bcs[:, 0:64], ALU.subtract, [asb, bcs], [wtok])
    act(wtok[:], wtok[:], AF.Exp, [wtok], [wtok])
    clamp = sb("m_clamp", [128, 64])
    tt(clamp[:], cumL[:], bcs[:, 0:64], ALU.subtract, [cumL, bcs], [clamp])
    act(clamp[:], clamp[:], AF.Exp, [clamp], [clamp])

    vext = sb("m_vext", [128, 2, 4, 65], BF16)
    P.op("vector", lambda e: e.memset(vext[:], 1.0), [], [vext])
    vw = sb("m_vw", [128, 2, 4, 65], BF16)
    gso = sb("m_gso", [128, 2, 256])
    ktm = sb("m_ktm", [128, 2, 256], BF16)
    WM = sb("m_WM", [128, 2, 4, 128])
    PT = sb("m_PT", [128, 2, 4, 128], BF16)
    Cn = sb("m_Cn", [128, 2, 65])
    P.op("vector", lambda e: e.memset(Cn[:], 0.0), [], [Cn])
    Cd = sb("m_Cd", [128, 2, 65])
    Cdbf = sb("m_Cdbf", [128, 2, 2, 65], BF16)
    nd = sb("m_nd", [128, 2, 4, 65])
    hsb = sb("m_h", [128, 2, 256])
    small = sb("m_small", [128, 2, 8])
    obf = sb("m_obf", [128, 2, 256], BF16)
    ohT = sb("m_ohT", [128, 2, 2, 128], BF16)
    sbf = dict(hn_st=sb("m_hn_st", [128, 2, 8]), hn_cen=sb("m_hn_cen", [128, 2, 256]),
               hn_sq=sb("m_hn_sq", [128, 2, 256]), gs=gso, obf=obf)
    PA, PB, PC, PD, PE_, PO0, PO1, PX = ps
    for t in range(int(os.environ.get("DBG_TILES", NT))):
        b = t % 2
        tok = slice(t * 128, (t + 1) * 128)
        g4 = slice(t * 4, t * 4 + 4)
        for kc in range(8):
            mm(PA[:, :], xT[:, kc, tok], win[:, kc, 512:1024], kc == 0, kc == 7, [win, xT], [PA])
        v4 = PA[:, 0:256].rearrange("p (h e) -> p h e", h=4)
        cp(vext[:, b, :, 0:64], v4, [PA], [(vext, b)], eng="scalar")
        tt(vw[:, b, :, 0:64], v4, bcast(wtok[:, g4], [128, 4, 64], 2), ALU.mult, [PA, wtok], [(vw, b)])
        cp(vw[:, b, :, 64], wtok[:, g4], [wtok], [(vw, b)])
        act(gso[:, b, :], PA[:, 256:512], AF.Sigmoid, [PA], [(gso, b)])
        tt(gso[:, b, :], gso[:, b, :], mng[:, :], ALU.mult, [(gso, b), mng], [(gso, b)])
        pb = ps_bf[1]
        for hc in range(2):
            tr(pb[:, hc * 128:(hc + 1) * 128], kT[:, hc, tok], cmat_bf[:, IDENT, :], [kT, cmat_bf], [PB])
        cp(ktm[:, b, :], pb[:, 0:256], [PB], [(ktm, b)])
        for h in range(4):
            hp, hc = h % 2, h // 2
            mm(PC[:, h * 128:(h + 1) * 128], kT[:, hc, tok], q[hp][:, hc, tok], True, True, [kT, q[hp]], [PC])
        tt(WM[:, b, :, :], bcast(wtok[:, g4], [128, 4, 128], 2), bcast(cmat[:, TRII, :], [128, 4, 128], 1), ALU.mult,
           [wtok, cmat], [(WM, b)])
        tt(PT[:, b, :, :], PC[:, :].rearrange("p (h n) -> p h n", h=4), WM[:, b, :, :], ALU.mult, [PC, (WM, b)], [(PT, b)])
        for h in range(4):
            hc = h // 2
            mm(PD[:, h * 65:(h + 1) * 65], ktm[:, b, hc * 128:(hc + 1) * 128], vw[:, b, h, :], True, True,
               [(ktm, b), (vw, b)], [PD])
        for h in range(4):
            hp, hc = h % 2, h // 2
            rws = slice(hp * 64, (hp + 1) * 64)
            ts(Cd[rws, hc, :], Cn[rws, hc, :], bcs[rws, 64 + t * 4 + h:64 + t * 4 + h + 1], ALU.mult,
               [(Cn, h), bcs], [(Cd, h)])
        cp(Cdbf[:, b, :, :], Cd[:], [Cd], [(Cdbf, b)])
        for h in range(4):
            hp, hc = h % 2, h // 2
            mm(PE_[:, h * 65:(h + 1) * 65], PT[:, b, h, :], vext[:, b, h, :], True, False, [(PT, b), (vext, b)], [PE_])
            mm(PE_[:, h * 65:(h + 1) * 65], q[hp][:, hc, tok], Cdbf[:, b, hc, :], False, True, [q[hp], (Cdbf, b)], [PE_])
        for h in range(4):
            hp, hc = h % 2, h // 2
            rws = slice(hp * 64, (hp + 1) * 64)
            tt(Cn[rws, hc, :], Cd[rws, hc, :], PD[rws, h * 65:(h + 1) * 65], ALU.add, [(Cd, h), PD], [(Cn, h)])
        cp(nd[:, b, :, :], PE_[:, 0:260].rearrange("p (h e) -> p h e", h=4), [PE_], [(nd, b)], eng="scalar")
        stt(small[:, b, 0:4], nd[:, b, :, 64], -1.0, nd[:, b, :, 64], ALU.mult, ALU.max, [(nd, b)], [(small, b)])
        tt(small[:, b, 0:4], small[:, b, 0:4], clamp[:, g4], ALU.max, [(small, b), clamp], [(small, b)])
        P.op("vector", lambda e, b=b: e.reciprocal(small[:, b, 4:8], small[:, b, 0:4]), [(small, b)], [(small, b)])
        tt(hsb[:, b, :].rearrange("p (h e) -> p h e", h=4), nd[:, b, :, 0:64], bcast(small[:, b, 4:8], [128, 4, 64], 2),
           ALU.mult, [(nd, b), (small, b)], [(hsb, b)])
        head_norm_gate(c, sbf, "m", hsb[:, b, :], gso[:, b, :], obf[:, b, :], b, (hsb, b))
        pbx = ps_bf[7]
        for ch in range(2):
            tr(pbx[:, ch * 128:(ch + 1) * 128], obf[:, b, ch * 128:(ch + 1) * 128], cmat_bf[:, IDENT, :],
               [(obf, b), cmat_bf], [PX])
        cp(ohT[:, b, :, :], pbx[:, 0:256].rearrange("p (c n) -> p c n", c=2), [PX], [(ohT, b)])
        for hf in range(2):
            po = PO0 if hf == 0 else PO1
            for ch in range(2):
                mm(po[:, :], ohT[:, b, ch, :], wout[:, ch, hf * 512:(hf + 1) * 512], ch == 0, ch == 1, [(ohT, b), wout], [po])
            xs = x_tm[:, t, hf * 512:(hf + 1) * 512]
            if c["first_mixer"]:
                stt(xs, xs, ALPHA, po[:, :], ALU.mult, ALU.add, [(x_tm, t), po], [(x_tm, t)])
            else:
                tt(xs, xs, po[:, :], ALU.add, [(x_tm, t), po], [(x_tm, t)])
    P.barrier()
    ph.close()


def mixer_mla(c, l):
    from contextlib import ExitStack
    import os
    nc, P, w = c["nc"], c["P"], c["w"]
    x_tm, xT, ps, cmat, cmat_bf, cs = c["x_tm"], c["xT"], c["ps"], c["cmat"], c["cmat_bf"], c["cs"]
    mm, act, tt, ts, stt, cp = c["mm"], c["act"], c["tt"], c["ts"], c["stt"], c["cp"]
    load, load_cast = c["load"], c["load_cast"]
    MMLA, ONES = 3, 4
    R_ = slice(64, 96)
    ph = ExitStack()

    def sb(name, shape, dt=F32, st=None):
        return P.tile(name, (st or ph).enter_context(nc.sbuf_tensor("%s_%d" % (name, l), list(shape), dt)))

    cqnT = sb("a_cqnT", [128, 2, S], BF16)
    ckvnT = sb("a_ckvnT", [128, S], BF16)
    krope = sb("a_krope", [96, S], BF16)
    v_all = sb("a_vall", [128, NT, 512], BF16)
    gq = sb("a_gq", [128, 2])
    gkv = sb("a_gkv", [128, 1])
    load(gq, gq[:], w["mla_q_norm_g"][l].rearrange("(rc p) -> p rc", p=128))
    load(gkv, gkv[:], w["mla_kv_norm_g"][l].rearrange("(o p) -> p o", o=1))

    p1 = ExitStack()
    win = sb("a_win", [128, 8, 416], BF16, p1)
    for kc in range(8):
        load_cast(win, win[:, kc, :], w["w_in"][l, kc * 128:(kc + 1) * 128, 2072:2488], sub=kc)
    wkr = sb("a_wkr", [128, 8, 2, 96], BF16, p1)
    P.op("vector", lambda e: e.memset(wkr[:], 0.0), [], [wkr])
    cp(wkr[:, :, 0, 64:96], win[:, :, 384:416], [win], [wkr])
    ts(wkr[:, :, 1, 64:80], win[:, :, 400:416], -1.0, ALU.mult, [win], [wkr])
    cp(wkr[:, :, 1, 80:96], win[:, :, 384:400], [win], [wkr])
    sq = sb("a_sq", [128, 2, 512], BF16, p1)
    rstd = sb("a_rstd", [128, 2, 512], F32, p1)
    tA = sb("a_tA", [96, 512], F32, p1)
    tB = sb("a_tB", [96, 512], F32, p1)
    for g in range(4):
        tok = slice(g * 512, (g + 1) * 512)
        for rc in range(2):
            for kc in range(8):
                mm(ps[rc][:, :], win[:, kc, rc * 128:(rc + 1) * 128], xT[:, kc, tok], kc == 0, kc == 7, [win, xT], [ps[rc]])
        for rc in range(2):
            act(sq[:, rc, :], ps[rc][:, :], AF.Square, [ps[rc]], [(sq, rc)])
        for rc in range(2):
            mm(ps[2][:, :], cmat_bf[:, ONES, :], sq[:, rc, :], rc == 0, rc == 1, [cmat_bf, (sq, rc)], [ps[2]])
        ts(rstd[:, 0, :], ps[2][:, :], 1.0 / 256, ALU.mult, [ps[2]], [(rstd, 0)], s2=LN_EPS, op1=ALU.add)
        act(rstd[:, 0, :], rstd[:, 0, :], AF.Ln, [(rstd, 0)], [(rstd, 0)])
        act(rstd[:, 0, :], rstd[:, 0, :], AF.Exp, [(rstd, 0)], [(rstd, 0)], scale=-0.5)
        for rc in range(2):
            stt(cqnT[:, rc, tok], ps[rc][:, :], gq[:, rc:rc + 1], rstd[:, 0, :], ALU.mult, ALU.mult,
                [ps[rc], gq, (rstd, 0)], [(cqnT, (rc, g))])
        for kc in range(8):
            mm(ps[3][:, :], win[:, kc, 256:384], xT[:, kc, tok], kc == 0, kc == 7, [win, xT], [ps[3]])
        act(sq[:, 0, :], ps[3][:, :], AF.Square, [ps[3]], [(sq, 0)])
        mm(ps[4][:, :], cmat_bf[:, ONES, :], sq[:, 0, :], True, True, [cmat_bf, (sq, 0)], [ps[4]])
        ts(rstd[:, 1, :], ps[4][:, :], 1.0 / 128, ALU.mult, [ps[4]], [(rstd, 1)], s2=LN_EPS, op1=ALU.add)
        act(rstd[:, 1, :], rstd[:, 1, :], AF.Ln, [(rstd, 1)], [(rstd, 1)])
        act(rstd[:, 1, :], rstd[:, 1, :], AF.Exp, [(rstd, 1)], [(rstd, 1)], scale=-0.5)
        stt(ckvnT[:, tok], ps[3][:, :], gkv[:, 0:1], rstd[:, 1, :], ALU.mult, ALU.mult, [ps[3], gkv, (rstd, 1)], [(ckvnT, g)])
        for r2 in range(2):
            for kc in range(8):
                mm(ps[5 + r2][0:96, :], wkr[:, kc, r2, :], xT[:, kc, tok], kc == 0, kc == 7, [wkr, xT], [ps[5 + r2]])
        tt(tA[R_, :], ps[5][R_, :], cs[R_, 0, tok], ALU.mult, [ps[5], cs], [tA])
        tt(tB[R_, :], ps[6][R_, :], cs[R_, 1, tok], ALU.mult, [ps[6], cs], [tB])
        tt(krope[R_, tok], tA[R_, :], tB[R_, :], ALU.add, [tA, tB], [(krope, g)])
    P.barrier()
    p1.close()

    wuk = sb("a_wuk", [128, 8, 64], BF16)
    wuv = sb("a_wuv", [128, 8, 64], BF16)
    ukv = w["mla_w_ukv"][l].rearrange("p (h two d) -> p h two d", h=8, two=2)
    load_cast(wuk, wuk[:], ukv[:, :, 0, :])
    load_cast(wuv, wuv[:], ukv[:, :, 1, :])
    wuq = sb("a_wuq", [128, 2, 768], BF16)
    load_cast(wuq, wuq[:], w["mla_w_uq"][l].rearrange("(rc p) n -> p rc n", p=128))
    wuqr = sb("a_wuqr", [128, 2, 8, 96], BF16)
    P.op("vector", lambda e: e.memset(wuqr[:], 0.0), [], [wuqr])
    wq4 = wuq[:].rearrange("p r (h c) -> p r h c", h=8)
    ts(wuqr[:, :, :, 64:80], wq4[:, :, :, 80:96], -1.0, ALU.mult, [wuq], [wuqr])
    cp(wuqr[:, :, :, 80:96], wq4[:, :, :, 64:80], [wuq], [wuqr])
    wout = sb("a_wout", [128, 4, D], BF16)
    load_cast(wout, wout[:], w["w_out"][l, 512:1024, :].rearrange("(k p) n -> p k n", p=128))
    qTh = sb("a_qTh", [96, 2, 512], BF16)
    PTb = sb("a_PT", [128, 3, 512], BF16)
    mlaT = sb("a_mlaT", [128, 4, 512], BF16)
    rden = sb("a_rden", [128, 2, 512])
    tA2 = sb("a_tA2", [96, 2, 512])
    tB2 = sb("a_tB2", [96, 2, 512])
    KH = [("k", h) for h in range(8)]
    for g in range(4):
        tok = slice(g * 512, (g + 1) * 512)
        for h in range(8):
            pk = ps[h % 2]
            mm(pk[0:64, :], wuk[:, h, :], ckvnT[:, tok], True, True, [wuk, ckvnT], [pk])
            cp(xT[0:64, h, tok], pk[0:64, :], [pk], [(xT, KH[h])], eng="scalar" if h % 2 else "vector")
        cp(xT[R_, :, tok], bcast(krope[R_, tok], [32, 8, 512], 1), [krope], [(xT, k) for k in KH])
    for t in range(NT):
        pv = ps[2 + t % 2]
        mm(pv[:, :], ckvnT[:, t * 128:(t + 1) * 128], wuv[:].rearrange("p h d -> p (h d)"), True, True, [ckvnT, wuv], [pv])
        cp(v_all[:, t, :], pv[:, :], [pv], [(v_all, t)], eng="scalar" if t % 2 else "vector")
    scale = 96.0 ** -0.5
    NG = int(os.environ.get("DBG_GROUPS", 4))

    def qproj(g, h):
        tok = slice(g * 512, (g + 1) * 512)
        qb = h % 2
        pq, pr = ps[0], ps[1]
        for rc in range(2):
            mm(pq[0:96, :], wuq[:, rc, h * 96:(h + 1) * 96], cqnT[:, rc, tok], rc == 0, rc == 1, [wuq, cqnT], [pq])
        for rc in range(2):
            mm(pr[0:96, :], wuqr[:, rc, h, :], cqnT[:, rc, tok], rc == 0, rc == 1, [wuqr, cqnT], [pr])
        cp(qTh[0:64, qb, :], pq[0:64, :], [pq], [(qTh, qb)], eng="scalar")
        tt(tA2[R_, qb, :], pq[R_, :], cs[R_, 0, tok], ALU.mult, [pq, cs], [(tA2, qb)])
        tt(tB2[R_, qb, :], pr[R_, :], cs[R_, 1, tok], ALU.mult, [pr, cs], [(tB2, qb)])
        tt(qTh[R_, qb, :], tA2[R_, qb, :], tB2[R_, qb, :], ALU.add, [(tA2, qb), (tB2, qb)], [(qTh, qb)])

    def attend(g, h, nxt):
        nkt = 4 * g + 4
        hp, pair, qb = h % 2, h // 2, h % 2
        po, pd = ps[4 + h % 2], ps[6 + h % 2]

        def qk(kt):
            qlo = max(kt - 4 * g, 0) * 128
            N = 512 - qlo
            pst = ps[2 + kt % 2]
            mm(pst[:, 0:N], xT[0:96, h, kt * 128:(kt + 1) * 128], qTh[0:96, qb, qlo:512], True, True,
               [(xT, KH[h]), (qTh, qb)], [pst])

        qk(0)
        for kt in range(nkt):
            qlo = max(kt - 4 * g, 0) * 128
            N = 512 - qlo
            pst = ps[2 + kt % 2]
            pb = kt % 3
            if kt + 1 < nkt:
                qk(kt + 1)
            elif nxt is not None:
                qproj(*nxt)
            act(PTb[:, pb, 0:N], pst[:, 0:N], AF.Exp, [pst], [(PTb, pb)], scale=scale)
            if kt >= 4 * g:
                tt(PTb[:, pb, 0:128], PTb[:, pb, 0:128], cmat_bf[:, MMLA, :], ALU.mult, [(PTb, pb), cmat_bf], [(PTb, pb)])
            mm(po[:, qlo:512], v_all[:, kt, pair * 128:(pair + 1) * 128], PTb[:, pb, 0:N], kt == 0, kt == nkt - 1,
               [(v_all, kt), (PTb, pb)], [po])
            mm(pd[:, qlo:512], cmat_bf[:, ONES, :], PTb[:, pb, 0:N], kt == 0, kt == nkt - 1, [cmat_bf, (PTb, pb)], [pd])
        rws = slice(hp * 64, (hp + 1) * 64)
        act(rden[rws, qb, :], pd[rws, :], AF.Ln, [pd], [(rden, qb)])
        act(rden[rws, qb, :], rden[rws, qb, :], AF.Exp, [(rden, qb)], [(rden, qb)], scale=-1.0)
        tt(mlaT[rws, pair, :], po[rws, :], rden[rws, qb, :], ALU.mult, [po, (rden, qb)], [(mlaT, h)])

    work = [(g, h) for g in range(NG) for h in range(8)]
    qproj(*work[0])
    for i, (g, h) in enumerate(work):
        nxt = work[i + 1] if i + 1 < len(work) else None
        if nxt is not None and nxt[0] != g:
            attend(g, h, None)
        else:
            attend(g, h, nxt)
        if h != 7:
            continue
        if nxt is not None:
            qproj(*nxt)
        for tl in range(4):
            t = g * 4 + tl
            for hf in range(2):
                pp = ps[5 + 2 * hf]
                for pr_ in range(4):
                    mm(pp[:, :], mlaT[:, pr_, tl * 128:(tl + 1) * 128], wout[:, pr_, hf * 512:(hf + 1) * 512], pr_ == 0, pr_ == 3,
                       [mlaT, wout], [pp])
                xs = x_tm[:, t, hf * 512:(hf + 1) * 512]
                if c["first_mixer"]:
                    stt(xs, xs, ALPHA, pp[:, :], ALU.mult, ALU.add, [(x_tm, t), pp], [(x_tm, t)])
                else:
                    tt(xs, xs, pp[:, :], ALU.add, [(x_tm, t), pp], [(x_tm, t)])
    P.barrier()
    ph.close()
```

```python
import numpy as np
import ml_dtypes
import concourse.bass as bass
import concourse.mybir as mybir
from concourse.bass_utils import run_bass_kernel_spmd

F32 = mybir.dt.float32
BF16 = mybir.dt.bfloat16
I32 = mybir.dt.int32
AF = mybir.ActivationFunctionType
ALU = mybir.AluOpType
AX = mybir.AxisListType

S = 2048
D = 1024
NT = 16
DEPTH = 2
ALPHA = (2 * DEPTH) ** 0.25
LN_EPS = 1e-5
IN_W = 2488


class Tile:
    def __init__(self, name, h):
        self.name = name
        self.h = h
        self.st = {}
        self.dsem = None
        self.dcount = 0
        self.psum = False

    def __getitem__(self, k):
        return self.h[k]


class Op:
    __slots__ = ("eng", "fn", "deps", "ddeps", "inc", "dma", "cost", "unit", "seg", "oi", "fin", "pos")

    def __init__(self, eng, fn, deps, ddeps, dma=None, cost=0.2):
        self.eng = eng
        self.fn = fn
        self.deps = deps
        self.ddeps = ddeps
        self.inc = False
        self.dma = dma
        self.cost = cost
        self.unit = None
        self.seg = 0
        self.oi = 0
        self.fin = 0.0
        self.pos = 0


class Prog:
    ENG = ("sync", "scalar", "vector", "gpsimd", "tensor")
    REORDER = ("scalar", "vector", "tensor")

    def __init__(self, nc):
        self.nc = nc
        self.ops = {e: [] for e in self.ENG}
        self.all = []
        self.dsems = {}
        self.dtotal = {}
        self.tiles = []
        self.seg = 0
        self.cur_unit = None
        self.nunits = 0
        self.dma_by = {}

    def tile(self, name, h):
        t = Tile(name, h)
        try:
            ml = self.nc.lookup_mloc(h)
            sbuf = "SB" in str(ml.type)
            t.lo, t.hi = int(ml.addr), int(ml.addr) + int(ml.dims[1])
        except Exception:
            sbuf = False
        t.sbuf = sbuf
        if sbuf:
            inh = []
            for o in self.tiles:
                if getattr(o, "sbuf", False) and o.lo < t.hi and t.lo < o.hi:
                    for st in o.st.values():
                        if st[0] is not None:
                            inh.append(st[0])
                        inh.extend(st[1])
            if inh:
                seen = set()
                uniq = []
                for x in inh:
                    k = id(x) if isinstance(x, Op) else x
                    if k not in seen:
                        seen.add(k)
                        uniq.append(x)
                t.st[None] = [None, uniq]
        self.tiles.append(t)
        return t

    def merge(self, t):
        allr = []
        for st in t.st.values():
            if st[0] is not None:
                allr.append(st[0])
            allr.extend(st[1])
        seen, uniq = set(), []
        for x in allr:
            k = id(x) if isinstance(x, Op) else x
            if k not in seen:
                seen.add(k)
                uniq.append(x)
        t.st = {None: [None, uniq]}

    def soft_barrier(self):
        return

    @staticmethod
    def _norm(lst):
        out = []
        for x in lst:
            if not isinstance(x, tuple):
                x = (x, None)
            if x[0].psum:
                x = (x[0], None)
            out.append(x)
        return out

    @staticmethod
    def _states(t, s):
        if s is None:
            return list(t.st.values())
        r = []
        if None in t.st:
            r.append(t.st[None])
        if s in t.st:
            r.append(t.st[s])
        return r

    def _collect(self, reads, writes):
        deps, ddeps = [], {}

        def add(ev):
            if ev is None:
                return
            if isinstance(ev, Op):
                deps.append(ev)
            else:
                k = ev
                ddeps[k] = self.dtotal[k]

        for (t, s) in reads:
            for st in self._states(t, s):
                add(st[0])
        for (t, s) in writes:
            for st in self._states(t, s):
                add(st[0])
                for r in st[1]:
                    add(r)
        return deps, ddeps

    def _update(self, ev, reads, writes):
        for (t, s) in reads:
            st = t.st.setdefault(s, [None, []])
            st[1].append(ev)
        for (t, s) in writes:
            if s is None:
                t.st = {None: [ev, []]}
            else:
                t.st[s] = [ev, []]

    def _add(self, o):
        o.seg = self.seg
        o.oi = len(self.all)
        self.all.append(o)
        self.ops[o.eng].append(o)

    def op(self, eng, fn, reads=(), writes=(), cost=0.2, start=None, stop=None):
        reads = self._norm(reads)
        writes = self._norm(writes)
        writes = writes + [r for r in reads if r[0].psum]
        deps, ddeps = self._collect(reads, writes)
        o = Op(eng, fn, deps, ddeps, cost=cost)
        if eng == "tensor":
            if self.cur_unit is None or start is None or start:
                self.nunits += 1
                self.cur_unit = self.nunits
            o.unit = self.cur_unit
            if stop is None or stop:
                self.cur_unit = None
        self._add(o)
        self._update(o, reads, writes)

    def dma(self, eng, out, in_, reads=(), writes=(), semtile=None):
        assert eng in ("sync", "gpsimd")
        reads = self._norm(reads)
        writes = self._norm(writes)
        deps, ddeps = self._collect(reads, writes)
        if semtile.dsem is None:
            semtile.dsem = ("D", semtile.name)
            self.dsems[semtile.dsem] = None
        semtile.dcount = self.dtotal.get(semtile.dsem, 0) + 16
        self.dtotal[semtile.dsem] = semtile.dcount
        o = Op(eng, None, deps, ddeps, dma=(out, in_, semtile.dsem), cost=0.1)
        self.dma_by[(semtile.dsem, semtile.dcount)] = o
        self._add(o)
        self._update(semtile.dsem, reads, writes)

    def barrier(self):
        for e in self.ENG:
            o = Op(e, "bar", [], {}, cost=0.05)
            self._add(o)
        self.seg += 1
        for t in self.tiles:
            t.st = {}

    def finish(self, eng="sync"):
        self.barrier()

    def schedule(self):
        LAT = 0.25
        W = 64
        order = {e: [] for e in self.ENG}
        nseg = self.seg + 1
        byseg = [{e: [] for e in self.ENG} for _ in range(nseg)]
        for o in self.all:
            byseg[o.seg][o.eng].append(o)
        for sg in range(nseg):
            lists = byseg[sg]
            items = {e: [] for e in self.ENG}
            item_of = {}
            for e in self.ENG:
                cur = None
                for o in lists[e]:
                    if o.unit is not None and cur is not None and cur["unit"] == o.unit:
                        cur["ops"].append(o)
                    else:
                        cur = {"unit": o.unit, "ops": [o], "nrem": 0, "rt": 0.0, "succ": [], "eng": e, "done": False,
                               "bar": o.fn == "bar"}
                        items[e].append(cur)
                    item_of[id(o)] = cur
            dmafin = {}
            for e in self.ENG:
                for it in items[e]:
                    preds = set()
                    for o in it["ops"]:
                        dl = list(o.deps)
                        for k, v in o.ddeps.items():
                            dd = self.dma_by.get((k, v))
                            if dd is not None:
                                dl.append(dd)
                        for d in dl:
                            if d.seg != sg:
                                continue
                            p = item_of[id(d)]
                            if p is it:
                                continue
                            preds.add(id(p))
                            if id(p) not in it.setdefault("pset", {}):
                                it["pset"][id(p)] = p
                    for p in it.get("pset", {}).values():
                        p["succ"].append(it)
                    it["nrem"] = len(it.get("pset", {}))
            free = {e: 0.0 for e in self.ENG}
            heads = {e: 0 for e in self.ENG}
            npend = sum(len(v) for v in items.values())
            while npend > 0:
                progressed = False
                for e in self.ENG:
                    L = items[e]
                    while heads[e] < len(L) and L[heads[e]]["done"]:
                        heads[e] += 1
                    if heads[e] >= len(L):
                        continue
                    wmax = W if e in self.REORDER else 1
                    pick, pick_t = None, None
                    i, cnt = heads[e], 0
                    while i < len(L) and cnt < wmax:
                        it = L[i]
                        if not it["done"]:
                            cnt += 1
                            if it["bar"]:
                                if i == heads[e] and it["nrem"] == 0:
                                    pick, pick_t = it, free[e]
                                break
                            if it["nrem"] == 0:
                                rt = it["rt"]
                                for o in it["ops"]:
                                    for k in o.ddeps:
                                        rt = max(rt, dmafin.get(k, 0.0))
                                st_t = max(rt, free[e])
                                if pick is None or st_t < pick_t - 1e-9:
                                    pick, pick_t = it, st_t
                                if st_t <= free[e] + 1e-9:
                                    break
                        i += 1
                    if pick is None:
                        continue
                    tcur = pick_t
                    for o in pick["ops"]:
                        tcur += o.cost
                        o.fin = tcur
                        order[e].append(o)
                        if o.dma is not None:
                            dmafin[o.dma[2]] = max(dmafin.get(o.dma[2], 0.0), tcur + 2.5)
                    pick["done"] = True
                    npend -= 1
                    free[e] = tcur
                    for sc in pick["succ"]:
                        sc["nrem"] -= 1
                        lat = 0.0 if (sc["eng"] == "tensor" and e == "tensor") else LAT
                        if sc["rt"] < tcur + lat:
                            sc["rt"] = tcur + lat
                    progressed = True
                if not progressed:
                    raise RuntimeError("scheduler deadlock in segment %d" % sg)
        for e in self.ENG:
            assert len(order[e]) == len(self.ops[e]), (e, len(order[e]), len(self.ops[e]))
            for i, o in enumerate(order[e]):
                o.pos = i
        self.order = order

    def emit(self, stack):
        nc = self.nc
        self.schedule()
        order = self.order
        for o in self.all:
            for d in o.deps:
                if d.eng == "tensor" and o.eng == "tensor":
                    continue
                d.inc = True
        barlast = {}
        for e in self.ENG:
            last = None
            for o in order[e]:
                if o.fn == "bar":
                    barlast[(e, o.seg)] = last
                elif o.dma is None:
                    last = o
        for v in barlast.values():
            if v is not None:
                v.inc = True
        esem = {e: stack.enter_context(nc.semaphore("es_" + e)) for e in self.ENG}
        for k in self.dsems:
            self.dsems[k] = stack.enter_context(nc.semaphore("ds_" + k[1]))
        cnt = {}
        for e in self.ENG:
            c_ = 0
            for o in order[e]:
                if o.inc and o.dma is None:
                    c_ += 1
                cnt[id(o)] = c_
        dma_at_bar = {}
        run_tot = {}
        segs = {}
        for o in self.all:
            if o.dma is not None:
                segs.setdefault(o.seg, {})
        tot = {}
        for sg in range(self.seg + 1):
            for o in self.all:
                pass
        cum = {}
        per_seg_tot = []
        cur = {}
        last_seg = 0
        for o in self.all:
            while last_seg < o.seg:
                per_seg_tot.append(dict(cur))
                last_seg += 1
            if o.dma is not None:
                cur[o.dma[2]] = cur.get(o.dma[2], 0) + 16
        while len(per_seg_tot) <= self.seg:
            per_seg_tot.append(dict(cur))
        prog = self

        def run(ename, eng):
            waited = {}

            def wait(key, sem, val):
                if val <= 0 or waited.get(key, 0) >= val:
                    return
                waited[key] = val
                eng.wait_ge(sem, val)

            for o in order[ename]:
                if o.fn == "bar":
                    for e2 in prog.ENG:
                        lo = barlast.get((e2, o.seg))
                        if lo is not None:
                            wait(e2, esem[e2], cnt[id(lo)])
                    for k, v in per_seg_tot[o.seg].items():
                        wait(k, prog.dsems[k], v)
                    continue
                need = {}
                for d in o.deps:
                    if d.eng == "tensor" and ename == "tensor":
                        continue
                    v = cnt[id(d)]
                    if need.get(d.eng, 0) < v:
                        need[d.eng] = v
                for k, v in need.items():
                    wait(k, esem[k], v)
                for k, v in o.ddeps.items():
                    wait(k, prog.dsems[k], v)
                if o.dma is not None:
                    out, in_, dk = o.dma
                    eng.dma_start(out=out, in_=in_).then_inc(prog.dsems[dk], 16)
                    continue
                ins = o.fn(eng)
                if o.inc:
                    ins.then_inc(esem[ename], 1)

        stack.enter_context(nc.allow_non_contiguous_dma("tiny strided parameter loads"))
        block = stack.enter_context(nc.Block())

        @block.sync
        def _(e):
            run("sync", e)

        @block.scalar
        def _(e):
            run("scalar", e)

        @block.vector
        def _(e):
            run("vector", e)

        @block.gpsimd
        def _(e):
            run("gpsimd", e)

        @block.tensor
        def _(e):
            run("tensor", e)


def bcast(ap, shape, axis):
    return ap.unsqueeze(axis).to_broadcast(list(shape))


class K:
    def __init__(self, layers=(0, 1), phases="ABC", dbg=()):
        self.layers = layers
        self.phases = phases
        self.dbg = dbg


def build(layers=(0, 1), phases="ABC", dbg=(), sub="GML"):
    from contextlib import ExitStack
    nc = bass.Bass("TRN2", target_bir_lowering=False)
    P = Prog(nc)
    stack = ExitStack()

    def din(name, shape, dt=F32):
        return nc.dram_tensor(name, list(shape), dt, kind="ExternalInput").ap()

    x_d = din("x", [S, D])
    mem_d = din("mem", [256, D])
    pos_d = din("positions", [1, S], I32)
    w = {}
    for name, shape in [
        ("w_in", [2, D, IN_W]), ("w_out", [2, D, D]), ("gla_w_a2", [2, 16, 256]), ("gla_b_a", [2, 256]),
        ("gla_norm_g", [2, 256]), ("ml_conv_w", [2, 4, 512]), ("ml_b_i", [2, 4]), ("ml_b_f", [2, 4]),
        ("ml_norm_g", [2, 256]), ("mla_q_norm_g", [2, 256]), ("mla_w_uq", [2, 256, 768]),
        ("mla_kv_norm_g", [2, 128]), ("mla_w_ukv", [2, 128, 1024]), ("xa_w_q", [2, D, D]),
        ("xa_w_kv", [2, D, 2 * D]), ("xa_w_o", [2, D, D]), ("moe_w_group", [2, D, 4]), ("moe_b_group", [2, 4]),
        ("moe_w_router", [2, D, 32]), ("moe_b_router", [2, 32]), ("moe_w_gate", [2, 32, D, 256]),
        ("moe_w_up", [2, 32, D, 256]), ("moe_w_down", [2, 32, 256, D]),
        ("ln1_g", [2, D]), ("ln1_b", [2, D]), ("ln2_g", [2, D]), ("ln2_b", [2, D]), ("ln3_g", [2, D]), ("ln3_b", [2, D]),
    ]:
        w[name] = din(name, shape)
    cmat_d = din("cmat", [128, 5, 128])
    sel_d = din("sel", [32, 32, 128])
    ropeinv_d = din("ropeinv", [96, 1])
    out_d = nc.dram_tensor("out", [S, D], F32, kind="ExternalOutput").ap()
    dbg_d = {}
    for name, shape in dbg:
        dbg_d[name] = nc.dram_tensor(name, list(shape), F32, kind="ExternalOutput").ap()

    def sb(name, shape, dt=F32):
        return P.tile(name, stack.enter_context(nc.sbuf_tensor(name, list(shape), dt)))

    x_tm = sb("x_tm", [128, NT, D])
    xT = sb("xT", [128, 8, S], BF16)
    cmat = sb("cmat_sb", [128, 5, 128])
    cmat_bf = sb("cmat_bf", [128, 5, 128], BF16)
    lnp = sb("lnp", [128, 2, D])
    ps = [P.tile("ps%d" % i, stack.enter_context(nc.psum_tensor("ps%d" % i, [128, 512], F32))) for i in range(8)]
    for p_ in ps:
        p_.psum = True
    IDENT, TRII, TRIS, MMLA, ONES = range(5)

    def fsz(ap):
        try:
            return int(ap.free_size())
        except Exception:
            return 256

    def mm(out, lhsT, rhs, start, stop, reads, writes):
        n = fsz(rhs)
        cst = max(64, n) / 2400.0 * (4.0 if rhs.dtype == F32 else 1.0) + 0.012
        P.op("tensor", lambda e: e.matmul(out, lhsT, rhs, start=start, stop=stop), reads, writes, cost=cst,
             start=start, stop=stop)

    def tr(out, in_, ident, reads, writes):
        P.op("tensor", lambda e: e.transpose(out, in_, ident), reads, writes, cost=0.08)

    def act(out, in_, func, reads, writes, bias=None, scale=None, accum_out=None):
        kw = {}
        if bias is not None:
            kw["bias"] = bias
        if scale is not None:
            kw["scale"] = scale
        if accum_out is not None:
            kw["accum_out"] = accum_out
        P.op("scalar", lambda e: e.activation(out, in_, func, **kw), reads, writes, cost=0.25 + fsz(out) * 0.00085)

    def vcost(out, f=1.0):
        return 0.12 + fsz(out) * 0.00105 * f

    def tt(out, a, b, op, reads, writes, eng="vector"):
        P.op(eng, lambda e: e.tensor_tensor(out, a, b, op), reads, writes, cost=vcost(out))

    def ts(out, a, s1, op0, reads, writes, s2=None, op1=None, eng="vector"):
        if op1 is None:
            P.op(eng, lambda e: e.tensor_scalar(out, a, s1, None, op0), reads, writes, cost=vcost(out, 0.6))
        else:
            P.op(eng, lambda e: e.tensor_scalar(out, a, s1, s2, op0, op1), reads, writes, cost=vcost(out, 0.6))

    def stt(out, a, s, b, op0, op1, reads, writes):
        P.op("vector", lambda e: e.scalar_tensor_tensor(out, a, s, b, op0, op1), reads, writes, cost=vcost(out))

    def cp(out, in_, reads, writes, eng="vector"):
        if eng == "scalar":
            P.op("scalar", lambda e: e.copy(out, in_), reads, writes, cost=0.25 + fsz(out) * 0.00085)
        else:
            P.op(eng, lambda e: e.tensor_copy(out, in_), reads, writes, cost=vcost(out, 0.6))

    def red(out, in_, op, reads, writes, axis=AX.X):
        P.op("vector", lambda e: e.tensor_reduce(out, in_, axis, op), reads, writes, cost=vcost(in_))

    def load_cast(dst_tile, dst_ap, src_ap, sub=None):
        P.dma("gpsimd", dst_ap, src_ap, writes=[(dst_tile, sub)], semtile=dst_tile)

    def load(dst_tile, dst_ap, src_ap, sub=None, eng="sync"):
        P.dma(eng, dst_ap, src_ap, writes=[(dst_tile, sub)], semtile=dst_tile)

    load(cmat, cmat[:], cmat_d)
    cp(cmat_bf[:], cmat[:], [cmat], [cmat_bf])
    for t in range(NT):
        load(x_tm, x_tm[:, t, :], x_d[t * 128:(t + 1) * 128, :], sub=t, eng="sync")

    memT = sb("memT", [128, 8, 256], BF16)
    if "B" in phases:
        from contextlib import ExitStack as _ES
        pre = _ES()
        mem_f = P.tile("mem_f", pre.enter_context(nc.sbuf_tensor("mem_f", [128, 2, D], F32)))
        mem_b = P.tile("mem_b", pre.enter_context(nc.sbuf_tensor("mem_b", [128, 2, D], BF16)))
        load(mem_f, mem_f[:], mem_d.rearrange("(t p) d -> p t d", p=128))
        cp(mem_b[:], mem_f[:], [mem_f], [mem_b])
        pbm = ps[7].h.bitcast(BF16)
        for mt in range(2):
            for c8 in range(8):
                tr(pbm[:, c8 * 128:(c8 + 1) * 128], mem_b[:, mt, c8 * 128:(c8 + 1) * 128], cmat_bf[:, 0, :],
                   [mem_b, cmat_bf], [(ps[7], c8)])
            cp(memT[:, :, mt * 128:(mt + 1) * 128], pbm[:, :].rearrange("p (c n) -> p c n", c=8), [ps[7]], [(memT, mt)])
        P.soft_barrier()
        pre.close()

    cs = None
    if "A" in phases and "L" in sub:
        import math
        from contextlib import ExitStack as _ES2
        cs = sb("rope_cs", [96, 2, S], BF16)
        pre2 = _ES2()

        def tmp(name, dt=F32):
            return P.tile(name, pre2.enter_context(nc.sbuf_tensor(name, [96, S], dt)))
        posi, ang, rr, kf, ki, mk = tmp("rp_posi", I32), tmp("rp_ang"), tmp("rp_r"), tmp("rp_kf"), tmp("rp_ki", I32), tmp("rp_m")
        rinv = P.tile("rp_inv", pre2.enter_context(nc.sbuf_tensor("rp_inv", [96, 1], F32)))
        R_ = slice(64, 96)
        load(rinv, rinv[:], ropeinv_d)
        P.dma("sync", posi[R_, :].unsqueeze(1), pos_d[0:1, :].partition_broadcast(32), writes=[posi], semtile=posi)
        cp(ang[R_, :], posi[R_, :], [posi], [ang])
        ts(ang[R_, :], ang[R_, :], rinv[R_, 0:1], ALU.mult, [ang, rinv], [ang])
        TWO_PI = 2.0 * math.pi
        C1 = 6.28125
        C2 = TWO_PI - C1
        for which, shift in ((1, 0.0), (0, math.pi / 2)):
            ts(rr[R_, :], ang[R_, :], shift, ALU.add, [ang], [rr])
            ts(kf[R_, :], rr[R_, :], 1.0 / TWO_PI, ALU.mult, [rr], [kf])
            cp(ki[R_, :], kf[R_, :], [kf], [ki])
            cp(kf[R_, :], ki[R_, :], [ki], [kf])
            stt(rr[R_, :], kf[R_, :], -C1, rr[R_, :], ALU.mult, ALU.add, [kf, rr], [rr])
            stt(rr[R_, :], kf[R_, :], -C2, rr[R_, :], ALU.mult, ALU.add, [kf, rr], [rr])
            ts(mk[R_, :], rr[R_, :], math.pi, ALU.is_gt, [rr], [mk])
            stt(rr[R_, :], mk[R_, :], -TWO_PI, rr[R_, :], ALU.mult, ALU.add, [mk, rr], [rr])
            ts(mk[R_, :], rr[R_, :], -math.pi, ALU.is_lt, [rr], [mk])
            stt(rr[R_, :], mk[R_, :], TWO_PI, rr[R_, :], ALU.mult, ALU.add, [mk, rr], [rr])
            ts(rr[R_, :], rr[R_, :], 3.141592, ALU.min, [rr], [rr], s2=-3.141592, op1=ALU.max)
            act(cs[R_, which, :], rr[R_, :], AF.Sin, [rr], [(cs, which)])
        P.soft_barrier()
        pre2.close()

    lnw = sb("ln_work", [128, 16])
    xbf = sb("ln_xbf", [128, 2, D], BF16)
    ps_bf = [ps[i].h.bitcast(BF16) for i in range(8)]

    def load_ln(gname, bname, l):
        load(lnp, lnp[:, 0, :].unsqueeze(1), w[gname][l:l + 1, :].partition_broadcast(128), sub=0)
        load(lnp, lnp[:, 1, :].unsqueeze(1), w[bname][l:l + 1, :].partition_broadcast(128), sub=1)

    def layer_norm_tile(t, pbank):
        xt = x_tm[:, t, :]
        st = lnw[:, 0:12].rearrange("p (a b) -> p a b", a=2)
        for hh in range(2):
            P.op("vector", lambda e, hh=hh: e.bn_stats(st[:, hh, :], x_tm[:, t, hh * 512:(hh + 1) * 512]),
                 [(x_tm, t)], [(lnw, "st%d" % hh)])
        P.op("vector", lambda e: e.bn_aggr(lnw[:, 12:14], lnw[:, 0:12]), [(lnw, "st0"), (lnw, "st1")], [(lnw, "mv")])
        ts(lnw[:, 14:15], lnw[:, 13:14], LN_EPS, ALU.add, [(lnw, "mv")], [(lnw, "sd")])
        act(lnw[:, 14:15], lnw[:, 14:15], AF.Sqrt, [(lnw, "sd")], [(lnw, "sd")])
        P.op("vector", lambda e: e.reciprocal(lnw[:, 15:16], lnw[:, 14:15]), [(lnw, "sd")], [(lnw, "rs")])
        ts(xt, xt, lnw[:, 12:13], ALU.subtract, [(x_tm, t), (lnw, "mv"), (lnw, "rs")], [(x_tm, t)],
           s2=lnw[:, 15:16], op1=ALU.mult)
        tt(xt, xt, lnp[:, 0, :], ALU.mult, [(x_tm, t), (lnp, 0)], [(x_tm, t)])
        tt(xt, xt, lnp[:, 1, :], ALU.add, [(x_tm, t), (lnp, 1)], [(x_tm, t)])
        refresh_xT(t, pbank)

    def refresh_xT(t, pbank):
        xt = x_tm[:, t, :]
        b = t % 2
        cp(xbf[:, b, :], xt, [(x_tm, t)], [(xbf, b)], eng="scalar")
        pb = ps_bf[pbank]
        for c in range(8):
            tr(pb[:, c * 128:(c + 1) * 128], xbf[:, b, c * 128:(c + 1) * 128], cmat_bf[:, IDENT, :],
               [(xbf, b), cmat_bf], [(ps[pbank], c)])
        cp(xT[:, :, t * 128:(t + 1) * 128], pb[:, :].rearrange("p (c n) -> p c n", c=8),
           [ps[pbank]], [(xT, t)])

    def store_out():
        for t in range(NT):
            P.dma("sync", out_d[t * 128:(t + 1) * 128, :], x_tm[:, t, :],
                  reads=[(x_tm, t)], semtile=x_tm)

    ctx = dict(nc=nc, P=P, stack=stack, w=w, x_tm=x_tm, xT=xT, cmat=cmat, cmat_bf=cmat_bf, lnp=lnp, ps=ps,
               ps_bf=ps_bf, sb=sb, mm=mm, tr=tr, act=act, tt=tt, ts=ts, stt=stt, cp=cp, red=red,
               load=load, load_cast=load_cast, load_ln=load_ln, layer_norm_tile=layer_norm_tile,
               sel_d=sel_d, ropeinv_d=ropeinv_d, memT=memT, sub=sub, cs=cs, mem_d=mem_d, pos_d=pos_d, dbg_d=dbg_d)

    first = True
    for l in layers:
        if first:
            for t in range(NT):
                refresh_xT(t, 5 + t % 3)
        if "A" in phases:
            phase_A(ctx, l)
        if "B" in phases:
            phase_B(ctx, l)
        if "C" in phases:
            phase_C(ctx, l)
        first = False
    store_out()
    P.finish("sync")
    P.emit(stack)
    stack.close()
    return nc


def phase_C(c, l):
    from contextlib import ExitStack
    nc, P, w = c["nc"], c["P"], c["w"]
    x_tm, xT, ps, cmat, cmat_bf = c["x_tm"], c["xT"], c["ps"], c["cmat"], c["cmat_bf"]
    mm, tr, act, tt, ts, stt, cp, red = c["mm"], c["tr"], c["act"], c["tt"], c["ts"], c["stt"], c["cp"], c["red"]
    load, load_cast = c["load"], c["load_cast"]
    IDENT = 0
    ph = ExitStack()

    def sb(name, shape, dt=F32):
        return P.tile(name, ph.enter_context(nc.sbuf_tensor("%s_%d" % (name, l), list(shape), dt)))

    c["load_ln"]("ln3_g", "ln3_b", l)
    gateT = sb("c_gateT", [32, S], BF16)
    sel = sb("c_sel", [32, 32, 128], BF16)
    load_cast(sel, sel[:], c["sel_d"])
    ph_r = ExitStack()
    _sb_outer = sb

    def sb(name, shape, dt=F32):
        return P.tile(name, ph_r.enter_context(nc.sbuf_tensor("%s_%d" % (name, l), list(shape), dt)))
    wr = sb("c_wr", [128, 8, 36], BF16)
    load_cast(wr, wr[:, :, 0:4], w["moe_w_group"][l].rearrange("(kc p) n -> p kc n", p=128), sub="g")
    load_cast(wr, wr[:, :, 4:36], w["moe_w_router"][l].rearrange("(kc p) n -> p kc n", p=128), sub="r")
    rb = sb("c_rb", [128, 36])
    load(rb, rb[:, 0:4].unsqueeze(1), w["moe_b_group"][l:l + 1, :].partition_broadcast(128), sub="g")
    load(rb, rb[:, 4:36].unsqueeze(1), w["moe_b_router"][l:l + 1, :].partition_broadcast(128), sub="r")
    lg = sb("c_lg", [128, NT, 36])
    for half in range(2):
        pr = ps[half]
        for tl in range(8):
            t = half * 8 + tl
            for kc in range(8):
                mm(pr[:, tl * 36:(tl + 1) * 36], xT[:, kc, t * 128:(t + 1) * 128], wr[:, kc, :], kc == 0, kc == 7,
                   [(xT, t), wr], [(pr, tl)])
        tt(lg[:, half * 8:(half + 1) * 8, :], pr[:, 0:288].rearrange("p (t n) -> p t n", t=8),
           bcast(rb[:, :], [128, 8, 36], 1), ALU.add, [pr, rb], [(lg, half)])
    r1 = sb("c_r1", [128, NT, 64])
    lgg = lg[:, :, 0:4]
    lge = lg[:, :, 4:36].rearrange("p t (g e) -> p t g e", g=4)
    gmax, gsum, ohg, eg = r1[:, :, 0], r1[:, :, 1], r1[:, :, 4:8], r1[:, :, 8:12]
    red(gmax, lgg, ALU.max, [lg], [(r1, "gmax")])
    tt(eg, lgg, bcast(gmax, [128, NT, 4], 2), ALU.subtract, [lg, (r1, "gmax")], [(r1, "eg")])
    tt(ohg, lgg, bcast(gmax, [128, NT, 4], 2), ALU.is_equal, [lg, (r1, "gmax")], [(r1, "ohg")])
    act(eg, eg, AF.Exp, [(r1, "eg")], [(r1, "eg")])
    red(gsum, eg, ALU.add, [(r1, "eg")], [(r1, "gsum")])
    gp = r1[:, :, 2]
    P.op("vector", lambda e: e.reciprocal(gp, gsum), [(r1, "gsum")], [(r1, "gp")])
    tmp = sb("c_tmp", [128, NT, 4, 8])
    tt(tmp[:], lge, bcast(ohg, [128, NT, 4, 8], 3), ALU.mult, [lg, (r1, "ohg")], [tmp])
    esel = r1[:, :, 16:24]
    red(esel, tmp[:].rearrange("p t g e -> p t e g"), ALU.add, [tmp], [(r1, "esel")])
    m1, m2, dd = r1[:, :, 3], r1[:, :, 12], r1[:, :, 13]
    mk1, mk2, e2 = r1[:, :, 24:32], r1[:, :, 32:40], r1[:, :, 40:48]
    red(m1, esel, ALU.max, [(r1, "esel")], [(r1, "m1")])
    tt(mk1, esel, bcast(m1, [128, NT, 8], 2), ALU.is_equal, [(r1, "esel"), (r1, "m1")], [(r1, "mk1")])
    stt(e2, mk1, -1e30, esel, ALU.mult, ALU.add, [(r1, "mk1"), (r1, "esel")], [(r1, "e2")])
    red(m2, e2, ALU.max, [(r1, "e2")], [(r1, "m2")])
    tt(mk2, e2, bcast(m2, [128, NT, 8], 2), ALU.is_equal, [(r1, "e2"), (r1, "m2")], [(r1, "mk2")])
    tt(dd, m2, m1, ALU.subtract, [(r1, "m1"), (r1, "m2")], [(r1, "dd")])
    act(dd, dd, AF.Exp, [(r1, "dd")], [(r1, "dd")])
    w1, w2 = r1[:, :, 14], r1[:, :, 15]
    ts(w1, dd, 1.0, ALU.add, [(r1, "dd")], [(r1, "w1")])
    P.op("vector", lambda e: e.reciprocal(w1, w1), [(r1, "w1")], [(r1, "w1")])
    tt(w1, w1, gp, ALU.mult, [(r1, "w1"), (r1, "gp")], [(r1, "w1")])
    tt(w2, w1, dd, ALU.mult, [(r1, "w1"), (r1, "dd")], [(r1, "w2")])
    comb = r1[:, :, 48:56]
    tt(comb, mk1, bcast(w1, [128, NT, 8], 2), ALU.mult, [(r1, "mk1"), (r1, "w1")], [(r1, "comb")])
    tt(mk2, mk2, bcast(w2, [128, NT, 8], 2), ALU.mult, [(r1, "mk2"), (r1, "w2")], [(r1, "mk2")])
    tt(comb, comb, mk2, ALU.add, [(r1, "comb"), (r1, "mk2")], [(r1, "comb")])
    gate = sb("c_gate", [128, NT, 4, 8])
    tt(gate[:], bcast(ohg, [128, NT, 4, 8], 3), bcast(comb, [128, NT, 4, 8], 2), ALU.mult,
       [(r1, "ohg"), (r1, "comb")], [gate])
    for g in range(4):
        pg = ps[2 + g % 2]
        for tl in range(4):
            t = g * 4 + tl
            tr(pg[0:32, tl * 128:(tl + 1) * 128], gate[:, t, :, :].rearrange("p g e -> p (g e)"), cmat[:, IDENT, :],
               [gate, cmat], [(pg, tl)])
        cp(gateT[:, g * 512:(g + 1) * 512], pg[0:32, :], [pg], [(gateT, g)], eng="scalar")
    if "c_gate" in c["dbg_d"]:
        P.dma("sync", c["dbg_d"]["c_gate"].rearrange("(t p) n -> p t n", p=128),
              gate[:].rearrange("p t g e -> p t (g e)"), reads=[gate], semtile=gate)

    P.soft_barrier()
    ph_r.close()
    sb = _sb_outer
    NSLOT = 4
    wg = sb("c_wg", [128, NSLOT, 8, 256], BF16)
    wu = sb("c_wu", [128, NSLOT, 8, 256], BF16)
    wd = sb("c_wd", [128, NSLOT, 2, D], BF16)
    wsem = [sb("c_wsem%d" % i, [1, 1]) for i in range(NSLOT)]
    hT = sb("c_hT", [128, 2, 2, S], BF16)
    sg = sb("c_sg", [128, 2, 512], BF16)
    gb = sb("c_gb", [128, 2, 512], BF16)

    def load_expert(e):
        s = e % NSLOT
        P.dma("gpsimd", wg[:, s, :, :], w["moe_w_gate"][l, e].rearrange("(kc p) n -> p kc n", p=128),
              writes=[(wg, s)], semtile=wsem[s])
        P.dma("gpsimd", wu[:, s, :, :], w["moe_w_up"][l, e].rearrange("(kc p) n -> p kc n", p=128),
              writes=[(wu, s)], semtile=wsem[s])
        P.dma("gpsimd", wd[:, s, :, :], w["moe_w_down"][l, e].rearrange("(kc p) n -> p kc n", p=128),
              writes=[(wd, s)], semtile=wsem[s])

    for e in range(2):
        load_expert(e)
    unit = 0
    for blk in range(16):
        for ei in range(2):
            e = blk * 2 + ei
            s = e % NSLOT
            if e + 2 < 32:
                load_expert(e + 2)
            for g in range(4):
                tok = slice(g * 512, (g + 1) * 512)
                pgb = ps[4]
                mm(pgb[:, :], sel[:, e, :], gateT[:, tok], True, True, [sel, (gateT, g)], [pgb])
                ub = (e * 4 + g) % 2
                cp(gb[:, ub, :], pgb[:, :], [pgb], [(gb, ub)], eng="scalar")
                for fc in range(2):
                    pgt, put = ps[(unit % 2) * 2], ps[(unit % 2) * 2 + 1]
                    for kc in range(8):
                        mm(pgt[:, :], wg[:, s, kc, fc * 128:(fc + 1) * 128], xT[:, kc, tok], kc == 0, kc == 7,
                           [(wg, s), xT], [pgt])
                    for kc in range(8):
                        mm(put[:, :], wu[:, s, kc, fc * 128:(fc + 1) * 128], xT[:, kc, tok], kc == 0, kc == 7,
                           [(wu, s), xT], [put])
                    u2 = unit % 2
                    act(sg[:, u2, :], pgt[:, :], AF.Silu, [pgt], [(sg, u2)])
                    tt(sg[:, u2, :], put[:, :], sg[:, u2, :], ALU.mult, [put, (sg, u2)], [(sg, u2)])
                    tt(hT[:, ei, fc, tok], sg[:, u2, :], gb[:, ub, :], ALU.mult, [(sg, u2), (gb, ub)],
                       [(hT, (ei, g))])
                    unit += 1
        for t in range(NT):
            g = t // 4
            for hf in range(2):
                po = ps[5 + (t * 2 + hf) % 3]
                k = 0
                for ei in range(2):
                    s = (blk * 2 + ei) % NSLOT
                    for fc in range(2):
                        mm(po[:, :], hT[:, ei, fc, t * 128:(t + 1) * 128], wd[:, s, fc, hf * 512:(hf + 1) * 512],
                           k == 0, k == 3, [(hT, (ei, g)), (wd, s)], [po])
                        k += 1
                xs = x_tm[:, t, hf * 512:(hf + 1) * 512]
                if blk == 0:
                    stt(xs, xs, ALPHA, po[:, :], ALU.mult, ALU.add, [(x_tm, t), po], [(x_tm, t)])
                else:
                    tt(xs, xs, po[:, :], ALU.add, [(x_tm, t), po], [(x_tm, t)])
    for t in range(NT):
        c["layer_norm_tile"](t, 5 + t % 3)
    P.soft_barrier()
    ph.close()


def host_consts():
    cm = np.zeros((128, 5, 128), np.float32)
    i = np.arange(128)
    cm[:, 0, :] = np.eye(128, dtype=np.float32)
    cm[:, 1, :] = (i[:, None] <= i[None, :]).astype(np.float32)
    cm[:, 2, :] = (i[:, None] > i[None, :]).astype(np.float32)
    cm[:, 3, :] = ((i[:, None] // 64) <= (i[None, :] // 64)).astype(np.float32)
    cm[:, 4, :] = 1.0
    sel = np.zeros((32, 32, 128), np.float32)
    for e in range(32):
        sel[e, e, :] = 1.0
    inv = (10000.0 ** (-np.arange(16, dtype=np.float32) / 16)).astype(np.float32)
    ri = np.zeros((96, 1), np.float32)
    ri[64:80, 0] = inv
    ri[80:96, 0] = inv
    return {"cmat": cm, "sel": sel, "ropeinv": ri}


_NC_CACHE = {}


def run_cores(inputs, n_cores=8, layers=(0, 1), phases="ABC", dbg=(), sub="GML"):
    key = (tuple(layers), phases, tuple(dbg), sub)
    if key not in _NC_CACHE:
        _NC_CACHE[key] = build(layers, phases, dbg, sub)
    nc = _NC_CACHE[key]
    consts = host_consts()
    shared = {k: np.ascontiguousarray(v) for k, v in inputs.items() if k not in ("x", "mem", "positions")}
    shared.update(consts)
    in_maps = []
    for b in range(n_cores):
        m = dict(shared)
        m["x"] = np.ascontiguousarray(inputs["x"][b])
        m["mem"] = np.ascontiguousarray(inputs["mem"][b])
        m["positions"] = np.ascontiguousarray(inputs["positions"][b:b + 1]).astype(np.int32)
        in_maps.append(m)
    res = run_bass_kernel_spmd(nc, in_maps, core_ids=list(range(n_cores)))
    return res.results


def kernel(**inputs):
    inputs = {k: np.asarray(v) for k, v in inputs.items()}
    res = run_cores(inputs, 8)
    return np.stack([r["out"] for r in res], axis=0).astype(np.float32)


def phase_B(c, l):
    from contextlib import ExitStack
    nc, P, w = c["nc"], c["P"], c["w"]
    x_tm, xT, ps, cmat_bf, memT = c["x_tm"], c["xT"], c["ps"], c["cmat_bf"], c["memT"]
    mm, act, tt, stt, cp = c["mm"], c["act"], c["tt"], c["stt"], c["cp"]
    load_cast = c["load_cast"]
    ONES = 4
    ph = ExitStack()

    def sb(name, shape, dt=F32):
        return P.tile(name, ph.enter_context(nc.sbuf_tensor("%s_%d" % (name, l), list(shape), dt)))

    c["load_ln"]("ln2_g", "ln2_b", l)
    kT = sb("b_kT", [128, 8, 256], BF16)
    vx = sb("b_v", [128, 2, D], BF16)
    ph2 = ExitStack()
    wkv = P.tile("b_wkv", ph2.enter_context(nc.sbuf_tensor("b_wkv_%d" % l, [128, 8, 2 * D], BF16)))
    for kc in range(8):
        load_cast(wkv, wkv[:, kc, :], w["xa_w_kv"][l, kc * 128:(kc + 1) * 128, :], sub=kc)
    for cc in range(8):
        pk = ps[cc % 2]
        for kc in range(8):
            mm(pk[:, 0:256], wkv[:, kc, cc * 128:(cc + 1) * 128], memT[:, kc, :], kc == 0, kc == 7, [wkv, memT], [pk])
        cp(kT[:, cc, :], pk[:, 0:256], [pk], [(kT, cc)], eng="scalar" if cc % 2 else "vector")
    for mt in range(2):
        for hf in range(2):
            pv = ps[2 + hf]
            for kc in range(8):
                mm(pv[:, :], memT[:, kc, mt * 128:(mt + 1) * 128], wkv[:, kc, D + hf * 512:D + (hf + 1) * 512],
                   kc == 0, kc == 7, [wkv, memT], [pv])
            cp(vx[:, mt, hf * 512:(hf + 1) * 512], pv[:, :], [pv], [(vx, (mt, hf))], eng="scalar" if hf else "vector")
    P.soft_barrier()
    ph2.close()
    wq = sb("b_wq", [128, 8, D], BF16)
    wo = sb("b_wo", [128, 8, D], BF16)
    for kc in range(0, 8, 2):
        load_cast(wq, wq[:, kc:kc + 2, :], w["xa_w_q"][l, kc * 128:(kc + 2) * 128, :].rearrange("(k p) n -> p k n", p=128), sub=kc)
    for kc in range(0, 8, 2):
        load_cast(wo, wo[:, kc:kc + 2, :], w["xa_w_o"][l, kc * 128:(kc + 2) * 128, :].rearrange("(k p) n -> p k n", p=128), sub=kc)
    qT = sb("b_qT", [128, 8, 512], BF16)
    xaT = sb("b_xaT", [128, 8, 512], BF16)
    PT = sb("b_PT", [128, 2, 512], BF16)
    rden = sb("b_rden", [128, 2, 512])
    scale = 256 ** -0.5
    for g in range(4):
        tok = slice(g * 512, (g + 1) * 512)
        for cc in range(8):
            pq = ps[cc % 2]
            for kc in range(8):
                mm(pq[:, :], wq[:, kc, cc * 128:(cc + 1) * 128], xT[:, kc, tok], kc == 0, kc == 7, [wq, xT], [pq])
            cp(qT[:, cc, :], pq[:, :], [pq], [(qT, cc)], eng="scalar" if cc % 2 else "vector")
        for h in range(4):
            for mt in range(2):
                pst = ps[2 + mt]
                for j in range(2):
                    mm(pst[:, :], kT[:, h * 2 + j, mt * 128:(mt + 1) * 128], qT[:, h * 2 + j, :], j == 0, j == 1,
                       [(kT, h * 2 + j), (qT, h * 2 + j)], [pst])
                act(PT[:, mt, :], pst[:, :], AF.Exp, [pst], [(PT, mt)], scale=scale)
            pden = ps[4]
            for mt in range(2):
                mm(pden[:, :], cmat_bf[:, ONES, :], PT[:, mt, :], mt == 0, mt == 1, [cmat_bf, (PT, mt)], [pden])
            rb = h % 2
            act(rden[:, rb, :], pden[:, :], AF.Ln, [pden], [(rden, rb)])
            act(rden[:, rb, :], rden[:, rb, :], AF.Exp, [(rden, rb)], [(rden, rb)], scale=-1.0)
            for j in range(2):
                po = ps[5 + j]
                for mt in range(2):
                    mm(po[:, :], vx[:, mt, h * 256 + j * 128:h * 256 + (j + 1) * 128], PT[:, mt, :], mt == 0, mt == 1,
                       [vx, (PT, mt)], [po])
                tt(xaT[:, h * 2 + j, :], po[:, :], rden[:, rb, :], ALU.mult, [po, (rden, rb)], [(xaT, h * 2 + j)])
        for tl in range(4):
            t = g * 4 + tl
            for hf in range(2):
                pp = ps[hf]
                for cc in range(8):
                    mm(pp[:, :], xaT[:, cc, tl * 128:(tl + 1) * 128], wo[:, cc, hf * 512:(hf + 1) * 512], cc == 0, cc == 7,
                       [xaT, wo], [pp])
                xs = x_tm[:, t, hf * 512:(hf + 1) * 512]
                stt(xs, xs, ALPHA, pp[:, :], ALU.mult, ALU.add, [(x_tm, t), pp], [(x_tm, t)])
            c["layer_norm_tile"](t, 7)
    P.soft_barrier()
    ph.close()


def head_norm_gate(c, sbf, name, src, gs, out_bf, b, keyp):
    P, tt, ts, red, act = c["P"], c["tt"], c["ts"], c["red"], c["act"]
    st = sbf["hn_st"]
    cen = sbf["hn_cen"]
    sq = sbf["hn_sq"]
    s4 = src.rearrange("p (h e) -> p h e", h=4)
    mean = st[:, b, 0:4]
    var = st[:, b, 4:8]
    red(mean, s4, ALU.add, [keyp], [(st, (b, "m"))])
    ts(mean, mean, -1.0 / 64, ALU.mult, [(st, (b, "m"))], [(st, (b, "m"))])
    c4 = cen[:, b, :].rearrange("p (h e) -> p h e", h=4)
    tt(c4, s4, bcast(mean, [128, 4, 64], 2), ALU.add, [keyp, (st, (b, "m"))], [(cen, b)])
    tt(sq[:, b, :], cen[:, b, :], cen[:, b, :], ALU.mult, [(cen, b)], [(sq, b)])
    red(var, sq[:, b, :].rearrange("p (h e) -> p h e", h=4), ALU.add, [(sq, b)], [(st, (b, "v"))])
    ts(var, var, 1.0 / 64, ALU.mult, [(st, (b, "v"))], [(st, (b, "v"))], s2=LN_EPS, op1=ALU.add)
    act(var, var, AF.Ln, [(st, (b, "v"))], [(st, (b, "v"))])
    act(var, var, AF.Exp, [(st, (b, "v"))], [(st, (b, "v"))], scale=-0.5)
    tt(c4, c4, bcast(var, [128, 4, 64], 2), ALU.mult, [(cen, b), (st, (b, "v"))], [(cen, b)])
    tt(out_bf, cen[:, b, :], gs, ALU.mult, [(cen, b), (sbf["gs"], b)], [(sbf["obf"], b)])


def phase_A(c, l):
    from contextlib import ExitStack
    nc, P, w = c["nc"], c["P"], c["w"]
    sub = c.get("sub", "GML")
    c["first_mixer"] = True
    if "G" in sub:
        mixer_gla(c, l)
        c["first_mixer"] = False
    if "M" in sub:
        mixer_mlstm(c, l)
        c["first_mixer"] = False
    if "L" in sub:
        mixer_mla(c, l)
    c["load_ln"]("ln1_g", "ln1_b", l)
    for t in range(NT):
        c["layer_norm_tile"](t, 5 + t % 3)
    P.soft_barrier()


def mixer_gla(c, l):
    from contextlib import ExitStack
    nc, P, w = c["nc"], c["P"], c["w"]
    x_tm, xT, ps, ps_bf, cmat, cmat_bf = c["x_tm"], c["xT"], c["ps"], c["ps_bf"], c["cmat"], c["cmat_bf"]
    mm, tr, act, tt, ts, stt, cp, red = c["mm"], c["tr"], c["act"], c["tt"], c["ts"], c["stt"], c["cp"], c["red"]
    load, load_cast = c["load"], c["load_cast"]
    IDENT, TRII, TRIS = 0, 1, 2
    ph = ExitStack()

    def sb(name, shape, dt=F32):
        return P.tile(name, ph.enter_context(nc.sbuf_tensor("%s_%d" % (name, l), list(shape), dt)))

    win = sb("g_win", [128, 8, 1040], BF16)
    for kc in range(8):
        load_cast(win, win[:, kc, :], w["w_in"][l, kc * 128:(kc + 1) * 128, 0:1040], sub=kc)
    wa2 = sb("g_wa2", [16, 256], BF16)
    load_cast(wa2, wa2[0:16, :], w["gla_w_a2"][l])
    babc = sb("g_babc", [128, 256])
    load(babc, babc[:].unsqueeze(1), w["gla_b_a"][l:l + 1, :].partition_broadcast(128))
    wout = sb("g_wout", [128, 2, D], BF16)
    load_cast(wout, wout[:], w["w_out"][l, 0:256, :].rearrange("(k p) n -> p k n", p=128))
    gng = sb("g_gng", [128, 256])
    load(gng, gng[:].unsqueeze(1), w["gla_norm_g"][l:l + 1, :].partition_broadcast(128))
    NB = 3
    gaT = sb("g_gaT", [16, NB, 128], BF16)
    Lsb = sb("g_L", [128, NB, 256])
    E1 = sb("g_E1", [128, NB, 256])
    E2 = sb("g_E2", [128, NB, 256])
    E3 = sb("g_E3", [128, NB, 256])
    qs = [sb("g_qs0", [128, NB, 256], BF16), sb("g_qs1", [128, NB, 256], BF16)]
    for i in range(2):
        P.op("vector", lambda e, i=i: e.memset(qs[i][:], 0.0), [], [qs[i]])
    ksT = sb("g_ksT", [128, NB, 256], BF16)
    k2 = sb("g_k2", [128, NB, 256], BF16)
    vsb = sb("g_v", [128, NB, 256], BF16)
    gs = sb("g_gs", [128, NB, 256])
    PT = sb("g_PT", [128, 2, 4, 128], BF16)
    osb = sb("g_osb", [128, NB, 256])
    obf = sb("g_obf", [128, NB, 256], BF16)
    ogT = sb("g_ogT", [128, 2, 2, 128], BF16)
    Dend = sb("g_Dend", [128, NT, 2])
    Sst = sb("g_S", [128, 2, 64])
    Sbf = sb("g_Sbf", [128, 2, 2, 64], BF16)
    P.op("vector", lambda e: e.memset(Sst[:], 0.0), [], [Sst])
    P.op("vector", lambda e: e.memset(Sbf[:], 0.0), [], [Sbf])
    sbf = dict(hn_st=sb("g_hn_st", [128, NB, 8]), hn_cen=sb("g_hn_cen", [128, NB, 256]),
               hn_sq=sb("g_hn_sq", [128, NB, 256]), gs=gs, obf=obf)
    import os
    NTL = int(os.environ.get("DBG_TILES", NT))
    A_, B_, C_, D_, E_, F_, G_, H_ = ps
    pbH = ps_bf[7]

    def S0(t):
        b = t % NB
        tok = slice(t * 128, (t + 1) * 128)
        for cc in range(4):
            for kc in range(8):
                mm(A_[:, cc * 128:(cc + 1) * 128], win[:, kc, cc * 128:(cc + 1) * 128], xT[:, kc, tok], kc == 0, kc == 7,
                   [win, xT], [A_])
        for kc in range(8):
            mm(B_[0:16, 256:384], win[:, kc, 1024:1040], xT[:, kc, tok], kc == 0, kc == 7, [win, xT], [B_])
        cp(gaT[0:16, b, :], B_[0:16, 256:384], [B_], [(gaT, b)])
        mm(B_[:, 0:256], gaT[0:16, b, :], wa2[0:16, :], True, True, [(gaT, b), wa2], [B_])
        tt(Lsb[:, b, :], B_[:, 0:256], babc[:, :], ALU.add, [B_, babc], [(Lsb, b)])
        act(Lsb[:, b, :], Lsb[:, b, :], AF.Exp, [(Lsb, b)], [(Lsb, b)], scale=-1.0)
        ts(Lsb[:, b, :], Lsb[:, b, :], 1.0, ALU.add, [(Lsb, b)], [(Lsb, b)])
        act(Lsb[:, b, :], Lsb[:, b, :], AF.Ln, [(Lsb, b)], [(Lsb, b)])
        for kc in range(8):
            mm(D_[:, :], xT[:, kc, tok], win[:, kc, 256:768], kc == 0, kc == 7, [win, xT], [D_])
        for kc in range(8):
            mm(E_[:, 0:256], xT[:, kc, tok], win[:, kc, 768:1024], kc == 0, kc == 7, [win, xT], [E_])
        cp(vsb[:, b, :], D_[:, 256:512], [D_], [(vsb, b)], eng="scalar")
        act(gs[:, b, :], E_[:, 0:256], AF.Silu, [E_], [(gs, b)])
        tt(gs[:, b, :], gs[:, b, :], gng[:, :], ALU.mult, [(gs, b), gng], [(gs, b)])
        for ch in range(2):
            mm(C_[:, ch * 128:(ch + 1) * 128], Lsb[:, b, ch * 128:(ch + 1) * 128], cmat[:, TRII, :], True, True,
               [(Lsb, b), cmat], [C_])
        mm(C_[:, 256:512], cmat[:, TRIS, :], Lsb[:, b, :], True, True, [(Lsb, b), cmat], [C_])
        act(E1[:, b, :], C_[:, 0:256], AF.Exp, [C_], [(E1, b)], scale=-1.0 / 16)
        act(E2[:, b, :], C_[:, 0:256], AF.Exp, [C_], [(E2, b)], scale=1.0 / 16)
        act(E3[:, b, :], C_[:, 256:512], AF.Exp, [C_], [(E3, b)], scale=-1.0 / 16)
        cp(Dend[:, t, :], E1[:, b, :].rearrange("p (c n) -> p c n", c=2)[:, :, 127], [(E1, b)], [(Dend, t)])
        for hp in range(2):
            rws = slice(hp * 64, (hp + 1) * 64)
            stt(qs[hp][rws, b, :], A_[rws, 0:256], 0.125, E1[rws, b, :], ALU.mult, ALU.mult, [A_, (E1, b)], [(qs[hp], b)])
        tt(ksT[:, b, :], A_[:, 256:512], E2[:, b, :], ALU.mult, [A_, (E2, b)], [(ksT, b)])
        tt(k2[:, b, :], D_[:, 0:256], E3[:, b, :], ALU.mult, [D_, (E3, b)], [(k2, b)])

    def S1(t):
        b = t % NB
        b2 = t % 2
        for h in range(4):
            hp, hc = h % 2, h // 2
            mm(F_[:, h * 128:(h + 1) * 128], ksT[:, b, hc * 128:(hc + 1) * 128],
               qs[hp][:, b, hc * 128:(hc + 1) * 128], True, True, [(ksT, b), (qs[hp], b)], [F_])
        for h in range(4):
            hc = h // 2
            mm(G_[:, h * 64:(h + 1) * 64], k2[:, b, hc * 128:(hc + 1) * 128], vsb[:, b, h * 64:(h + 1) * 64],
               True, True, [(k2, b), (vsb, b)], [G_])
        tt(PT[:, b2, :, :], F_[:, :].rearrange("p (h n) -> p h n", h=4), bcast(cmat[:, TRII, :], [128, 4, 128], 1),
           ALU.mult, [F_, cmat], [(PT, b2)])
        for h in range(4):
            hp, hc = h % 2, h // 2
            mm(G_[:, 256 + h * 64:256 + (h + 1) * 64], PT[:, b2, h, :], vsb[:, b, h * 64:(h + 1) * 64], True, False,
               [(PT, b2), (vsb, b)], [G_])
            mm(G_[:, 256 + h * 64:256 + (h + 1) * 64], qs[hp][:, b, hc * 128:(hc + 1) * 128],
               Sbf[:, b2, hc, :], False, True, [(qs[hp], b), (Sbf, b2)], [G_])
        for h in range(4):
            hp, hc = h % 2, h // 2
            rows = slice(hp * 64, (hp + 1) * 64)
            stt(Sst[rows, hc, :], Sst[rows, hc, :], Dend[rows, t, hc:hc + 1], G_[rows, h * 64:(h + 1) * 64],
                ALU.mult, ALU.add, [(Sst, h), (Dend, t), G_], [(Sst, h)])
        cp(Sbf[:, 1 - b2, :, :], Sst[:, :, :], [Sst], [(Sbf, 1 - b2)])
        cp(osb[:, b, :], G_[:, 256:512], [G_], [(osb, b)], eng="scalar")

    def S2(t):
        b = t % NB
        b2 = t % 2
        head_norm_gate(c, sbf, "g", osb[:, b, :], gs[:, b, :], obf[:, b, :], b, (osb, b))
        for ch in range(2):
            tr(pbH[:, ch * 128:(ch + 1) * 128], obf[:, b, ch * 128:(ch + 1) * 128], cmat_bf[:, IDENT, :],
               [(obf, b), cmat_bf], [H_])
        cp(ogT[:, b2, :, :], pbH[:, 0:256].rearrange("p (c n) -> p c n", c=2), [H_], [(ogT, b2)])
        for q4 in range(4):
            pq = H_[:, 128:384]
            for ch in range(2):
                mm(pq, ogT[:, b2, ch, :], wout[:, ch, q4 * 256:(q4 + 1) * 256], ch == 0, ch == 1, [(ogT, b2), wout], [H_])
            xs = x_tm[:, t, q4 * 256:(q4 + 1) * 256]
            if c["first_mixer"]:
                stt(xs, xs, ALPHA, pq, ALU.mult, ALU.add, [(x_tm, t), H_], [(x_tm, t)])
            else:
                tt(xs, xs, pq, ALU.add, [(x_tm, t), H_], [(x_tm, t)])

    for step in range(NTL + 2):
        if 0 <= step - 2 < NTL:
            S2(step - 2)
        if 0 <= step - 1 < NTL:
            S1(step - 1)
        if step < NTL:
            S0(step)
    P.soft_barrier()
    ph.close()


def mixer_mlstm(c, l):
    from contextlib import ExitStack
    import os
    nc, P, w = c["nc"], c["P"], c["w"]
    x_tm, xT, ps, ps_bf, cmat, cmat_bf = c["x_tm"], c["xT"], c["ps"], c["ps_bf"], c["cmat"], c["cmat_bf"]
    mm, tr, act, tt, ts, stt, cp, red = c["mm"], c["tr"], c["act"], c["tt"], c["ts"], c["stt"], c["cp"], c["red"]
    load, load_cast = c["load"], c["load_cast"]
    IDENT, TRII, ONES = 0, 1, 4
    ph = ExitStack()

    def sb(name, shape, dt=F32):
        return P.tile(name, ph.enter_context(nc.sbuf_tensor("%s_%d" % (name, l), list(shape), dt)))

    win = sb("m_win", [128, 8, 1032], BF16)
    for kc in range(8):
        load_cast(win, win[:, kc, :], w["w_in"][l, kc * 128:(kc + 1) * 128, 1040:2072], sub=kc)
    cw = sb("m_cw", [128, 4, 4])
    for j in range(4):
        load(cw, cw[:, :, j], w["ml_conv_w"][l, j, :].rearrange("(c p) -> p c", p=128), sub=j)
    bif = sb("m_bif", [128, 8])
    load(bif, bif[:, 0:4].unsqueeze(1), w["ml_b_i"][l:l + 1, :].partition_broadcast(128), sub=0)
    load(bif, bif[:, 4:8].unsqueeze(1), w["ml_b_f"][l:l + 1, :].partition_broadcast(128), sub=1)
    mng = sb("m_mng", [128, 256])
    load(mng, mng[:].unsqueeze(1), w["ml_norm_g"][l:l + 1, :].partition_broadcast(128))
    wout = sb("m_wout", [128, 2, D], BF16)
    load_cast(wout, wout[:], w["w_out"][l, 256:512, :].rearrange("(k p) n -> p k n", p=128))

    q = [sb("m_q0", [128, 2, S], BF16), sb("m_q1", [128, 2, S], BF16)]
    kT = sb("m_kT", [128, 2, S], BF16)
    ph1 = ExitStack()
    mqk = P.tile("m_mqk", ph1.enter_context(nc.sbuf_tensor("m_mqk_%d" % l, [128, 4, S + 3], BF16)))
    acc = P.tile("m_acc", ph1.enter_context(nc.sbuf_tensor("m_acc_%d" % l, [128, 2, 1024], F32)))
    P.op("vector", lambda e: e.memset(mqk[:, :, 0:3], 0.0), [], [mqk])
    for g in range(4):
        tok = slice(g * 512, (g + 1) * 512)
        for ch in range(4):
            pp = ps[(g * 4 + ch) % 2]
            for kc in range(8):
                mm(pp[:, :], win[:, kc, ch * 128:(ch + 1) * 128], xT[:, kc, tok], kc == 0, kc == 7, [win, xT], [pp])
            cp(mqk[:, ch, 3 + g * 512:3 + (g + 1) * 512], pp[:, :], [pp], [(mqk, ch)], eng="scalar" if ch % 2 else "vector")
    for i in range(2):
        P.op("vector", lambda e, i=i: e.memset(q[i][:], 0.0), [], [q[i]])
    for ch in range(4):
        for half in range(2):
            ai = half
            a = acc[:, ai, :]
            off = half * 1024
            ts(a, mqk[:, ch, off:off + 1024], cw[:, ch, 0:1], ALU.mult, [(mqk, ch), cw], [(acc, ai)])
            for j in range(1, 4):
                stt(a, mqk[:, ch, off + j:off + j + 1024], cw[:, ch, j:j + 1], a, ALU.mult, ALU.add,
                    [(mqk, ch), cw, (acc, ai)], [(acc, ai)])
            tokh = slice(off, off + 1024)
            if ch < 2:
                for hp in range(2):
                    rws = slice(hp * 64, (hp + 1) * 64)
                    act(q[hp][rws, ch, tokh], acc[rws, ai, :], AF.Silu, [(acc, ai)], [(q[hp], (ch, half))])
            else:
                act(kT[:, ch - 2, tokh], a, AF.Silu, [(acc, ai)], [(kT, (ch, half))])
    for hp in range(2):
        ts(q[hp][:], q[hp][:], 0.125, ALU.mult, [q[hp]], [q[hp]])
    P.soft_barrier()
    ph1.close()

    gates = sb("m_gates", [128, NT, 8])
    pg = ps[2]
    for t in range(NT):
        for kc in range(8):
            mm(pg[:, t * 8:(t + 1) * 8], xT[:, kc, t * 128:(t + 1) * 128], win[:, kc, 1024:1032], kc == 0, kc == 7,
               [win, xT], [pg])
    tt(gates[:], pg[:, 0:128].rearrange("p (t n) -> p t n", t=NT), bcast(bif[:, :], [128, NT, 8], 1), ALU.add,
       [pg, bif], [gates])
    Lf = sb("m_Lf", [128, NT, 4])
    act(Lf[:], gates[:, :, 4:8], AF.Exp, [gates], [Lf], scale=-1.0)
    ts(Lf[:], Lf[:], 1.0, ALU.add, [Lf], [Lf])
    act(Lf[:], Lf[:], AF.Ln, [Lf], [Lf])
    Lf2 = Lf[:].rearrange("p t n -> p (t n)")
    p3 = ps[3]
    mm(p3[:, 0:64], cmat[:, TRII, :], Lf2, True, True, [cmat, Lf], [p3])
    asb = sb("m_a", [128, NT, 4])
    tt(asb[:], p3[:, 0:64].rearrange("p (t n) -> p t n", t=NT), gates[:, :, 0:4], ALU.add, [p3, gates], [asb])
    cumL = sb("m_cumL", [128, 64])
    cp(cumL[:], p3[:, 0:64], [p3], [cumL])
    a2 = asb[:].rearrange("p t n -> p (t n)")
    p4 = ps[4]
    tr(p4[0:64, 0:128], a2, cmat[:, IDENT, :], [asb, cmat], [p4])
    Acol = sb("m_Acol", [64, 1])
    red(Acol[:, 0:1], p4[0:64, 0:128], ALU.max, [p4], [Acol])
    rows = sb("m_rows", [1, 5, 64])
    p5 = ps[5]
    mm(p5[0:1, 0:64], Acol[0:64, 0:1], cmat[0:64, IDENT, 0:64], True, True, [Acol, cmat], [p5])
    mm(p5[0:1, 64:128], cmat[:, ONES, 0:1], Lf2, True, True, [cmat, Lf], [p5])
    cp(rows[0:1, 0:2, :], p5[0:1, 0:128].rearrange("p (a n) -> p a n", a=2), [p5], [rows])
    P.op("vector", lambda e: e.memset(rows[0:1, 2, 0:4], 0.0), [rows], [rows])
    for cc in range(NT):
        sl = slice(cc * 4, cc * 4 + 4)
        tt(rows[0:1, 3, sl], rows[0:1, 2, sl], rows[0:1, 0, sl], ALU.max, [rows], [rows])
        if cc < NT - 1:
            tt(rows[0:1, 2, (cc + 1) * 4:(cc + 1) * 4 + 4], rows[0:1, 3, sl], rows[0:1, 1, sl], ALU.subtract, [rows], [rows])
    tt(rows[0:1, 4, :], rows[0:1, 2, :], rows[0:1, 3, :], ALU.subtract, [rows], [rows])
    act(rows[0:1, 4, :], rows[0:1, 4, :], AF.Exp, [rows], [rows])
    p6 = ps[6]
    mm(p6[:, 0:128], cmat[0:1, ONES, :], rows[0:1, 3:5, :].rearrange("p a n -> p (a n)"), True, True, [cmat, rows], [p6])
    bcs = sb("m_bcs", [128, 128])
    cp(bcs[:], p6[:, 0:128], [p6], [bcs])
    wtok = sb("m_wtok", [128, 64])
    tt(wtok[:], a2, bcs[:, 0:64], ALU.subtract, [asb, bcs], [wtok])
    act(wtok[:], wtok[:], AF.Exp, [wtok], [wtok])
    clamp = sb("m_clamp", [128, 64])
    tt(clamp[:], cumL[:], bcs[:, 0:64], ALU.subtract, [cumL, bcs], [clamp])
    act(clamp[:], clamp[:], AF.Exp, [clamp], [clamp])

    vext = sb("m_vext", [128, 2, 4, 65], BF16)
    P.op("vector", lambda e: e.memset(vext[:], 1.0), [], [vext])
    vw = sb("m_vw", [128, 2, 4, 65], BF16)
    gso = sb("m_gso", [128, 2, 256])
    ktm = sb("m_ktm", [128, 2, 256], BF16)
    WM = sb("m_WM", [128, 2, 4, 128])
    PT = sb("m_PT", [128, 2, 4, 128], BF16)
    Cn = sb("m_Cn", [128, 2, 65])
    P.op("vector", lambda e: e.memset(Cn[:], 0.0), [], [Cn])
    Cd = sb("m_Cd", [128, 2, 65])
    Cdbf = sb("m_Cdbf", [128, 2, 2, 65], BF16)
    nd = sb("m_nd", [128, 2, 4, 65])
    hsb = sb("m_h", [128, 2, 256])
    small = sb("m_small", [128, 2, 8])
    obf = sb("m_obf", [128, 2, 256], BF16)
    ohT = sb("m_ohT", [128, 2, 2, 128], BF16)
    sbf = dict(hn_st=sb("m_hn_st", [128, 2, 8]), hn_cen=sb("m_hn_cen", [128, 2, 256]),
               hn_sq=sb("m_hn_sq", [128, 2, 256]), gs=gso, obf=obf)
    PA, PB, PC, PD, PE_, PO0, PO1, PX = ps
    for t in range(int(os.environ.get("DBG_TILES", NT))):
        b = t % 2
        tok = slice(t * 128, (t + 1) * 128)
        g4 = slice(t * 4, t * 4 + 4)
        for kc in range(8):
            mm(PA[:, :], xT[:, kc, tok], win[:, kc, 512:1024], kc == 0, kc == 7, [win, xT], [PA])
        v4 = PA[:, 0:256].rearrange("p (h e) -> p h e", h=4)
        cp(vext[:, b, :, 0:64], v4, [PA], [(vext, b)], eng="scalar")
        tt(vw[:, b, :, 0:64], v4, bcast(wtok[:, g4], [128, 4, 64], 2), ALU.mult, [PA, wtok], [(vw, b)])
        cp(vw[:, b, :, 64], wtok[:, g4], [wtok], [(vw, b)])
        act(gso[:, b, :], PA[:, 256:512], AF.Sigmoid, [PA], [(gso, b)])
        tt(gso[:, b, :], gso[:, b, :], mng[:, :], ALU.mult, [(gso, b), mng], [(gso, b)])
        pb = ps_bf[1]
        for hc in range(2):
            tr(pb[:, hc * 128:(hc + 1) * 128], kT[:, hc, tok], cmat_bf[:, IDENT, :], [kT, cmat_bf], [PB])
        cp(ktm[:, b, :], pb[:, 0:256], [PB], [(ktm, b)])
        for h in range(4):
            hp, hc = h % 2, h // 2
            mm(PC[:, h * 128:(h + 1) * 128], kT[:, hc, tok], q[hp][:, hc, tok], True, True, [kT, q[hp]], [PC])
        tt(WM[:, b, :, :], bcast(wtok[:, g4], [128, 4, 128], 2), bcast(cmat[:, TRII, :], [128, 4, 128], 1), ALU.mult,
           [wtok, cmat], [(WM, b)])
        tt(PT[:, b, :, :], PC[:, :].rearrange("p (h n) -> p h n", h=4), WM[:, b, :, :], ALU.mult, [PC, (WM, b)], [(PT, b)])
        for h in range(4):
            hc = h // 2
            mm(PD[:, h * 65:(h + 1) * 65], ktm[:, b, hc * 128:(hc + 1) * 128], vw[:, b, h, :], True, True,
               [(ktm, b), (vw, b)], [PD])
        for h in range(4):
            hp, hc = h % 2, h // 2
            rws = slice(hp * 64, (hp + 1) * 64)
            ts(Cd[rws, hc, :], Cn[rws, hc, :], bcs[rws, 64 + t * 4 + h:64 + t * 4 + h + 1], ALU.mult,
               [(Cn, h), bcs], [(Cd, h)])
        cp(Cdbf[:, b, :, :], Cd[:], [Cd], [(Cdbf, b)])
        for h in range(4):
            hp, hc = h % 2, h // 2
            mm(PE_[:, h * 65:(h + 1) * 65], PT[:, b, h, :], vext[:, b, h, :], True, False, [(PT, b), (vext, b)], [PE_])
            mm(PE_[:, h * 65:(h + 1) * 65], q[hp][:, hc, tok], Cdbf[:, b, hc, :], False, True, [q[hp], (Cdbf, b)], [PE_])
        for h in range(4):
            hp, hc = h % 2, h // 2
            rws = slice(hp * 64, (hp + 1) * 64)
            tt(Cn[rws, hc, :], Cd[rws, hc, :], PD[rws, h * 65:(h + 1) * 65], ALU.add, [(Cd, h), PD], [(Cn, h)])
        cp(nd[:, b, :, :], PE_[:, 0:260].rearrange("p (h e) -> p h e", h=4), [PE_], [(nd, b)], eng="scalar")
        stt(small[:, b, 0:4], nd[:, b, :, 64], -1.0, nd[:, b, :, 64], ALU.mult, ALU.max, [(nd, b)], [(small, b)])
        tt(small[:, b, 0:4], small[:, b, 0:4], clamp[:, g4], ALU.max, [(small, b), clamp], [(small, b)])
        P.op("vector", lambda e, b=b: e.reciprocal(small[:, b, 4:8], small[:, b, 0:4]), [(small, b)], [(small, b)])
        tt(hsb[:, b, :].rearrange("p (h e) -> p h e", h=4), nd[:, b, :, 0:64], bcast(small[:, b, 4:8], [128, 4, 64], 2),
           ALU.mult, [(nd, b), (small, b)], [(hsb, b)])
        head_norm_gate(c, sbf, "m", hsb[:, b, :], gso[:, b, :], obf[:, b, :], b, (hsb, b))
        pbx = ps_bf[7]
        for ch in range(2):
            tr(pbx[:, ch * 128:(ch + 1) * 128], obf[:, b, ch * 128:(ch + 1) * 128], cmat_bf[:, IDENT, :],
               [(obf, b), cmat_bf], [PX])
        cp(ohT[:, b, :, :], pbx[:, 0:256].rearrange("p (c n) -> p c n", c=2), [PX], [(ohT, b)])
        for hf in range(2):
            po = PO0 if hf == 0 else PO1
            for ch in range(2):
                mm(po[:, :], ohT[:, b, ch, :], wout[:, ch, hf * 512:(hf + 1) * 512], ch == 0, ch == 1, [(ohT, b), wout], [po])
            xs = x_tm[:, t, hf * 512:(hf + 1) * 512]
            if c["first_mixer"]:
                stt(xs, xs, ALPHA, po[:, :], ALU.mult, ALU.add, [(x_tm, t), po], [(x_tm, t)])
            else:
                tt(xs, xs, po[:, :], ALU.add, [(x_tm, t), po], [(x_tm, t)])
    P.soft_barrier()
    ph.close()


def mixer_mla(c, l):
    from contextlib import ExitStack
    import os
    nc, P, w = c["nc"], c["P"], c["w"]
    x_tm, xT, ps, cmat, cmat_bf, cs = c["x_tm"], c["xT"], c["ps"], c["cmat"], c["cmat_bf"], c["cs"]
    mm, act, tt, ts, stt, cp = c["mm"], c["act"], c["tt"], c["ts"], c["stt"], c["cp"]
    load, load_cast = c["load"], c["load_cast"]
    MMLA, ONES = 3, 4
    R_ = slice(64, 96)
    ph = ExitStack()

    def sb(name, shape, dt=F32, st=None):
        return P.tile(name, (st or ph).enter_context(nc.sbuf_tensor("%s_%d" % (name, l), list(shape), dt)))

    cqnT = sb("a_cqnT", [128, 2, S], BF16)
    ckvnT = sb("a_ckvnT", [128, S], BF16)
    krope = sb("a_krope", [96, S], BF16)
    v_all = sb("a_vall", [128, NT, 512], BF16)
    gq = sb("a_gq", [128, 2])
    gkv = sb("a_gkv", [128, 1])
    load(gq, gq[:], w["mla_q_norm_g"][l].rearrange("(rc p) -> p rc", p=128))
    load(gkv, gkv[:], w["mla_kv_norm_g"][l].rearrange("(o p) -> p o", o=1))

    p1 = ExitStack()
    win = sb("a_win", [128, 8, 416], BF16, p1)
    for kc in range(8):
        load_cast(win, win[:, kc, :], w["w_in"][l, kc * 128:(kc + 1) * 128, 2072:2488], sub=kc)
    wkr = sb("a_wkr", [128, 8, 2, 96], BF16, p1)
    P.op("vector", lambda e: e.memset(wkr[:], 0.0), [], [wkr])
    cp(wkr[:, :, 0, 64:96], win[:, :, 384:416], [win], [wkr])
    ts(wkr[:, :, 1, 64:80], win[:, :, 400:416], -1.0, ALU.mult, [win], [wkr])
    cp(wkr[:, :, 1, 80:96], win[:, :, 384:400], [win], [wkr])
    sq = sb("a_sq", [128, 2, 512], BF16, p1)
    rstd = sb("a_rstd", [128, 2, 512], F32, p1)
    tA = sb("a_tA", [96, 512], F32, p1)
    tB = sb("a_tB", [96, 512], F32, p1)
    for g in range(4):
        tok = slice(g * 512, (g + 1) * 512)
        for rc in range(2):
            for kc in range(8):
                mm(ps[rc][:, :], win[:, kc, rc * 128:(rc + 1) * 128], xT[:, kc, tok], kc == 0, kc == 7, [win, xT], [ps[rc]])
        for rc in range(2):
            act(sq[:, rc, :], ps[rc][:, :], AF.Square, [ps[rc]], [(sq, rc)])
        for rc in range(2):
            mm(ps[2][:, :], cmat_bf[:, ONES, :], sq[:, rc, :], rc == 0, rc == 1, [cmat_bf, (sq, rc)], [ps[2]])
        ts(rstd[:, 0, :], ps[2][:, :], 1.0 / 256, ALU.mult, [ps[2]], [(rstd, 0)], s2=LN_EPS, op1=ALU.add)
        act(rstd[:, 0, :], rstd[:, 0, :], AF.Ln, [(rstd, 0)], [(rstd, 0)])
        act(rstd[:, 0, :], rstd[:, 0, :], AF.Exp, [(rstd, 0)], [(rstd, 0)], scale=-0.5)
        for rc in range(2):
            stt(cqnT[:, rc, tok], ps[rc][:, :], gq[:, rc:rc + 1], rstd[:, 0, :], ALU.mult, ALU.mult,
                [ps[rc], gq, (rstd, 0)], [(cqnT, (rc, g))])
        for kc in range(8):
            mm(ps[3][:, :], win[:, kc, 256:384], xT[:, kc, tok], kc == 0, kc == 7, [win, xT], [ps[3]])
        act(sq[:, 0, :], ps[3][:, :], AF.Square, [ps[3]], [(sq, 0)])
        mm(ps[4][:, :], cmat_bf[:, ONES, :], sq[:, 0, :], True, True, [cmat_bf, (sq, 0)], [ps[4]])
        ts(rstd[:, 1, :], ps[4][:, :], 1.0 / 128, ALU.mult, [ps[4]], [(rstd, 1)], s2=LN_EPS, op1=ALU.add)
        act(rstd[:, 1, :], rstd[:, 1, :], AF.Ln, [(rstd, 1)], [(rstd, 1)])
        act(rstd[:, 1, :], rstd[:, 1, :], AF.Exp, [(rstd, 1)], [(rstd, 1)], scale=-0.5)
        stt(ckvnT[:, tok], ps[3][:, :], gkv[:, 0:1], rstd[:, 1, :], ALU.mult, ALU.mult, [ps[3], gkv, (rstd, 1)], [(ckvnT, g)])
        for r2 in range(2):
            for kc in range(8):
                mm(ps[5 + r2][0:96, :], wkr[:, kc, r2, :], xT[:, kc, tok], kc == 0, kc == 7, [wkr, xT], [ps[5 + r2]])
        tt(tA[R_, :], ps[5][R_, :], cs[R_, 0, tok], ALU.mult, [ps[5], cs], [tA])
        tt(tB[R_, :], ps[6][R_, :], cs[R_, 1, tok], ALU.mult, [ps[6], cs], [tB])
        tt(krope[R_, tok], tA[R_, :], tB[R_, :], ALU.add, [tA, tB], [(krope, g)])
    P.soft_barrier()
    p1.close()

    wuk = sb("a_wuk", [128, 8, 64], BF16)
    wuv = sb("a_wuv", [128, 8, 64], BF16)
    ukv = w["mla_w_ukv"][l].rearrange("p (h two d) -> p h two d", h=8, two=2)
    load_cast(wuk, wuk[:], ukv[:, :, 0, :])
    load_cast(wuv, wuv[:], ukv[:, :, 1, :])
    wuq = sb("a_wuq", [128, 2, 768], BF16)
    load_cast(wuq, wuq[:], w["mla_w_uq"][l].rearrange("(rc p) n -> p rc n", p=128))
    wuqr = sb("a_wuqr", [128, 2, 8, 96], BF16)
    P.op("vector", lambda e: e.memset(wuqr[:], 0.0), [], [wuqr])
    wq4 = wuq[:].rearrange("p r (h c) -> p r h c", h=8)
    ts(wuqr[:, :, :, 64:80], wq4[:, :, :, 80:96], -1.0, ALU.mult, [wuq], [wuqr])
    cp(wuqr[:, :, :, 80:96], wq4[:, :, :, 64:80], [wuq], [wuqr])
    wout = sb("a_wout", [128, 4, D], BF16)
    load_cast(wout, wout[:], w["w_out"][l, 512:1024, :].rearrange("(k p) n -> p k n", p=128))
    qTh = sb("a_qTh", [96, 2, 512], BF16)
    PTb = sb("a_PT", [128, 3, 512], BF16)
    mlaT = sb("a_mlaT", [128, 4, 512], BF16)
    rden = sb("a_rden", [128, 2, 512])
    tA2 = sb("a_tA2", [96, 2, 512])
    tB2 = sb("a_tB2", [96, 2, 512])
    KH = [("k", h) for h in range(8)]
    P.merge(xT)
    for g in range(4):
        tok = slice(g * 512, (g + 1) * 512)
        for h in range(8):
            pk = ps[h % 2]
            mm(pk[0:64, :], wuk[:, h, :], ckvnT[:, tok], True, True, [wuk, ckvnT], [pk])
            cp(xT[0:64, h, tok], pk[0:64, :], [pk], [(xT, KH[h])], eng="scalar" if h % 2 else "vector")
        cp(xT[R_, :, tok], bcast(krope[R_, tok], [32, 8, 512], 1), [krope], [(xT, k) for k in KH])
    for t in range(NT):
        pv = ps[2 + t % 2]
        mm(pv[:, :], ckvnT[:, t * 128:(t + 1) * 128], wuv[:].rearrange("p h d -> p (h d)"), True, True, [ckvnT, wuv], [pv])
        cp(v_all[:, t, :], pv[:, :], [pv], [(v_all, t)], eng="scalar" if t % 2 else "vector")
    scale = 96.0 ** -0.5
    NG = int(os.environ.get("DBG_GROUPS", 4))

    def qproj(g, h):
        tok = slice(g * 512, (g + 1) * 512)
        qb = h % 2
        pq, pr = ps[0], ps[1]
        for rc in range(2):
            mm(pq[0:96, :], wuq[:, rc, h * 96:(h + 1) * 96], cqnT[:, rc, tok], rc == 0, rc == 1, [wuq, cqnT], [pq])
        for rc in range(2):
            mm(pr[0:96, :], wuqr[:, rc, h, :], cqnT[:, rc, tok], rc == 0, rc == 1, [wuqr, cqnT], [pr])
        cp(qTh[0:64, qb, :], pq[0:64, :], [pq], [(qTh, qb)], eng="scalar")
        tt(tA2[R_, qb, :], pq[R_, :], cs[R_, 0, tok], ALU.mult, [pq, cs], [(tA2, qb)])
        tt(tB2[R_, qb, :], pr[R_, :], cs[R_, 1, tok], ALU.mult, [pr, cs], [(tB2, qb)])
        tt(qTh[R_, qb, :], tA2[R_, qb, :], tB2[R_, qb, :], ALU.add, [(tA2, qb), (tB2, qb)], [(qTh, qb)])

    def attend(g, h, nxt):
        nkt = 4 * g + 4
        hp, pair, qb = h % 2, h // 2, h % 2
        po, pd = ps[4 + h % 2], ps[6 + h % 2]

        def qk(kt):
            qlo = max(kt - 4 * g, 0) * 128
            N = 512 - qlo
            pst = ps[2 + kt % 2]
            mm(pst[:, 0:N], xT[0:96, h, kt * 128:(kt + 1) * 128], qTh[0:96, qb, qlo:512], True, True,
               [(xT, KH[h]), (qTh, qb)], [pst])

        qk(0)
        for kt in range(nkt):
            qlo = max(kt - 4 * g, 0) * 128
            N = 512 - qlo
            pst = ps[2 + kt % 2]
            pb = kt % 3
            if kt + 1 < nkt:
                qk(kt + 1)
            elif nxt is not None:
                qproj(*nxt)
            act(PTb[:, pb, 0:N], pst[:, 0:N], AF.Exp, [pst], [(PTb, pb)], scale=scale)
            if kt >= 4 * g:
                tt(PTb[:, pb, 0:128], PTb[:, pb, 0:128], cmat_bf[:, MMLA, :], ALU.mult, [(PTb, pb), cmat_bf], [(PTb, pb)])
            mm(po[:, qlo:512], v_all[:, kt, pair * 128:(pair + 1) * 128], PTb[:, pb, 0:N], kt == 0, kt == nkt - 1,
               [(v_all, kt), (PTb, pb)], [po])
            mm(pd[:, qlo:512], cmat_bf[:, ONES, :], PTb[:, pb, 0:N], kt == 0, kt == nkt - 1, [cmat_bf, (PTb, pb)], [pd])
        rws = slice(hp * 64, (hp + 1) * 64)
        act(rden[rws, qb, :], pd[rws, :], AF.Ln, [pd], [(rden, qb)])
        act(rden[rws, qb, :], rden[rws, qb, :], AF.Exp, [(rden, qb)], [(rden, qb)], scale=-1.0)
        tt(mlaT[rws, pair, :], po[rws, :], rden[rws, qb, :], ALU.mult, [po, (rden, qb)], [(mlaT, h)])

    work = [(g, h) for g in range(NG) for h in range(8)]
    qproj(*work[0])
    for i, (g, h) in enumerate(work):
        nxt = work[i + 1] if i + 1 < len(work) else None
        if nxt is not None and nxt[0] != g:
            attend(g, h, None)
        else:
            attend(g, h, nxt)
        if h != 7:
            continue
        if nxt is not None:
            qproj(*nxt)
        for tl in range(4):
            t = g * 4 + tl
            for hf in range(2):
                pp = ps[5 + 2 * hf]
                for pr_ in range(4):
                    mm(pp[:, :], mlaT[:, pr_, tl * 128:(tl + 1) * 128], wout[:, pr_, hf * 512:(hf + 1) * 512], pr_ == 0, pr_ == 3,
                       [mlaT, wout], [pp])
                xs = x_tm[:, t, hf * 512:(hf + 1) * 512]
                if c["first_mixer"]:
                    stt(xs, xs, ALPHA, pp[:, :], ALU.mult, ALU.add, [(x_tm, t), pp], [(x_tm, t)])
                else:
                    tt(xs, xs, pp[:, :], ALU.add, [(x_tm, t), pp], [(x_tm, t)])
    P.merge(xT)
    P.soft_barrier()
    ph.close()
```

```python
import numpy as np
import ml_dtypes
import concourse.bass as bass
import concourse.mybir as mybir
from concourse.bass_utils import run_bass_kernel_spmd

F32 = mybir.dt.float32
BF16 = mybir.dt.bfloat16
I32 = mybir.dt.int32
AF = mybir.ActivationFunctionType
ALU = mybir.AluOpType
AX = mybir.AxisListType

S = 2048
D = 1024
NT = 16
DEPTH = 2
ALPHA = (2 * DEPTH) ** 0.25
LN_EPS = 1e-5
IN_W = 2488


class Tile:
    def __init__(self, name, h):
        self.name = name
        self.h = h
        self.st = {}
        self.dsem = None
        self.dcount = 0
        self.psum = False

    def __getitem__(self, k):
        return self.h[k]


class Op:
    __slots__ = ("eng", "fn", "deps", "ddeps", "inc", "dma", "cost", "unit", "seg", "oi", "fin", "pos", "tbl")

    def __init__(self, eng, fn, deps, ddeps, dma=None, cost=0.2):
        self.eng = eng
        self.fn = fn
        self.deps = deps
        self.ddeps = ddeps
        self.inc = False
        self.dma = dma
        self.cost = cost
        self.unit = None
        self.seg = 0
        self.oi = 0
        self.fin = 0.0
        self.pos = 0
        self.tbl = None


class Prog:
    ENG = ("sync", "scalar", "vector", "gpsimd", "tensor")
    REORDER = ("scalar", "vector", "tensor")

    def __init__(self, nc):
        self.nc = nc
        self.ops = {e: [] for e in self.ENG}
        self.all = []
        self.dsems = {}
        self.dtotal = {}
        self.tiles = []
        self.seg = 0
        self.cur_unit = None
        self.nunits = 0
        self.dma_by = {}

    def tile(self, name, h):
        t = Tile(name, h)
        try:
            ml = self.nc.lookup_mloc(h)
            sbuf = "SB" in str(ml.type)
            t.lo, t.hi = int(ml.addr), int(ml.addr) + int(ml.dims[1])
        except Exception:
            sbuf = False
        t.sbuf = sbuf
        if sbuf:
            inh = []
            for o in self.tiles:
                if getattr(o, "sbuf", False) and o.lo < t.hi and t.lo < o.hi:
                    for st in o.st.values():
                        if st[0] is not None:
                            inh.append(st[0])
                        inh.extend(st[1])
            if inh:
                seen = set()
                uniq = []
                for x in inh:
                    k = id(x) if isinstance(x, Op) else x
                    if k not in seen:
                        seen.add(k)
                        uniq.append(x)
                t.st[None] = [None, uniq]
        self.tiles.append(t)
        return t

    def merge(self, t):
        allr = []
        for st in t.st.values():
            if st[0] is not None:
                allr.append(st[0])
            allr.extend(st[1])
        seen, uniq = set(), []
        for x in allr:
            k = id(x) if isinstance(x, Op) else x
            if k not in seen:
                seen.add(k)
                uniq.append(x)
        t.st = {None: [None, uniq]}

    def soft_barrier(self):
        return

    @staticmethod
    def _norm(lst):
        out = []
        for x in lst:
            if not isinstance(x, tuple):
                x = (x, None)
            if x[0].psum:
                x = (x[0], None)
            out.append(x)
        return out

    @staticmethod
    def _states(t, s):
        if s is None:
            return list(t.st.values())
        r = []
        if None in t.st:
            r.append(t.st[None])
        if s in t.st:
            r.append(t.st[s])
        return r

    def _collect(self, reads, writes):
        deps, ddeps = [], {}

        def add(ev):
            if ev is None:
                return
            if isinstance(ev, Op):
                deps.append(ev)
            else:
                k = ev
                ddeps[k] = self.dtotal[k]

        for (t, s) in reads:
            for st in self._states(t, s):
                add(st[0])
        for (t, s) in writes:
            for st in self._states(t, s):
                add(st[0])
                for r in st[1]:
                    add(r)
        return deps, ddeps

    def _update(self, ev, reads, writes):
        for (t, s) in reads:
            st = t.st.setdefault(s, [None, []])
            st[1].append(ev)
        for (t, s) in writes:
            if s is None:
                t.st = {None: [ev, []]}
            else:
                t.st[s] = [ev, []]

    def _add(self, o):
        o.seg = self.seg
        o.oi = len(self.all)
        self.all.append(o)
        self.ops[o.eng].append(o)

    def op(self, eng, fn, reads=(), writes=(), cost=0.2, start=None, stop=None, tbl=None):
        reads = self._norm(reads)
        writes = self._norm(writes)
        writes = writes + [r for r in reads if r[0].psum]
        deps, ddeps = self._collect(reads, writes)
        o = Op(eng, fn, deps, ddeps, cost=cost)
        o.tbl = tbl
        if eng == "tensor":
            if self.cur_unit is None or start is None or start:
                self.nunits += 1
                self.cur_unit = self.nunits
            o.unit = self.cur_unit
            if stop is None or stop:
                self.cur_unit = None
        self._add(o)
        self._update(o, reads, writes)

    def dma(self, eng, out, in_, reads=(), writes=(), semtile=None):
        assert eng in ("sync", "gpsimd")
        reads = self._norm(reads)
        writes = self._norm(writes)
        deps, ddeps = self._collect(reads, writes)
        if semtile.dsem is None:
            semtile.dsem = ("D", semtile.name)
            self.dsems[semtile.dsem] = None
        semtile.dcount = self.dtotal.get(semtile.dsem, 0) + 16
        self.dtotal[semtile.dsem] = semtile.dcount
        o = Op(eng, None, deps, ddeps, dma=(out, in_, semtile.dsem), cost=0.1)
        self.dma_by[(semtile.dsem, semtile.dcount)] = o
        self._add(o)
        self._update(semtile.dsem, reads, writes)

    def barrier(self):
        for e in self.ENG:
            o = Op(e, "bar", [], {}, cost=0.05)
            self._add(o)
        self.seg += 1
        for t in self.tiles:
            t.st = {}

    def finish(self, eng="sync"):
        self.barrier()

    def schedule(self):
        LAT = 0.25
        W = 64
        order = {e: [] for e in self.ENG}
        nseg = self.seg + 1
        byseg = [{e: [] for e in self.ENG} for _ in range(nseg)]
        for o in self.all:
            byseg[o.seg][o.eng].append(o)
        for sg in range(nseg):
            lists = byseg[sg]
            items = {e: [] for e in self.ENG}
            item_of = {}
            for e in self.ENG:
                cur = None
                for o in lists[e]:
                    if o.unit is not None and cur is not None and cur["unit"] == o.unit:
                        cur["ops"].append(o)
                    else:
                        cur = {"unit": o.unit, "ops": [o], "nrem": 0, "rt": 0.0, "succ": [], "eng": e, "done": False,
                               "bar": o.fn == "bar"}
                        items[e].append(cur)
                    item_of[id(o)] = cur
            dmafin = {}
            for e in self.ENG:
                for it in items[e]:
                    preds = set()
                    for o in it["ops"]:
                        dl = list(o.deps)
                        for k, v in o.ddeps.items():
                            dd = self.dma_by.get((k, v))
                            if dd is not None:
                                dl.append(dd)
                        for d in dl:
                            if d.seg != sg:
                                continue
                            p = item_of[id(d)]
                            if p is it:
                                continue
                            preds.add(id(p))
                            if id(p) not in it.setdefault("pset", {}):
                                it["pset"][id(p)] = p
                    for p in it.get("pset", {}).values():
                        p["succ"].append(it)
                    it["nrem"] = len(it.get("pset", {}))
            free = {e: 0.0 for e in self.ENG}
            heads = {e: 0 for e in self.ENG}
            cur_tbl = None
            npend = sum(len(v) for v in items.values())
            while npend > 0:
                progressed = False
                for e in self.ENG:
                    L = items[e]
                    while heads[e] < len(L) and L[heads[e]]["done"]:
                        heads[e] += 1
                    if heads[e] >= len(L):
                        continue
                    wmax = W if e in self.REORDER else 1
                    pick, pick_t = None, None
                    i, cnt = heads[e], 0
                    while i < len(L) and cnt < wmax:
                        it = L[i]
                        if not it["done"]:
                            cnt += 1
                            if it["bar"]:
                                if i == heads[e] and it["nrem"] == 0:
                                    pick, pick_t = it, free[e]
                                break
                            if it["nrem"] == 0:
                                rt = it["rt"]
                                for o in it["ops"]:
                                    for k in o.ddeps:
                                        rt = max(rt, dmafin.get(k, 0.0))
                                st_t = max(rt, free[e])
                                if e == "scalar":
                                    tb = it["ops"][0].tbl
                                    if tb is not None and tb != cur_tbl:
                                        st_t += 1.4
                                if pick is None or st_t < pick_t - 1e-9:
                                    pick, pick_t = it, st_t
                                if st_t <= free[e] + 1e-9:
                                    break
                        i += 1
                    if pick is None:
                        continue
                    tcur = pick_t
                    if e == "scalar" and pick["ops"][0].tbl is not None:
                        cur_tbl = pick["ops"][0].tbl
                    for o in pick["ops"]:
                        tcur += o.cost
                        o.fin = tcur
                        order[e].append(o)
                        if o.dma is not None:
                            dmafin[o.dma[2]] = max(dmafin.get(o.dma[2], 0.0), tcur + 2.5)
                    pick["done"] = True
                    npend -= 1
                    free[e] = tcur
                    for sc in pick["succ"]:
                        sc["nrem"] -= 1
                        lat = 0.0 if (sc["eng"] == "tensor" and e == "tensor") else LAT
                        if sc["rt"] < tcur + lat:
                            sc["rt"] = tcur + lat
                    progressed = True
                if not progressed:
                    raise RuntimeError("scheduler deadlock in segment %d" % sg)
        for e in self.ENG:
            assert len(order[e]) == len(self.ops[e]), (e, len(order[e]), len(self.ops[e]))
            for i, o in enumerate(order[e]):
                o.pos = i
        self.order = order

    def emit(self, stack):
        nc = self.nc
        self.schedule()
        order = self.order
        for o in self.all:
            for d in o.deps:
                if d.eng == "tensor" and o.eng == "tensor":
                    continue
                d.inc = True
        barlast = {}
        for e in self.ENG:
            last = None
            for o in order[e]:
                if o.fn == "bar":
                    barlast[(e, o.seg)] = last
                elif o.dma is None:
                    last = o
        for v in barlast.values():
            if v is not None:
                v.inc = True
        esem = {e: stack.enter_context(nc.semaphore("es_" + e)) for e in self.ENG}
        for k in self.dsems:
            self.dsems[k] = stack.enter_context(nc.semaphore("ds_" + k[1]))
        cnt = {}
        for e in self.ENG:
            c_ = 0
            for o in order[e]:
                if o.inc and o.dma is None:
                    c_ += 1
                cnt[id(o)] = c_
        dma_at_bar = {}
        run_tot = {}
        segs = {}
        for o in self.all:
            if o.dma is not None:
                segs.setdefault(o.seg, {})
        tot = {}
        for sg in range(self.seg + 1):
            for o in self.all:
                pass
        cum = {}
        per_seg_tot = []
        cur = {}
        last_seg = 0
        for o in self.all:
            while last_seg < o.seg:
                per_seg_tot.append(dict(cur))
                last_seg += 1
            if o.dma is not None:
                cur[o.dma[2]] = cur.get(o.dma[2], 0) + 16
        while len(per_seg_tot) <= self.seg:
            per_seg_tot.append(dict(cur))
        prog = self

        def run(ename, eng):
            waited = {}

            def wait(key, sem, val):
                if val <= 0 or waited.get(key, 0) >= val:
                    return
                waited[key] = val
                eng.wait_ge(sem, val)

            for o in order[ename]:
                if o.fn == "bar":
                    for e2 in prog.ENG:
                        lo = barlast.get((e2, o.seg))
                        if lo is not None:
                            wait(e2, esem[e2], cnt[id(lo)])
                    for k, v in per_seg_tot[o.seg].items():
                        wait(k, prog.dsems[k], v)
                    continue
                need = {}
                for d in o.deps:
                    if d.eng == "tensor" and ename == "tensor":
                        continue
                    v = cnt[id(d)]
                    if need.get(d.eng, 0) < v:
                        need[d.eng] = v
                for k, v in need.items():
                    wait(k, esem[k], v)
                for k, v in o.ddeps.items():
                    wait(k, prog.dsems[k], v)
                if o.dma is not None:
                    out, in_, dk = o.dma
                    eng.dma_start(out=out, in_=in_).then_inc(prog.dsems[dk], 16)
                    continue
                ins = o.fn(eng)
                if o.inc:
                    ins.then_inc(esem[ename], 1)

        stack.enter_context(nc.allow_non_contiguous_dma("tiny strided parameter loads"))
        block = stack.enter_context(nc.Block())

        @block.sync
        def _(e):
            run("sync", e)

        @block.scalar
        def _(e):
            run("scalar", e)

        @block.vector
        def _(e):
            run("vector", e)

        @block.gpsimd
        def _(e):
            run("gpsimd", e)

        @block.tensor
        def _(e):
            run("tensor", e)


def bcast(ap, shape, axis):
    return ap.unsqueeze(axis).to_broadcast(list(shape))


class K:
    def __init__(self, layers=(0, 1), phases="ABC", dbg=()):
        self.layers = layers
        self.phases = phases
        self.dbg = dbg


def build(layers=(0, 1), phases="ABC", dbg=(), sub="GML"):
    from contextlib import ExitStack
    nc = bass.Bass("TRN2", target_bir_lowering=False)
    P = Prog(nc)
    stack = ExitStack()

    def din(name, shape, dt=F32):
        return nc.dram_tensor(name, list(shape), dt, kind="ExternalInput").ap()

    x_d = din("x", [S, D])
    mem_d = din("mem", [256, D])
    pos_d = din("positions", [1, S], I32)
    w = {}
    for name, shape in [
        ("w_in", [2, D, IN_W]), ("w_out", [2, D, D]), ("gla_w_a2", [2, 16, 256]), ("gla_b_a", [2, 256]),
        ("gla_norm_g", [2, 256]), ("ml_conv_w", [2, 4, 512]), ("ml_b_i", [2, 4]), ("ml_b_f", [2, 4]),
        ("ml_norm_g", [2, 256]), ("mla_q_norm_g", [2, 256]), ("mla_w_uq", [2, 256, 768]),
        ("mla_kv_norm_g", [2, 128]), ("mla_w_ukv", [2, 128, 1024]), ("xa_w_q", [2, D, D]),
        ("xa_w_kv", [2, D, 2 * D]), ("xa_w_o", [2, D, D]), ("moe_w_group", [2, D, 4]), ("moe_b_group", [2, 4]),
        ("moe_w_router", [2, D, 32]), ("moe_b_router", [2, 32]), ("moe_w_gate", [2, 32, D, 256]),
        ("moe_w_up", [2, 32, D, 256]), ("moe_w_down", [2, 32, 256, D]),
        ("ln1_g", [2, D]), ("ln1_b", [2, D]), ("ln2_g", [2, D]), ("ln2_b", [2, D]), ("ln3_g", [2, D]), ("ln3_b", [2, D]),
    ]:
        w[name] = din(name, shape)
    cmat_d = din("cmat", [128, 5, 128])
    sel_d = din("sel", [32, 32, 128])
    ropeinv_d = din("ropeinv", [96, 1])
    out_d = nc.dram_tensor("out", [S, D], F32, kind="ExternalOutput").ap()
    dbg_d = {}
    for name, shape in dbg:
        dbg_d[name] = nc.dram_tensor(name, list(shape), F32, kind="ExternalOutput").ap()

    def sb(name, shape, dt=F32):
        return P.tile(name, stack.enter_context(nc.sbuf_tensor(name, list(shape), dt)))

    x_tm = sb("x_tm", [128, NT, D])
    xT = sb("xT", [128, 8, S], BF16)
    cmat = sb("cmat_sb", [128, 5, 128])
    cmat_bf = sb("cmat_bf", [128, 5, 128], BF16)
    lnp = sb("lnp", [128, 2, D])
    ps = [P.tile("ps%d" % i, stack.enter_context(nc.psum_tensor("ps%d" % i, [128, 512], F32))) for i in range(8)]
    for p_ in ps:
        p_.psum = True
    IDENT, TRII, TRIS, MMLA, ONES = range(5)

    def fsz(ap):
        try:
            return int(ap.free_size())
        except Exception:
            return 256

    def mm(out, lhsT, rhs, start, stop, reads, writes):
        n = fsz(rhs)
        cst = max(64, n) / 2400.0 * (4.0 if rhs.dtype == F32 else 1.0) + 0.012
        P.op("tensor", lambda e: e.matmul(out, lhsT, rhs, start=start, stop=stop), reads, writes, cost=cst,
             start=start, stop=stop)

    def tr(out, in_, ident, reads, writes):
        P.op("tensor", lambda e: e.transpose(out, in_, ident), reads, writes, cost=0.08)

    def act(out, in_, func, reads, writes, bias=None, scale=None, accum_out=None):
        kw = {}
        if bias is not None:
            kw["bias"] = bias
        if scale is not None:
            kw["scale"] = scale
        if accum_out is not None:
            kw["accum_out"] = accum_out
        tb = {AF.Silu: "s", AF.Sigmoid: "s", AF.Sqrt: "q", AF.Sin: "n", AF.Copy: None, AF.Identity: None}.get(func, "e")
        P.op("scalar", lambda e: e.activation(out, in_, func, **kw), reads, writes, cost=0.25 + fsz(out) * 0.00085, tbl=tb)

    def vcost(out, f=1.0):
        return 0.12 + fsz(out) * 0.00105 * f

    def tt(out, a, b, op, reads, writes, eng="vector"):
        P.op(eng, lambda e: e.tensor_tensor(out, a, b, op), reads, writes, cost=vcost(out))

    def ts(out, a, s1, op0, reads, writes, s2=None, op1=None, eng="vector"):
        if op1 is None:
            P.op(eng, lambda e: e.tensor_scalar(out, a, s1, None, op0), reads, writes, cost=vcost(out, 0.6))
        else:
            P.op(eng, lambda e: e.tensor_scalar(out, a, s1, s2, op0, op1), reads, writes, cost=vcost(out, 0.6))

    def stt(out, a, s, b, op0, op1, reads, writes):
        P.op("vector", lambda e: e.scalar_tensor_tensor(out, a, s, b, op0, op1), reads, writes, cost=vcost(out))

    def cp(out, in_, reads, writes, eng="vector"):
        if eng == "scalar":
            P.op("scalar", lambda e: e.copy(out, in_), reads, writes, cost=0.25 + fsz(out) * 0.00085)
        else:
            P.op(eng, lambda e: e.tensor_copy(out, in_), reads, writes, cost=vcost(out, 0.6))

    def red(out, in_, op, reads, writes, axis=AX.X):
        P.op("vector", lambda e: e.tensor_reduce(out, in_, axis, op), reads, writes, cost=vcost(in_))

    def load_cast(dst_tile, dst_ap, src_ap, sub=None):
        P.dma("gpsimd", dst_ap, src_ap, writes=[(dst_tile, sub)], semtile=dst_tile)

    def load(dst_tile, dst_ap, src_ap, sub=None, eng="sync"):
        P.dma(eng, dst_ap, src_ap, writes=[(dst_tile, sub)], semtile=dst_tile)

    load(cmat, cmat[:], cmat_d)
    cp(cmat_bf[:], cmat[:], [cmat], [cmat_bf])
    for t in range(NT):
        load(x_tm, x_tm[:, t, :], x_d[t * 128:(t + 1) * 128, :], sub=t, eng="sync")

    memT = sb("memT", [128, 8, 256], BF16)
    if "B" in phases:
        from contextlib import ExitStack as _ES
        pre = _ES()
        mem_f = P.tile("mem_f", pre.enter_context(nc.sbuf_tensor("mem_f", [128, 2, D], F32)))
        mem_b = P.tile("mem_b", pre.enter_context(nc.sbuf_tensor("mem_b", [128, 2, D], BF16)))
        load(mem_f, mem_f[:], mem_d.rearrange("(t p) d -> p t d", p=128))
        cp(mem_b[:], mem_f[:], [mem_f], [mem_b])
        pbm = ps[7].h.bitcast(BF16)
        for mt in range(2):
            for c8 in range(8):
                tr(pbm[:, c8 * 128:(c8 + 1) * 128], mem_b[:, mt, c8 * 128:(c8 + 1) * 128], cmat_bf[:, 0, :],
                   [mem_b, cmat_bf], [(ps[7], c8)])
            cp(memT[:, :, mt * 128:(mt + 1) * 128], pbm[:, :].rearrange("p (c n) -> p c n", c=8), [ps[7]], [(memT, mt)])
        P.soft_barrier()
        pre.close()

    cs = None
    if "A" in phases and "L" in sub:
        import math
        from contextlib import ExitStack as _ES2
        cs = sb("rope_cs", [96, 2, S], BF16)
        pre2 = _ES2()

        def tmp(name, dt=F32):
            return P.tile(name, pre2.enter_context(nc.sbuf_tensor(name, [96, S], dt)))
        posi, ang, rr, kf, ki, mk = tmp("rp_posi", I32), tmp("rp_ang"), tmp("rp_r"), tmp("rp_kf"), tmp("rp_ki", I32), tmp("rp_m")
        rinv = P.tile("rp_inv", pre2.enter_context(nc.sbuf_tensor("rp_inv", [96, 1], F32)))
        R_ = slice(64, 96)
        load(rinv, rinv[:], ropeinv_d)
        P.dma("sync", posi[R_, :].unsqueeze(1), pos_d[0:1, :].partition_broadcast(32), writes=[posi], semtile=posi)
        cp(ang[R_, :], posi[R_, :], [posi], [ang])
        ts(ang[R_, :], ang[R_, :], rinv[R_, 0:1], ALU.mult, [ang, rinv], [ang])
        TWO_PI = 2.0 * math.pi
        C1 = 6.28125
        C2 = TWO_PI - C1
        for which, shift in ((1, 0.0), (0, math.pi / 2)):
            ts(rr[R_, :], ang[R_, :], shift, ALU.add, [ang], [rr])
            ts(kf[R_, :], rr[R_, :], 1.0 / TWO_PI, ALU.mult, [rr], [kf])
            cp(ki[R_, :], kf[R_, :], [kf], [ki])
            cp(kf[R_, :], ki[R_, :], [ki], [kf])
            stt(rr[R_, :], kf[R_, :], -C1, rr[R_, :], ALU.mult, ALU.add, [kf, rr], [rr])
            stt(rr[R_, :], kf[R_, :], -C2, rr[R_, :], ALU.mult, ALU.add, [kf, rr], [rr])
            ts(mk[R_, :], rr[R_, :], math.pi, ALU.is_gt, [rr], [mk])
            stt(rr[R_, :], mk[R_, :], -TWO_PI, rr[R_, :], ALU.mult, ALU.add, [mk, rr], [rr])
            ts(mk[R_, :], rr[R_, :], -math.pi, ALU.is_lt, [rr], [mk])
            stt(rr[R_, :], mk[R_, :], TWO_PI, rr[R_, :], ALU.mult, ALU.add, [mk, rr], [rr])
            ts(rr[R_, :], rr[R_, :], 3.141592, ALU.min, [rr], [rr], s2=-3.141592, op1=ALU.max)
            act(cs[R_, which, :], rr[R_, :], AF.Sin, [rr], [(cs, which)])
        P.soft_barrier()
        pre2.close()

    lnw = sb("ln_work", [128, 16])
    xbf = sb("ln_xbf", [128, 2, D], BF16)
    ps_bf = [ps[i].h.bitcast(BF16) for i in range(8)]

    def load_ln(gname, bname, l):
        load(lnp, lnp[:, 0, :].unsqueeze(1), w[gname][l:l + 1, :].partition_broadcast(128), sub=0)
        load(lnp, lnp[:, 1, :].unsqueeze(1), w[bname][l:l + 1, :].partition_broadcast(128), sub=1)

    def layer_norm_tile(t, pbank):
        xt = x_tm[:, t, :]
        st = lnw[:, 0:12].rearrange("p (a b) -> p a b", a=2)
        for hh in range(2):
            P.op("vector", lambda e, hh=hh: e.bn_stats(st[:, hh, :], x_tm[:, t, hh * 512:(hh + 1) * 512]),
                 [(x_tm, t)], [(lnw, "st%d" % hh)])
        P.op("vector", lambda e: e.bn_aggr(lnw[:, 12:14], lnw[:, 0:12]), [(lnw, "st0"), (lnw, "st1")], [(lnw, "mv")])
        ts(lnw[:, 14:15], lnw[:, 13:14], LN_EPS, ALU.add, [(lnw, "mv")], [(lnw, "sd")])
        act(lnw[:, 14:15], lnw[:, 14:15], AF.Ln, [(lnw, "sd")], [(lnw, "sd")])
        act(lnw[:, 15:16], lnw[:, 14:15], AF.Exp, [(lnw, "sd")], [(lnw, "rs")], scale=-0.5)
        ts(xt, xt, lnw[:, 12:13], ALU.subtract, [(x_tm, t), (lnw, "mv"), (lnw, "rs")], [(x_tm, t)],
           s2=lnw[:, 15:16], op1=ALU.mult)
        tt(xt, xt, lnp[:, 0, :], ALU.mult, [(x_tm, t), (lnp, 0)], [(x_tm, t)])
        tt(xt, xt, lnp[:, 1, :], ALU.add, [(x_tm, t), (lnp, 1)], [(x_tm, t)])
        refresh_xT(t, pbank)

    def refresh_xT(t, pbank):
        xt = x_tm[:, t, :]
        b = t % 2
        cp(xbf[:, b, :], xt, [(x_tm, t)], [(xbf, b)], eng="scalar")
        pb = ps_bf[pbank]
        for c in range(8):
            tr(pb[:, c * 128:(c + 1) * 128], xbf[:, b, c * 128:(c + 1) * 128], cmat_bf[:, IDENT, :],
               [(xbf, b), cmat_bf], [(ps[pbank], c)])
        cp(xT[:, :, t * 128:(t + 1) * 128], pb[:, :].rearrange("p (c n) -> p c n", c=8),
           [ps[pbank]], [(xT, t)])

    def store_out():
        for t in range(NT):
            P.dma("sync", out_d[t * 128:(t + 1) * 128, :], x_tm[:, t, :],
                  reads=[(x_tm, t)], semtile=x_tm)

    ctx = dict(nc=nc, P=P, stack=stack, w=w, x_tm=x_tm, xT=xT, cmat=cmat, cmat_bf=cmat_bf, lnp=lnp, ps=ps,
               ps_bf=ps_bf, sb=sb, mm=mm, tr=tr, act=act, tt=tt, ts=ts, stt=stt, cp=cp, red=red,
               load=load, load_cast=load_cast, load_ln=load_ln, layer_norm_tile=layer_norm_tile,
               sel_d=sel_d, ropeinv_d=ropeinv_d, memT=memT, sub=sub, cs=cs, mem_d=mem_d, pos_d=pos_d, dbg_d=dbg_d)

    first = True
    for l in layers:
        if first:
            for t in range(NT):
                refresh_xT(t, 5 + t % 3)
        if "A" in phases:
            phase_A(ctx, l)
        if "B" in phases:
            phase_B(ctx, l)
        if "C" in phases:
            phase_C(ctx, l)
        first = False
    store_out()
    P.finish("sync")
    P.emit(stack)
    stack.close()
    return nc


def phase_C(c, l):
    from contextlib import ExitStack
    nc, P, w = c["nc"], c["P"], c["w"]
    x_tm, xT, ps, cmat, cmat_bf = c["x_tm"], c["xT"], c["ps"], c["cmat"], c["cmat_bf"]
    mm, tr, act, tt, ts, stt, cp, red = c["mm"], c["tr"], c["act"], c["tt"], c["ts"], c["stt"], c["cp"], c["red"]
    load, load_cast = c["load"], c["load_cast"]
    IDENT = 0
    ph = ExitStack()

    def sb(name, shape, dt=F32):
        return P.tile(name, ph.enter_context(nc.sbuf_tensor("%s_%d" % (name, l), list(shape), dt)))

    c["load_ln"]("ln3_g", "ln3_b", l)
    gateT = sb("c_gateT", [32, S], BF16)
    sel = sb("c_sel", [32, 32, 128], BF16)
    load_cast(sel, sel[:], c["sel_d"])
    ph_r = ExitStack()
    _sb_outer = sb

    def sb(name, shape, dt=F32):
        return P.tile(name, ph_r.enter_context(nc.sbuf_tensor("%s_%d" % (name, l), list(shape), dt)))
    wr = sb("c_wr", [128, 8, 36], BF16)
    load_cast(wr, wr[:, :, 0:4], w["moe_w_group"][l].rearrange("(kc p) n -> p kc n", p=128), sub="g")
    load_cast(wr, wr[:, :, 4:36], w["moe_w_router"][l].rearrange("(kc p) n -> p kc n", p=128), sub="r")
    rb = sb("c_rb", [128, 36])
    load(rb, rb[:, 0:4].unsqueeze(1), w["moe_b_group"][l:l + 1, :].partition_broadcast(128), sub="g")
    load(rb, rb[:, 4:36].unsqueeze(1), w["moe_b_router"][l:l + 1, :].partition_broadcast(128), sub="r")
    lg = sb("c_lg", [128, NT, 36])
    for half in range(2):
        pr = ps[half]
        for tl in range(8):
            t = half * 8 + tl
            for kc in range(8):
                mm(pr[:, tl * 36:(tl + 1) * 36], xT[:, kc, t * 128:(t + 1) * 128], wr[:, kc, :], kc == 0, kc == 7,
                   [(xT, t), wr], [(pr, tl)])
        tt(lg[:, half * 8:(half + 1) * 8, :], pr[:, 0:288].rearrange("p (t n) -> p t n", t=8),
           bcast(rb[:, :], [128, 8, 36], 1), ALU.add, [pr, rb], [(lg, half)])
    r1 = sb("c_r1", [128, NT, 64])
    lgg = lg[:, :, 0:4]
    lge = lg[:, :, 4:36].rearrange("p t (g e) -> p t g e", g=4)
    gmax, gsum, ohg, eg = r1[:, :, 0], r1[:, :, 1], r1[:, :, 4:8], r1[:, :, 8:12]
    red(gmax, lgg, ALU.max, [lg], [(r1, "gmax")])
    tt(eg, lgg, bcast(gmax, [128, NT, 4], 2), ALU.subtract, [lg, (r1, "gmax")], [(r1, "eg")])
    tt(ohg, lgg, bcast(gmax, [128, NT, 4], 2), ALU.is_equal, [lg, (r1, "gmax")], [(r1, "ohg")])
    act(eg, eg, AF.Exp, [(r1, "eg")], [(r1, "eg")])
    red(gsum, eg, ALU.add, [(r1, "eg")], [(r1, "gsum")])
    gp = r1[:, :, 2]
    P.op("vector", lambda e: e.reciprocal(gp, gsum), [(r1, "gsum")], [(r1, "gp")])
    tmp = sb("c_tmp", [128, NT, 4, 8])
    tt(tmp[:], lge, bcast(ohg, [128, NT, 4, 8], 3), ALU.mult, [lg, (r1, "ohg")], [tmp])
    esel = r1[:, :, 16:24]
    red(esel, tmp[:].rearrange("p t g e -> p t e g"), ALU.add, [tmp], [(r1, "esel")])
    m1, m2, dd = r1[:, :, 3], r1[:, :, 12], r1[:, :, 13]
    mk1, mk2, e2 = r1[:, :, 24:32], r1[:, :, 32:40], r1[:, :, 40:48]
    red(m1, esel, ALU.max, [(r1, "esel")], [(r1, "m1")])
    tt(mk1, esel, bcast(m1, [128, NT, 8], 2), ALU.is_equal, [(r1, "esel"), (r1, "m1")], [(r1, "mk1")])
    stt(e2, mk1, -1e30, esel, ALU.mult, ALU.add, [(r1, "mk1"), (r1, "esel")], [(r1, "e2")])
    red(m2, e2, ALU.max, [(r1, "e2")], [(r1, "m2")])
    tt(mk2, e2, bcast(m2, [128, NT, 8], 2), ALU.is_equal, [(r1, "e2"), (r1, "m2")], [(r1, "mk2")])
    tt(dd, m2, m1, ALU.subtract, [(r1, "m1"), (r1, "m2")], [(r1, "dd")])
    act(dd, dd, AF.Exp, [(r1, "dd")], [(r1, "dd")])
    w1, w2 = r1[:, :, 14], r1[:, :, 15]
    ts(w1, dd, 1.0, ALU.add, [(r1, "dd")], [(r1, "w1")])
    P.op("vector", lambda e: e.reciprocal(w1, w1), [(r1, "w1")], [(r1, "w1")])
    tt(w1, w1, gp, ALU.mult, [(r1, "w1"), (r1, "gp")], [(r1, "w1")])
    tt(w2, w1, dd, ALU.mult, [(r1, "w1"), (r1, "dd")], [(r1, "w2")])
    comb = r1[:, :, 48:56]
    tt(comb, mk1, bcast(w1, [128, NT, 8], 2), ALU.mult, [(r1, "mk1"), (r1, "w1")], [(r1, "comb")])
    tt(mk2, mk2, bcast(w2, [128, NT, 8], 2), ALU.mult, [(r1, "mk2"), (r1, "w2")], [(r1, "mk2")])
    tt(comb, comb, mk2, ALU.add, [(r1, "comb"), (r1, "mk2")], [(r1, "comb")])
    gate = sb("c_gate", [128, NT, 4, 8])
    tt(gate[:], bcast(ohg, [128, NT, 4, 8], 3), bcast(comb, [128, NT, 4, 8], 2), ALU.mult,
       [(r1, "ohg"), (r1, "comb")], [gate])
    for g in range(4):
        pg = ps[2 + g % 2]
        for tl in range(4):
            t = g * 4 + tl
            tr(pg[0:32, tl * 128:(tl + 1) * 128], gate[:, t, :, :].rearrange("p g e -> p (g e)"), cmat[:, IDENT, :],
               [gate, cmat], [(pg, tl)])
        cp(gateT[:, g * 512:(g + 1) * 512], pg[0:32, :], [pg], [(gateT, g)], eng="scalar")
    if "c_gate" in c["dbg_d"]:
        P.dma("sync", c["dbg_d"]["c_gate"].rearrange("(t p) n -> p t n", p=128),
              gate[:].rearrange("p t g e -> p t (g e)"), reads=[gate], semtile=gate)

    P.soft_barrier()
    ph_r.close()
    sb = _sb_outer
    NSLOT = 4
    wg = sb("c_wg", [128, NSLOT, 8, 256], BF16)
    wu = sb("c_wu", [128, NSLOT, 8, 256], BF16)
    wd = sb("c_wd", [128, NSLOT, 2, D], BF16)
    wsem = [sb("c_wsem%d" % i, [1, 1]) for i in range(NSLOT)]
    hT = sb("c_hT", [128, 2, 2, S], BF16)
    sg = sb("c_sg", [128, 2, 512], BF16)
    gb = sb("c_gb", [128, 2, 512], BF16)

    def load_expert(e):
        s = e % NSLOT
        P.dma("gpsimd", wg[:, s, :, :], w["moe_w_gate"][l, e].rearrange("(kc p) n -> p kc n", p=128),
              writes=[(wg, s)], semtile=wsem[s])
        P.dma("gpsimd", wu[:, s, :, :], w["moe_w_up"][l, e].rearrange("(kc p) n -> p kc n", p=128),
              writes=[(wu, s)], semtile=wsem[s])
        P.dma("gpsimd", wd[:, s, :, :], w["moe_w_down"][l, e].rearrange("(kc p) n -> p kc n", p=128),
              writes=[(wd, s)], semtile=wsem[s])

    for e in range(2):
        load_expert(e)
    unit = 0
    for blk in range(16):
        for ei in range(2):
            e = blk * 2 + ei
            s = e % NSLOT
            if e + 2 < 32:
                load_expert(e + 2)
            for g in range(4):
                tok = slice(g * 512, (g + 1) * 512)
                pgb = ps[4]
                mm(pgb[:, :], sel[:, e, :], gateT[:, tok], True, True, [sel, (gateT, g)], [pgb])
                ub = (e * 4 + g) % 2
                cp(gb[:, ub, :], pgb[:, :], [pgb], [(gb, ub)], eng="scalar")
                for fc in range(2):
                    pgt, put = ps[(unit % 2) * 2], ps[(unit % 2) * 2 + 1]
                    for kc in range(8):
                        mm(pgt[:, :], wg[:, s, kc, fc * 128:(fc + 1) * 128], xT[:, kc, tok], kc == 0, kc == 7,
                           [(wg, s), xT], [pgt])
                    for kc in range(8):
                        mm(put[:, :], wu[:, s, kc, fc * 128:(fc + 1) * 128], xT[:, kc, tok], kc == 0, kc == 7,
                           [(wu, s), xT], [put])
                    u2 = unit % 2
                    act(sg[:, u2, :], pgt[:, :], AF.Silu, [pgt], [(sg, u2)])
                    tt(sg[:, u2, :], put[:, :], sg[:, u2, :], ALU.mult, [put, (sg, u2)], [(sg, u2)])
                    tt(hT[:, ei, fc, tok], sg[:, u2, :], gb[:, ub, :], ALU.mult, [(sg, u2), (gb, ub)],
                       [(hT, (ei, g))])
                    unit += 1
        for t in range(NT):
            g = t // 4
            for hf in range(2):
                po = ps[5 + (t * 2 + hf) % 3]
                k = 0
                for ei in range(2):
                    s = (blk * 2 + ei) % NSLOT
                    for fc in range(2):
                        mm(po[:, :], hT[:, ei, fc, t * 128:(t + 1) * 128], wd[:, s, fc, hf * 512:(hf + 1) * 512],
                           k == 0, k == 3, [(hT, (ei, g)), (wd, s)], [po])
                        k += 1
                xs = x_tm[:, t, hf * 512:(hf + 1) * 512]
                if blk == 0:
                    stt(xs, xs, ALPHA, po[:, :], ALU.mult, ALU.add, [(x_tm, t), po], [(x_tm, t)])
                else:
                    tt(xs, xs, po[:, :], ALU.add, [(x_tm, t), po], [(x_tm, t)])
    for t in range(NT):
        c["layer_norm_tile"](t, 5 + t % 3)
    P.soft_barrier()
    ph.close()


def host_consts():
    cm = np.zeros((128, 5, 128), np.float32)
    i = np.arange(128)
    cm[:, 0, :] = np.eye(128, dtype=np.float32)
    cm[:, 1, :] = (i[:, None] <= i[None, :]).astype(np.float32)
    cm[:, 2, :] = (i[:, None] > i[None, :]).astype(np.float32)
    cm[:, 3, :] = ((i[:, None] // 64) <= (i[None, :] // 64)).astype(np.float32)
    cm[:, 4, :] = 1.0
    sel = np.zeros((32, 32, 128), np.float32)
    for e in range(32):
        sel[e, e, :] = 1.0
    inv = (10000.0 ** (-np.arange(16, dtype=np.float32) / 16)).astype(np.float32)
    ri = np.zeros((96, 1), np.float32)
    ri[64:80, 0] = inv
    ri[80:96, 0] = inv
    return {"cmat": cm, "sel": sel, "ropeinv": ri}


_NC_CACHE = {}


def run_cores(inputs, n_cores=8, layers=(0, 1), phases="ABC", dbg=(), sub="GML"):
    key = (tuple(layers), phases, tuple(dbg), sub)
    if key not in _NC_CACHE:
        _NC_CACHE[key] = build(layers, phases, dbg, sub)
    nc = _NC_CACHE[key]
    consts = host_consts()
    shared = {k: np.ascontiguousarray(v) for k, v in inputs.items() if k not in ("x", "mem", "positions")}
    shared.update(consts)
    in_maps = []
    for b in range(n_cores):
        m = dict(shared)
        m["x"] = np.ascontiguousarray(inputs["x"][b])
        m["mem"] = np.ascontiguousarray(inputs["mem"][b])
        m["positions"] = np.ascontiguousarray(inputs["positions"][b:b + 1]).astype(np.int32)
        in_maps.append(m)
    res = run_bass_kernel_spmd(nc, in_maps, core_ids=list(range(n_cores)))
    return res.results


def kernel(**inputs):
    inputs = {k: np.asarray(v) for k, v in inputs.items()}
    res = run_cores(inputs, 8)
    return np.stack([r["out"] for r in res], axis=0).astype(np.float32)


def phase_B(c, l):
    from contextlib import ExitStack
    nc, P, w = c["nc"], c["P"], c["w"]
    x_tm, xT, ps, cmat_bf, memT = c["x_tm"], c["xT"], c["ps"], c["cmat_bf"], c["memT"]
    mm, act, tt, stt, cp = c["mm"], c["act"], c["tt"], c["stt"], c["cp"]
    load_cast = c["load_cast"]
    ONES = 4
    ph = ExitStack()

    def sb(name, shape, dt=F32):
        return P.tile(name, ph.enter_context(nc.sbuf_tensor("%s_%d" % (name, l), list(shape), dt)))

    c["load_ln"]("ln2_g", "ln2_b", l)
    kT = sb("b_kT", [128, 8, 256], BF16)
    vx = sb("b_v", [128, 2, D], BF16)
    ph2 = ExitStack()
    wkv = P.tile("b_wkv", ph2.enter_context(nc.sbuf_tensor("b_wkv_%d" % l, [128, 8, 2 * D], BF16)))
    for kc in range(8):
        load_cast(wkv, wkv[:, kc, :], w["xa_w_kv"][l, kc * 128:(kc + 1) * 128, :], sub=kc)
    for cc in range(8):
        pk = ps[cc % 2]
        for kc in range(8):
            mm(pk[:, 0:256], wkv[:, kc, cc * 128:(cc + 1) * 128], memT[:, kc, :], kc == 0, kc == 7, [wkv, memT], [pk])
        cp(kT[:, cc, :], pk[:, 0:256], [pk], [(kT, cc)], eng="scalar" if cc % 2 else "vector")
    for mt in range(2):
        for hf in range(2):
            pv = ps[2 + hf]
            for kc in range(8):
                mm(pv[:, :], memT[:, kc, mt * 128:(mt + 1) * 128], wkv[:, kc, D + hf * 512:D + (hf + 1) * 512],
                   kc == 0, kc == 7, [wkv, memT], [pv])
            cp(vx[:, mt, hf * 512:(hf + 1) * 512], pv[:, :], [pv], [(vx, (mt, hf))], eng="scalar" if hf else "vector")
    P.soft_barrier()
    ph2.close()
    wq = sb("b_wq", [128, 8, D], BF16)
    wo = sb("b_wo", [128, 8, D], BF16)
    for kc in range(0, 8, 2):
        load_cast(wq, wq[:, kc:kc + 2, :], w["xa_w_q"][l, kc * 128:(kc + 2) * 128, :].rearrange("(k p) n -> p k n", p=128), sub=kc)
    for kc in range(0, 8, 2):
        load_cast(wo, wo[:, kc:kc + 2, :], w["xa_w_o"][l, kc * 128:(kc + 2) * 128, :].rearrange("(k p) n -> p k n", p=128), sub=kc)
    qT = sb("b_qT", [128, 8, 512], BF16)
    xaT = sb("b_xaT", [128, 8, 512], BF16)
    PT = sb("b_PT", [128, 2, 512], BF16)
    rden = sb("b_rden", [128, 2, 512])
    scale = 256 ** -0.5
    for g in range(4):
        tok = slice(g * 512, (g + 1) * 512)
        for cc in range(8):
            pq = ps[cc % 2]
            for kc in range(8):
                mm(pq[:, :], wq[:, kc, cc * 128:(cc + 1) * 128], xT[:, kc, tok], kc == 0, kc == 7, [wq, xT], [pq])
            cp(qT[:, cc, :], pq[:, :], [pq], [(qT, cc)], eng="scalar" if cc % 2 else "vector")
        for h in range(4):
            for mt in range(2):
                pst = ps[2 + mt]
                for j in range(2):
                    mm(pst[:, :], kT[:, h * 2 + j, mt * 128:(mt + 1) * 128], qT[:, h * 2 + j, :], j == 0, j == 1,
                       [(kT, h * 2 + j), (qT, h * 2 + j)], [pst])
                act(PT[:, mt, :], pst[:, :], AF.Exp, [pst], [(PT, mt)], scale=scale)
            pden = ps[4]
            for mt in range(2):
                mm(pden[:, :], cmat_bf[:, ONES, :], PT[:, mt, :], mt == 0, mt == 1, [cmat_bf, (PT, mt)], [pden])
            rb = h % 2
            act(rden[:, rb, :], pden[:, :], AF.Ln, [pden], [(rden, rb)])
            act(rden[:, rb, :], rden[:, rb, :], AF.Exp, [(rden, rb)], [(rden, rb)], scale=-1.0)
            for j in range(2):
                po = ps[5 + j]
                for mt in range(2):
                    mm(po[:, :], vx[:, mt, h * 256 + j * 128:h * 256 + (j + 1) * 128], PT[:, mt, :], mt == 0, mt == 1,
                       [vx, (PT, mt)], [po])
                tt(xaT[:, h * 2 + j, :], po[:, :], rden[:, rb, :], ALU.mult, [po, (rden, rb)], [(xaT, h * 2 + j)])
        for tl in range(4):
            t = g * 4 + tl
            for hf in range(2):
                pp = ps[hf]
                for cc in range(8):
                    mm(pp[:, :], xaT[:, cc, tl * 128:(tl + 1) * 128], wo[:, cc, hf * 512:(hf + 1) * 512], cc == 0, cc == 7,
                       [xaT, wo], [pp])
                xs = x_tm[:, t, hf * 512:(hf + 1) * 512]
                stt(xs, xs, ALPHA, pp[:, :], ALU.mult, ALU.add, [(x_tm, t), pp], [(x_tm, t)])
            c["layer_norm_tile"](t, 7)
    P.soft_barrier()
    ph.close()


def head_norm_gate(c, sbf, name, src, gs, out_bf, b, keyp):
    P, tt, ts, red, act = c["P"], c["tt"], c["ts"], c["red"], c["act"]
    st = sbf["hn_st"]
    cen = sbf["hn_cen"]
    sq = sbf["hn_sq"]
    s4 = src.rearrange("p (h e) -> p h e", h=4)
    mean = st[:, b, 0:4]
    var = st[:, b, 4:8]
    red(mean, s4, ALU.add, [keyp], [(st, (b, "m"))])
    ts(mean, mean, -1.0 / 64, ALU.mult, [(st, (b, "m"))], [(st, (b, "m"))])
    c4 = cen[:, b, :].rearrange("p (h e) -> p h e", h=4)
    tt(c4, s4, bcast(mean, [128, 4, 64], 2), ALU.add, [keyp, (st, (b, "m"))], [(cen, b)])
    tt(sq[:, b, :], cen[:, b, :], cen[:, b, :], ALU.mult, [(cen, b)], [(sq, b)])
    red(var, sq[:, b, :].rearrange("p (h e) -> p h e", h=4), ALU.add, [(sq, b)], [(st, (b, "v"))])
    ts(var, var, 1.0 / 64, ALU.mult, [(st, (b, "v"))], [(st, (b, "v"))], s2=LN_EPS, op1=ALU.add)
    act(var, var, AF.Ln, [(st, (b, "v"))], [(st, (b, "v"))])
    act(var, var, AF.Exp, [(st, (b, "v"))], [(st, (b, "v"))], scale=-0.5)
    tt(c4, c4, bcast(var, [128, 4, 64], 2), ALU.mult, [(cen, b), (st, (b, "v"))], [(cen, b)])
    tt(out_bf, cen[:, b, :], gs, ALU.mult, [(cen, b), (sbf["gs"], b)], [(sbf["obf"], b)])


def phase_A(c, l):
    from contextlib import ExitStack
    nc, P, w = c["nc"], c["P"], c["w"]
    sub = c.get("sub", "GML")
    c["first_mixer"] = True
    if "G" in sub:
        mixer_gla(c, l)
        c["first_mixer"] = False
    if "M" in sub:
        mixer_mlstm(c, l)
        c["first_mixer"] = False
    if "L" in sub:
        mixer_mla(c, l)
    c["load_ln"]("ln1_g", "ln1_b", l)
    for t in range(NT):
        c["layer_norm_tile"](t, 5 + t % 3)
    P.soft_barrier()


def mixer_gla(c, l):
    from contextlib import ExitStack
    nc, P, w = c["nc"], c["P"], c["w"]
    x_tm, xT, ps, ps_bf, cmat, cmat_bf = c["x_tm"], c["xT"], c["ps"], c["ps_bf"], c["cmat"], c["cmat_bf"]
    mm, tr, act, tt, ts, stt, cp, red = c["mm"], c["tr"], c["act"], c["tt"], c["ts"], c["stt"], c["cp"], c["red"]
    load, load_cast = c["load"], c["load_cast"]
    IDENT, TRII, TRIS = 0, 1, 2
    ph = ExitStack()

    def sb(name, shape, dt=F32):
        return P.tile(name, ph.enter_context(nc.sbuf_tensor("%s_%d" % (name, l), list(shape), dt)))

    win = sb("g_win", [128, 8, 1040], BF16)
    for kc in range(8):
        load_cast(win, win[:, kc, :], w["w_in"][l, kc * 128:(kc + 1) * 128, 0:1040], sub=kc)
    wa2 = sb("g_wa2", [16, 256], BF16)
    load_cast(wa2, wa2[0:16, :], w["gla_w_a2"][l])
    babc = sb("g_babc", [128, 256])
    load(babc, babc[:].unsqueeze(1), w["gla_b_a"][l:l + 1, :].partition_broadcast(128))
    wout = sb("g_wout", [128, 2, D], BF16)
    load_cast(wout, wout[:], w["w_out"][l, 0:256, :].rearrange("(k p) n -> p k n", p=128))
    gng = sb("g_gng", [128, 256])
    load(gng, gng[:].unsqueeze(1), w["gla_norm_g"][l:l + 1, :].partition_broadcast(128))
    NB = 3
    gaT = sb("g_gaT", [16, NB, 128], BF16)
    Lsb = sb("g_L", [128, NB, 256])
    E1 = sb("g_E1", [128, NB, 256])
    E2 = sb("g_E2", [128, NB, 256])
    E3 = sb("g_E3", [128, NB, 256])
    qs = [sb("g_qs0", [128, NB, 256], BF16), sb("g_qs1", [128, NB, 256], BF16)]
    for i in range(2):
        P.op("vector", lambda e, i=i: e.memset(qs[i][:], 0.0), [], [qs[i]])
    ksT = sb("g_ksT", [128, NB, 256], BF16)
    k2 = sb("g_k2", [128, NB, 256], BF16)
    vsb = sb("g_v", [128, NB, 256], BF16)
    gs = sb("g_gs", [128, NB, 256])
    PT = sb("g_PT", [128, 2, 4, 128], BF16)
    osb = sb("g_osb", [128, NB, 256])
    obf = sb("g_obf", [128, NB, 256], BF16)
    ogT = sb("g_ogT", [128, 2, 2, 128], BF16)
    Dend = sb("g_Dend", [128, NT, 2])
    Sst = sb("g_S", [128, 2, 64])
    Sbf = sb("g_Sbf", [128, 2, 2, 64], BF16)
    P.op("vector", lambda e: e.memset(Sst[:], 0.0), [], [Sst])
    P.op("vector", lambda e: e.memset(Sbf[:], 0.0), [], [Sbf])
    sbf = dict(hn_st=sb("g_hn_st", [128, NB, 8]), hn_cen=sb("g_hn_cen", [128, NB, 256]),
               hn_sq=sb("g_hn_sq", [128, NB, 256]), gs=gs, obf=obf)
    import os
    NTL = int(os.environ.get("DBG_TILES", NT))
    A_, B_, C_, D_, E_, F_, G_, H_ = ps
    pbH = ps_bf[7]

    def S0(t):
        b = t % NB
        tok = slice(t * 128, (t + 1) * 128)
        for cc in range(4):
            for kc in range(8):
                mm(A_[:, cc * 128:(cc + 1) * 128], win[:, kc, cc * 128:(cc + 1) * 128], xT[:, kc, tok], kc == 0, kc == 7,
                   [win, xT], [A_])
        for kc in range(8):
            mm(B_[0:16, 256:384], win[:, kc, 1024:1040], xT[:, kc, tok], kc == 0, kc == 7, [win, xT], [B_])
        cp(gaT[0:16, b, :], B_[0:16, 256:384], [B_], [(gaT, b)])
        mm(B_[:, 0:256], gaT[0:16, b, :], wa2[0:16, :], True, True, [(gaT, b), wa2], [B_])
        tt(Lsb[:, b, :], B_[:, 0:256], babc[:, :], ALU.add, [B_, babc], [(Lsb, b)])
        act(Lsb[:, b, :], Lsb[:, b, :], AF.Exp, [(Lsb, b)], [(Lsb, b)], scale=-1.0)
        ts(Lsb[:, b, :], Lsb[:, b, :], 1.0, ALU.add, [(Lsb, b)], [(Lsb, b)])
        act(Lsb[:, b, :], Lsb[:, b, :], AF.Ln, [(Lsb, b)], [(Lsb, b)])
        for kc in range(8):
            mm(D_[:, :], xT[:, kc, tok], win[:, kc, 256:768], kc == 0, kc == 7, [win, xT], [D_])
        for kc in range(8):
            mm(E_[:, 0:256], xT[:, kc, tok], win[:, kc, 768:1024], kc == 0, kc == 7, [win, xT], [E_])
        cp(vsb[:, b, :], D_[:, 256:512], [D_], [(vsb, b)], eng="scalar")
        act(gs[:, b, :], E_[:, 0:256], AF.Exp, [E_], [(gs, b)], scale=-1.0)
        ts(gs[:, b, :], gs[:, b, :], 1.0, ALU.add, [(gs, b)], [(gs, b)])
        act(gs[:, b, :], gs[:, b, :], AF.Ln, [(gs, b)], [(gs, b)])
        act(gs[:, b, :], gs[:, b, :], AF.Exp, [(gs, b)], [(gs, b)], scale=-1.0)
        stt(gs[:, b, :], E_[:, 0:256], 1.0, gs[:, b, :], ALU.mult, ALU.mult, [E_, (gs, b)], [(gs, b)])
        tt(gs[:, b, :], gs[:, b, :], gng[:, :], ALU.mult, [(gs, b), gng], [(gs, b)])
        for ch in range(2):
            mm(C_[:, ch * 128:(ch + 1) * 128], Lsb[:, b, ch * 128:(ch + 1) * 128], cmat[:, TRII, :], True, True,
               [(Lsb, b), cmat], [C_])
        mm(C_[:, 256:512], cmat[:, TRIS, :], Lsb[:, b, :], True, True, [(Lsb, b), cmat], [C_])
        act(E1[:, b, :], C_[:, 0:256], AF.Exp, [C_], [(E1, b)], scale=-1.0 / 16)
        act(E2[:, b, :], C_[:, 0:256], AF.Exp, [C_], [(E2, b)], scale=1.0 / 16)
        act(E3[:, b, :], C_[:, 256:512], AF.Exp, [C_], [(E3, b)], scale=-1.0 / 16)
        cp(Dend[:, t, :], E1[:, b, :].rearrange("p (c n) -> p c n", c=2)[:, :, 127], [(E1, b)], [(Dend, t)])
        for hp in range(2):
            rws = slice(hp * 64, (hp + 1) * 64)
            stt(qs[hp][rws, b, :], A_[rws, 0:256], 0.125, E1[rws, b, :], ALU.mult, ALU.mult, [A_, (E1, b)], [(qs[hp], b)])
        tt(ksT[:, b, :], A_[:, 256:512], E2[:, b, :], ALU.mult, [A_, (E2, b)], [(ksT, b)])
        tt(k2[:, b, :], D_[:, 0:256], E3[:, b, :], ALU.mult, [D_, (E3, b)], [(k2, b)])

    def S1(t):
        b = t % NB
        b2 = t % 2
        for h in range(4):
            hp, hc = h % 2, h // 2
            mm(F_[:, h * 128:(h + 1) * 128], ksT[:, b, hc * 128:(hc + 1) * 128],
               qs[hp][:, b, hc * 128:(hc + 1) * 128], True, True, [(ksT, b), (qs[hp], b)], [F_])
        for h in range(4):
            hc = h // 2
            mm(G_[:, h * 64:(h + 1) * 64], k2[:, b, hc * 128:(hc + 1) * 128], vsb[:, b, h * 64:(h + 1) * 64],
               True, True, [(k2, b), (vsb, b)], [G_])
        tt(PT[:, b2, :, :], F_[:, :].rearrange("p (h n) -> p h n", h=4), bcast(cmat[:, TRII, :], [128, 4, 128], 1),
           ALU.mult, [F_, cmat], [(PT, b2)])
        for h in range(4):
            hp, hc = h % 2, h // 2
            mm(G_[:, 256 + h * 64:256 + (h + 1) * 64], PT[:, b2, h, :], vsb[:, b, h * 64:(h + 1) * 64], True, False,
               [(PT, b2), (vsb, b)], [G_])
            mm(G_[:, 256 + h * 64:256 + (h + 1) * 64], qs[hp][:, b, hc * 128:(hc + 1) * 128],
               Sbf[:, b2, hc, :], False, True, [(qs[hp], b), (Sbf, b2)], [G_])
        for h in range(4):
            hp, hc = h % 2, h // 2
            rows = slice(hp * 64, (hp + 1) * 64)
            stt(Sst[rows, hc, :], Sst[rows, hc, :], Dend[rows, t, hc:hc + 1], G_[rows, h * 64:(h + 1) * 64],
                ALU.mult, ALU.add, [(Sst, h), (Dend, t), G_], [(Sst, h)])
        cp(Sbf[:, 1 - b2, :, :], Sst[:, :, :], [Sst], [(Sbf, 1 - b2)])
        cp(osb[:, b, :], G_[:, 256:512], [G_], [(osb, b)], eng="scalar")

    def S2(t):
        b = t % NB
        b2 = t % 2
        head_norm_gate(c, sbf, "g", osb[:, b, :], gs[:, b, :], obf[:, b, :], b, (osb, b))
        for ch in range(2):
            tr(pbH[:, ch * 128:(ch + 1) * 128], obf[:, b, ch * 128:(ch + 1) * 128], cmat_bf[:, IDENT, :],
               [(obf, b), cmat_bf], [H_])
        cp(ogT[:, b2, :, :], pbH[:, 0:256].rearrange("p (c n) -> p c n", c=2), [H_], [(ogT, b2)])
        for q4 in range(4):
            pq = H_[:, 128:384]
            for ch in range(2):
                mm(pq, ogT[:, b2, ch, :], wout[:, ch, q4 * 256:(q4 + 1) * 256], ch == 0, ch == 1, [(ogT, b2), wout], [H_])
            xs = x_tm[:, t, q4 * 256:(q4 + 1) * 256]
            if c["first_mixer"]:
                stt(xs, xs, ALPHA, pq, ALU.mult, ALU.add, [(x_tm, t), H_], [(x_tm, t)])
            else:
                tt(xs, xs, pq, ALU.add, [(x_tm, t), H_], [(x_tm, t)])

    for step in range(NTL + 2):
        if 0 <= step - 2 < NTL:
            S2(step - 2)
        if 0 <= step - 1 < NTL:
            S1(step - 1)
        if step < NTL:
            S0(step)
    P.soft_barrier()
    ph.close()


def mixer_mlstm(c, l):
    from contextlib import ExitStack
    import os
    nc, P, w = c["nc"], c["P"], c["w"]
    x_tm, xT, ps, ps_bf, cmat, cmat_bf = c["x_tm"], c["xT"], c["ps"], c["ps_bf"], c["cmat"], c["cmat_bf"]
    mm, tr, act, tt, ts, stt, cp, red = c["mm"], c["tr"], c["act"], c["tt"], c["ts"], c["stt"], c["cp"], c["red"]
    load, load_cast = c["load"], c["load_cast"]
    IDENT, TRII, ONES = 0, 1, 4
    ph = ExitStack()

    def sb(name, shape, dt=F32):
        return P.tile(name, ph.enter_context(nc.sbuf_tensor("%s_%d" % (name, l), list(shape), dt)))

    win = sb("m_win", [128, 8, 1032], BF16)
    for kc in range(8):
        load_cast(win, win[:, kc, :], w["w_in"][l, kc * 128:(kc + 1) * 128, 1040:2072], sub=kc)
    cw = sb("m_cw", [128, 4, 4])
    for j in range(4):
        load(cw, cw[:, :, j], w["ml_conv_w"][l, j, :].rearrange("(c p) -> p c", p=128), sub=j)
    bif = sb("m_bif", [128, 8])
    load(bif, bif[:, 0:4].unsqueeze(1), w["ml_b_i"][l:l + 1, :].partition_broadcast(128), sub=0)
    load(bif, bif[:, 4:8].unsqueeze(1), w["ml_b_f"][l:l + 1, :].partition_broadcast(128), sub=1)
    mng = sb("m_mng", [128, 256])
    load(mng, mng[:].unsqueeze(1), w["ml_norm_g"][l:l + 1, :].partition_broadcast(128))
    wout = sb("m_wout", [128, 2, D], BF16)
    load_cast(wout, wout[:], w["w_out"][l, 256:512, :].rearrange("(k p) n -> p k n", p=128))

    q = [sb("m_q0", [128, 2, S], BF16), sb("m_q1", [128, 2, S], BF16)]
    kT = sb("m_kT", [128, 2, S], BF16)
    ph1 = ExitStack()
    mqk = P.tile("m_mqk", ph1.enter_context(nc.sbuf_tensor("m_mqk_%d" % l, [128, 4, S + 3], BF16)))
    acc = P.tile("m_acc", ph1.enter_context(nc.sbuf_tensor("m_acc_%d" % l, [128, 2, 1024], F32)))
    P.op("vector", lambda e: e.memset(mqk[:, :, 0:3], 0.0), [], [mqk])
    for g in range(4):
        tok = slice(g * 512, (g + 1) * 512)
        for ch in range(4):
            pp = ps[(g * 4 + ch) % 2]
            for kc in range(8):
                mm(pp[:, :], win[:, kc, ch * 128:(ch + 1) * 128], xT[:, kc, tok], kc == 0, kc == 7, [win, xT], [pp])
            cp(mqk[:, ch, 3 + g * 512:3 + (g + 1) * 512], pp[:, :], [pp], [(mqk, ch)], eng="scalar" if ch % 2 else "vector")
    for i in range(2):
        P.op("vector", lambda e, i=i: e.memset(q[i][:], 0.0), [], [q[i]])
    for ch in range(4):
        for half in range(2):
            ai = half
            a = acc[:, ai, :]
            off = half * 1024
            ts(a, mqk[:, ch, off:off + 1024], cw[:, ch, 0:1], ALU.mult, [(mqk, ch), cw], [(acc, ai)])
            for j in range(1, 4):
                stt(a, mqk[:, ch, off + j:off + j + 1024], cw[:, ch, j:j + 1], a, ALU.mult, ALU.add,
                    [(mqk, ch), cw, (acc, ai)], [(acc, ai)])
            tokh = slice(off, off + 1024)
            if ch < 2:
                for hp in range(2):
                    rws = slice(hp * 64, (hp + 1) * 64)
                    act(q[hp][rws, ch, tokh], acc[rws, ai, :], AF.Silu, [(acc, ai)], [(q[hp], (ch, half))])
            else:
                act(kT[:, ch - 2, tokh], a, AF.Silu, [(acc, ai)], [(kT, (ch, half))])
    for hp in range(2):
        ts(q[hp][:], q[hp][:], 0.125, ALU.mult, [q[hp]], [q[hp]])
    P.soft_barrier()
    ph1.close()

    gates = sb("m_gates", [128, NT, 8])
    pg = ps[2]
    for t in range(NT):
        for kc in range(8):
            mm(pg[:, t * 8:(t + 1) * 8], xT[:, kc, t * 128:(t + 1) * 128], win[:, kc, 1024:1032], kc == 0, kc == 7,
               [win, xT], [pg])
    tt(gates[:], pg[:, 0:128].rearrange("p (t n) -> p t n", t=NT), bcast(bif[:, :], [128, NT, 8], 1), ALU.add,
       [pg, bif], [gates])
    Lf = sb("m_Lf", [128, NT, 4])
    act(Lf[:], gates[:, :, 4:8], AF.Exp, [gates], [Lf], scale=-1.0)
    ts(Lf[:], Lf[:], 1.0, ALU.add, [Lf], [Lf])
    act(Lf[:], Lf[:], AF.Ln, [Lf], [Lf])
    Lf2 = Lf[:].rearrange("p t n -> p (t n)")
    p3 = ps[3]
    mm(p3[:, 0:64], cmat[:, TRII, :], Lf2, True, True, [cmat, Lf], [p3])
    asb = sb("m_a", [128, NT, 4])
    tt(asb[:], p3[:, 0:64].rearrange("p (t n) -> p t n", t=NT), gates[:, :, 0:4], ALU.add, [p3, gates], [asb])
    cumL = sb("m_cumL", [128, 64])
    cp(cumL[:], p3[:, 0:64], [p3], [cumL])
    a2 = asb[:].rearrange("p t n -> p (t n)")
    p4 = ps[4]
    tr(p4[0:64, 0:128], a2, cmat[:, IDENT, :], [asb, cmat], [p4])
    Acol = sb("m_Acol", [64, 1])
    red(Acol[:, 0:1], p4[0:64, 0:128], ALU.max, [p4], [Acol])
    rows = sb("m_rows", [1, 5, 64])
    p5 = ps[5]
    mm(p5[0:1, 0:64], Acol[0:64, 0:1], cmat[0:64, IDENT, 0:64], True, True, [Acol, cmat], [p5])
    mm(p5[0:1, 64:128], cmat[:, ONES, 0:1], Lf2, True, True, [cmat, Lf], [p5])
    cp(rows[0:1, 0:2, :], p5[0:1, 0:128].rearrange("p (a n) -> p a n", a=2), [p5], [rows])
    P.op("vector", lambda e: e.memset(rows[0:1, 2, 0:4], 0.0), [rows], [rows])
    for cc in range(NT):
        sl = slice(cc * 4, cc * 4 + 4)
        tt(rows[0:1, 3, sl], rows[0:1, 2, sl], rows[0:1, 0, sl], ALU.max, [rows], [rows])
        if cc < NT - 1:
            tt(rows[0:1, 2, (cc + 1) * 4:(cc + 1) * 4 + 4], rows[0:1, 3, sl], rows[0:1, 1, sl], ALU.subtract, [rows], [rows])
    tt(rows[0:1, 4, :], rows[0:1, 2, :], rows[0:1, 3, :], ALU.subtract, [rows], [rows])
    act(rows[0:1, 4, :], rows[0:1, 4, :], AF.Exp, [rows], [rows])
    p6 = ps[6]
    mm(p6[:, 0:128], cmat[0:1, ONES, :], rows[0:1, 3:5, :].rearrange("p a n -> p (a n)"), True, True, [cmat, rows], [p6])
    bcs = sb("m_bcs", [128, 128])
    cp(bcs[:], p6[:, 0:128], [p6], [bcs])
    wtok = sb("m_wtok", [128, 64])
    tt(wtok[:], a2, bcs[:, 0:64], ALU.subtract, [asb, bcs], [wtok])
    act(wtok[:], wtok[:], AF.Exp, [wtok], [wtok])
    clamp = sb("m_clamp", [128, 64])
    tt(clamp[:], cumL[:], bcs[:, 0:64], ALU.subtract, [cumL, bcs], [clamp])
    act(clamp[:], clamp[:], AF.Exp, [clamp], [clamp])

    vext = sb("m_vext", [128, 2, 4, 65], BF16)
    P.op("vector", lambda e: e.memset(vext[:], 1.0), [], [vext])
    vw = sb("m_vw", [128, 2, 4, 65], BF16)
    gso = sb("m_gso", [128, 2, 256])
    ktm = sb("m_ktm", [128, 2, 256], BF16)
    WM = sb("m_WM", [128, 2, 4, 128])
    PT = sb("m_PT", [128, 2, 4, 128], BF16)
    Cn = sb("m_Cn", [128, 2, 65])
    P.op("vector", lambda e: e.memset(Cn[:], 0.0), [], [Cn])
    Cd = sb("m_Cd", [128, 2, 65])
    Cdbf = sb("m_Cdbf", [128, 2, 2, 65], BF16)
    nd = sb("m_nd", [128, 2, 4, 65])
    hsb = sb("m_h", [128, 2, 256])
    small = sb("m_small", [128, 2, 8])
    obf = sb("m_obf", [128, 2, 256], BF16)
    ohT = sb("m_ohT", [128, 2, 2, 128], BF16)
    sbf = dict(hn_st=sb("m_hn_st", [128, 2, 8]), hn_cen=sb("m_hn_cen", [128, 2, 256]),
               hn_sq=sb("m_hn_sq", [128, 2, 256]), gs=gso, obf=obf)
    PA, PB, PC, PD, PE_, PO0, PO1, PX = ps
    for t in range(int(os.environ.get("DBG_TILES", NT))):
        b = t % 2
        tok = slice(t * 128, (t + 1) * 128)
        g4 = slice(t * 4, t * 4 + 4)
        for kc in range(8):
            mm(PA[:, :], xT[:, kc, tok], win[:, kc, 512:1024], kc == 0, kc == 7, [win, xT], [PA])
        v4 = PA[:, 0:256].rearrange("p (h e) -> p h e", h=4)
        cp(vext[:, b, :, 0:64], v4, [PA], [(vext, b)], eng="scalar")
        tt(vw[:, b, :, 0:64], v4, bcast(wtok[:, g4], [128, 4, 64], 2), ALU.mult, [PA, wtok], [(vw, b)])
        cp(vw[:, b, :, 64], wtok[:, g4], [wtok], [(vw, b)])
        act(gso[:, b, :], PA[:, 256:512], AF.Exp, [PA], [(gso, b)], scale=-1.0)
        ts(gso[:, b, :], gso[:, b, :], 1.0, ALU.add, [(gso, b)], [(gso, b)])
        act(gso[:, b, :], gso[:, b, :], AF.Ln, [(gso, b)], [(gso, b)])
        act(gso[:, b, :], gso[:, b, :], AF.Exp, [(gso, b)], [(gso, b)], scale=-1.0)
        tt(gso[:, b, :], gso[:, b, :], mng[:, :], ALU.mult, [(gso, b), mng], [(gso, b)])
        pb = ps_bf[1]
        for hc in range(2):
            tr(pb[:, hc * 128:(hc + 1) * 128], kT[:, hc, tok], cmat_bf[:, IDENT, :], [kT, cmat_bf], [PB])
        cp(ktm[:, b, :], pb[:, 0:256], [PB], [(ktm, b)])
        for h in range(4):
            hp, hc = h % 2, h // 2
            mm(PC[:, h * 128:(h + 1) * 128], kT[:, hc, tok], q[hp][:, hc, tok], True, True, [kT, q[hp]], [PC])
        tt(WM[:, b, :, :], bcast(wtok[:, g4], [128, 4, 128], 2), bcast(cmat[:, TRII, :], [128, 4, 128], 1), ALU.mult,
           [wtok, cmat], [(WM, b)])
        tt(PT[:, b, :, :], PC[:, :].rearrange("p (h n) -> p h n", h=4), WM[:, b, :, :], ALU.mult, [PC, (WM, b)], [(PT, b)])
        for h in range(4):
            hc = h // 2
            mm(PD[:, h * 65:(h + 1) * 65], ktm[:, b, hc * 128:(hc + 1) * 128], vw[:, b, h, :], True, True,
               [(ktm, b), (vw, b)], [PD])
        for h in range(4):
            hp, hc = h % 2, h // 2
            rws = slice(hp * 64, (hp + 1) * 64)
            ts(Cd[rws, hc, :], Cn[rws, hc, :], bcs[rws, 64 + t * 4 + h:64 + t * 4 + h + 1], ALU.mult,
               [(Cn, h), bcs], [(Cd, h)])
        cp(Cdbf[:, b, :, :], Cd[:], [Cd], [(Cdbf, b)])
        for h in range(4):
            hp, hc = h % 2, h // 2
            mm(PE_[:, h * 65:(h + 1) * 65], PT[:, b, h, :], vext[:, b, h, :], True, False, [(PT, b), (vext, b)], [PE_])
            mm(PE_[:, h * 65:(h + 1) * 65], q[hp][:, hc, tok], Cdbf[:, b, hc, :], False, True, [q[hp], (Cdbf, b)], [PE_])
        for h in range(4):
            hp, hc = h % 2, h // 2
            rws = slice(hp * 64, (hp + 1) * 64)
            tt(Cn[rws, hc, :], Cd[rws, hc, :], PD[rws, h * 65:(h + 1) * 65], ALU.add, [(Cd, h), PD], [(Cn, h)])
        cp(nd[:, b, :, :], PE_[:, 0:260].rearrange("p (h e) -> p h e", h=4), [PE_], [(nd, b)], eng="scalar")
        stt(small[:, b, 0:4], nd[:, b, :, 64], -1.0, nd[:, b, :, 64], ALU.mult, ALU.max, [(nd, b)], [(small, b)])
        tt(small[:, b, 0:4], small[:, b, 0:4], clamp[:, g4], ALU.max, [(small, b), clamp], [(small, b)])
        P.op("vector", lambda e, b=b: e.reciprocal(small[:, b, 4:8], small[:, b, 0:4]), [(small, b)], [(small, b)])
        tt(hsb[:, b, :].rearrange("p (h e) -> p h e", h=4), nd[:, b, :, 0:64], bcast(small[:, b, 4:8], [128, 4, 64], 2),
           ALU.mult, [(nd, b), (small, b)], [(hsb, b)])
        head_norm_gate(c, sbf, "m", hsb[:, b, :], gso[:, b, :], obf[:, b, :], b, (hsb, b))
        pbx = ps_bf[7]
        for ch in range(2):
            tr(pbx[:, ch * 128:(ch + 1) * 128], obf[:, b, ch * 128:(ch + 1) * 128], cmat_bf[:, IDENT, :],
               [(obf, b), cmat_bf], [PX])
        cp(ohT[:, b, :, :], pbx[:, 0:256].rearrange("p (c n) -> p c n", c=2), [PX], [(ohT, b)])
        for hf in range(2):
            po = PO0 if hf == 0 else PO1
            for ch in range(2):
                mm(po[:, :], ohT[:, b, ch, :], wout[:, ch, hf * 512:(hf + 1) * 512], ch == 0, ch == 1, [(ohT, b), wout], [po])
            xs = x_tm[:, t, hf * 512:(hf + 1) * 512]
            if c["first_mixer"]:
                stt(xs, xs, ALPHA, po[:, :], ALU.mult, ALU.add, [(x_tm, t), po], [(x_tm, t)])
            else:
                tt(xs, xs, po[:, :], ALU.add, [(x_tm, t), po], [(x_tm, t)])
    P.soft_barrier()
    ph.close()


def mixer_mla(c, l):
    from contextlib import ExitStack
    import os
    nc, P, w = c["nc"], c["P"], c["w"]
    x_tm, xT, ps, cmat, cmat_bf, cs = c["x_tm"], c["xT"], c["ps"], c["cmat"], c["cmat_bf"], c["cs"]
    mm, act, tt, ts, stt, cp = c["mm"], c["act"], c["tt"], c["ts"], c["stt"], c["cp"]
    load, load_cast = c["load"], c["load_cast"]
    MMLA, ONES = 3, 4
    R_ = slice(64, 96)
    ph = ExitStack()

    def sb(name, shape, dt=F32, st=None):
        return P.tile(name, (st or ph).enter_context(nc.sbuf_tensor("%s_%d" % (name, l), list(shape), dt)))

    cqnT = sb("a_cqnT", [128, 2, S], BF16)
    ckvnT = sb("a_ckvnT", [128, S], BF16)
    krope = sb("a_krope", [96, S], BF16)
    v_all = sb("a_vall", [128, NT, 512], BF16)
    gq = sb("a_gq", [128, 2])
    gkv = sb("a_gkv", [128, 1])
    load(gq, gq[:], w["mla_q_norm_g"][l].rearrange("(rc p) -> p rc", p=128))
    load(gkv, gkv[:], w["mla_kv_norm_g"][l].rearrange("(o p) -> p o", o=1))

    p1 = ExitStack()
    win = sb("a_win", [128, 8, 416], BF16, p1)
    for kc in range(8):
        load_cast(win, win[:, kc, :], w["w_in"][l, kc * 128:(kc + 1) * 128, 2072:2488], sub=kc)
    wkr = sb("a_wkr", [128, 8, 2, 96], BF16, p1)
    P.op("vector", lambda e: e.memset(wkr[:], 0.0), [], [wkr])
    cp(wkr[:, :, 0, 64:96], win[:, :, 384:416], [win], [wkr])
    ts(wkr[:, :, 1, 64:80], win[:, :, 400:416], -1.0, ALU.mult, [win], [wkr])
    cp(wkr[:, :, 1, 80:96], win[:, :, 384:400], [win], [wkr])
    sq = sb("a_sq", [128, 2, 512], BF16, p1)
    rstd = sb("a_rstd", [128, 2, 512], F32, p1)
    tA = sb("a_tA", [96, 512], F32, p1)
    tB = sb("a_tB", [96, 512], F32, p1)
    for g in range(4):
        tok = slice(g * 512, (g + 1) * 512)
        for rc in range(2):
            for kc in range(8):
                mm(ps[rc][:, :], win[:, kc, rc * 128:(rc + 1) * 128], xT[:, kc, tok], kc == 0, kc == 7, [win, xT], [ps[rc]])
        for rc in range(2):
            act(sq[:, rc, :], ps[rc][:, :], AF.Square, [ps[rc]], [(sq, rc)])
        for rc in range(2):
            mm(ps[2][:, :], cmat_bf[:, ONES, :], sq[:, rc, :], rc == 0, rc == 1, [cmat_bf, (sq, rc)], [ps[2]])
        ts(rstd[:, 0, :], ps[2][:, :], 1.0 / 256, ALU.mult, [ps[2]], [(rstd, 0)], s2=LN_EPS, op1=ALU.add)
        act(rstd[:, 0, :], rstd[:, 0, :], AF.Ln, [(rstd, 0)], [(rstd, 0)])
        act(rstd[:, 0, :], rstd[:, 0, :], AF.Exp, [(rstd, 0)], [(rstd, 0)], scale=-0.5)
        for rc in range(2):
            stt(cqnT[:, rc, tok], ps[rc][:, :], gq[:, rc:rc + 1], rstd[:, 0, :], ALU.mult, ALU.mult,
                [ps[rc], gq, (rstd, 0)], [(cqnT, (rc, g))])
        for kc in range(8):
            mm(ps[3][:, :], win[:, kc, 256:384], xT[:, kc, tok], kc == 0, kc == 7, [win, xT], [ps[3]])
        act(sq[:, 0, :], ps[3][:, :], AF.Square, [ps[3]], [(sq, 0)])
        mm(ps[4][:, :], cmat_bf[:, ONES, :], sq[:, 0, :], True, True, [cmat_bf, (sq, 0)], [ps[4]])
        ts(rstd[:, 1, :], ps[4][:, :], 1.0 / 128, ALU.mult, [ps[4]], [(rstd, 1)], s2=LN_EPS, op1=ALU.add)
        act(rstd[:, 1, :], rstd[:, 1, :], AF.Ln, [(rstd, 1)], [(rstd, 1)])
        act(rstd[:, 1, :], rstd[:, 1, :], AF.Exp, [(rstd, 1)], [(rstd, 1)], scale=-0.5)
        stt(ckvnT[:, tok], ps[3][:, :], gkv[:, 0:1], rstd[:, 1, :], ALU.mult, ALU.mult, [ps[3], gkv, (rstd, 1)], [(ckvnT, g)])
        for r2 in range(2):
            for kc in range(8):
                mm(ps[5 + r2][0:96, :], wkr[:, kc, r2, :], xT[:, kc, tok], kc == 0, kc == 7, [wkr, xT], [ps[5 + r2]])
        tt(tA[R_, :], ps[5][R_, :], cs[R_, 0, tok], ALU.mult, [ps[5], cs], [tA])
        tt(tB[R_, :], ps[6][R_, :], cs[R_, 1, tok], ALU.mult, [ps[6], cs], [tB])
        tt(krope[R_, tok], tA[R_, :], tB[R_, :], ALU.add, [tA, tB], [(krope, g)])
    P.soft_barrier()
    p1.close()

    wuk = sb("a_wuk", [128, 8, 64], BF16)
    wuv = sb("a_wuv", [128, 8, 64], BF16)
    ukv = w["mla_w_ukv"][l].rearrange("p (h two d) -> p h two d", h=8, two=2)
    load_cast(wuk, wuk[:], ukv[:, :, 0, :])
    load_cast(wuv, wuv[:], ukv[:, :, 1, :])
    wuq = sb("a_wuq", [128, 2, 768], BF16)
    load_cast(wuq, wuq[:], w["mla_w_uq"][l].rearrange("(rc p) n -> p rc n", p=128))
    wuqr = sb("a_wuqr", [128, 2, 8, 96], BF16)
    P.op("vector", lambda e: e.memset(wuqr[:], 0.0), [], [wuqr])
    wq4 = wuq[:].rearrange("p r (h c) -> p r h c", h=8)
    ts(wuqr[:, :, :, 64:80], wq4[:, :, :, 80:96], -1.0, ALU.mult, [wuq], [wuqr])
    cp(wuqr[:, :, :, 80:96], wq4[:, :, :, 64:80], [wuq], [wuqr])
    wout = sb("a_wout", [128, 4, D], BF16)
    load_cast(wout, wout[:], w["w_out"][l, 512:1024, :].rearrange("(k p) n -> p k n", p=128))
    qTh = sb("a_qTh", [96, 2, 512], BF16)
    PTb = sb("a_PT", [128, 3, 512], BF16)
    mlaT = sb("a_mlaT", [128, 4, 512], BF16)
    rden = sb("a_rden", [128, 2, 512])
    tA2 = sb("a_tA2", [96, 2, 512])
    tB2 = sb("a_tB2", [96, 2, 512])
    KH = [("k", h) for h in range(8)]
    P.merge(xT)
    for g in range(4):
        tok = slice(g * 512, (g + 1) * 512)
        for h in range(8):
            pk = ps[h % 2]
            mm(pk[0:64, :], wuk[:, h, :], ckvnT[:, tok], True, True, [wuk, ckvnT], [pk])
            cp(xT[0:64, h, tok], pk[0:64, :], [pk], [(xT, KH[h])], eng="scalar" if h % 2 else "vector")
        cp(xT[R_, :, tok], bcast(krope[R_, tok], [32, 8, 512], 1), [krope], [(xT, k) for k in KH])
    for t in range(NT):
        pv = ps[2 + t % 2]
        mm(pv[:, :], ckvnT[:, t * 128:(t + 1) * 128], wuv[:].rearrange("p h d -> p (h d)"), True, True, [ckvnT, wuv], [pv])
        cp(v_all[:, t, :], pv[:, :], [pv], [(v_all, t)], eng="scalar" if t % 2 else "vector")
    scale = 96.0 ** -0.5
    NG = int(os.environ.get("DBG_GROUPS", 4))

    def qproj(g, h):
        tok = slice(g * 512, (g + 1) * 512)
        qb = h % 2
        pq, pr = ps[0], ps[1]
        for rc in range(2):
            mm(pq[0:96, :], wuq[:, rc, h * 96:(h + 1) * 96], cqnT[:, rc, tok], rc == 0, rc == 1, [wuq, cqnT], [pq])
        for rc in range(2):
            mm(pr[0:96, :], wuqr[:, rc, h, :], cqnT[:, rc, tok], rc == 0, rc == 1, [wuqr, cqnT], [pr])
        cp(qTh[0:64, qb, :], pq[0:64, :], [pq], [(qTh, qb)], eng="scalar")
        tt(tA2[R_, qb, :], pq[R_, :], cs[R_, 0, tok], ALU.mult, [pq, cs], [(tA2, qb)])
        tt(tB2[R_, qb, :], pr[R_, :], cs[R_, 1, tok], ALU.mult, [pr, cs], [(tB2, qb)])
        tt(qTh[R_, qb, :], tA2[R_, qb, :], tB2[R_, qb, :], ALU.add, [(tA2, qb), (tB2, qb)], [(qTh, qb)])

    def attend(g, h, nxt):
        nkt = 4 * g + 4
        hp, pair, qb = h % 2, h // 2, h % 2
        po, pd = ps[4 + h % 2], ps[6 + h % 2]

        def qk(kt):
            qlo = max(kt - 4 * g, 0) * 128
            N = 512 - qlo
            pst = ps[2 + kt % 2]
            mm(pst[:, 0:N], xT[0:96, h, kt * 128:(kt + 1) * 128], qTh[0:96, qb, qlo:512], True, True,
               [(xT, KH[h]), (qTh, qb)], [pst])

        qk(0)
        for kt in range(nkt):
            qlo = max(kt - 4 * g, 0) * 128
            N = 512 - qlo
            pst = ps[2 + kt % 2]
            pb = kt % 3
            if kt + 1 < nkt:
                qk(kt + 1)
            elif nxt is not None:
                qproj(*nxt)
            act(PTb[:, pb, 0:N], pst[:, 0:N], AF.Exp, [pst], [(PTb, pb)], scale=scale)
            if kt >= 4 * g:
                tt(PTb[:, pb, 0:128], PTb[:, pb, 0:128], cmat_bf[:, MMLA, :], ALU.mult, [(PTb, pb), cmat_bf], [(PTb, pb)])
            mm(po[:, qlo:512], v_all[:, kt, pair * 128:(pair + 1) * 128], PTb[:, pb, 0:N], kt == 0, kt == nkt - 1,
               [(v_all, kt), (PTb, pb)], [po])
            mm(pd[:, qlo:512], cmat_bf[:, ONES, :], PTb[:, pb, 0:N], kt == 0, kt == nkt - 1, [cmat_bf, (PTb, pb)], [pd])
        rws = slice(hp * 64, (hp + 1) * 64)
        act(rden[rws, qb, :], pd[rws, :], AF.Ln, [pd], [(rden, qb)])
        act(rden[rws, qb, :], rden[rws, qb, :], AF.Exp, [(rden, qb)], [(rden, qb)], scale=-1.0)
        tt(mlaT[rws, pair, :], po[rws, :], rden[rws, qb, :], ALU.mult, [po, (rden, qb)], [(mlaT, h)])

    work = [(g, h) for g in range(NG) for h in range(8)]
    qproj(*work[0])
    for i, (g, h) in enumerate(work):
        nxt = work[i + 1] if i + 1 < len(work) else None
        if nxt is not None and nxt[0] != g:
            attend(g, h, None)
        else:
            attend(g, h, nxt)
        if h != 7:
            continue
        if nxt is not None:
            qproj(*nxt)
        for tl in range(4):
            t = g * 4 + tl
            for hf in range(2):
                pp = ps[5 + 2 * hf]
                for pr_ in range(4):
                    mm(pp[:, :], mlaT[:, pr_, tl * 128:(tl + 1) * 128], wout[:, pr_, hf * 512:(hf + 1) * 512], pr_ == 0, pr_ == 3,
                       [mlaT, wout], [pp])
                xs = x_tm[:, t, hf * 512:(hf + 1) * 512]
                if c["first_mixer"]:
                    stt(xs, xs, ALPHA, pp[:, :], ALU.mult, ALU.add, [(x_tm, t), pp], [(x_tm, t)])
                else:
                    tt(xs, xs, pp[:, :], ALU.add, [(x_tm, t), pp], [(x_tm, t)])
    P.merge(xT)
    P.soft_barrier()
    ph.close()
```

```python
import numpy as np
import ml_dtypes
import concourse.bass as bass
import concourse.mybir as mybir
from concourse.bass_utils import run_bass_kernel_spmd

F32 = mybir.dt.float32
BF16 = mybir.dt.bfloat16
I32 = mybir.dt.int32
AF = mybir.ActivationFunctionType
ALU = mybir.AluOpType
AX = mybir.AxisListType

S = 2048
D = 1024
NT = 16
DEPTH = 2
ALPHA = (2 * DEPTH) ** 0.25
LN_EPS = 1e-5
IN_W = 2488


class Tile:
    def __init__(self, name, h):
        self.name = name
        self.h = h
        self.st = {}
        self.dsem = None
        self.dcount = 0
        self.psum = False

    def __getitem__(self, k):
        return self.h[k]


class Op:
    __slots__ = ("eng", "fn", "deps", "ddeps", "inc", "dma", "cost", "unit", "seg", "oi", "fin", "pos", "tbl")

    def __init__(self, eng, fn, deps, ddeps, dma=None, cost=0.2):
        self.eng = eng
        self.fn = fn
        self.deps = deps
        self.ddeps = ddeps
        self.inc = False
        self.dma = dma
        self.cost = cost
        self.unit = None
        self.seg = 0
        self.oi = 0
        self.fin = 0.0
        self.pos = 0
        self.tbl = None


class Prog:
    ENG = ("sync", "scalar", "vector", "gpsimd", "tensor")
    REORDER = ("scalar", "vector", "tensor")

    def __init__(self, nc):
        self.nc = nc
        self.ops = {e: [] for e in self.ENG}
        self.all = []
        self.dsems = {}
        self.dtotal = {}
        self.tiles = []
        self.seg = 0
        self.cur_unit = None
        self.nunits = 0
        self.dma_by = {}

    def tile(self, name, h):
        t = Tile(name, h)
        try:
            ml = self.nc.lookup_mloc(h)
            sbuf = "SB" in str(ml.type)
            t.lo, t.hi = int(ml.addr), int(ml.addr) + int(ml.dims[1])
        except Exception:
            sbuf = False
        t.sbuf = sbuf
        if sbuf:
            inh = []
            for o in self.tiles:
                if getattr(o, "sbuf", False) and o.lo < t.hi and t.lo < o.hi:
                    for st in o.st.values():
                        if st[0] is not None:
                            inh.append(st[0])
                        inh.extend(st[1])
            if inh:
                seen = set()
                uniq = []
                for x in inh:
                    k = id(x) if isinstance(x, Op) else x
                    if k not in seen:
                        seen.add(k)
                        uniq.append(x)
                t.st[None] = [None, uniq]
        self.tiles.append(t)
        return t

    def merge(self, t):
        allr = []
        for st in t.st.values():
            if st[0] is not None:
                allr.append(st[0])
            allr.extend(st[1])
        seen, uniq = set(), []
        for x in allr:
            k = id(x) if isinstance(x, Op) else x
            if k not in seen:
                seen.add(k)
                uniq.append(x)
        t.st = {None: [None, uniq]}

    def soft_barrier(self):
        return

    @staticmethod
    def _norm(lst):
        out = []
        for x in lst:
            if not isinstance(x, tuple):
                x = (x, None)
            if x[0].psum:
                x = (x[0], None)
            out.append(x)
        return out

    @staticmethod
    def _states(t, s):
        if s is None:
            return list(t.st.values())
        r = []
        if None in t.st:
            r.append(t.st[None])
        if s in t.st:
            r.append(t.st[s])
        return r

    def _collect(self, reads, writes):
        deps, ddeps = [], {}

        def add(ev):
            if ev is None:
                return
            if isinstance(ev, Op):
                deps.append(ev)
            else:
                k = ev
                ddeps[k] = self.dtotal[k]

        for (t, s) in reads:
            for st in self._states(t, s):
                add(st[0])
        for (t, s) in writes:
            for st in self._states(t, s):
                add(st[0])
                for r in st[1]:
                    add(r)
        return deps, ddeps

    def _update(self, ev, reads, writes):
        for (t, s) in reads:
            st = t.st.setdefault(s, [None, []])
            st[1].append(ev)
        for (t, s) in writes:
            if s is None:
                t.st = {None: [ev, []]}
            else:
                t.st[s] = [ev, []]

    def _add(self, o):
        o.seg = self.seg
        o.oi = len(self.all)
        self.all.append(o)
        self.ops[o.eng].append(o)

    def op(self, eng, fn, reads=(), writes=(), cost=0.2, start=None, stop=None, tbl=None):
        reads = self._norm(reads)
        writes = self._norm(writes)
        writes = writes + [r for r in reads if r[0].psum]
        deps, ddeps = self._collect(reads, writes)
        o = Op(eng, fn, deps, ddeps, cost=cost)
        o.tbl = tbl
        if eng == "tensor":
            if self.cur_unit is None or start is None or start:
                self.nunits += 1
                self.cur_unit = self.nunits
            o.unit = self.cur_unit
            if stop is None or stop:
                self.cur_unit = None
        self._add(o)
        self._update(o, reads, writes)

    def dma(self, eng, out, in_, reads=(), writes=(), semtile=None):
        assert eng in ("sync", "gpsimd")
        reads = self._norm(reads)
        writes = self._norm(writes)
        deps, ddeps = self._collect(reads, writes)
        if semtile.dsem is None:
            semtile.dsem = ("D", semtile.name)
            self.dsems[semtile.dsem] = None
        semtile.dcount = self.dtotal.get(semtile.dsem, 0) + 16
        self.dtotal[semtile.dsem] = semtile.dcount
        o = Op(eng, None, deps, ddeps, dma=(out, in_, semtile.dsem), cost=0.1)
        self.dma_by[(semtile.dsem, semtile.dcount)] = o
        self._add(o)
        self._update(semtile.dsem, reads, writes)

    def barrier(self):
        for e in self.ENG:
            o = Op(e, "bar", [], {}, cost=0.05)
            self._add(o)
        self.seg += 1
        for t in self.tiles:
            t.st = {}

    def finish(self, eng="sync"):
        self.barrier()

    def schedule(self):
        LAT = 0.25
        W = 64
        order = {e: [] for e in self.ENG}
        nseg = self.seg + 1
        byseg = [{e: [] for e in self.ENG} for _ in range(nseg)]
        for o in self.all:
            byseg[o.seg][o.eng].append(o)
        for sg in range(nseg):
            lists = byseg[sg]
            items = {e: [] for e in self.ENG}
            item_of = {}
            for e in self.ENG:
                cur = None
                for o in lists[e]:
                    if o.unit is not None and cur is not None and cur["unit"] == o.unit:
                        cur["ops"].append(o)
                    else:
                        cur = {"unit": o.unit, "ops": [o], "nrem": 0, "rt": 0.0, "succ": [], "eng": e, "done": False,
                               "bar": o.fn == "bar"}
                        items[e].append(cur)
                    item_of[id(o)] = cur
            dmafin = {}
            for e in self.ENG:
                for it in items[e]:
                    preds = set()
                    for o in it["ops"]:
                        dl = list(o.deps)
                        for k, v in o.ddeps.items():
                            dd = self.dma_by.get((k, v))
                            if dd is not None:
                                dl.append(dd)
                        for d in dl:
                            if d.seg != sg:
                                continue
                            p = item_of[id(d)]
                            if p is it:
                                continue
                            preds.add(id(p))
                            if id(p) not in it.setdefault("pset", {}):
                                it["pset"][id(p)] = p
                    for p in it.get("pset", {}).values():
                        p["succ"].append(it)
                    it["nrem"] = len(it.get("pset", {}))
            free = {e: 0.0 for e in self.ENG}
            heads = {e: 0 for e in self.ENG}
            cur_tbl = None
            npend = sum(len(v) for v in items.values())
            while npend > 0:
                progressed = False
                for e in self.ENG:
                    L = items[e]
                    while heads[e] < len(L) and L[heads[e]]["done"]:
                        heads[e] += 1
                    if heads[e] >= len(L):
                        continue
                    wmax = W if e in self.REORDER else 1
                    pick, pick_t = None, None
                    i, cnt = heads[e], 0
                    while i < len(L) and cnt < wmax:
                        it = L[i]
                        if not it["done"]:
                            cnt += 1
                            if it["bar"]:
                                if i == heads[e] and it["nrem"] == 0:
                                    pick, pick_t = it, free[e]
                                break
                            if it["nrem"] == 0:
                                rt = it["rt"]
                                for o in it["ops"]:
                                    for k in o.ddeps:
                                        rt = max(rt, dmafin.get(k, 0.0))
                                st_t = max(rt, free[e])
                                if e == "scalar":
                                    tb = it["ops"][0].tbl
                                    if tb is not None and tb != cur_tbl:
                                        st_t += 1.4
                                if pick is None or st_t < pick_t - 1e-9:
                                    pick, pick_t = it, st_t
                                if st_t <= free[e] + 1e-9:
                                    break
                        i += 1
                    if pick is None:
                        continue
                    tcur = pick_t
                    if e == "scalar" and pick["ops"][0].tbl is not None:
                        cur_tbl = pick["ops"][0].tbl
                    for o in pick["ops"]:
                        tcur += o.cost
                        o.fin = tcur
                        order[e].append(o)
                        if o.dma is not None:
                            dmafin[o.dma[2]] = max(dmafin.get(o.dma[2], 0.0), tcur + 2.5)
                    pick["done"] = True
                    npend -= 1
                    free[e] = tcur
                    for sc in pick["succ"]:
                        sc["nrem"] -= 1
                        lat = 0.0 if (sc["eng"] == "tensor" and e == "tensor") else LAT
                        if sc["rt"] < tcur + lat:
                            sc["rt"] = tcur + lat
                    progressed = True
                if not progressed:
                    raise RuntimeError("scheduler deadlock in segment %d" % sg)
        for e in self.ENG:
            assert len(order[e]) == len(self.ops[e]), (e, len(order[e]), len(self.ops[e]))
            for i, o in enumerate(order[e]):
                o.pos = i
        self.order = order

    def emit(self, stack):
        nc = self.nc
        self.schedule()
        order = self.order
        for o in self.all:
            for d in o.deps:
                if d.eng == "tensor" and o.eng == "tensor":
                    continue
                d.inc = True
        barlast = {}
        for e in self.ENG:
            last = None
            for o in order[e]:
                if o.fn == "bar":
                    barlast[(e, o.seg)] = last
                elif o.dma is None:
                    last = o
        for v in barlast.values():
            if v is not None:
                v.inc = True
        esem = {e: stack.enter_context(nc.semaphore("es_" + e)) for e in self.ENG}
        for k in self.dsems:
            self.dsems[k] = stack.enter_context(nc.semaphore("ds_" + k[1]))
        cnt = {}
        for e in self.ENG:
            c_ = 0
            for o in order[e]:
                if o.inc and o.dma is None:
                    c_ += 1
                cnt[id(o)] = c_
        dma_at_bar = {}
        run_tot = {}
        segs = {}
        for o in self.all:
            if o.dma is not None:
                segs.setdefault(o.seg, {})
        tot = {}
        for sg in range(self.seg + 1):
            for o in self.all:
                pass
        cum = {}
        per_seg_tot = []
        cur = {}
        last_seg = 0
        for o in self.all:
            while last_seg < o.seg:
                per_seg_tot.append(dict(cur))
                last_seg += 1
            if o.dma is not None:
                cur[o.dma[2]] = cur.get(o.dma[2], 0) + 16
        while len(per_seg_tot) <= self.seg:
            per_seg_tot.append(dict(cur))
        prog = self

        def run(ename, eng):
            waited = {}

            def wait(key, sem, val):
                if val <= 0 or waited.get(key, 0) >= val:
                    return
                waited[key] = val
                eng.wait_ge(sem, val)

            for o in order[ename]:
                if o.fn == "bar":
                    for e2 in prog.ENG:
                        lo = barlast.get((e2, o.seg))
                        if lo is not None:
                            wait(e2, esem[e2], cnt[id(lo)])
                    for k, v in per_seg_tot[o.seg].items():
                        wait(k, prog.dsems[k], v)
                    continue
                need = {}
                for d in o.deps:
                    if d.eng == "tensor" and ename == "tensor":
                        continue
                    v = cnt[id(d)]
                    if need.get(d.eng, 0) < v:
                        need[d.eng] = v
                for k, v in need.items():
                    wait(k, esem[k], v)
                for k, v in o.ddeps.items():
                    wait(k, prog.dsems[k], v)
                if o.dma is not None:
                    out, in_, dk = o.dma
                    eng.dma_start(out=out, in_=in_).then_inc(prog.dsems[dk], 16)
                    continue
                ins = o.fn(eng)
                if o.inc:
                    ins.then_inc(esem[ename], 1)

        stack.enter_context(nc.allow_non_contiguous_dma("tiny strided parameter loads"))
        block = stack.enter_context(nc.Block())

        @block.sync
        def _(e):
            run("sync", e)

        @block.scalar
        def _(e):
            run("scalar", e)

        @block.vector
        def _(e):
            run("vector", e)

        @block.gpsimd
        def _(e):
            run("gpsimd", e)

        @block.tensor
        def _(e):
            run("tensor", e)


def bcast(ap, shape, axis):
    return ap.unsqueeze(axis).to_broadcast(list(shape))


class K:
    def __init__(self, layers=(0, 1), phases="ABC", dbg=()):
        self.layers = layers
        self.phases = phases
        self.dbg = dbg


def build(layers=(0, 1), phases="ABC", dbg=(), sub="GML"):
    from contextlib import ExitStack
    nc = bass.Bass("TRN2", target_bir_lowering=False)
    P = Prog(nc)
    stack = ExitStack()

    def din(name, shape, dt=F32):
        return nc.dram_tensor(name, list(shape), dt, kind="ExternalInput").ap()

    x_d = din("x", [S, D])
    mem_d = din("mem", [256, D])
    pos_d = din("positions", [1, S], I32)
    w = {}
    for name, shape in [
        ("w_in", [2, D, IN_W]), ("w_out", [2, D, D]), ("gla_w_a2", [2, 16, 256]), ("gla_b_a", [2, 256]),
        ("gla_norm_g", [2, 256]), ("ml_conv_w", [2, 4, 512]), ("ml_b_i", [2, 4]), ("ml_b_f", [2, 4]),
        ("ml_norm_g", [2, 256]), ("mla_q_norm_g", [2, 256]), ("mla_w_uq", [2, 256, 768]),
        ("mla_kv_norm_g", [2, 128]), ("mla_w_ukv", [2, 128, 1024]), ("xa_w_q", [2, D, D]),
        ("xa_w_kv", [2, D, 2 * D]), ("xa_w_o", [2, D, D]), ("moe_w_group", [2, D, 4]), ("moe_b_group", [2, 4]),
        ("moe_w_router", [2, D, 32]), ("moe_b_router", [2, 32]), ("moe_w_gate", [2, 32, D, 256]),
        ("moe_w_up", [2, 32, D, 256]), ("moe_w_down", [2, 32, 256, D]),
        ("ln1_g", [2, D]), ("ln1_b", [2, D]), ("ln2_g", [2, D]), ("ln2_b", [2, D]), ("ln3_g", [2, D]), ("ln3_b", [2, D]),
    ]:
        w[name] = din(name, shape)
    cmat_d = din("cmat", [128, 5, 128])
    sel_d = din("sel", [32, 32, 128])
    ropeinv_d = din("ropeinv", [96, 1])
    out_d = nc.dram_tensor("out", [S, D], F32, kind="ExternalOutput").ap()
    dbg_d = {}
    for name, shape in dbg:
        dbg_d[name] = nc.dram_tensor(name, list(shape), F32, kind="ExternalOutput").ap()

    def sb(name, shape, dt=F32):
        return P.tile(name, stack.enter_context(nc.sbuf_tensor(name, list(shape), dt)))

    x_tm = sb("x_tm", [128, NT, D])
    xT = sb("xT", [128, 8, S], BF16)
    cmat = sb("cmat_sb", [128, 5, 128])
    cmat_bf = sb("cmat_bf", [128, 5, 128], BF16)
    lnp = sb("lnp", [128, 2, D])
    ps = [P.tile("ps%d" % i, stack.enter_context(nc.psum_tensor("ps%d" % i, [128, 512], F32))) for i in range(8)]
    for p_ in ps:
        p_.psum = True
    IDENT, TRII, TRIS, MMLA, ONES = range(5)

    def fsz(ap):
        try:
            return int(ap.free_size())
        except Exception:
            return 256

    def mm(out, lhsT, rhs, start, stop, reads, writes):
        n = fsz(rhs)
        cst = max(64, n) / 2400.0 * (4.0 if rhs.dtype == F32 else 1.0) + 0.012
        P.op("tensor", lambda e: e.matmul(out, lhsT, rhs, start=start, stop=stop), reads, writes, cost=cst,
             start=start, stop=stop)

    def tr(out, in_, ident, reads, writes):
        P.op("tensor", lambda e: e.transpose(out, in_, ident), reads, writes, cost=0.08)

    def act(out, in_, func, reads, writes, bias=None, scale=None, accum_out=None):
        kw = {}
        if bias is not None:
            kw["bias"] = bias
        if scale is not None:
            kw["scale"] = scale
        if accum_out is not None:
            kw["accum_out"] = accum_out
        tb = {AF.Silu: "s", AF.Sigmoid: "s", AF.Sqrt: "q", AF.Sin: "n", AF.Copy: None, AF.Identity: None}.get(func, "e")
        P.op("scalar", lambda e: e.activation(out, in_, func, **kw), reads, writes, cost=0.25 + fsz(out) * 0.00085, tbl=tb)

    def vcost(out, f=1.0):
        return 0.12 + fsz(out) * 0.00105 * f

    def tt(out, a, b, op, reads, writes, eng="vector"):
        P.op(eng, lambda e: e.tensor_tensor(out, a, b, op), reads, writes, cost=vcost(out))

    def ts(out, a, s1, op0, reads, writes, s2=None, op1=None, eng="vector"):
        if op1 is None:
            P.op(eng, lambda e: e.tensor_scalar(out, a, s1, None, op0), reads, writes, cost=vcost(out, 0.6))
        else:
            P.op(eng, lambda e: e.tensor_scalar(out, a, s1, s2, op0, op1), reads, writes, cost=vcost(out, 0.6))

    def stt(out, a, s, b, op0, op1, reads, writes):
        P.op("vector", lambda e: e.scalar_tensor_tensor(out, a, s, b, op0, op1), reads, writes, cost=vcost(out))

    def cp(out, in_, reads, writes, eng="vector"):
        if eng == "scalar":
            P.op("scalar", lambda e: e.copy(out, in_), reads, writes, cost=0.25 + fsz(out) * 0.00085)
        else:
            P.op(eng, lambda e: e.tensor_copy(out, in_), reads, writes, cost=vcost(out, 0.6))

    def red(out, in_, op, reads, writes, axis=AX.X):
        P.op("vector", lambda e: e.tensor_reduce(out, in_, axis, op), reads, writes, cost=vcost(in_))

    def load_cast(dst_tile, dst_ap, src_ap, sub=None):
        P.dma("gpsimd", dst_ap, src_ap, writes=[(dst_tile, sub)], semtile=dst_tile)

    def load(dst_tile, dst_ap, src_ap, sub=None, eng="sync"):
        P.dma(eng, dst_ap, src_ap, writes=[(dst_tile, sub)], semtile=dst_tile)

    load(cmat, cmat[:], cmat_d)
    cp(cmat_bf[:], cmat[:], [cmat], [cmat_bf])
    for t in range(NT):
        load(x_tm, x_tm[:, t, :], x_d[t * 128:(t + 1) * 128, :], sub=t, eng="sync")

    memT = sb("memT", [128, 8, 256], BF16)
    if "B" in phases:
        from contextlib import ExitStack as _ES
        pre = _ES()
        mem_f = P.tile("mem_f", pre.enter_context(nc.sbuf_tensor("mem_f", [128, 2, D], F32)))
        mem_b = P.tile("mem_b", pre.enter_context(nc.sbuf_tensor("mem_b", [128, 2, D], BF16)))
        load(mem_f, mem_f[:], mem_d.rearrange("(t p) d -> p t d", p=128))
        cp(mem_b[:], mem_f[:], [mem_f], [mem_b])
        pbm = ps[7].h.bitcast(BF16)
        for mt in range(2):
            for c8 in range(8):
                tr(pbm[:, c8 * 128:(c8 + 1) * 128], mem_b[:, mt, c8 * 128:(c8 + 1) * 128], cmat_bf[:, 0, :],
                   [mem_b, cmat_bf], [(ps[7], c8)])
            cp(memT[:, :, mt * 128:(mt + 1) * 128], pbm[:, :].rearrange("p (c n) -> p c n", c=8), [ps[7]], [(memT, mt)])
        P.soft_barrier()
        pre.close()

    cs = None
    if "A" in phases and "L" in sub:
        import math
        from contextlib import ExitStack as _ES2
        cs = sb("rope_cs", [96, 2, S], BF16)
        pre2 = _ES2()

        def tmp(name, dt=F32):
            return P.tile(name, pre2.enter_context(nc.sbuf_tensor(name, [96, S], dt)))
        posi, ang, rr, kf, ki, mk = tmp("rp_posi", I32), tmp("rp_ang"), tmp("rp_r"), tmp("rp_kf"), tmp("rp_ki", I32), tmp("rp_m")
        rinv = P.tile("rp_inv", pre2.enter_context(nc.sbuf_tensor("rp_inv", [96, 1], F32)))
        R_ = slice(64, 96)
        load(rinv, rinv[:], ropeinv_d)
        P.dma("sync", posi[R_, :].unsqueeze(1), pos_d[0:1, :].partition_broadcast(32), writes=[posi], semtile=posi)
        cp(ang[R_, :], posi[R_, :], [posi], [ang])
        ts(ang[R_, :], ang[R_, :], rinv[R_, 0:1], ALU.mult, [ang, rinv], [ang])
        TWO_PI = 2.0 * math.pi
        C1 = 6.28125
        C2 = TWO_PI - C1
        for which, shift in ((1, 0.0), (0, math.pi / 2)):
            ts(rr[R_, :], ang[R_, :], shift, ALU.add, [ang], [rr])
            ts(kf[R_, :], rr[R_, :], 1.0 / TWO_PI, ALU.mult, [rr], [kf])
            cp(ki[R_, :], kf[R_, :], [kf], [ki])
            cp(kf[R_, :], ki[R_, :], [ki], [kf])
            stt(rr[R_, :], kf[R_, :], -C1, rr[R_, :], ALU.mult, ALU.add, [kf, rr], [rr])
            stt(rr[R_, :], kf[R_, :], -C2, rr[R_, :], ALU.mult, ALU.add, [kf, rr], [rr])
            ts(mk[R_, :], rr[R_, :], math.pi, ALU.is_gt, [rr], [mk])
            stt(rr[R_, :], mk[R_, :], -TWO_PI, rr[R_, :], ALU.mult, ALU.add, [mk, rr], [rr])
            ts(mk[R_, :], rr[R_, :], -math.pi, ALU.is_lt, [rr], [mk])
            stt(rr[R_, :], mk[R_, :], TWO_PI, rr[R_, :], ALU.mult, ALU.add, [mk, rr], [rr])
            ts(rr[R_, :], rr[R_, :], 3.141592, ALU.min, [rr], [rr], s2=-3.141592, op1=ALU.max)
            act(cs[R_, which, :], rr[R_, :], AF.Sin, [rr], [(cs, which)])
        P.soft_barrier()
        pre2.close()

    lnw = sb("ln_work", [128, 16])
    xbf = sb("ln_xbf", [128, 2, D], BF16)
    ps_bf = [ps[i].h.bitcast(BF16) for i in range(8)]

    def load_ln(gname, bname, l):
        load(lnp, lnp[:, 0, :].unsqueeze(1), w[gname][l:l + 1, :].partition_broadcast(128), sub=0)
        load(lnp, lnp[:, 1, :].unsqueeze(1), w[bname][l:l + 1, :].partition_broadcast(128), sub=1)

    def layer_norm_tile(t, pbank):
        xt = x_tm[:, t, :]
        st = lnw[:, 0:12].rearrange("p (a b) -> p a b", a=2)
        for hh in range(2):
            P.op("vector", lambda e, hh=hh: e.bn_stats(st[:, hh, :], x_tm[:, t, hh * 512:(hh + 1) * 512]),
                 [(x_tm, t)], [(lnw, "st%d" % hh)])
        P.op("vector", lambda e: e.bn_aggr(lnw[:, 12:14], lnw[:, 0:12]), [(lnw, "st0"), (lnw, "st1")], [(lnw, "mv")])
        ts(lnw[:, 14:15], lnw[:, 13:14], LN_EPS, ALU.add, [(lnw, "mv")], [(lnw, "sd")])
        act(lnw[:, 14:15], lnw[:, 14:15], AF.Ln, [(lnw, "sd")], [(lnw, "sd")])
        act(lnw[:, 15:16], lnw[:, 14:15], AF.Exp, [(lnw, "sd")], [(lnw, "rs")], scale=-0.5)
        ts(xt, xt, lnw[:, 12:13], ALU.subtract, [(x_tm, t), (lnw, "mv"), (lnw, "rs")], [(x_tm, t)],
           s2=lnw[:, 15:16], op1=ALU.mult)
        tt(xt, xt, lnp[:, 0, :], ALU.mult, [(x_tm, t), (lnp, 0)], [(x_tm, t)])
        tt(xt, xt, lnp[:, 1, :], ALU.add, [(x_tm, t), (lnp, 1)], [(x_tm, t)])
        refresh_xT(t, pbank)

    def refresh_xT(t, pbank):
        xt = x_tm[:, t, :]
        b = t % 2
        cp(xbf[:, b, :], xt, [(x_tm, t)], [(xbf, b)], eng="scalar")
        pb = ps_bf[pbank]
        for c in range(8):
            tr(pb[:, c * 128:(c + 1) * 128], xbf[:, b, c * 128:(c + 1) * 128], cmat_bf[:, IDENT, :],
               [(xbf, b), cmat_bf], [(ps[pbank], c)])
        cp(xT[:, :, t * 128:(t + 1) * 128], pb[:, :].rearrange("p (c n) -> p c n", c=8),
           [ps[pbank]], [(xT, t)], eng="scalar" if t % 2 else "vector")

    def store_out():
        for t in range(NT):
            P.dma("sync", out_d[t * 128:(t + 1) * 128, :], x_tm[:, t, :],
                  reads=[(x_tm, t)], semtile=x_tm)

    ctx = dict(nc=nc, P=P, stack=stack, w=w, x_tm=x_tm, xT=xT, cmat=cmat, cmat_bf=cmat_bf, lnp=lnp, ps=ps,
               ps_bf=ps_bf, sb=sb, mm=mm, tr=tr, act=act, tt=tt, ts=ts, stt=stt, cp=cp, red=red,
               load=load, load_cast=load_cast, load_ln=load_ln, layer_norm_tile=layer_norm_tile,
               sel_d=sel_d, ropeinv_d=ropeinv_d, memT=memT, sub=sub, cs=cs, mem_d=mem_d, pos_d=pos_d, dbg_d=dbg_d)

    first = True
    for l in layers:
        if first:
            for t in range(NT):
                refresh_xT(t, 5 + t % 3)
        if "A" in phases:
            phase_A(ctx, l)
        if "B" in phases:
            phase_B(ctx, l)
        if "C" in phases:
            phase_C(ctx, l)
        first = False
    store_out()
    P.finish("sync")
    P.emit(stack)
    stack.close()
    return nc


def phase_C(c, l):
    from contextlib import ExitStack
    nc, P, w = c["nc"], c["P"], c["w"]
    x_tm, xT, ps, cmat, cmat_bf = c["x_tm"], c["xT"], c["ps"], c["cmat"], c["cmat_bf"]
    mm, tr, act, tt, ts, stt, cp, red = c["mm"], c["tr"], c["act"], c["tt"], c["ts"], c["stt"], c["cp"], c["red"]
    load, load_cast = c["load"], c["load_cast"]
    IDENT = 0
    ph = ExitStack()

    def sb(name, shape, dt=F32):
        return P.tile(name, ph.enter_context(nc.sbuf_tensor("%s_%d" % (name, l), list(shape), dt)))

    c["load_ln"]("ln3_g", "ln3_b", l)
    gateT = sb("c_gateT", [32, S], BF16)
    sel = sb("c_sel", [32, 32, 128], BF16)
    load_cast(sel, sel[:], c["sel_d"])
    ph_r = ExitStack()
    _sb_outer = sb

    def sb(name, shape, dt=F32):
        return P.tile(name, ph_r.enter_context(nc.sbuf_tensor("%s_%d" % (name, l), list(shape), dt)))
    wr = sb("c_wr", [128, 8, 36], BF16)
    load_cast(wr, wr[:, :, 0:4], w["moe_w_group"][l].rearrange("(kc p) n -> p kc n", p=128), sub="g")
    load_cast(wr, wr[:, :, 4:36], w["moe_w_router"][l].rearrange("(kc p) n -> p kc n", p=128), sub="r")
    rb = sb("c_rb", [128, 36])
    load(rb, rb[:, 0:4].unsqueeze(1), w["moe_b_group"][l:l + 1, :].partition_broadcast(128), sub="g")
    load(rb, rb[:, 4:36].unsqueeze(1), w["moe_b_router"][l:l + 1, :].partition_broadcast(128), sub="r")
    lg = sb("c_lg", [128, NT, 36])
    for half in range(2):
        pr = ps[half]
        for tl in range(8):
            t = half * 8 + tl
            for kc in range(8):
                mm(pr[:, tl * 36:(tl + 1) * 36], xT[:, kc, t * 128:(t + 1) * 128], wr[:, kc, :], kc == 0, kc == 7,
                   [(xT, t), wr], [(pr, tl)])
        tt(lg[:, half * 8:(half + 1) * 8, :], pr[:, 0:288].rearrange("p (t n) -> p t n", t=8),
           bcast(rb[:, :], [128, 8, 36], 1), ALU.add, [pr, rb], [(lg, half)])
    r1 = sb("c_r1", [128, NT, 64])
    lgg = lg[:, :, 0:4]
    lge = lg[:, :, 4:36].rearrange("p t (g e) -> p t g e", g=4)
    gmax, gsum, ohg, eg = r1[:, :, 0], r1[:, :, 1], r1[:, :, 4:8], r1[:, :, 8:12]
    red(gmax, lgg, ALU.max, [lg], [(r1, "gmax")])
    tt(eg, lgg, bcast(gmax, [128, NT, 4], 2), ALU.subtract, [lg, (r1, "gmax")], [(r1, "eg")])
    tt(ohg, lgg, bcast(gmax, [128, NT, 4], 2), ALU.is_equal, [lg, (r1, "gmax")], [(r1, "ohg")])
    act(eg, eg, AF.Exp, [(r1, "eg")], [(r1, "eg")])
    red(gsum, eg, ALU.add, [(r1, "eg")], [(r1, "gsum")])
    gp = r1[:, :, 2]
    P.op("vector", lambda e: e.reciprocal(gp, gsum), [(r1, "gsum")], [(r1, "gp")])
    tmp = sb("c_tmp", [128, NT, 4, 8])
    tt(tmp[:], lge, bcast(ohg, [128, NT, 4, 8], 3), ALU.mult, [lg, (r1, "ohg")], [tmp])
    esel = r1[:, :, 16:24]
    red(esel, tmp[:].rearrange("p t g e -> p t e g"), ALU.add, [tmp], [(r1, "esel")])
    m1, m2, dd = r1[:, :, 3], r1[:, :, 12], r1[:, :, 13]
    mk1, mk2, e2 = r1[:, :, 24:32], r1[:, :, 32:40], r1[:, :, 40:48]
    red(m1, esel, ALU.max, [(r1, "esel")], [(r1, "m1")])
    tt(mk1, esel, bcast(m1, [128, NT, 8], 2), ALU.is_equal, [(r1, "esel"), (r1, "m1")], [(r1, "mk1")])
    stt(e2, mk1, -1e30, esel, ALU.mult, ALU.add, [(r1, "mk1"), (r1, "esel")], [(r1, "e2")])
    red(m2, e2, ALU.max, [(r1, "e2")], [(r1, "m2")])
    tt(mk2, e2, bcast(m2, [128, NT, 8], 2), ALU.is_equal, [(r1, "e2"), (r1, "m2")], [(r1, "mk2")])
    tt(dd, m2, m1, ALU.subtract, [(r1, "m1"), (r1, "m2")], [(r1, "dd")])
    act(dd, dd, AF.Exp, [(r1, "dd")], [(r1, "dd")])
    w1, w2 = r1[:, :, 14], r1[:, :, 15]
    ts(w1, dd, 1.0, ALU.add, [(r1, "dd")], [(r1, "w1")])
    P.op("vector", lambda e: e.reciprocal(w1, w1), [(r1, "w1")], [(r1, "w1")])
    tt(w1, w1, gp, ALU.mult, [(r1, "w1"), (r1, "gp")], [(r1, "w1")])
    tt(w2, w1, dd, ALU.mult, [(r1, "w1"), (r1, "dd")], [(r1, "w2")])
    comb = r1[:, :, 48:56]
    tt(comb, mk1, bcast(w1, [128, NT, 8], 2), ALU.mult, [(r1, "mk1"), (r1, "w1")], [(r1, "comb")])
    tt(mk2, mk2, bcast(w2, [128, NT, 8], 2), ALU.mult, [(r1, "mk2"), (r1, "w2")], [(r1, "mk2")])
    tt(comb, comb, mk2, ALU.add, [(r1, "comb"), (r1, "mk2")], [(r1, "comb")])
    gate = sb("c_gate", [128, NT, 4, 8])
    tt(gate[:], bcast(ohg, [128, NT, 4, 8], 3), bcast(comb, [128, NT, 4, 8], 2), ALU.mult,
       [(r1, "ohg"), (r1, "comb")], [gate])
    for g in range(4):
        pg = ps[2 + g % 2]
        for tl in range(4):
            t = g * 4 + tl
            tr(pg[0:32, tl * 128:(tl + 1) * 128], gate[:, t, :, :].rearrange("p g e -> p (g e)"), cmat[:, IDENT, :],
               [gate, cmat], [(pg, tl)])
        cp(gateT[:, g * 512:(g + 1) * 512], pg[0:32, :], [pg], [(gateT, g)], eng="scalar")
    if "c_gate" in c["dbg_d"]:
        P.dma("sync", c["dbg_d"]["c_gate"].rearrange("(t p) n -> p t n", p=128),
              gate[:].rearrange("p t g e -> p t (g e)"), reads=[gate], semtile=gate)

    P.soft_barrier()
    ph_r.close()
    sb = _sb_outer
    NSLOT = 4
    wg = sb("c_wg", [128, NSLOT, 8, 256], BF16)
    wu = sb("c_wu", [128, NSLOT, 8, 256], BF16)
    wd = sb("c_wd", [128, NSLOT, 2, D], BF16)
    wsem = [sb("c_wsem%d" % i, [1, 1]) for i in range(NSLOT)]
    hT = sb("c_hT", [128, 2, 2, S], BF16)
    sg = sb("c_sg", [128, 2, 512], BF16)
    gb = sb("c_gb", [128, 2, 512], BF16)

    def load_expert(e):
        s = e % NSLOT
        P.dma("gpsimd", wg[:, s, :, :], w["moe_w_gate"][l, e].rearrange("(kc p) n -> p kc n", p=128),
              writes=[(wg, s)], semtile=wsem[s])
        P.dma("gpsimd", wu[:, s, :, :], w["moe_w_up"][l, e].rearrange("(kc p) n -> p kc n", p=128),
              writes=[(wu, s)], semtile=wsem[s])
        P.dma("gpsimd", wd[:, s, :, :], w["moe_w_down"][l, e].rearrange("(kc p) n -> p kc n", p=128),
              writes=[(wd, s)], semtile=wsem[s])

    for e in range(2):
        load_expert(e)
    unit = 0
    for blk in range(16):
        for ei in range(2):
            e = blk * 2 + ei
            s = e % NSLOT
            if e + 2 < 32:
                load_expert(e + 2)
            for g in range(4):
                tok = slice(g * 512, (g + 1) * 512)
                pgb = ps[4]
                mm(pgb[:, :], sel[:, e, :], gateT[:, tok], True, True, [sel, (gateT, g)], [pgb])
                ub = (e * 4 + g) % 2
                cp(gb[:, ub, :], pgb[:, :], [pgb], [(gb, ub)], eng="scalar")
                for fc in range(2):
                    pgt, put = ps[(unit % 2) * 2], ps[(unit % 2) * 2 + 1]
                    for kc in range(8):
                        mm(pgt[:, :], wg[:, s, kc, fc * 128:(fc + 1) * 128], xT[:, kc, tok], kc == 0, kc == 7,
                           [(wg, s), xT], [pgt])
                    for kc in range(8):
                        mm(put[:, :], wu[:, s, kc, fc * 128:(fc + 1) * 128], xT[:, kc, tok], kc == 0, kc == 7,
                           [(wu, s), xT], [put])
                    u2 = unit % 2
                    act(sg[:, u2, :], pgt[:, :], AF.Silu, [pgt], [(sg, u2)])
                    tt(sg[:, u2, :], put[:, :], sg[:, u2, :], ALU.mult, [put, (sg, u2)], [(sg, u2)])
                    tt(hT[:, ei, fc, tok], sg[:, u2, :], gb[:, ub, :], ALU.mult, [(sg, u2), (gb, ub)],
                       [(hT, (ei, g))])
                    unit += 1
        for t in range(NT):
            g = t // 4
            for hf in range(2):
                po = ps[5 + (t * 2 + hf) % 3]
                k = 0
                for ei in range(2):
                    s = (blk * 2 + ei) % NSLOT
                    for fc in range(2):
                        mm(po[:, :], hT[:, ei, fc, t * 128:(t + 1) * 128], wd[:, s, fc, hf * 512:(hf + 1) * 512],
                           k == 0, k == 3, [(hT, (ei, g)), (wd, s)], [po])
                        k += 1
                xs = x_tm[:, t, hf * 512:(hf + 1) * 512]
                if blk == 0:
                    stt(xs, xs, ALPHA, po[:, :], ALU.mult, ALU.add, [(x_tm, t), po], [(x_tm, t)])
                else:
                    tt(xs, xs, po[:, :], ALU.add, [(x_tm, t), po], [(x_tm, t)])
    for t in range(NT):
        c["layer_norm_tile"](t, 5 + t % 3)
    P.soft_barrier()
    ph.close()


def host_consts():
    cm = np.zeros((128, 5, 128), np.float32)
    i = np.arange(128)
    cm[:, 0, :] = np.eye(128, dtype=np.float32)
    cm[:, 1, :] = (i[:, None] <= i[None, :]).astype(np.float32)
    cm[:, 2, :] = (i[:, None] > i[None, :]).astype(np.float32)
    cm[:, 3, :] = ((i[:, None] // 64) <= (i[None, :] // 64)).astype(np.float32)
    cm[:, 4, :] = 1.0
    sel = np.zeros((32, 32, 128), np.float32)
    for e in range(32):
        sel[e, e, :] = 1.0
    inv = (10000.0 ** (-np.arange(16, dtype=np.float32) / 16)).astype(np.float32)
    ri = np.zeros((96, 1), np.float32)
    ri[64:80, 0] = inv
    ri[80:96, 0] = inv
    return {"cmat": cm, "sel": sel, "ropeinv": ri}


_NC_CACHE = {}


def run_cores(inputs, n_cores=8, layers=(0, 1), phases="ABC", dbg=(), sub="GML"):
    key = (tuple(layers), phases, tuple(dbg), sub)
    if key not in _NC_CACHE:
        _NC_CACHE[key] = build(layers, phases, dbg, sub)
    nc = _NC_CACHE[key]
    consts = host_consts()
    shared = {k: np.ascontiguousarray(v) for k, v in inputs.items() if k not in ("x", "mem", "positions")}
    shared.update(consts)
    in_maps = []
    for b in range(n_cores):
        m = dict(shared)
        m["x"] = np.ascontiguousarray(inputs["x"][b])
        m["mem"] = np.ascontiguousarray(inputs["mem"][b])
        m["positions"] = np.ascontiguousarray(inputs["positions"][b:b + 1]).astype(np.int32)
        in_maps.append(m)
    res = run_bass_kernel_spmd(nc, in_maps, core_ids=list(range(n_cores)))
    return res.results


def kernel(**inputs):
    inputs = {k: np.asarray(v) for k, v in inputs.items()}
    res = run_cores(inputs, 8)
    return np.stack([r["out"] for r in res], axis=0).astype(np.float32)


def phase_B(c, l):
    from contextlib import ExitStack
    nc, P, w = c["nc"], c["P"], c["w"]
    x_tm, xT, ps, cmat_bf, memT = c["x_tm"], c["xT"], c["ps"], c["cmat_bf"], c["memT"]
    mm, act, tt, stt, cp = c["mm"], c["act"], c["tt"], c["stt"], c["cp"]
    load_cast = c["load_cast"]
    ONES = 4
    ph = ExitStack()

    def sb(name, shape, dt=F32):
        return P.tile(name, ph.enter_context(nc.sbuf_tensor("%s_%d" % (name, l), list(shape), dt)))

    c["load_ln"]("ln2_g", "ln2_b", l)
    kT = sb("b_kT", [128, 8, 256], BF16)
    vx = sb("b_v", [128, 2, D], BF16)
    ph2 = ExitStack()
    wkv = P.tile("b_wkv", ph2.enter_context(nc.sbuf_tensor("b_wkv_%d" % l, [128, 8, 2 * D], BF16)))
    for kc in range(8):
        load_cast(wkv, wkv[:, kc, :], w["xa_w_kv"][l, kc * 128:(kc + 1) * 128, :], sub=kc)
    for cc in range(8):
        pk = ps[cc % 2]
        for kc in range(8):
            mm(pk[:, 0:256], wkv[:, kc, cc * 128:(cc + 1) * 128], memT[:, kc, :], kc == 0, kc == 7, [wkv, memT], [pk])
        cp(kT[:, cc, :], pk[:, 0:256], [pk], [(kT, cc)], eng="scalar" if cc % 2 else "vector")
    for mt in range(2):
        for hf in range(2):
            pv = ps[2 + hf]
            for kc in range(8):
                mm(pv[:, :], memT[:, kc, mt * 128:(mt + 1) * 128], wkv[:, kc, D + hf * 512:D + (hf + 1) * 512],
                   kc == 0, kc == 7, [wkv, memT], [pv])
            cp(vx[:, mt, hf * 512:(hf + 1) * 512], pv[:, :], [pv], [(vx, (mt, hf))], eng="scalar" if hf else "vector")
    P.soft_barrier()
    ph2.close()
    wq = sb("b_wq", [128, 8, D], BF16)
    wo = sb("b_wo", [128, 8, D], BF16)
    for kc in range(0, 8, 2):
        load_cast(wq, wq[:, kc:kc + 2, :], w["xa_w_q"][l, kc * 128:(kc + 2) * 128, :].rearrange("(k p) n -> p k n", p=128), sub=kc)
    for kc in range(0, 8, 2):
        load_cast(wo, wo[:, kc:kc + 2, :], w["xa_w_o"][l, kc * 128:(kc + 2) * 128, :].rearrange("(k p) n -> p k n", p=128), sub=kc)
    qT = sb("b_qT", [128, 8, 512], BF16)
    xaT = sb("b_xaT", [128, 8, 512], BF16)
    PT = sb("b_PT", [128, 2, 512], BF16)
    rden = sb("b_rden", [128, 2, 512])
    scale = 256 ** -0.5
    for g in range(4):
        tok = slice(g * 512, (g + 1) * 512)
        for cc in range(8):
            pq = ps[cc % 2]
            for kc in range(8):
                mm(pq[:, :], wq[:, kc, cc * 128:(cc + 1) * 128], xT[:, kc, tok], kc == 0, kc == 7, [wq, xT], [pq])
            cp(qT[:, cc, :], pq[:, :], [pq], [(qT, cc)], eng="scalar" if cc % 2 else "vector")
        for h in range(4):
            for mt in range(2):
                pst = ps[2 + mt]
                for j in range(2):
                    mm(pst[:, :], kT[:, h * 2 + j, mt * 128:(mt + 1) * 128], qT[:, h * 2 + j, :], j == 0, j == 1,
                       [(kT, h * 2 + j), (qT, h * 2 + j)], [pst])
                act(PT[:, mt, :], pst[:, :], AF.Exp, [pst], [(PT, mt)], scale=scale)
            pden = ps[4]
            for mt in range(2):
                mm(pden[:, :], cmat_bf[:, ONES, :], PT[:, mt, :], mt == 0, mt == 1, [cmat_bf, (PT, mt)], [pden])
            rb = h % 2
            act(rden[:, rb, :], pden[:, :], AF.Ln, [pden], [(rden, rb)])
            act(rden[:, rb, :], rden[:, rb, :], AF.Exp, [(rden, rb)], [(rden, rb)], scale=-1.0)
            for j in range(2):
                po = ps[5 + j]
                for mt in range(2):
                    mm(po[:, :], vx[:, mt, h * 256 + j * 128:h * 256 + (j + 1) * 128], PT[:, mt, :], mt == 0, mt == 1,
                       [vx, (PT, mt)], [po])
                tt(xaT[:, h * 2 + j, :], po[:, :], rden[:, rb, :], ALU.mult, [po, (rden, rb)], [(xaT, h * 2 + j)])
        for tl in range(4):
            t = g * 4 + tl
            for hf in range(2):
                pp = ps[hf]
                for cc in range(8):
                    mm(pp[:, :], xaT[:, cc, tl * 128:(tl + 1) * 128], wo[:, cc, hf * 512:(hf + 1) * 512], cc == 0, cc == 7,
                       [xaT, wo], [pp])
                xs = x_tm[:, t, hf * 512:(hf + 1) * 512]
                stt(xs, xs, ALPHA, pp[:, :], ALU.mult, ALU.add, [(x_tm, t), pp], [(x_tm, t)])
            c["layer_norm_tile"](t, 7)
    P.soft_barrier()
    ph.close()


def head_norm_gate(c, sbf, name, src, gs, out_bf, b, keyp):
    P, tt, ts, red, act = c["P"], c["tt"], c["ts"], c["red"], c["act"]
    st = sbf["hn_st"]
    cen = sbf["hn_cen"]
    sq = sbf["hn_sq"]
    s4 = src.rearrange("p (h e) -> p h e", h=4)
    mean = st[:, b, 0:4]
    var = st[:, b, 4:8]
    red(mean, s4, ALU.add, [keyp], [(st, (b, "m"))])
    ts(mean, mean, -1.0 / 64, ALU.mult, [(st, (b, "m"))], [(st, (b, "m"))])
    c4 = cen[:, b, :].rearrange("p (h e) -> p h e", h=4)
    tt(c4, s4, bcast(mean, [128, 4, 64], 2), ALU.add, [keyp, (st, (b, "m"))], [(cen, b)])
    tt(sq[:, b, :], cen[:, b, :], cen[:, b, :], ALU.mult, [(cen, b)], [(sq, b)])
    red(var, sq[:, b, :].rearrange("p (h e) -> p h e", h=4), ALU.add, [(sq, b)], [(st, (b, "v"))])
    ts(var, var, 1.0 / 64, ALU.mult, [(st, (b, "v"))], [(st, (b, "v"))], s2=LN_EPS, op1=ALU.add)
    act(var, var, AF.Ln, [(st, (b, "v"))], [(st, (b, "v"))])
    act(var, var, AF.Exp, [(st, (b, "v"))], [(st, (b, "v"))], scale=-0.5)
    tt(c4, c4, bcast(var, [128, 4, 64], 2), ALU.mult, [(cen, b), (st, (b, "v"))], [(cen, b)])
    tt(out_bf, cen[:, b, :], gs, ALU.mult, [(cen, b), (sbf["gs"], b)], [(sbf["obf"], b)])


def phase_A(c, l):
    from contextlib import ExitStack
    nc, P, w = c["nc"], c["P"], c["w"]
    sub = c.get("sub", "GML")
    c["first_mixer"] = True
    if "G" in sub:
        mixer_gla(c, l)
        c["first_mixer"] = False
    if "M" in sub:
        mixer_mlstm(c, l)
        c["first_mixer"] = False
    if "L" in sub:
        mixer_mla(c, l)
    c["load_ln"]("ln1_g", "ln1_b", l)
    for t in range(NT):
        c["layer_norm_tile"](t, 5 + t % 3)
    P.soft_barrier()


def mixer_gla(c, l):
    from contextlib import ExitStack
    nc, P, w = c["nc"], c["P"], c["w"]
    x_tm, xT, ps, ps_bf, cmat, cmat_bf = c["x_tm"], c["xT"], c["ps"], c["ps_bf"], c["cmat"], c["cmat_bf"]
    mm, tr, act, tt, ts, stt, cp, red = c["mm"], c["tr"], c["act"], c["tt"], c["ts"], c["stt"], c["cp"], c["red"]
    load, load_cast = c["load"], c["load_cast"]
    IDENT, TRII, TRIS = 0, 1, 2
    ph = ExitStack()

    def sb(name, shape, dt=F32):
        return P.tile(name, ph.enter_context(nc.sbuf_tensor("%s_%d" % (name, l), list(shape), dt)))

    win = sb("g_win", [128, 8, 1040], BF16)
    for kc in range(8):
        load_cast(win, win[:, kc, :], w["w_in"][l, kc * 128:(kc + 1) * 128, 0:1040], sub=kc)
    wa2 = sb("g_wa2", [16, 256], BF16)
    load_cast(wa2, wa2[0:16, :], w["gla_w_a2"][l])
    barow = sb("g_barow", [1, 256], BF16)
    load_cast(barow, barow[:], w["gla_b_a"][l:l + 1, :])
    wout = sb("g_wout", [128, 2, D], BF16)
    load_cast(wout, wout[:], w["w_out"][l, 0:256, :].rearrange("(k p) n -> p k n", p=128))
    gng = sb("g_gng", [128, 256])
    load(gng, gng[:].unsqueeze(1), w["gla_norm_g"][l:l + 1, :].partition_broadcast(128))
    NB = 3
    gaT = sb("g_gaT", [16, NB, 128], BF16)
    Lsb = sb("g_L", [128, NB, 256])
    E1 = sb("g_E1", [128, NB, 256])
    E2 = sb("g_E2", [128, NB, 256])
    E3 = sb("g_E3", [128, NB, 256])
    qs = [sb("g_qs0", [128, NB, 256], BF16), sb("g_qs1", [128, NB, 256], BF16)]
    for i in range(2):
        P.op("vector", lambda e, i=i: e.memset(qs[i][:], 0.0), [], [qs[i]])
    ksT = sb("g_ksT", [128, NB, 256], BF16)
    k2 = sb("g_k2", [128, NB, 256], BF16)
    vsb = sb("g_v", [128, NB, 256], BF16)
    gs = sb("g_gs", [128, NB, 256])
    PT = sb("g_PT", [128, 2, 4, 128], BF16)
    osb = sb("g_osb", [128, NB, 256])
    obf = sb("g_obf", [128, NB, 256], BF16)
    ogT = sb("g_ogT", [128, 2, 2, 128], BF16)
    Dend = sb("g_Dend", [128, NT, 2])
    Sst = sb("g_S", [128, 2, 64])
    Sbf = sb("g_Sbf", [128, 2, 2, 64], BF16)
    P.op("vector", lambda e: e.memset(Sst[:], 0.0), [], [Sst])
    P.op("vector", lambda e: e.memset(Sbf[:], 0.0), [], [Sbf])
    sbf = dict(hn_st=sb("g_hn_st", [128, NB, 8]), hn_cen=sb("g_hn_cen", [128, NB, 256]),
               hn_sq=sb("g_hn_sq", [128, NB, 256]), gs=gs, obf=obf)
    import os
    NTL = int(os.environ.get("DBG_TILES", NT))
    A_, B_, C_, D_, E_, F_, G_, H_ = ps
    pbH = ps_bf[7]

    def S0(t):
        b = t % NB
        tok = slice(t * 128, (t + 1) * 128)
        for cc in range(4):
            for kc in range(8):
                mm(A_[:, cc * 128:(cc + 1) * 128], win[:, kc, cc * 128:(cc + 1) * 128], xT[:, kc, tok], kc == 0, kc == 7,
                   [win, xT], [A_])
        for kc in range(8):
            mm(B_[0:16, 256:384], win[:, kc, 1024:1040], xT[:, kc, tok], kc == 0, kc == 7, [win, xT], [B_])
        cp(gaT[0:16, b, :], B_[0:16, 256:384], [B_], [(gaT, b)], eng="scalar")
        mm(B_[:, 0:256], gaT[0:16, b, :], wa2[0:16, :], True, False, [(gaT, b), wa2], [B_])
        mm(B_[:, 0:256], cmat_bf[0:1, 4, :], barow[0:1, :], False, True, [cmat_bf, barow], [B_])
        act(Lsb[:, b, :], B_[:, 0:256], AF.Exp, [B_], [(Lsb, b)], scale=-1.0)
        act(Lsb[:, b, :], Lsb[:, b, :], AF.Ln, [(Lsb, b)], [(Lsb, b)], bias=1.0)
        for kc in range(8):
            mm(D_[:, :], xT[:, kc, tok], win[:, kc, 256:768], kc == 0, kc == 7, [win, xT], [D_])
        for kc in range(8):
            mm(E_[:, 0:256], xT[:, kc, tok], win[:, kc, 768:1024], kc == 0, kc == 7, [win, xT], [E_])
        cp(vsb[:, b, :], D_[:, 256:512], [D_], [(vsb, b)], eng="scalar")
        act(gs[:, b, :], E_[:, 0:256], AF.Exp, [E_], [(gs, b)], scale=-1.0)
        act(gs[:, b, :], gs[:, b, :], AF.Ln, [(gs, b)], [(gs, b)], bias=1.0)
        act(gs[:, b, :], gs[:, b, :], AF.Exp, [(gs, b)], [(gs, b)], scale=-1.0)
        stt(gs[:, b, :], E_[:, 0:256], 1.0, gs[:, b, :], ALU.mult, ALU.mult, [E_, (gs, b)], [(gs, b)])
        tt(gs[:, b, :], gs[:, b, :], gng[:, :], ALU.mult, [(gs, b), gng], [(gs, b)])
        for ch in range(2):
            mm(C_[:, ch * 128:(ch + 1) * 128], Lsb[:, b, ch * 128:(ch + 1) * 128], cmat[:, TRII, :], True, True,
               [(Lsb, b), cmat], [C_])
        mm(C_[:, 256:512], cmat[:, TRIS, :], Lsb[:, b, :], True, True, [(Lsb, b), cmat], [C_])
        act(E1[:, b, :], C_[:, 0:256], AF.Exp, [C_], [(E1, b)], scale=-1.0 / 16)
        act(E2[:, b, :], C_[:, 0:256], AF.Exp, [C_], [(E2, b)], scale=1.0 / 16)
        act(E3[:, b, :], C_[:, 256:512], AF.Exp, [C_], [(E3, b)], scale=-1.0 / 16)
        cp(Dend[:, t, :], E1[:, b, :].rearrange("p (c n) -> p c n", c=2)[:, :, 127], [(E1, b)], [(Dend, t)])
        for hp in range(2):
            rws = slice(hp * 64, (hp + 1) * 64)
            stt(qs[hp][rws, b, :], A_[rws, 0:256], 0.125, E1[rws, b, :], ALU.mult, ALU.mult, [A_, (E1, b)], [(qs[hp], b)])
        tt(ksT[:, b, :], A_[:, 256:512], E2[:, b, :], ALU.mult, [A_, (E2, b)], [(ksT, b)])
        tt(k2[:, b, :], D_[:, 0:256], E3[:, b, :], ALU.mult, [D_, (E3, b)], [(k2, b)])

    def S1(t):
        b = t % NB
        b2 = t % 2
        for h in range(4):
            hp, hc = h % 2, h // 2
            mm(F_[:, h * 128:(h + 1) * 128], ksT[:, b, hc * 128:(hc + 1) * 128],
               qs[hp][:, b, hc * 128:(hc + 1) * 128], True, True, [(ksT, b), (qs[hp], b)], [F_])
        for h in range(4):
            hc = h // 2
            mm(G_[:, h * 64:(h + 1) * 64], k2[:, b, hc * 128:(hc + 1) * 128], vsb[:, b, h * 64:(h + 1) * 64],
               True, True, [(k2, b), (vsb, b)], [G_])
        tt(PT[:, b2, :, :], F_[:, :].rearrange("p (h n) -> p h n", h=4), bcast(cmat[:, TRII, :], [128, 4, 128], 1),
           ALU.mult, [F_, cmat], [(PT, b2)])
        for h in range(4):
            hp, hc = h % 2, h // 2
            mm(G_[:, 256 + h * 64:256 + (h + 1) * 64], PT[:, b2, h, :], vsb[:, b, h * 64:(h + 1) * 64], True, False,
               [(PT, b2), (vsb, b)], [G_])
            mm(G_[:, 256 + h * 64:256 + (h + 1) * 64], qs[hp][:, b, hc * 128:(hc + 1) * 128],
               Sbf[:, b2, hc, :], False, True, [(qs[hp], b), (Sbf, b2)], [G_])
        for h in range(4):
            hp, hc = h % 2, h // 2
            rows = slice(hp * 64, (hp + 1) * 64)
            stt(Sst[rows, hc, :], Sst[rows, hc, :], Dend[rows, t, hc:hc + 1], G_[rows, h * 64:(h + 1) * 64],
                ALU.mult, ALU.add, [(Sst, h), (Dend, t), G_], [(Sst, h)])
        cp(Sbf[:, 1 - b2, :, :], Sst[:, :, :], [Sst], [(Sbf, 1 - b2)], eng="scalar")
        cp(osb[:, b, :], G_[:, 256:512], [G_], [(osb, b)], eng="scalar")

    def S2(t):
        b = t % NB
        b2 = t % 2
        head_norm_gate(c, sbf, "g", osb[:, b, :], gs[:, b, :], obf[:, b, :], b, (osb, b))
        for ch in range(2):
            tr(pbH[:, ch * 128:(ch + 1) * 128], obf[:, b, ch * 128:(ch + 1) * 128], cmat_bf[:, IDENT, :],
               [(obf, b), cmat_bf], [H_])
        cp(ogT[:, b2, :, :], pbH[:, 0:256].rearrange("p (c n) -> p c n", c=2), [H_], [(ogT, b2)], eng="scalar")
        for q4 in range(4):
            pq = H_[:, 128:384]
            for ch in range(2):
                mm(pq, ogT[:, b2, ch, :], wout[:, ch, q4 * 256:(q4 + 1) * 256], ch == 0, ch == 1, [(ogT, b2), wout], [H_])
            xs = x_tm[:, t, q4 * 256:(q4 + 1) * 256]
            if c["first_mixer"]:
                stt(xs, xs, ALPHA, pq, ALU.mult, ALU.add, [(x_tm, t), H_], [(x_tm, t)])
            else:
                tt(xs, xs, pq, ALU.add, [(x_tm, t), H_], [(x_tm, t)])

    for step in range(NTL + 2):
        if 0 <= step - 2 < NTL:
            S2(step - 2)
        if 0 <= step - 1 < NTL:
            S1(step - 1)
        if step < NTL:
            S0(step)
    P.soft_barrier()
    ph.close()


def mixer_mlstm(c, l):
    from contextlib import ExitStack
    import os
    nc, P, w = c["nc"], c["P"], c["w"]
    x_tm, xT, ps, ps_bf, cmat, cmat_bf = c["x_tm"], c["xT"], c["ps"], c["ps_bf"], c["cmat"], c["cmat_bf"]
    mm, tr, act, tt, ts, stt, cp, red = c["mm"], c["tr"], c["act"], c["tt"], c["ts"], c["stt"], c["cp"], c["red"]
    load, load_cast = c["load"], c["load_cast"]
    IDENT, TRII, ONES = 0, 1, 4
    ph = ExitStack()

    def sb(name, shape, dt=F32):
        return P.tile(name, ph.enter_context(nc.sbuf_tensor("%s_%d" % (name, l), list(shape), dt)))

    win = sb("m_win", [128, 8, 1032], BF16)
    for kc in range(8):
        load_cast(win, win[:, kc, :], w["w_in"][l, kc * 128:(kc + 1) * 128, 1040:2072], sub=kc)
    cw = sb("m_cw", [128, 4, 4])
    for j in range(4):
        load(cw, cw[:, :, j], w["ml_conv_w"][l, j, :].rearrange("(c p) -> p c", p=128), sub=j)
    bif = sb("m_bif", [128, 8])
    load(bif, bif[:, 0:4].unsqueeze(1), w["ml_b_i"][l:l + 1, :].partition_broadcast(128), sub=0)
    load(bif, bif[:, 4:8].unsqueeze(1), w["ml_b_f"][l:l + 1, :].partition_broadcast(128), sub=1)
    mng = sb("m_mng", [128, 256])
    load(mng, mng[:].unsqueeze(1), w["ml_norm_g"][l:l + 1, :].partition_broadcast(128))
    wout = sb("m_wout", [128, 2, D], BF16)
    load_cast(wout, wout[:], w["w_out"][l, 256:512, :].rearrange("(k p) n -> p k n", p=128))

    q = [sb("m_q0", [128, 2, S], BF16), sb("m_q1", [128, 2, S], BF16)]
    kT = sb("m_kT", [128, 2, S], BF16)
    ph1 = ExitStack()
    mqk = P.tile("m_mqk", ph1.enter_context(nc.sbuf_tensor("m_mqk_%d" % l, [128, 4, S + 3], BF16)))
    acc = P.tile("m_acc", ph1.enter_context(nc.sbuf_tensor("m_acc_%d" % l, [128, 2, 1024], F32)))
    P.op("vector", lambda e: e.memset(mqk[:, :, 0:3], 0.0), [], [mqk])
    for g in range(4):
        tok = slice(g * 512, (g + 1) * 512)
        for ch in range(4):
            pp = ps[(g * 4 + ch) % 2]
            for kc in range(8):
                mm(pp[:, :], win[:, kc, ch * 128:(ch + 1) * 128], xT[:, kc, tok], kc == 0, kc == 7, [win, xT], [pp])
            cp(mqk[:, ch, 3 + g * 512:3 + (g + 1) * 512], pp[:, :], [pp], [(mqk, ch)], eng="scalar" if ch % 2 else "vector")
    for i in range(2):
        P.op("vector", lambda e, i=i: e.memset(q[i][:], 0.0), [], [q[i]])
    for ch in range(4):
        for half in range(2):
            ai = half
            a = acc[:, ai, :]
            off = half * 1024
            ts(a, mqk[:, ch, off:off + 1024], cw[:, ch, 0:1], ALU.mult, [(mqk, ch), cw], [(acc, ai)])
            for j in range(1, 4):
                stt(a, mqk[:, ch, off + j:off + j + 1024], cw[:, ch, j:j + 1], a, ALU.mult, ALU.add,
                    [(mqk, ch), cw, (acc, ai)], [(acc, ai)])
            tokh = slice(off, off + 1024)
            if ch < 2:
                for hp in range(2):
                    rws = slice(hp * 64, (hp + 1) * 64)
                    act(q[hp][rws, ch, tokh], acc[rws, ai, :], AF.Silu, [(acc, ai)], [(q[hp], (ch, half))])
            else:
                act(kT[:, ch - 2, tokh], a, AF.Silu, [(acc, ai)], [(kT, (ch, half))])
    for hp in range(2):
        ts(q[hp][:], q[hp][:], 0.125, ALU.mult, [q[hp]], [q[hp]])
    P.soft_barrier()
    ph1.close()

    gates = sb("m_gates", [128, NT, 8])
    pg = ps[2]
    for t in range(NT):
        for kc in range(8):
            mm(pg[:, t * 8:(t + 1) * 8], xT[:, kc, t * 128:(t + 1) * 128], win[:, kc, 1024:1032], kc == 0, kc == 7,
               [win, xT], [pg])
    tt(gates[:], pg[:, 0:128].rearrange("p (t n) -> p t n", t=NT), bcast(bif[:, :], [128, NT, 8], 1), ALU.add,
       [pg, bif], [gates])
    Lf = sb("m_Lf", [128, NT, 4])
    act(Lf[:], gates[:, :, 4:8], AF.Exp, [gates], [Lf], scale=-1.0)
    ts(Lf[:], Lf[:], 1.0, ALU.add, [Lf], [Lf])
    act(Lf[:], Lf[:], AF.Ln, [Lf], [Lf])
    Lf2 = Lf[:].rearrange("p t n -> p (t n)")
    p3 = ps[3]
    mm(p3[:, 0:64], cmat[:, TRII, :], Lf2, True, True, [cmat, Lf], [p3])
    asb = sb("m_a", [128, NT, 4])
    tt(asb[:], p3[:, 0:64].rearrange("p (t n) -> p t n", t=NT), gates[:, :, 0:4], ALU.add, [p3, gates], [asb])
    cumL = sb("m_cumL", [128, 64])
    cp(cumL[:], p3[:, 0:64], [p3], [cumL])
    a2 = asb[:].rearrange("p t n -> p (t n)")
    p4 = ps[4]
    tr(p4[0:64, 0:128], a2, cmat[:, IDENT, :], [asb, cmat], [p4])
    Acol = sb("m_Acol", [64, 1])
    red(Acol[:, 0:1], p4[0:64, 0:128], ALU.max, [p4], [Acol])
    rows = sb("m_rows", [1, 5, 64])
    p5 = ps[5]
    mm(p5[0:1, 0:64], Acol[0:64, 0:1], cmat[0:64, IDENT, 0:64], True, True, [Acol, cmat], [p5])
    mm(p5[0:1, 64:128], cmat[:, ONES, 0:1], Lf2, True, True, [cmat, Lf], [p5])
    cp(rows[0:1, 0:2, :], p5[0:1, 0:128].rearrange("p (a n) -> p a n", a=2), [p5], [rows])
    P.op("vector", lambda e: e.memset(rows[0:1, 2, 0:4], 0.0), [rows], [rows])
    for cc in range(NT):
        sl = slice(cc * 4, cc * 4 + 4)
        tt(rows[0:1, 3, sl], rows[0:1, 2, sl], rows[0:1, 0, sl], ALU.max, [rows], [rows])
        if cc < NT - 1:
            tt(rows[0:1, 2, (cc + 1) * 4:(cc + 1) * 4 + 4], rows[0:1, 3, sl], rows[0:1, 1, sl], ALU.subtract, [rows], [rows])
    tt(rows[0:1, 4, :], rows[0:1, 2, :], rows[0:1, 3, :], ALU.subtract, [rows], [rows])
    act(rows[0:1, 4, :], rows[0:1, 4, :], AF.Exp, [rows], [rows])
    p6 = ps[6]
    mm(p6[:, 0:128], cmat[0:1, ONES, :], rows[0:1, 3:5, :].rearrange("p a n -> p (a n)"), True, True, [cmat, rows], [p6])
    bcs = sb("m_bcs", [128, 128])
    cp(bcs[:], p6[:, 0:128], [p6], [bcs])
    wtok = sb("m_wtok", [128, 64])
    tt(wtok[:], a2, bcs[:, 0:64], ALU.subtract, [asb, bcs], [wtok])
    act(wtok[:], wtok[:], AF.Exp, [wtok], [wtok])
    clamp = sb("m_clamp", [128, 64])
    tt(clamp[:], cumL[:], bcs[:, 0:64], ALU.subtract, [cumL, bcs], [clamp])
    act(clamp[:], clamp[:], AF.Exp, [clamp], [clamp])

    vext = sb("m_vext", [128, 2, 4, 65], BF16)
    P.op("vector", lambda e: e.memset(vext[:], 1.0), [], [vext])
    vw = sb("m_vw", [128, 2, 4, 65], BF16)
    gso = sb("m_gso", [128, 2, 256])
    ktm = sb("m_ktm", [128, 2, 256], BF16)
    WM = sb("m_WM", [128, 2, 4, 128])
    PT = sb("m_PT", [128, 2, 4, 128], BF16)
    Cn = sb("m_Cn", [128, 2, 65])
    P.op("vector", lambda e: e.memset(Cn[:], 0.0), [], [Cn])
    Cd = sb("m_Cd", [128, 2, 65])
    Cdbf = sb("m_Cdbf", [128, 2, 2, 65], BF16)
    nd = sb("m_nd", [128, 2, 4, 65])
    hsb = sb("m_h", [128, 2, 256])
    small = sb("m_small", [128, 2, 8])
    obf = sb("m_obf", [128, 2, 256], BF16)
    ohT = sb("m_ohT", [128, 2, 2, 128], BF16)
    sbf = dict(hn_st=sb("m_hn_st", [128, 2, 8]), hn_cen=sb("m_hn_cen", [128, 2, 256]),
               hn_sq=sb("m_hn_sq", [128, 2, 256]), gs=gso, obf=obf)
    PA, PB, PC, PD, PE_, PO0, PO1, PX = ps
    for t in range(int(os.environ.get("DBG_TILES", NT))):
        b = t % 2
        tok = slice(t * 128, (t + 1) * 128)
        g4 = slice(t * 4, t * 4 + 4)
        for kc in range(8):
            mm(PA[:, :], xT[:, kc, tok], win[:, kc, 512:1024], kc == 0, kc == 7, [win, xT], [PA])
        v4 = PA[:, 0:256].rearrange("p (h e) -> p h e", h=4)
        cp(vext[:, b, :, 0:64], v4, [PA], [(vext, b)], eng="scalar")
        tt(vw[:, b, :, 0:64], v4, bcast(wtok[:, g4], [128, 4, 64], 2), ALU.mult, [PA, wtok], [(vw, b)])
        cp(vw[:, b, :, 64], wtok[:, g4], [wtok], [(vw, b)])
        act(gso[:, b, :], PA[:, 256:512], AF.Exp, [PA], [(gso, b)], scale=-1.0)
        act(gso[:, b, :], gso[:, b, :], AF.Ln, [(gso, b)], [(gso, b)], bias=1.0)
        act(gso[:, b, :], gso[:, b, :], AF.Exp, [(gso, b)], [(gso, b)], scale=-1.0)
        tt(gso[:, b, :], gso[:, b, :], mng[:, :], ALU.mult, [(gso, b), mng], [(gso, b)])
        pb = ps_bf[1]
        for hc in range(2):
            tr(pb[:, hc * 128:(hc + 1) * 128], kT[:, hc, tok], cmat_bf[:, IDENT, :], [kT, cmat_bf], [PB])
        cp(ktm[:, b, :], pb[:, 0:256], [PB], [(ktm, b)], eng="scalar")
        for h in range(4):
            hp, hc = h % 2, h // 2
            mm(PC[:, h * 128:(h + 1) * 128], kT[:, hc, tok], q[hp][:, hc, tok], True, True, [kT, q[hp]], [PC])
        tt(WM[:, b, :, :], bcast(wtok[:, g4], [128, 4, 128], 2), bcast(cmat[:, TRII, :], [128, 4, 128], 1), ALU.mult,
           [wtok, cmat], [(WM, b)])
        tt(PT[:, b, :, :], PC[:, :].rearrange("p (h n) -> p h n", h=4), WM[:, b, :, :], ALU.mult, [PC, (WM, b)], [(PT, b)])
        for h in range(4):
            hc = h // 2
            mm(PD[:, h * 65:(h + 1) * 65], ktm[:, b, hc * 128:(hc + 1) * 128], vw[:, b, h, :], True, True,
               [(ktm, b), (vw, b)], [PD])
        for h in range(4):
            hp, hc = h % 2, h // 2
            rws = slice(hp * 64, (hp + 1) * 64)
            ts(Cd[rws, hc, :], Cn[rws, hc, :], bcs[rws, 64 + t * 4 + h:64 + t * 4 + h + 1], ALU.mult,
               [(Cn, h), bcs], [(Cd, h)])
        cp(Cdbf[:, b, :, :], Cd[:], [Cd], [(Cdbf, b)], eng="scalar")
        for h in range(4):
            hp, hc = h % 2, h // 2
            mm(PE_[:, h * 65:(h + 1) * 65], PT[:, b, h, :], vext[:, b, h, :], True, False, [(PT, b), (vext, b)], [PE_])
            mm(PE_[:, h * 65:(h + 1) * 65], q[hp][:, hc, tok], Cdbf[:, b, hc, :], False, True, [q[hp], (Cdbf, b)], [PE_])
        for h in range(4):
            hp, hc = h % 2, h // 2
            rws = slice(hp * 64, (hp + 1) * 64)
            tt(Cn[rws, hc, :], Cd[rws, hc, :], PD[rws, h * 65:(h + 1) * 65], ALU.add, [(Cd, h), PD], [(Cn, h)])
        cp(nd[:, b, :, :], PE_[:, 0:260].rearrange("p (h e) -> p h e", h=4), [PE_], [(nd, b)], eng="scalar")
        stt(small[:, b, 0:4], nd[:, b, :, 64], -1.0, nd[:, b, :, 64], ALU.mult, ALU.max, [(nd, b)], [(small, b)])
        tt(small[:, b, 0:4], small[:, b, 0:4], clamp[:, g4], ALU.max, [(small, b), clamp], [(small, b)])
        P.op("vector", lambda e, b=b: e.reciprocal(small[:, b, 4:8], small[:, b, 0:4]), [(small, b)], [(small, b)])
        tt(hsb[:, b, :].rearrange("p (h e) -> p h e", h=4), nd[:, b, :, 0:64], bcast(small[:, b, 4:8], [128, 4, 64], 2),
           ALU.mult, [(nd, b), (small, b)], [(hsb, b)])
        head_norm_gate(c, sbf, "m", hsb[:, b, :], gso[:, b, :], obf[:, b, :], b, (hsb, b))
        pbx = ps_bf[7]
        for ch in range(2):
            tr(pbx[:, ch * 128:(ch + 1) * 128], obf[:, b, ch * 128:(ch + 1) * 128], cmat_bf[:, IDENT, :],
               [(obf, b), cmat_bf], [PX])
        cp(ohT[:, b, :, :], pbx[:, 0:256].rearrange("p (c n) -> p c n", c=2), [PX], [(ohT, b)], eng="scalar")
        for hf in range(2):
            po = PO0 if hf == 0 else PO1
            for ch in range(2):
                mm(po[:, :], ohT[:, b, ch, :], wout[:, ch, hf * 512:(hf + 1) * 512], ch == 0, ch == 1, [(ohT, b), wout], [po])
            xs = x_tm[:, t, hf * 512:(hf + 1) * 512]
            if c["first_mixer"]:
                stt(xs, xs, ALPHA, po[:, :], ALU.mult, ALU.add, [(x_tm, t), po], [(x_tm, t)])
            else:
                tt(xs, xs, po[:, :], ALU.add, [(x_tm, t), po], [(x_tm, t)])
    P.soft_barrier()
    ph.close()


def mixer_mla(c, l):
    from contextlib import ExitStack
    import os
    nc, P, w = c["nc"], c["P"], c["w"]
    x_tm, xT, ps, cmat, cmat_bf, cs = c["x_tm"], c["xT"], c["ps"], c["cmat"], c["cmat_bf"], c["cs"]
    mm, act, tt, ts, stt, cp = c["mm"], c["act"], c["tt"], c["ts"], c["stt"], c["cp"]
    load, load_cast = c["load"], c["load_cast"]
    MMLA, ONES = 3, 4
    R_ = slice(64, 96)
    ph = ExitStack()

    def sb(name, shape, dt=F32, st=None):
        return P.tile(name, (st or ph).enter_context(nc.sbuf_tensor("%s_%d" % (name, l), list(shape), dt)))

    cqnT = sb("a_cqnT", [128, 2, S], BF16)
    ckvnT = sb("a_ckvnT", [128, S], BF16)
    krope = sb("a_krope", [96, S], BF16)
    v_all = sb("a_vall", [128, NT, 512], BF16)
    gq = sb("a_gq", [128, 2])
    gkv = sb("a_gkv", [128, 1])
    load(gq, gq[:], w["mla_q_norm_g"][l].rearrange("(rc p) -> p rc", p=128))
    load(gkv, gkv[:], w["mla_kv_norm_g"][l].rearrange("(o p) -> p o", o=1))

    p1 = ExitStack()
    win = sb("a_win", [128, 8, 416], BF16, p1)
    for kc in range(8):
        load_cast(win, win[:, kc, :], w["w_in"][l, kc * 128:(kc + 1) * 128, 2072:2488], sub=kc)
    wkr = sb("a_wkr", [128, 8, 2, 96], BF16, p1)
    P.op("vector", lambda e: e.memset(wkr[:], 0.0), [], [wkr])
    cp(wkr[:, :, 0, 64:96], win[:, :, 384:416], [win], [wkr])
    ts(wkr[:, :, 1, 64:80], win[:, :, 400:416], -1.0, ALU.mult, [win], [wkr])
    cp(wkr[:, :, 1, 80:96], win[:, :, 384:400], [win], [wkr])
    sq = sb("a_sq", [128, 2, 512], BF16, p1)
    rstd = sb("a_rstd", [128, 2, 512], F32, p1)
    tA = sb("a_tA", [96, 512], F32, p1)
    tB = sb("a_tB", [96, 512], F32, p1)
    for g in range(4):
        tok = slice(g * 512, (g + 1) * 512)
        for rc in range(2):
            for kc in range(8):
                mm(ps[rc][:, :], win[:, kc, rc * 128:(rc + 1) * 128], xT[:, kc, tok], kc == 0, kc == 7, [win, xT], [ps[rc]])
        for rc in range(2):
            act(sq[:, rc, :], ps[rc][:, :], AF.Square, [ps[rc]], [(sq, rc)])
        for rc in range(2):
            mm(ps[2][:, :], cmat_bf[:, ONES, :], sq[:, rc, :], rc == 0, rc == 1, [cmat_bf, (sq, rc)], [ps[2]])
        ts(rstd[:, 0, :], ps[2][:, :], 1.0 / 256, ALU.mult, [ps[2]], [(rstd, 0)], s2=LN_EPS, op1=ALU.add)
        act(rstd[:, 0, :], rstd[:, 0, :], AF.Ln, [(rstd, 0)], [(rstd, 0)])
        act(rstd[:, 0, :], rstd[:, 0, :], AF.Exp, [(rstd, 0)], [(rstd, 0)], scale=-0.5)
        for rc in range(2):
            stt(cqnT[:, rc, tok], ps[rc][:, :], gq[:, rc:rc + 1], rstd[:, 0, :], ALU.mult, ALU.mult,
                [ps[rc], gq, (rstd, 0)], [(cqnT, (rc, g))])
        for kc in range(8):
            mm(ps[3][:, :], win[:, kc, 256:384], xT[:, kc, tok], kc == 0, kc == 7, [win, xT], [ps[3]])
        act(sq[:, 0, :], ps[3][:, :], AF.Square, [ps[3]], [(sq, 0)])
        mm(ps[4][:, :], cmat_bf[:, ONES, :], sq[:, 0, :], True, True, [cmat_bf, (sq, 0)], [ps[4]])
        ts(rstd[:, 1, :], ps[4][:, :], 1.0 / 128, ALU.mult, [ps[4]], [(rstd, 1)], s2=LN_EPS, op1=ALU.add)
        act(rstd[:, 1, :], rstd[:, 1, :], AF.Ln, [(rstd, 1)], [(rstd, 1)])
        act(rstd[:, 1, :], rstd[:, 1, :], AF.Exp, [(rstd, 1)], [(rstd, 1)], scale=-0.5)
        stt(ckvnT[:, tok], ps[3][:, :], gkv[:, 0:1], rstd[:, 1, :], ALU.mult, ALU.mult, [ps[3], gkv, (rstd, 1)], [(ckvnT, g)])
        for r2 in range(2):
            for kc in range(8):
                mm(ps[5 + r2][0:96, :], wkr[:, kc, r2, :], xT[:, kc, tok], kc == 0, kc == 7, [wkr, xT], [ps[5 + r2]])
        tt(tA[R_, :], ps[5][R_, :], cs[R_, 0, tok], ALU.mult, [ps[5], cs], [tA])
        tt(tB[R_, :], ps[6][R_, :], cs[R_, 1, tok], ALU.mult, [ps[6], cs], [tB])
        tt(krope[R_, tok], tA[R_, :], tB[R_, :], ALU.add, [tA, tB], [(krope, g)])
    P.soft_barrier()
    p1.close()

    wuk = sb("a_wuk", [128, 8, 64], BF16)
    wuv = sb("a_wuv", [128, 8, 64], BF16)
    ukv = w["mla_w_ukv"][l].rearrange("p (h two d) -> p h two d", h=8, two=2)
    load_cast(wuk, wuk[:], ukv[:, :, 0, :])
    load_cast(wuv, wuv[:], ukv[:, :, 1, :])
    wuq = sb("a_wuq", [128, 2, 768], BF16)
    load_cast(wuq, wuq[:], w["mla_w_uq"][l].rearrange("(rc p) n -> p rc n", p=128))
    wuqr = sb("a_wuqr", [128, 2, 8, 96], BF16)
    P.op("vector", lambda e: e.memset(wuqr[:], 0.0), [], [wuqr])
    wq4 = wuq[:].rearrange("p r (h c) -> p r h c", h=8)
    ts(wuqr[:, :, :, 64:80], wq4[:, :, :, 80:96], -1.0, ALU.mult, [wuq], [wuqr])
    cp(wuqr[:, :, :, 80:96], wq4[:, :, :, 64:80], [wuq], [wuqr])
    wout = sb("a_wout", [128, 4, D], BF16)
    load_cast(wout, wout[:], w["w_out"][l, 512:1024, :].rearrange("(k p) n -> p k n", p=128))
    qTh = sb("a_qTh", [96, 2, 512], BF16)
    PTb = sb("a_PT", [128, 3, 512], BF16)
    mlaT = sb("a_mlaT", [128, 4, 512], BF16)
    rden = sb("a_rden", [128, 2, 512])
    tA2 = sb("a_tA2", [96, 2, 512])
    tB2 = sb("a_tB2", [96, 2, 512])
    KH = [("k", h) for h in range(8)]
    P.merge(xT)
    for g in range(4):
        tok = slice(g * 512, (g + 1) * 512)
        for h in range(8):
            pk = ps[h % 2]
            mm(pk[0:64, :], wuk[:, h, :], ckvnT[:, tok], True, True, [wuk, ckvnT], [pk])
            cp(xT[0:64, h, tok], pk[0:64, :], [pk], [(xT, KH[h])], eng="scalar" if h % 2 else "vector")
        cp(xT[R_, :, tok], bcast(krope[R_, tok], [32, 8, 512], 1), [krope], [(xT, k) for k in KH])
    for t in range(NT):
        pv = ps[2 + t % 2]
        mm(pv[:, :], ckvnT[:, t * 128:(t + 1) * 128], wuv[:].rearrange("p h d -> p (h d)"), True, True, [ckvnT, wuv], [pv])
        cp(v_all[:, t, :], pv[:, :], [pv], [(v_all, t)], eng="scalar" if t % 2 else "vector")
    scale = 96.0 ** -0.5
    NG = int(os.environ.get("DBG_GROUPS", 4))

    def qproj(g, h):
        tok = slice(g * 512, (g + 1) * 512)
        qb = h % 2
        pq, pr = ps[0], ps[1]
        for rc in range(2):
            mm(pq[0:96, :], wuq[:, rc, h * 96:(h + 1) * 96], cqnT[:, rc, tok], rc == 0, rc == 1, [wuq, cqnT], [pq])
        for rc in range(2):
            mm(pr[0:96, :], wuqr[:, rc, h, :], cqnT[:, rc, tok], rc == 0, rc == 1, [wuqr, cqnT], [pr])
        cp(qTh[0:64, qb, :], pq[0:64, :], [pq], [(qTh, qb)], eng="scalar")
        tt(tA2[R_, qb, :], pq[R_, :], cs[R_, 0, tok], ALU.mult, [pq, cs], [(tA2, qb)])
        tt(tB2[R_, qb, :], pr[R_, :], cs[R_, 1, tok], ALU.mult, [pr, cs], [(tB2, qb)])
        tt(qTh[R_, qb, :], tA2[R_, qb, :], tB2[R_, qb, :], ALU.add, [(tA2, qb), (tB2, qb)], [(qTh, qb)])

    def attend(g, h, nxt):
        nkt = 4 * g + 4
        hp, pair, qb = h % 2, h // 2, h % 2
        po, pd = ps[4 + h % 2], ps[6 + h % 2]

        def qk(kt):
            qlo = max(kt - 4 * g, 0) * 128
            N = 512 - qlo
            pst = ps[2 + kt % 2]
            mm(pst[:, 0:N], xT[0:96, h, kt * 128:(kt + 1) * 128], qTh[0:96, qb, qlo:512], True, True,
               [(xT, KH[h]), (qTh, qb)], [pst])

        qk(0)
        for kt in range(nkt):
            qlo = max(kt - 4 * g, 0) * 128
            N = 512 - qlo
            pst = ps[2 + kt % 2]
            pb = kt % 3
            if kt + 1 < nkt:
                qk(kt + 1)
            elif nxt is not None:
                qproj(*nxt)
            act(PTb[:, pb, 0:N], pst[:, 0:N], AF.Exp, [pst], [(PTb, pb)], scale=scale)
            if kt >= 4 * g:
                tt(PTb[:, pb, 0:128], PTb[:, pb, 0:128], cmat_bf[:, MMLA, :], ALU.mult, [(PTb, pb), cmat_bf], [(PTb, pb)])
            mm(po[:, qlo:512], v_all[:, kt, pair * 128:(pair + 1) * 128], PTb[:, pb, 0:N], kt == 0, kt == nkt - 1,
               [(v_all, kt), (PTb, pb)], [po])
            mm(pd[:, qlo:512], cmat_bf[:, ONES, :], PTb[:, pb, 0:N], kt == 0, kt == nkt - 1, [cmat_bf, (PTb, pb)], [pd])
        rws = slice(hp * 64, (hp + 1) * 64)
        act(rden[rws, qb, :], pd[rws, :], AF.Ln, [pd], [(rden, qb)])
        act(rden[rws, qb, :], rden[rws, qb, :], AF.Exp, [(rden, qb)], [(rden, qb)], scale=-1.0)
        tt(mlaT[rws, pair, :], po[rws, :], rden[rws, qb, :], ALU.mult, [po, (rden, qb)], [(mlaT, h)])

    work = [(g, h) for g in range(NG) for h in range(8)]
    qproj(*work[0])
    for i, (g, h) in enumerate(work):
        nxt = work[i + 1] if i + 1 < len(work) else None
        if nxt is not None and nxt[0] != g:
            attend(g, h, None)
        else:
            attend(g, h, nxt)
        if h != 7:
            continue
        if nxt is not None:
            qproj(*nxt)
        for tl in range(4):
            t = g * 4 + tl
            for hf in range(2):
                pp = ps[5 + 2 * hf]
                for pr_ in range(4):
                    mm(pp[:, :], mlaT[:, pr_, tl * 128:(tl + 1) * 128], wout[:, pr_, hf * 512:(hf + 1) * 512], pr_ == 0, pr_ == 3,
                       [mlaT, wout], [pp])
                xs = x_tm[:, t, hf * 512:(hf + 1) * 512]
                if c["first_mixer"]:
                    stt(xs, xs, ALPHA, pp[:, :], ALU.mult, ALU.add, [(x_tm, t), pp], [(x_tm, t)])
                else:
                    tt(xs, xs, pp[:, :], ALU.add, [(x_tm, t), pp], [(x_tm, t)])
    P.merge(xT)
    P.soft_barrier()
    ph.close()
```

```python
import numpy as np
import ml_dtypes
import concourse.bass as bass
import concourse.mybir as mybir
from concourse.bass_utils import run_bass_kernel_spmd

F32 = mybir.dt.float32
BF16 = mybir.dt.bfloat16
I32 = mybir.dt.int32
AF = mybir.ActivationFunctionType
ALU = mybir.AluOpType
AX = mybir.AxisListType

S = 2048
D = 1024
NT = 16
DEPTH = 2
ALPHA = (2 * DEPTH) ** 0.25
LN_EPS = 1e-5
IN_W = 2488


class Tile:
    def __init__(self, name, h):
        self.name = name
        self.h = h
        self.st = {}
        self.dsem = None
        self.dcount = 0
        self.psum = False

    def __getitem__(self, k):
        return self.h[k]


class Op:
    __slots__ = ("eng", "fn", "deps", "ddeps", "inc", "dma", "cost", "unit", "seg", "oi", "fin", "pos", "tbl")

    def __init__(self, eng, fn, deps, ddeps, dma=None, cost=0.2):
        self.eng = eng
        self.fn = fn
        self.deps = deps
        self.ddeps = ddeps
        self.inc = False
        self.dma = dma
        self.cost = cost
        self.unit = None
        self.seg = 0
        self.oi = 0
        self.fin = 0.0
        self.pos = 0
        self.tbl = None


class Prog:
    ENG = ("sync", "scalar", "vector", "gpsimd", "tensor")
    REORDER = ("scalar", "vector", "tensor")

    def __init__(self, nc):
        self.nc = nc
        self.ops = {e: [] for e in self.ENG}
        self.all = []
        self.dsems = {}
        self.dtotal = {}
        self.tiles = []
        self.seg = 0
        self.cur_unit = None
        self.nunits = 0
        self.dma_by = {}

    def tile(self, name, h):
        t = Tile(name, h)
        try:
            ml = self.nc.lookup_mloc(h)
            sbuf = "SB" in str(ml.type)
            t.lo, t.hi = int(ml.addr), int(ml.addr) + int(ml.dims[1])
        except Exception:
            sbuf = False
        t.sbuf = sbuf
        if sbuf:
            inh = []
            for o in self.tiles:
                if getattr(o, "sbuf", False) and o.lo < t.hi and t.lo < o.hi:
                    for st in o.st.values():
                        if st[0] is not None:
                            inh.append(st[0])
                        inh.extend(st[1])
            if inh:
                seen = set()
                uniq = []
                for x in inh:
                    k = id(x) if isinstance(x, Op) else x
                    if k not in seen:
                        seen.add(k)
                        uniq.append(x)
                t.st[None] = [None, uniq]
        self.tiles.append(t)
        return t

    def merge(self, t):
        allr = []
        for st in t.st.values():
            if st[0] is not None:
                allr.append(st[0])
            allr.extend(st[1])
        seen, uniq = set(), []
        for x in allr:
            k = id(x) if isinstance(x, Op) else x
            if k not in seen:
                seen.add(k)
                uniq.append(x)
        t.st = {None: [None, uniq]}

    def soft_barrier(self):
        return

    @staticmethod
    def _norm(lst):
        out = []
        for x in lst:
            if not isinstance(x, tuple):
                x = (x, None)
            if x[0].psum:
                x = (x[0], None)
            out.append(x)
        return out

    @staticmethod
    def _states(t, s):
        if s is None:
            return list(t.st.values())
        r = []
        if None in t.st:
            r.append(t.st[None])
        if s in t.st:
            r.append(t.st[s])
        return r

    def _collect(self, reads, writes):
        deps, ddeps = [], {}

        def add(ev):
            if ev is None:
                return
            if isinstance(ev, Op):
                deps.append(ev)
            else:
                k = ev
                ddeps[k] = self.dtotal[k]

        for (t, s) in reads:
            for st in self._states(t, s):
                add(st[0])
        for (t, s) in writes:
            for st in self._states(t, s):
                add(st[0])
                for r in st[1]:
                    add(r)
        return deps, ddeps

    def _update(self, ev, reads, writes):
        for (t, s) in reads:
            st = t.st.setdefault(s, [None, []])
            st[1].append(ev)
        for (t, s) in writes:
            if s is None:
                t.st = {None: [ev, []]}
            else:
                t.st[s] = [ev, []]

    def _add(self, o):
        o.seg = self.seg
        o.oi = len(self.all)
        self.all.append(o)
        self.ops[o.eng].append(o)

    def op(self, eng, fn, reads=(), writes=(), cost=0.2, start=None, stop=None, tbl=None):
        reads = self._norm(reads)
        writes = self._norm(writes)
        writes = writes + [r for r in reads if r[0].psum]
        deps, ddeps = self._collect(reads, writes)
        o = Op(eng, fn, deps, ddeps, cost=cost)
        o.tbl = tbl
        if eng == "tensor":
            if self.cur_unit is None or start is None or start:
                self.nunits += 1
                self.cur_unit = self.nunits
            o.unit = self.cur_unit
            if stop is None or stop:
                self.cur_unit = None
        self._add(o)
        self._update(o, reads, writes)

    def dma(self, eng, out, in_, reads=(), writes=(), semtile=None):
        assert eng in ("sync", "gpsimd")
        reads = self._norm(reads)
        writes = self._norm(writes)
        deps, ddeps = self._collect(reads, writes)
        if semtile.dsem is None:
            semtile.dsem = ("D", semtile.name)
            self.dsems[semtile.dsem] = None
        semtile.dcount = self.dtotal.get(semtile.dsem, 0) + 16
        self.dtotal[semtile.dsem] = semtile.dcount
        o = Op(eng, None, deps, ddeps, dma=(out, in_, semtile.dsem), cost=0.1)
        self.dma_by[(semtile.dsem, semtile.dcount)] = o
        self._add(o)
        self._update(semtile.dsem, reads, writes)

    def barrier(self):
        for e in self.ENG:
            o = Op(e, "bar", [], {}, cost=0.05)
            self._add(o)
        self.seg += 1
        for t in self.tiles:
            t.st = {}

    def finish(self, eng="sync"):
        self.barrier()

    def schedule(self):
        LAT = 0.25
        W = 64
        order = {e: [] for e in self.ENG}
        nseg = self.seg + 1
        byseg = [{e: [] for e in self.ENG} for _ in range(nseg)]
        for o in self.all:
            byseg[o.seg][o.eng].append(o)
        for sg in range(nseg):
            lists = byseg[sg]
            items = {e: [] for e in self.ENG}
            item_of = {}
            for e in self.ENG:
                cur = None
                for o in lists[e]:
                    if o.unit is not None and cur is not None and cur["unit"] == o.unit:
                        cur["ops"].append(o)
                    else:
                        cur = {"unit": o.unit, "ops": [o], "nrem": 0, "rt": 0.0, "succ": [], "eng": e, "done": False,
                               "bar": o.fn == "bar"}
                        items[e].append(cur)
                    item_of[id(o)] = cur
            dmafin = {}
            for e in self.ENG:
                for it in items[e]:
                    preds = set()
                    for o in it["ops"]:
                        dl = list(o.deps)
                        for k, v in o.ddeps.items():
                            dd = self.dma_by.get((k, v))
                            if dd is not None:
                                dl.append(dd)
                        for d in dl:
                            if d.seg != sg:
                                continue
                            p = item_of[id(d)]
                            if p is it:
                                continue
                            preds.add(id(p))
                            if id(p) not in it.setdefault("pset", {}):
                                it["pset"][id(p)] = p
                    for p in it.get("pset", {}).values():
                        p["succ"].append(it)
                    it["nrem"] = len(it.get("pset", {}))
            free = {e: 0.0 for e in self.ENG}
            heads = {e: 0 for e in self.ENG}
            cur_tbl = None
            npend = sum(len(v) for v in items.values())
            while npend > 0:
                progressed = False
                for e in self.ENG:
                    L = items[e]
                    while heads[e] < len(L) and L[heads[e]]["done"]:
                        heads[e] += 1
                    if heads[e] >= len(L):
                        continue
                    wmax = W if e in self.REORDER else 1
                    pick, pick_t = None, None
                    i, cnt = heads[e], 0
                    while i < len(L) and cnt < wmax:
                        it = L[i]
                        if not it["done"]:
                            cnt += 1
                            if it["bar"]:
                                if i == heads[e] and it["nrem"] == 0:
                                    pick, pick_t = it, free[e]
                                break
                            if it["nrem"] == 0:
                                rt = it["rt"]
                                for o in it["ops"]:
                                    for k in o.ddeps:
                                        rt = max(rt, dmafin.get(k, 0.0))
                                st_t = max(rt, free[e])
                                if e == "scalar":
                                    tb = it["ops"][0].tbl
                                    if tb is not None and tb != cur_tbl:
                                        st_t += 1.4
                                if pick is None or st_t < pick_t - 1e-9:
                                    pick, pick_t = it, st_t
                                if st_t <= free[e] + 1e-9:
                                    break
                        i += 1
                    if pick is None:
                        continue
                    tcur = pick_t
                    if e == "scalar" and pick["ops"][0].tbl is not None:
                        cur_tbl = pick["ops"][0].tbl
                    for o in pick["ops"]:
                        tcur += o.cost
                        o.fin = tcur
                        order[e].append(o)
                        if o.dma is not None:
                            dmafin[o.dma[2]] = max(dmafin.get(o.dma[2], 0.0), tcur + 2.5)
                    pick["done"] = True
                    npend -= 1
                    free[e] = tcur
                    for sc in pick["succ"]:
                        sc["nrem"] -= 1
                        lat = 0.0 if (sc["eng"] == "tensor" and e == "tensor") else LAT
                        if sc["rt"] < tcur + lat:
                            sc["rt"] = tcur + lat
                    progressed = True
                if not progressed:
                    raise RuntimeError("scheduler deadlock in segment %d" % sg)
        for e in self.ENG:
            assert len(order[e]) == len(self.ops[e]), (e, len(order[e]), len(self.ops[e]))
            for i, o in enumerate(order[e]):
                o.pos = i
        self.order = order

    def emit(self, stack):
        nc = self.nc
        self.schedule()
        order = self.order
        for o in self.all:
            for d in o.deps:
                if d.eng == "tensor" and o.eng == "tensor":
                    continue
                d.inc = True
        barlast = {}
        for e in self.ENG:
            last = None
            for o in order[e]:
                if o.fn == "bar":
                    barlast[(e, o.seg)] = last
                elif o.dma is None:
                    last = o
        for v in barlast.values():
            if v is not None:
                v.inc = True
        esem = {e: stack.enter_context(nc.semaphore("es_" + e)) for e in self.ENG}
        for k in self.dsems:
            self.dsems[k] = stack.enter_context(nc.semaphore("ds_" + k[1]))
        cnt = {}
        for e in self.ENG:
            c_ = 0
            for o in order[e]:
                if o.inc and o.dma is None:
                    c_ += 1
                cnt[id(o)] = c_
        dma_at_bar = {}
        run_tot = {}
        segs = {}
        for o in self.all:
            if o.dma is not None:
                segs.setdefault(o.seg, {})
        tot = {}
        for sg in range(self.seg + 1):
            for o in self.all:
                pass
        cum = {}
        per_seg_tot = []
        cur = {}
        last_seg = 0
        for o in self.all:
            while last_seg < o.seg:
                per_seg_tot.append(dict(cur))
                last_seg += 1
            if o.dma is not None:
                cur[o.dma[2]] = cur.get(o.dma[2], 0) + 16
        while len(per_seg_tot) <= self.seg:
            per_seg_tot.append(dict(cur))
        prog = self

        def run(ename, eng):
            waited = {}

            def wait(key, sem, val):
                if val <= 0 or waited.get(key, 0) >= val:
                    return
                waited[key] = val
                eng.wait_ge(sem, val)

            for o in order[ename]:
                if o.fn == "bar":
                    for e2 in prog.ENG:
                        lo = barlast.get((e2, o.seg))
                        if lo is not None:
                            wait(e2, esem[e2], cnt[id(lo)])
                    for k, v in per_seg_tot[o.seg].items():
                        wait(k, prog.dsems[k], v)
                    continue
                need = {}
                for d in o.deps:
                    if d.eng == "tensor" and ename == "tensor":
                        continue
                    v = cnt[id(d)]
                    if need.get(d.eng, 0) < v:
                        need[d.eng] = v
                for k, v in need.items():
                    wait(k, esem[k], v)
                for k, v in o.ddeps.items():
                    wait(k, prog.dsems[k], v)
                if o.dma is not None:
                    out, in_, dk = o.dma
                    eng.dma_start(out=out, in_=in_).then_inc(prog.dsems[dk], 16)
                    continue
                ins = o.fn(eng)
                if o.inc:
                    ins.then_inc(esem[ename], 1)

        stack.enter_context(nc.allow_non_contiguous_dma("tiny strided parameter loads"))
        block = stack.enter_context(nc.Block())

        @block.sync
        def _(e):
            run("sync", e)

        @block.scalar
        def _(e):
            run("scalar", e)

        @block.vector
        def _(e):
            run("vector", e)

        @block.gpsimd
        def _(e):
            run("gpsimd", e)

        @block.tensor
        def _(e):
            run("tensor", e)


def bcast(ap, shape, axis):
    return ap.unsqueeze(axis).to_broadcast(list(shape))


class K:
    def __init__(self, layers=(0, 1), phases="ABC", dbg=()):
        self.layers = layers
        self.phases = phases
        self.dbg = dbg


def build(layers=(0, 1), phases="ABC", dbg=(), sub="GML"):
    from contextlib import ExitStack
    nc = bass.Bass("TRN2", target_bir_lowering=False)
    P = Prog(nc)
    stack = ExitStack()

    def din(name, shape, dt=F32):
        return nc.dram_tensor(name, list(shape), dt, kind="ExternalInput").ap()

    x_d = din("x", [S, D])
    mem_d = din("mem", [256, D])
    pos_d = din("positions", [1, S], I32)
    w = {}
    for name, shape in [
        ("w_in", [2, D, IN_W]), ("w_out", [2, D, D]), ("gla_w_a2", [2, 16, 256]), ("gla_b_a", [2, 256]),
        ("gla_norm_g", [2, 256]), ("ml_conv_w", [2, 4, 512]), ("ml_b_i", [2, 4]), ("ml_b_f", [2, 4]),
        ("ml_norm_g", [2, 256]), ("mla_q_norm_g", [2, 256]), ("mla_w_uq", [2, 256, 768]),
        ("mla_kv_norm_g", [2, 128]), ("mla_w_ukv", [2, 128, 1024]), ("xa_w_q", [2, D, D]),
        ("xa_w_kv", [2, D, 2 * D]), ("xa_w_o", [2, D, D]), ("moe_w_group", [2, D, 4]), ("moe_b_group", [2, 4]),
        ("moe_w_router", [2, D, 32]), ("moe_b_router", [2, 32]), ("moe_w_gate", [2, 32, D, 256]),
        ("moe_w_up", [2, 32, D, 256]), ("moe_w_down", [2, 32, 256, D]),
        ("ln1_g", [2, D]), ("ln1_b", [2, D]), ("ln2_g", [2, D]), ("ln2_b", [2, D]), ("ln3_g", [2, D]), ("ln3_b", [2, D]),
    ]:
        w[name] = din(name, shape)
    cmat_d = din("cmat", [128, 5, 128])
    sel_d = din("sel", [32, 32, 128])
    ropeinv_d = din("ropeinv", [96, 1])
    out_d = nc.dram_tensor("out", [S, D], F32, kind="ExternalOutput").ap()
    gscr_d = nc.dram_tensor("gate_scratch", [2, 32, S], BF16, kind="Internal").ap()
    dbg_d = {}
    for name, shape in dbg:
        dbg_d[name] = nc.dram_tensor(name, list(shape), F32, kind="ExternalOutput").ap()

    def sb(name, shape, dt=F32):
        return P.tile(name, stack.enter_context(nc.sbuf_tensor(name, list(shape), dt)))

    x_tm = sb("x_tm", [128, NT, D])
    xT = sb("xT", [128, 8, S], BF16)
    cmat = sb("cmat_sb", [128, 5, 128])
    cmat_bf = sb("cmat_bf", [128, 5, 128], BF16)
    lnp = sb("lnp", [128, 2, D])
    ps = [P.tile("ps%d" % i, stack.enter_context(nc.psum_tensor("ps%d" % i, [128, 512], F32))) for i in range(8)]
    for p_ in ps:
        p_.psum = True
    IDENT, TRII, TRIS, MMLA, ONES = range(5)

    def fsz(ap):
        try:
            return int(ap.free_size())
        except Exception:
            return 256

    def mm(out, lhsT, rhs, start, stop, reads, writes):
        n = fsz(rhs)
        cst = max(64, n) / 2400.0 * (4.0 if rhs.dtype == F32 else 1.0) + 0.012
        P.op("tensor", lambda e: e.matmul(out, lhsT, rhs, start=start, stop=stop), reads, writes, cost=cst,
             start=start, stop=stop)

    def tr(out, in_, ident, reads, writes):
        P.op("tensor", lambda e: e.transpose(out, in_, ident), reads, writes, cost=0.08)

    def act(out, in_, func, reads, writes, bias=None, scale=None, accum_out=None):
        kw = {}
        if bias is not None:
            kw["bias"] = bias
        if scale is not None:
            kw["scale"] = scale
        if accum_out is not None:
            kw["accum_out"] = accum_out
        tb = {AF.Silu: "s", AF.Sigmoid: "s", AF.Sqrt: "q", AF.Sin: "n", AF.Copy: None, AF.Identity: None}.get(func, "e")
        P.op("scalar", lambda e: e.activation(out, in_, func, **kw), reads, writes, cost=0.25 + fsz(out) * 0.00085, tbl=tb)

    def vcost(out, f=1.0):
        return 0.12 + fsz(out) * 0.00105 * f

    def tt(out, a, b, op, reads, writes, eng="vector"):
        P.op(eng, lambda e: e.tensor_tensor(out, a, b, op), reads, writes, cost=vcost(out))

    def ts(out, a, s1, op0, reads, writes, s2=None, op1=None, eng="vector"):
        if op1 is None:
            P.op(eng, lambda e: e.tensor_scalar(out, a, s1, None, op0), reads, writes, cost=vcost(out, 0.6))
        else:
            P.op(eng, lambda e: e.tensor_scalar(out, a, s1, s2, op0, op1), reads, writes, cost=vcost(out, 0.6))

    def stt(out, a, s, b, op0, op1, reads, writes):
        P.op("vector", lambda e: e.scalar_tensor_tensor(out, a, s, b, op0, op1), reads, writes, cost=vcost(out))

    def cp(out, in_, reads, writes, eng="vector"):
        if eng == "scalar":
            P.op("scalar", lambda e: e.copy(out, in_), reads, writes, cost=0.25 + fsz(out) * 0.00085)
        else:
            P.op(eng, lambda e: e.tensor_copy(out, in_), reads, writes, cost=vcost(out, 0.6))

    def red(out, in_, op, reads, writes, axis=AX.X):
        P.op("vector", lambda e: e.tensor_reduce(out, in_, axis, op), reads, writes, cost=vcost(in_))

    def load_cast(dst_tile, dst_ap, src_ap, sub=None):
        P.dma("gpsimd", dst_ap, src_ap, writes=[(dst_tile, sub)], semtile=dst_tile)

    def load(dst_tile, dst_ap, src_ap, sub=None, eng="sync"):
        P.dma(eng, dst_ap, src_ap, writes=[(dst_tile, sub)], semtile=dst_tile)

    load(cmat, cmat[:], cmat_d)
    cp(cmat_bf[:], cmat[:], [cmat], [cmat_bf])
    for t in range(NT):
        load(x_tm, x_tm[:, t, :], x_d[t * 128:(t + 1) * 128, :], sub=t, eng="sync")

    memT = sb("memT", [128, 8, 256], BF16)
    if "B" in phases:
        from contextlib import ExitStack as _ES
        pre = _ES()
        mem_f = P.tile("mem_f", pre.enter_context(nc.sbuf_tensor("mem_f", [128, 2, D], F32)))
        mem_b = P.tile("mem_b", pre.enter_context(nc.sbuf_tensor("mem_b", [128, 2, D], BF16)))
        load(mem_f, mem_f[:], mem_d.rearrange("(t p) d -> p t d", p=128))
        cp(mem_b[:], mem_f[:], [mem_f], [mem_b])
        pbm = ps[7].h.bitcast(BF16)
        for mt in range(2):
            for c8 in range(8):
                tr(pbm[:, c8 * 128:(c8 + 1) * 128], mem_b[:, mt, c8 * 128:(c8 + 1) * 128], cmat_bf[:, 0, :],
                   [mem_b, cmat_bf], [(ps[7], c8)])
            cp(memT[:, :, mt * 128:(mt + 1) * 128], pbm[:, :].rearrange("p (c n) -> p c n", c=8), [ps[7]], [(memT, mt)])
        P.soft_barrier()
        pre.close()

    cs = None
    if "A" in phases and "L" in sub:
        import math
        from contextlib import ExitStack as _ES2
        cs = sb("rope_cs", [96, 2, S], BF16)
        pre2 = _ES2()

        def tmp(name, dt=F32):
            return P.tile(name, pre2.enter_context(nc.sbuf_tensor(name, [96, S], dt)))
        posi, ang, rr, kf, ki, mk = tmp("rp_posi", I32), tmp("rp_ang"), tmp("rp_r"), tmp("rp_kf"), tmp("rp_ki", I32), tmp("rp_m")
        rinv = P.tile("rp_inv", pre2.enter_context(nc.sbuf_tensor("rp_inv", [96, 1], F32)))
        R_ = slice(64, 96)
        load(rinv, rinv[:], ropeinv_d)
        P.dma("sync", posi[R_, :].unsqueeze(1), pos_d[0:1, :].partition_broadcast(32), writes=[posi], semtile=posi)
        cp(ang[R_, :], posi[R_, :], [posi], [ang])
        ts(ang[R_, :], ang[R_, :], rinv[R_, 0:1], ALU.mult, [ang, rinv], [ang])
        TWO_PI = 2.0 * math.pi
        C1 = 6.28125
        C2 = TWO_PI - C1
        for which, shift in ((1, 0.0), (0, math.pi / 2)):
            ts(rr[R_, :], ang[R_, :], shift, ALU.add, [ang], [rr])
            ts(kf[R_, :], rr[R_, :], 1.0 / TWO_PI, ALU.mult, [rr], [kf])
            cp(ki[R_, :], kf[R_, :], [kf], [ki])
            cp(kf[R_, :], ki[R_, :], [ki], [kf])
            stt(rr[R_, :], kf[R_, :], -C1, rr[R_, :], ALU.mult, ALU.add, [kf, rr], [rr])
            stt(rr[R_, :], kf[R_, :], -C2, rr[R_, :], ALU.mult, ALU.add, [kf, rr], [rr])
            ts(mk[R_, :], rr[R_, :], math.pi, ALU.is_gt, [rr], [mk])
            stt(rr[R_, :], mk[R_, :], -TWO_PI, rr[R_, :], ALU.mult, ALU.add, [mk, rr], [rr])
            ts(mk[R_, :], rr[R_, :], -math.pi, ALU.is_lt, [rr], [mk])
            stt(rr[R_, :], mk[R_, :], TWO_PI, rr[R_, :], ALU.mult, ALU.add, [mk, rr], [rr])
            ts(rr[R_, :], rr[R_, :], 3.141592, ALU.min, [rr], [rr], s2=-3.141592, op1=ALU.max)
            act(cs[R_, which, :], rr[R_, :], AF.Sin, [rr], [(cs, which)])
        P.soft_barrier()
        pre2.close()

    lnw = sb("ln_work", [128, 16])
    xbf = sb("ln_xbf", [128, 2, D], BF16)
    ps_bf = [ps[i].h.bitcast(BF16) for i in range(8)]

    def load_ln(gname, bname, l):
        load(lnp, lnp[:, 0, :].unsqueeze(1), w[gname][l:l + 1, :].partition_broadcast(128), sub=0)
        load(lnp, lnp[:, 1, :].unsqueeze(1), w[bname][l:l + 1, :].partition_broadcast(128), sub=1)

    def layer_norm_tile(t, pbank):
        xt = x_tm[:, t, :]
        st = lnw[:, 0:12].rearrange("p (a b) -> p a b", a=2)
        for hh in range(2):
            P.op("vector", lambda e, hh=hh: e.bn_stats(st[:, hh, :], x_tm[:, t, hh * 512:(hh + 1) * 512]),
                 [(x_tm, t)], [(lnw, "st%d" % hh)])
        P.op("vector", lambda e: e.bn_aggr(lnw[:, 12:14], lnw[:, 0:12]), [(lnw, "st0"), (lnw, "st1")], [(lnw, "mv")])
        ts(lnw[:, 14:15], lnw[:, 13:14], LN_EPS, ALU.add, [(lnw, "mv")], [(lnw, "sd")])
        act(lnw[:, 14:15], lnw[:, 14:15], AF.Ln, [(lnw, "sd")], [(lnw, "sd")])
        act(lnw[:, 15:16], lnw[:, 14:15], AF.Exp, [(lnw, "sd")], [(lnw, "rs")], scale=-0.5)
        ts(xt, xt, lnw[:, 12:13], ALU.subtract, [(x_tm, t), (lnw, "mv"), (lnw, "rs")], [(x_tm, t)],
           s2=lnw[:, 15:16], op1=ALU.mult)
        tt(xt, xt, lnp[:, 0, :], ALU.mult, [(x_tm, t), (lnp, 0)], [(x_tm, t)])
        tt(xt, xt, lnp[:, 1, :], ALU.add, [(x_tm, t), (lnp, 1)], [(x_tm, t)])
        refresh_xT(t, pbank)

    def refresh_xT(t, pbank):
        xt = x_tm[:, t, :]
        b = t % 2
        cp(xbf[:, b, :], xt, [(x_tm, t)], [(xbf, b)], eng="scalar")
        pb = ps_bf[pbank]
        for c in range(8):
            tr(pb[:, c * 128:(c + 1) * 128], xbf[:, b, c * 128:(c + 1) * 128], cmat_bf[:, IDENT, :],
               [(xbf, b), cmat_bf], [(ps[pbank], c)])
        cp(xT[:, :, t * 128:(t + 1) * 128], pb[:, :].rearrange("p (c n) -> p c n", c=8),
           [ps[pbank]], [(xT, t)], eng="scalar" if t % 2 else "vector")

    def store_out():
        for t in range(NT):
            P.dma("sync", out_d[t * 128:(t + 1) * 128, :], x_tm[:, t, :],
                  reads=[(x_tm, t)], semtile=x_tm)

    ctx = dict(nc=nc, P=P, stack=stack, w=w, x_tm=x_tm, xT=xT, cmat=cmat, cmat_bf=cmat_bf, lnp=lnp, ps=ps,
               ps_bf=ps_bf, sb=sb, mm=mm, tr=tr, act=act, tt=tt, ts=ts, stt=stt, cp=cp, red=red,
               load=load, load_cast=load_cast, load_ln=load_ln, layer_norm_tile=layer_norm_tile,
               sel_d=sel_d, ropeinv_d=ropeinv_d, memT=memT, sub=sub, cs=cs, gscr_d=gscr_d, mem_d=mem_d, pos_d=pos_d, dbg_d=dbg_d)

    first = True
    for l in layers:
        if first:
            for t in range(NT):
                refresh_xT(t, 5 + t % 3)
        if "A" in phases:
            phase_A(ctx, l)
        if "B" in phases:
            phase_B(ctx, l)
        if "C" in phases:
            phase_C(ctx, l)
        first = False
    store_out()
    P.finish("sync")
    P.emit(stack)
    stack.close()
    return nc


def phase_C(c, l):
    from contextlib import ExitStack
    nc, P, w = c["nc"], c["P"], c["w"]
    x_tm, xT, ps, cmat, cmat_bf = c["x_tm"], c["xT"], c["ps"], c["cmat"], c["cmat_bf"]
    mm, tr, act, tt, ts, stt, cp, red = c["mm"], c["tr"], c["act"], c["tt"], c["ts"], c["stt"], c["cp"], c["red"]
    load, load_cast = c["load"], c["load_cast"]
    IDENT = 0
    ph = ExitStack()

    def sb(name, shape, dt=F32):
        return P.tile(name, ph.enter_context(nc.sbuf_tensor("%s_%d" % (name, l), list(shape), dt)))

    c["load_ln"]("ln3_g", "ln3_b", l)
    gateT = sb("c_gateT", [32, S], BF16)
    ph_r = ExitStack()
    _sb_outer = sb

    def sb(name, shape, dt=F32):
        return P.tile(name, ph_r.enter_context(nc.sbuf_tensor("%s_%d" % (name, l), list(shape), dt)))
    wr = sb("c_wr", [128, 8, 36], BF16)
    load_cast(wr, wr[:, :, 0:4], w["moe_w_group"][l].rearrange("(kc p) n -> p kc n", p=128), sub="g")
    load_cast(wr, wr[:, :, 4:36], w["moe_w_router"][l].rearrange("(kc p) n -> p kc n", p=128), sub="r")
    rb = sb("c_rb", [128, 36])
    load(rb, rb[:, 0:4].unsqueeze(1), w["moe_b_group"][l:l + 1, :].partition_broadcast(128), sub="g")
    load(rb, rb[:, 4:36].unsqueeze(1), w["moe_b_router"][l:l + 1, :].partition_broadcast(128), sub="r")
    lg = sb("c_lg", [128, NT, 36])
    for half in range(2):
        pr = ps[half]
        for tl in range(8):
            t = half * 8 + tl
            for kc in range(8):
                mm(pr[:, tl * 36:(tl + 1) * 36], xT[:, kc, t * 128:(t + 1) * 128], wr[:, kc, :], kc == 0, kc == 7,
                   [(xT, t), wr], [(pr, tl)])
        tt(lg[:, half * 8:(half + 1) * 8, :], pr[:, 0:288].rearrange("p (t n) -> p t n", t=8),
           bcast(rb[:, :], [128, 8, 36], 1), ALU.add, [pr, rb], [(lg, half)])
    r1 = sb("c_r1", [128, NT, 64])
    lgg = lg[:, :, 0:4]
    lge = lg[:, :, 4:36].rearrange("p t (g e) -> p t g e", g=4)
    gmax, gsum, ohg, eg = r1[:, :, 0], r1[:, :, 1], r1[:, :, 4:8], r1[:, :, 8:12]
    red(gmax, lgg, ALU.max, [lg], [(r1, "gmax")])
    tt(eg, lgg, bcast(gmax, [128, NT, 4], 2), ALU.subtract, [lg, (r1, "gmax")], [(r1, "eg")])
    tt(ohg, lgg, bcast(gmax, [128, NT, 4], 2), ALU.is_equal, [lg, (r1, "gmax")], [(r1, "ohg")])
    act(eg, eg, AF.Exp, [(r1, "eg")], [(r1, "eg")])
    red(gsum, eg, ALU.add, [(r1, "eg")], [(r1, "gsum")])
    gp = r1[:, :, 2]
    P.op("vector", lambda e: e.reciprocal(gp, gsum), [(r1, "gsum")], [(r1, "gp")])
    tmp = sb("c_tmp", [128, NT, 4, 8])
    tt(tmp[:], lge, bcast(ohg, [128, NT, 4, 8], 3), ALU.mult, [lg, (r1, "ohg")], [tmp])
    esel = r1[:, :, 16:24]
    red(esel, tmp[:].rearrange("p t g e -> p t e g"), ALU.add, [tmp], [(r1, "esel")])
    m1, m2, dd = r1[:, :, 3], r1[:, :, 12], r1[:, :, 13]
    mk1, mk2, e2 = r1[:, :, 24:32], r1[:, :, 32:40], r1[:, :, 40:48]
    red(m1, esel, ALU.max, [(r1, "esel")], [(r1, "m1")])
    tt(mk1, esel, bcast(m1, [128, NT, 8], 2), ALU.is_equal, [(r1, "esel"), (r1, "m1")], [(r1, "mk1")])
    stt(e2, mk1, -1e30, esel, ALU.mult, ALU.add, [(r1, "mk1"), (r1, "esel")], [(r1, "e2")])
    red(m2, e2, ALU.max, [(r1, "e2")], [(r1, "m2")])
    tt(mk2, e2, bcast(m2, [128, NT, 8], 2), ALU.is_equal, [(r1, "e2"), (r1, "m2")], [(r1, "mk2")])
    tt(dd, m2, m1, ALU.subtract, [(r1, "m1"), (r1, "m2")], [(r1, "dd")])
    act(dd, dd, AF.Exp, [(r1, "dd")], [(r1, "dd")])
    w1, w2 = r1[:, :, 14], r1[:, :, 15]
    ts(w1, dd, 1.0, ALU.add, [(r1, "dd")], [(r1, "w1")])
    P.op("vector", lambda e: e.reciprocal(w1, w1), [(r1, "w1")], [(r1, "w1")])
    tt(w1, w1, gp, ALU.mult, [(r1, "w1"), (r1, "gp")], [(r1, "w1")])
    tt(w2, w1, dd, ALU.mult, [(r1, "w1"), (r1, "dd")], [(r1, "w2")])
    comb = r1[:, :, 48:56]
    tt(comb, mk1, bcast(w1, [128, NT, 8], 2), ALU.mult, [(r1, "mk1"), (r1, "w1")], [(r1, "comb")])
    tt(mk2, mk2, bcast(w2, [128, NT, 8], 2), ALU.mult, [(r1, "mk2"), (r1, "w2")], [(r1, "mk2")])
    tt(comb, comb, mk2, ALU.add, [(r1, "comb"), (r1, "mk2")], [(r1, "comb")])
    gate = sb("c_gate", [128, NT, 4, 8])
    tt(gate[:], bcast(ohg, [128, NT, 4, 8], 3), bcast(comb, [128, NT, 4, 8], 2), ALU.mult,
       [(r1, "ohg"), (r1, "comb")], [gate])
    for g in range(4):
        pg = ps[2 + g % 2]
        for tl in range(4):
            t = g * 4 + tl
            tr(pg[0:32, tl * 128:(tl + 1) * 128], gate[:, t, :, :].rearrange("p g e -> p (g e)"), cmat[:, IDENT, :],
               [gate, cmat], [(pg, tl)])
        cp(gateT[:, g * 512:(g + 1) * 512], pg[0:32, :], [pg], [(gateT, g)], eng="scalar")
    if "c_gate" in c["dbg_d"]:
        P.dma("sync", c["dbg_d"]["c_gate"].rearrange("(t p) n -> p t n", p=128),
              gate[:].rearrange("p t g e -> p t (g e)"), reads=[gate], semtile=gate)

    gscr = P.tile("gscr%d" % l, c["gscr_d"])
    P.dma("sync", c["gscr_d"][l], gateT[:, :], reads=[gateT], writes=[gscr], semtile=gscr)
    P.soft_barrier()
    ph_r.close()
    sb = _sb_outer
    NSLOT = 4
    wg = sb("c_wg", [128, NSLOT, 8, 256], BF16)
    wu = sb("c_wu", [128, NSLOT, 8, 256], BF16)
    wd = sb("c_wd", [128, NSLOT, 2, D], BF16)
    wsem = [sb("c_wsem%d" % i, [1, 1]) for i in range(NSLOT)]
    hT = sb("c_hT", [128, 2, 2, S], BF16)
    sg = sb("c_sg", [128, 2, 512], BF16)
    gbc = sb("c_gbc", [128, 2, S], BF16)
    gbsem = [sb("c_gbsem%d" % i, [1, 1]) for i in range(2)]

    def load_gate(e):
        eb = e % 2
        P.dma("sync", gbc[:, eb, :].unsqueeze(1), c["gscr_d"][l, e:e + 1, :].partition_broadcast(128),
              reads=[gscr], writes=[(gbc, eb)], semtile=gbsem[eb])
    load_gate(0)

    def load_expert(e):
        s = e % NSLOT
        P.dma("gpsimd", wg[:, s, :, :], w["moe_w_gate"][l, e].rearrange("(kc p) n -> p kc n", p=128),
              writes=[(wg, s)], semtile=wsem[s])
        P.dma("gpsimd", wu[:, s, :, :], w["moe_w_up"][l, e].rearrange("(kc p) n -> p kc n", p=128),
              writes=[(wu, s)], semtile=wsem[s])
        P.dma("gpsimd", wd[:, s, :, :], w["moe_w_down"][l, e].rearrange("(kc p) n -> p kc n", p=128),
              writes=[(wd, s)], semtile=wsem[s])

    for e in range(2):
        load_expert(e)
    unit = 0
    for blk in range(16):
        for ei in range(2):
            e = blk * 2 + ei
            s = e % NSLOT
            if e + 2 < 32:
                load_expert(e + 2)
            if e + 1 < 32:
                load_gate(e + 1)
            eb = e % 2
            for g in range(4):
                tok = slice(g * 512, (g + 1) * 512)
                for fc in range(2):
                    pgt, put = ps[(unit % 2) * 2], ps[(unit % 2) * 2 + 1]
                    for kc in range(8):
                        mm(pgt[:, :], wg[:, s, kc, fc * 128:(fc + 1) * 128], xT[:, kc, tok], kc == 0, kc == 7,
                           [(wg, s), xT], [pgt])
                    for kc in range(8):
                        mm(put[:, :], wu[:, s, kc, fc * 128:(fc + 1) * 128], xT[:, kc, tok], kc == 0, kc == 7,
                           [(wu, s), xT], [put])
                    u2 = unit % 2
                    act(sg[:, u2, :], pgt[:, :], AF.Silu, [pgt], [(sg, u2)])
                    tt(sg[:, u2, :], put[:, :], sg[:, u2, :], ALU.mult, [put, (sg, u2)], [(sg, u2)])
                    tt(hT[:, ei, fc, tok], sg[:, u2, :], gbc[:, eb, tok], ALU.mult, [(sg, u2), (gbc, eb)],
                       [(hT, (ei, g))])
                    unit += 1
        for t in range(NT):
            g = t // 4
            for hf in range(2):
                po = ps[5 + (t * 2 + hf) % 3]
                k = 0
                for ei in range(2):
                    s = (blk * 2 + ei) % NSLOT
                    for fc in range(2):
                        mm(po[:, :], hT[:, ei, fc, t * 128:(t + 1) * 128], wd[:, s, fc, hf * 512:(hf + 1) * 512],
                           k == 0, k == 3, [(hT, (ei, g)), (wd, s)], [po])
                        k += 1
                xs = x_tm[:, t, hf * 512:(hf + 1) * 512]
                if blk == 0:
                    stt(xs, xs, ALPHA, po[:, :], ALU.mult, ALU.add, [(x_tm, t), po], [(x_tm, t)])
                else:
                    tt(xs, xs, po[:, :], ALU.add, [(x_tm, t), po], [(x_tm, t)])
    for t in range(NT):
        c["layer_norm_tile"](t, 5 + t % 3)
    P.soft_barrier()
    ph.close()


def host_consts():
    cm = np.zeros((128, 5, 128), np.float32)
    i = np.arange(128)
    cm[:, 0, :] = np.eye(128, dtype=np.float32)
    cm[:, 1, :] = (i[:, None] <= i[None, :]).astype(np.float32)
    cm[:, 2, :] = (i[:, None] > i[None, :]).astype(np.float32)
    cm[:, 3, :] = ((i[:, None] // 64) <= (i[None, :] // 64)).astype(np.float32)
    cm[:, 4, :] = 1.0
    sel = np.zeros((32, 32, 128), np.float32)
    for e in range(32):
        sel[e, e, :] = 1.0
    inv = (10000.0 ** (-np.arange(16, dtype=np.float32) / 16)).astype(np.float32)
    ri = np.zeros((96, 1), np.float32)
    ri[64:80, 0] = inv
    ri[80:96, 0] = inv
    return {"cmat": cm, "sel": sel, "ropeinv": ri}


_NC_CACHE = {}


def run_cores(inputs, n_cores=8, layers=(0, 1), phases="ABC", dbg=(), sub="GML"):
    key = (tuple(layers), phases, tuple(dbg), sub)
    if key not in _NC_CACHE:
        _NC_CACHE[key] = build(layers, phases, dbg, sub)
    nc = _NC_CACHE[key]
    consts = host_consts()
    shared = {k: np.ascontiguousarray(v) for k, v in inputs.items() if k not in ("x", "mem", "positions")}
    shared.update(consts)
    in_maps = []
    for b in range(n_cores):
        m = dict(shared)
        m["x"] = np.ascontiguousarray(inputs["x"][b])
        m["mem"] = np.ascontiguousarray(inputs["mem"][b])
        m["positions"] = np.ascontiguousarray(inputs["positions"][b:b + 1]).astype(np.int32)
        in_maps.append(m)
    res = run_bass_kernel_spmd(nc, in_maps, core_ids=list(range(n_cores)))
    return res.results


def kernel(**inputs):
    inputs = {k: np.asarray(v) for k, v in inputs.items()}
    res = run_cores(inputs, 8)
    return np.stack([r["out"] for r in res], axis=0).astype(np.float32)


def phase_B(c, l):
    from contextlib import ExitStack
    nc, P, w = c["nc"], c["P"], c["w"]
    x_tm, xT, ps, cmat_bf, memT = c["x_tm"], c["xT"], c["ps"], c["cmat_bf"], c["memT"]
    mm, act, tt, stt, cp = c["mm"], c["act"], c["tt"], c["stt"], c["cp"]
    load_cast = c["load_cast"]
    ONES = 4
    ph = ExitStack()

    def sb(name, shape, dt=F32):
        return P.tile(name, ph.enter_context(nc.sbuf_tensor("%s_%d" % (name, l), list(shape), dt)))

    c["load_ln"]("ln2_g", "ln2_b", l)
    kT = sb("b_kT", [128, 8, 256], BF16)
    vx = sb("b_v", [128, 2, D], BF16)
    ph2 = ExitStack()
    wkv = P.tile("b_wkv", ph2.enter_context(nc.sbuf_tensor("b_wkv_%d" % l, [128, 8, 2 * D], BF16)))
    for kc in range(8):
        load_cast(wkv, wkv[:, kc, :], w["xa_w_kv"][l, kc * 128:(kc + 1) * 128, :], sub=kc)
    for cc in range(8):
        pk = ps[cc % 2]
        for kc in range(8):
            mm(pk[:, 0:256], wkv[:, kc, cc * 128:(cc + 1) * 128], memT[:, kc, :], kc == 0, kc == 7, [wkv, memT], [pk])
        cp(kT[:, cc, :], pk[:, 0:256], [pk], [(kT, cc)], eng="scalar" if cc % 2 else "vector")
    for mt in range(2):
        for hf in range(2):
            pv = ps[2 + hf]
            for kc in range(8):
                mm(pv[:, :], memT[:, kc, mt * 128:(mt + 1) * 128], wkv[:, kc, D + hf * 512:D + (hf + 1) * 512],
                   kc == 0, kc == 7, [wkv, memT], [pv])
            cp(vx[:, mt, hf * 512:(hf + 1) * 512], pv[:, :], [pv], [(vx, (mt, hf))], eng="scalar" if hf else "vector")
    P.soft_barrier()
    ph2.close()
    wq = sb("b_wq", [128, 8, D], BF16)
    wo = sb("b_wo", [128, 8, D], BF16)
    for kc in range(0, 8, 2):
        load_cast(wq, wq[:, kc:kc + 2, :], w["xa_w_q"][l, kc * 128:(kc + 2) * 128, :].rearrange("(k p) n -> p k n", p=128), sub=kc)
    for kc in range(0, 8, 2):
        load_cast(wo, wo[:, kc:kc + 2, :], w["xa_w_o"][l, kc * 128:(kc + 2) * 128, :].rearrange("(k p) n -> p k n", p=128), sub=kc)
    qT2 = sb("b_qT", [128, 2, 8, 512], BF16)
    xaT2 = sb("b_xaT", [128, 2, 8, 512], BF16)
    PT = sb("b_PT", [128, 2, 512], BF16)
    rden = sb("b_rden", [128, 2, 512])
    scale = 256 ** -0.5
    for g in range(4):
        tok = slice(g * 512, (g + 1) * 512)
        gb_ = g % 2

        class _V:
            def __init__(self, t, i):
                self.t, self.i = t, i

            def __getitem__(self, k):
                return self.t[(k[0], self.i) + tuple(k[1:])]
        qT = _V(qT2, gb_)
        xaT = _V(xaT2, gb_)
        for cc in range(8):
            pq = ps[cc % 2]
            for kc in range(8):
                mm(pq[:, :], wq[:, kc, cc * 128:(cc + 1) * 128], xT[:, kc, tok], kc == 0, kc == 7, [wq, xT], [pq])
            cp(qT[:, cc, :], pq[:, :], [pq], [(qT2, (gb_, cc))], eng="scalar" if cc % 2 else "vector")
        for h in range(4):
            for mt in range(2):
                pst = ps[2 + mt]
                for j in range(2):
                    mm(pst[:, :], kT[:, h * 2 + j, mt * 128:(mt + 1) * 128], qT[:, h * 2 + j, :], j == 0, j == 1,
                       [(kT, h * 2 + j), (qT2, (gb_, h * 2 + j))], [pst])
                act(PT[:, mt, :], pst[:, :], AF.Exp, [pst], [(PT, mt)], scale=scale)
            pden = ps[4]
            for mt in range(2):
                mm(pden[:, :], cmat_bf[:, ONES, :], PT[:, mt, :], mt == 0, mt == 1, [cmat_bf, (PT, mt)], [pden])
            rb = h % 2
            act(rden[:, rb, :], pden[:, :], AF.Ln, [pden], [(rden, rb)])
            act(rden[:, rb, :], rden[:, rb, :], AF.Exp, [(rden, rb)], [(rden, rb)], scale=-1.0)
            for j in range(2):
                po = ps[5 + j]
                for mt in range(2):
                    mm(po[:, :], vx[:, mt, h * 256 + j * 128:h * 256 + (j + 1) * 128], PT[:, mt, :], mt == 0, mt == 1,
                       [vx, (PT, mt)], [po])
                tt(xaT[:, h * 2 + j, :], po[:, :], rden[:, rb, :], ALU.mult, [po, (rden, rb)], [(xaT2, (gb_, h * 2 + j))])
        for tl in range(4):
            t = g * 4 + tl
            for hf in range(2):
                pp = ps[hf]
                for cc in range(8):
                    mm(pp[:, :], xaT[:, cc, tl * 128:(tl + 1) * 128], wo[:, cc, hf * 512:(hf + 1) * 512], cc == 0, cc == 7,
                       [(xaT2, (gb_, cc)), wo], [pp])
                xs = x_tm[:, t, hf * 512:(hf + 1) * 512]
                stt(xs, xs, ALPHA, pp[:, :], ALU.mult, ALU.add, [(x_tm, t), pp], [(x_tm, t)])
            c["layer_norm_tile"](t, 7)
    P.soft_barrier()
    ph.close()


def head_norm_gate(c, sbf, name, src, gs, out_bf, b, keyp):
    P, tt, ts, red, act = c["P"], c["tt"], c["ts"], c["red"], c["act"]
    st = sbf["hn_st"]
    cen = sbf["hn_cen"]
    sq = sbf["hn_sq"]
    s4 = src.rearrange("p (h e) -> p h e", h=4)
    mean = st[:, b, 0:4]
    var = st[:, b, 4:8]
    red(mean, s4, ALU.add, [keyp], [(st, (b, "m"))])
    ts(mean, mean, -1.0 / 64, ALU.mult, [(st, (b, "m"))], [(st, (b, "m"))])
    c4 = cen[:, b, :].rearrange("p (h e) -> p h e", h=4)
    tt(c4, s4, bcast(mean, [128, 4, 64], 2), ALU.add, [keyp, (st, (b, "m"))], [(cen, b)])
    tt(sq[:, b, :], cen[:, b, :], cen[:, b, :], ALU.mult, [(cen, b)], [(sq, b)])
    red(var, sq[:, b, :].rearrange("p (h e) -> p h e", h=4), ALU.add, [(sq, b)], [(st, (b, "v"))])
    ts(var, var, 1.0 / 64, ALU.mult, [(st, (b, "v"))], [(st, (b, "v"))], s2=LN_EPS, op1=ALU.add)
    act(var, var, AF.Ln, [(st, (b, "v"))], [(st, (b, "v"))])
    act(var, var, AF.Exp, [(st, (b, "v"))], [(st, (b, "v"))], scale=-0.5)
    tt(c4, c4, bcast(var, [128, 4, 64], 2), ALU.mult, [(cen, b), (st, (b, "v"))], [(cen, b)])
    tt(out_bf, cen[:, b, :], gs, ALU.mult, [(cen, b), (sbf["gs"], b)], [(sbf["obf"], b)])


def phase_A(c, l):
    from contextlib import ExitStack
    nc, P, w = c["nc"], c["P"], c["w"]
    sub = c.get("sub", "GML")
    c["first_mixer"] = True
    if "G" in sub:
        mixer_gla(c, l)
        c["first_mixer"] = False
    if "M" in sub:
        mixer_mlstm(c, l)
        c["first_mixer"] = False
    if "L" in sub:
        mixer_mla(c, l)
    c["load_ln"]("ln1_g", "ln1_b", l)
    for t in range(NT):
        c["layer_norm_tile"](t, 5 + t % 3)
    P.soft_barrier()


def mixer_gla(c, l):
    from contextlib import ExitStack
    nc, P, w = c["nc"], c["P"], c["w"]
    x_tm, xT, ps, ps_bf, cmat, cmat_bf = c["x_tm"], c["xT"], c["ps"], c["ps_bf"], c["cmat"], c["cmat_bf"]
    mm, tr, act, tt, ts, stt, cp, red = c["mm"], c["tr"], c["act"], c["tt"], c["ts"], c["stt"], c["cp"], c["red"]
    load, load_cast = c["load"], c["load_cast"]
    IDENT, TRII, TRIS = 0, 1, 2
    ph = ExitStack()

    def sb(name, shape, dt=F32):
        return P.tile(name, ph.enter_context(nc.sbuf_tensor("%s_%d" % (name, l), list(shape), dt)))

    win = sb("g_win", [128, 8, 1040], BF16)
    for kc in range(8):
        load_cast(win, win[:, kc, :], w["w_in"][l, kc * 128:(kc + 1) * 128, 0:1040], sub=kc)
    wa2 = sb("g_wa2", [16, 256], BF16)
    load_cast(wa2, wa2[0:16, :], w["gla_w_a2"][l])
    barow = sb("g_barow", [1, 256], BF16)
    load_cast(barow, barow[:], w["gla_b_a"][l:l + 1, :])
    wout = sb("g_wout", [128, 2, D], BF16)
    load_cast(wout, wout[:], w["w_out"][l, 0:256, :].rearrange("(k p) n -> p k n", p=128))
    gng = sb("g_gng", [128, 256])
    load(gng, gng[:].unsqueeze(1), w["gla_norm_g"][l:l + 1, :].partition_broadcast(128))
    NB = 3
    gaT = sb("g_gaT", [16, NB, 128], BF16)
    Lsb = sb("g_L", [128, NB, 256])
    E1 = sb("g_E1", [128, NB, 256])
    E2 = sb("g_E2", [128, NB, 256])
    E3 = sb("g_E3", [128, NB, 256])
    qs = [sb("g_qs0", [128, NB, 256], BF16), sb("g_qs1", [128, NB, 256], BF16)]
    for i in range(2):
        P.op("vector", lambda e, i=i: e.memset(qs[i][:], 0.0), [], [qs[i]])
    ksT = sb("g_ksT", [128, NB, 256], BF16)
    k2 = sb("g_k2", [128, NB, 256], BF16)
    vsb = sb("g_v", [128, NB, 256], BF16)
    gs = sb("g_gs", [128, NB, 256])
    PT = sb("g_PT", [128, 2, 4, 128], BF16)
    osb = sb("g_osb", [128, NB, 256])
    obf = sb("g_obf", [128, NB, 256], BF16)
    ogT = sb("g_ogT", [128, 2, 2, 128], BF16)
    Dend = sb("g_Dend", [128, NT, 2])
    Sst = sb("g_S", [128, 2, 64])
    Sbf = sb("g_Sbf", [128, 2, 2, 64], BF16)
    P.op("vector", lambda e: e.memset(Sst[:], 0.0), [], [Sst])
    P.op("vector", lambda e: e.memset(Sbf[:], 0.0), [], [Sbf])
    sbf = dict(hn_st=sb("g_hn_st", [128, NB, 8]), hn_cen=sb("g_hn_cen", [128, NB, 256]),
               hn_sq=sb("g_hn_sq", [128, NB, 256]), gs=gs, obf=obf)
    import os
    NTL = int(os.environ.get("DBG_TILES", NT))
    A_, B_, C_, D_, E_, F_, G_, H_ = ps
    pbH = ps_bf[7]

    def S0(t):
        b = t % NB
        tok = slice(t * 128, (t + 1) * 128)
        for cc in range(4):
            for kc in range(8):
                mm(A_[:, cc * 128:(cc + 1) * 128], win[:, kc, cc * 128:(cc + 1) * 128], xT[:, kc, tok], kc == 0, kc == 7,
                   [win, xT], [A_])
        for kc in range(8):
            mm(B_[0:16, 256:384], win[:, kc, 1024:1040], xT[:, kc, tok], kc == 0, kc == 7, [win, xT], [B_])
        cp(gaT[0:16, b, :], B_[0:16, 256:384], [B_], [(gaT, b)], eng="scalar")
        mm(B_[:, 0:256], gaT[0:16, b, :], wa2[0:16, :], True, False, [(gaT, b), wa2], [B_])
        mm(B_[:, 0:256], cmat_bf[0:1, 4, :], barow[0:1, :], False, True, [cmat_bf, barow], [B_])
        act(Lsb[:, b, :], B_[:, 0:256], AF.Exp, [B_], [(Lsb, b)], scale=-1.0)
        act(Lsb[:, b, :], Lsb[:, b, :], AF.Ln, [(Lsb, b)], [(Lsb, b)], bias=1.0)
        for kc in range(8):
            mm(D_[:, :], xT[:, kc, tok], win[:, kc, 256:768], kc == 0, kc == 7, [win, xT], [D_])
        for kc in range(8):
            mm(E_[:, 0:256], xT[:, kc, tok], win[:, kc, 768:1024], kc == 0, kc == 7, [win, xT], [E_])
        cp(vsb[:, b, :], D_[:, 256:512], [D_], [(vsb, b)], eng="scalar")
        act(gs[:, b, :], E_[:, 0:256], AF.Exp, [E_], [(gs, b)], scale=-1.0)
        act(gs[:, b, :], gs[:, b, :], AF.Ln, [(gs, b)], [(gs, b)], bias=1.0)
        act(gs[:, b, :], gs[:, b, :], AF.Exp, [(gs, b)], [(gs, b)], scale=-1.0)
        stt(gs[:, b, :], E_[:, 0:256], 1.0, gs[:, b, :], ALU.mult, ALU.mult, [E_, (gs, b)], [(gs, b)])
        tt(gs[:, b, :], gs[:, b, :], gng[:, :], ALU.mult, [(gs, b), gng], [(gs, b)])
        for ch in range(2):
            mm(C_[:, ch * 128:(ch + 1) * 128], Lsb[:, b, ch * 128:(ch + 1) * 128], cmat[:, TRII, :], True, True,
               [(Lsb, b), cmat], [C_])
        mm(C_[:, 256:512], cmat[:, TRIS, :], Lsb[:, b, :], True, True, [(Lsb, b), cmat], [C_])
        act(E1[:, b, :], C_[:, 0:256], AF.Exp, [C_], [(E1, b)], scale=-1.0 / 16)
        act(E2[:, b, :], C_[:, 0:256], AF.Exp, [C_], [(E2, b)], scale=1.0 / 16)
        act(E3[:, b, :], C_[:, 256:512], AF.Exp, [C_], [(E3, b)], scale=-1.0 / 16)
        cp(Dend[:, t, :], E1[:, b, :].rearrange("p (c n) -> p c n", c=2)[:, :, 127], [(E1, b)], [(Dend, t)])
        for hp in range(2):
            rws = slice(hp * 64, (hp + 1) * 64)
            stt(qs[hp][rws, b, :], A_[rws, 0:256], 0.125, E1[rws, b, :], ALU.mult, ALU.mult, [A_, (E1, b)], [(qs[hp], b)])
        tt(ksT[:, b, :], A_[:, 256:512], E2[:, b, :], ALU.mult, [A_, (E2, b)], [(ksT, b)])
        tt(k2[:, b, :], D_[:, 0:256], E3[:, b, :], ALU.mult, [D_, (E3, b)], [(k2, b)])

    def S1(t):
        b = t % NB
        b2 = t % 2
        for h in range(4):
            hp, hc = h % 2, h // 2
            mm(F_[:, h * 128:(h + 1) * 128], ksT[:, b, hc * 128:(hc + 1) * 128],
               qs[hp][:, b, hc * 128:(hc + 1) * 128], True, True, [(ksT, b), (qs[hp], b)], [F_])
        for h in range(4):
            hc = h // 2
            mm(G_[:, h * 64:(h + 1) * 64], k2[:, b, hc * 128:(hc + 1) * 128], vsb[:, b, h * 64:(h + 1) * 64],
               True, True, [(k2, b), (vsb, b)], [G_])
        tt(PT[:, b2, :, :], F_[:, :].rearrange("p (h n) -> p h n", h=4), bcast(cmat[:, TRII, :], [128, 4, 128], 1),
           ALU.mult, [F_, cmat], [(PT, b2)])
        for h in range(4):
            hp, hc = h % 2, h // 2
            mm(G_[:, 256 + h * 64:256 + (h + 1) * 64], PT[:, b2, h, :], vsb[:, b, h * 64:(h + 1) * 64], True, False,
               [(PT, b2), (vsb, b)], [G_])
            mm(G_[:, 256 + h * 64:256 + (h + 1) * 64], qs[hp][:, b, hc * 128:(hc + 1) * 128],
               Sbf[:, b2, hc, :], False, True, [(qs[hp], b), (Sbf, b2)], [G_])
        for h in range(4):
            hp, hc = h % 2, h // 2
            rows = slice(hp * 64, (hp + 1) * 64)
            stt(Sst[rows, hc, :], Sst[rows, hc, :], Dend[rows, t, hc:hc + 1], G_[rows, h * 64:(h + 1) * 64],
                ALU.mult, ALU.add, [(Sst, h), (Dend, t), G_], [(Sst, h)])
        cp(Sbf[:, 1 - b2, :, :], Sst[:, :, :], [Sst], [(Sbf, 1 - b2)], eng="scalar")
        cp(osb[:, b, :], G_[:, 256:512], [G_], [(osb, b)], eng="scalar")

    def S2(t):
        b = t % NB
        b2 = t % 2
        head_norm_gate(c, sbf, "g", osb[:, b, :], gs[:, b, :], obf[:, b, :], b, (osb, b))
        for ch in range(2):
            tr(pbH[:, ch * 128:(ch + 1) * 128], obf[:, b, ch * 128:(ch + 1) * 128], cmat_bf[:, IDENT, :],
               [(obf, b), cmat_bf], [H_])
        cp(ogT[:, b2, :, :], pbH[:, 0:256].rearrange("p (c n) -> p c n", c=2), [H_], [(ogT, b2)], eng="scalar")
        for q4 in range(4):
            pq = H_[:, 128:384]
            for ch in range(2):
                mm(pq, ogT[:, b2, ch, :], wout[:, ch, q4 * 256:(q4 + 1) * 256], ch == 0, ch == 1, [(ogT, b2), wout], [H_])
            xs = x_tm[:, t, q4 * 256:(q4 + 1) * 256]
            if c["first_mixer"]:
                stt(xs, xs, ALPHA, pq, ALU.mult, ALU.add, [(x_tm, t), H_], [(x_tm, t)])
            else:
                tt(xs, xs, pq, ALU.add, [(x_tm, t), H_], [(x_tm, t)])

    for step in range(NTL + 2):
        if 0 <= step - 2 < NTL:
            S2(step - 2)
        if 0 <= step - 1 < NTL:
            S1(step - 1)
        if step < NTL:
            S0(step)
    P.soft_barrier()
    ph.close()


def mixer_mlstm(c, l):
    from contextlib import ExitStack
    import os
    nc, P, w = c["nc"], c["P"], c["w"]
    x_tm, xT, ps, ps_bf, cmat, cmat_bf = c["x_tm"], c["xT"], c["ps"], c["ps_bf"], c["cmat"], c["cmat_bf"]
    mm, tr, act, tt, ts, stt, cp, red = c["mm"], c["tr"], c["act"], c["tt"], c["ts"], c["stt"], c["cp"], c["red"]
    load, load_cast = c["load"], c["load_cast"]
    IDENT, TRII, ONES = 0, 1, 4
    ph = ExitStack()

    def sb(name, shape, dt=F32):
        return P.tile(name, ph.enter_context(nc.sbuf_tensor("%s_%d" % (name, l), list(shape), dt)))

    win = sb("m_win", [128, 8, 1032], BF16)
    for kc in range(8):
        load_cast(win, win[:, kc, :], w["w_in"][l, kc * 128:(kc + 1) * 128, 1040:2072], sub=kc)
    cw = sb("m_cw", [128, 4, 4])
    for j in range(4):
        load(cw, cw[:, :, j], w["ml_conv_w"][l, j, :].rearrange("(c p) -> p c", p=128), sub=j)
    bif = sb("m_bif", [128, 8])
    load(bif, bif[:, 0:4].unsqueeze(1), w["ml_b_i"][l:l + 1, :].partition_broadcast(128), sub=0)
    load(bif, bif[:, 4:8].unsqueeze(1), w["ml_b_f"][l:l + 1, :].partition_broadcast(128), sub=1)
    mng = sb("m_mng", [128, 256])
    load(mng, mng[:].unsqueeze(1), w["ml_norm_g"][l:l + 1, :].partition_broadcast(128))
    wout = sb("m_wout", [128, 2, D], BF16)
    load_cast(wout, wout[:], w["w_out"][l, 256:512, :].rearrange("(k p) n -> p k n", p=128))

    q = [sb("m_q0", [128, 2, S], BF16), sb("m_q1", [128, 2, S], BF16)]
    kT = sb("m_kT", [128, 2, S], BF16)
    ph1 = ExitStack()
    mqk = P.tile("m_mqk", ph1.enter_context(nc.sbuf_tensor("m_mqk_%d" % l, [128, 4, S + 3], BF16)))
    acc = P.tile("m_acc", ph1.enter_context(nc.sbuf_tensor("m_acc_%d" % l, [128, 2, 1024], F32)))
    P.op("vector", lambda e: e.memset(mqk[:, :, 0:3], 0.0), [], [mqk])
    for g in range(4):
        tok = slice(g * 512, (g + 1) * 512)
        for ch in range(4):
            pp = ps[(g * 4 + ch) % 2]
            for kc in range(8):
                mm(pp[:, :], win[:, kc, ch * 128:(ch + 1) * 128], xT[:, kc, tok], kc == 0, kc == 7, [win, xT], [pp])
            cp(mqk[:, ch, 3 + g * 512:3 + (g + 1) * 512], pp[:, :], [pp], [(mqk, ch)], eng="scalar" if ch % 2 else "vector")
    for i in range(2):
        P.op("vector", lambda e, i=i: e.memset(q[i][:], 0.0), [], [q[i]])
    for ch in range(4):
        for half in range(2):
            ai = half
            a = acc[:, ai, :]
            off = half * 1024
            ts(a, mqk[:, ch, off:off + 1024], cw[:, ch, 0:1], ALU.mult, [(mqk, ch), cw], [(acc, ai)])
            for j in range(1, 4):
                stt(a, mqk[:, ch, off + j:off + j + 1024], cw[:, ch, j:j + 1], a, ALU.mult, ALU.add,
                    [(mqk, ch), cw, (acc, ai)], [(acc, ai)])
            tokh = slice(off, off + 1024)
            if ch < 2:
                for hp in range(2):
                    rws = slice(hp * 64, (hp + 1) * 64)
                    act(q[hp][rws, ch, tokh], acc[rws, ai, :], AF.Silu, [(acc, ai)], [(q[hp], (ch, half))])
            else:
                act(kT[:, ch - 2, tokh], a, AF.Silu, [(acc, ai)], [(kT, (ch, half))])
    for hp in range(2):
        ts(q[hp][:], q[hp][:], 0.125, ALU.mult, [q[hp]], [q[hp]])
    P.soft_barrier()
    ph1.close()

    gates = sb("m_gates", [128, NT, 8])
    pg = ps[2]
    for t in range(NT):
        for kc in range(8):
            mm(pg[:, t * 8:(t + 1) * 8], xT[:, kc, t * 128:(t + 1) * 128], win[:, kc, 1024:1032], kc == 0, kc == 7,
               [win, xT], [pg])
    tt(gates[:], pg[:, 0:128].rearrange("p (t n) -> p t n", t=NT), bcast(bif[:, :], [128, NT, 8], 1), ALU.add,
       [pg, bif], [gates])
    Lf = sb("m_Lf", [128, NT, 4])
    act(Lf[:], gates[:, :, 4:8], AF.Exp, [gates], [Lf], scale=-1.0)
    ts(Lf[:], Lf[:], 1.0, ALU.add, [Lf], [Lf])
    act(Lf[:], Lf[:], AF.Ln, [Lf], [Lf])
    Lf2 = Lf[:].rearrange("p t n -> p (t n)")
    p3 = ps[3]
    mm(p3[:, 0:64], cmat[:, TRII, :], Lf2, True, True, [cmat, Lf], [p3])
    asb = sb("m_a", [128, NT, 4])
    tt(asb[:], p3[:, 0:64].rearrange("p (t n) -> p t n", t=NT), gates[:, :, 0:4], ALU.add, [p3, gates], [asb])
    cumL = sb("m_cumL", [128, 64])
    cp(cumL[:], p3[:, 0:64], [p3], [cumL])
    a2 = asb[:].rearrange("p t n -> p (t n)")
    p4 = ps[4]
    tr(p4[0:64, 0:128], a2, cmat[:, IDENT, :], [asb, cmat], [p4])
    Acol = sb("m_Acol", [64, 1])
    red(Acol[:, 0:1], p4[0:64, 0:128], ALU.max, [p4], [Acol])
    rows = sb("m_rows", [1, 5, 64])
    p5 = ps[5]
    mm(p5[0:1, 0:64], Acol[0:64, 0:1], cmat[0:64, IDENT, 0:64], True, True, [Acol, cmat], [p5])
    mm(p5[0:1, 64:128], cmat[:, ONES, 0:1], Lf2, True, True, [cmat, Lf], [p5])
    cp(rows[0:1, 0:2, :], p5[0:1, 0:128].rearrange("p (a n) -> p a n", a=2), [p5], [rows])
    P.op("vector", lambda e: e.memset(rows[0:1, 2, 0:4], 0.0), [rows], [rows])
    for cc in range(NT):
        sl = slice(cc * 4, cc * 4 + 4)
        tt(rows[0:1, 3, sl], rows[0:1, 2, sl], rows[0:1, 0, sl], ALU.max, [rows], [rows])
        if cc < NT - 1:
            tt(rows[0:1, 2, (cc + 1) * 4:(cc + 1) * 4 + 4], rows[0:1, 3, sl], rows[0:1, 1, sl], ALU.subtract, [rows], [rows])
    tt(rows[0:1, 4, :], rows[0:1, 2, :], rows[0:1, 3, :], ALU.subtract, [rows], [rows])
    act(rows[0:1, 4, :], rows[0:1, 4, :], AF.Exp, [rows], [rows])
    p6 = ps[6]
    mm(p6[:, 0:128], cmat[0:1, ONES, :], rows[0:1, 3:5, :].rearrange("p a n -> p (a n)"), True, True, [cmat, rows], [p6])
    bcs = sb("m_bcs", [128, 128])
    cp(bcs[:], p6[:, 0:128], [p6], [bcs])
    wtok = sb("m_wtok", [128, 64])
    tt(wtok[:], a2, bcs[:, 0:64], ALU.subtract, [asb, bcs], [wtok])
    act(wtok[:], wtok[:], AF.Exp, [wtok], [wtok])
    clamp = sb("m_clamp", [128, 64])
    tt(clamp[:], cumL[:], bcs[:, 0:64], ALU.subtract, [cumL, bcs], [clamp])
    act(clamp[:], clamp[:], AF.Exp, [clamp], [clamp])

    vext = sb("m_vext", [128, 2, 4, 65], BF16)
    P.op("vector", lambda e: e.memset(vext[:], 1.0), [], [vext])
    vw = sb("m_vw", [128, 2, 4, 65], BF16)
    gso = sb("m_gso", [128, 2, 256])
    ktm = sb("m_ktm", [128, 2, 256], BF16)
    WM = sb("m_WM", [128, 2, 4, 128])
    PT = sb("m_PT", [128, 2, 4, 128], BF16)
    Cn = sb("m_Cn", [128, 2, 65])
    P.op("vector", lambda e: e.memset(Cn[:], 0.0), [], [Cn])
    Cd = sb("m_Cd", [128, 2, 65])
    Cdbf = sb("m_Cdbf", [128, 2, 2, 65], BF16)
    nd = sb("m_nd", [128, 2, 4, 65])
    hsb = sb("m_h", [128, 2, 256])
    small = sb("m_small", [128, 2, 8])
    obf = sb("m_obf", [128, 2, 256], BF16)
    ohT = sb("m_ohT", [128, 2, 2, 128], BF16)
    sbf = dict(hn_st=sb("m_hn_st", [128, 2, 8]), hn_cen=sb("m_hn_cen", [128, 2, 256]),
               hn_sq=sb("m_hn_sq", [128, 2, 256]), gs=gso, obf=obf)
    PA, PB, PC, PD, PE_, PO0, PO1, PX = ps
    for t in range(int(os.environ.get("DBG_TILES", NT))):
        b = t % 2
        tok = slice(t * 128, (t + 1) * 128)
        g4 = slice(t * 4, t * 4 + 4)
        for kc in range(8):
            mm(PA[:, :], xT[:, kc, tok], win[:, kc, 512:1024], kc == 0, kc == 7, [win, xT], [PA])
        v4 = PA[:, 0:256].rearrange("p (h e) -> p h e", h=4)
        cp(vext[:, b, :, 0:64], v4, [PA], [(vext, b)], eng="scalar")
        tt(vw[:, b, :, 0:64], v4, bcast(wtok[:, g4], [128, 4, 64], 2), ALU.mult, [PA, wtok], [(vw, b)])
        cp(vw[:, b, :, 64], wtok[:, g4], [wtok], [(vw, b)])
        act(gso[:, b, :], PA[:, 256:512], AF.Exp, [PA], [(gso, b)], scale=-1.0)
        act(gso[:, b, :], gso[:, b, :], AF.Ln, [(gso, b)], [(gso, b)], bias=1.0)
        act(gso[:, b, :], gso[:, b, :], AF.Exp, [(gso, b)], [(gso, b)], scale=-1.0)
        tt(gso[:, b, :], gso[:, b, :], mng[:, :], ALU.mult, [(gso, b), mng], [(gso, b)])
        pb = ps_bf[1]
        for hc in range(2):
            tr(pb[:, hc * 128:(hc + 1) * 128], kT[:, hc, tok], cmat_bf[:, IDENT, :], [kT, cmat_bf], [PB])
        cp(ktm[:, b, :], pb[:, 0:256], [PB], [(ktm, b)], eng="scalar")
        for h in range(4):
            hp, hc = h % 2, h // 2
            mm(PC[:, h * 128:(h + 1) * 128], kT[:, hc, tok], q[hp][:, hc, tok], True, True, [kT, q[hp]], [PC])
        tt(WM[:, b, :, :], bcast(wtok[:, g4], [128, 4, 128], 2), bcast(cmat[:, TRII, :], [128, 4, 128], 1), ALU.mult,
           [wtok, cmat], [(WM, b)])
        tt(PT[:, b, :, :], PC[:, :].rearrange("p (h n) -> p h n", h=4), WM[:, b, :, :], ALU.mult, [PC, (WM, b)], [(PT, b)])
        for h in range(4):
            hc = h // 2
            mm(PD[:, h * 65:(h + 1) * 65], ktm[:, b, hc * 128:(hc + 1) * 128], vw[:, b, h, :], True, True,
               [(ktm, b), (vw, b)], [PD])
        for h in range(4):
            hp, hc = h % 2, h // 2
            rws = slice(hp * 64, (hp + 1) * 64)
            ts(Cd[rws, hc, :], Cn[rws, hc, :], bcs[rws, 64 + t * 4 + h:64 + t * 4 + h + 1], ALU.mult,
               [(Cn, h), bcs], [(Cd, h)])
        cp(Cdbf[:, b, :, :], Cd[:], [Cd], [(Cdbf, b)], eng="scalar")
        for h in range(4):
            hp, hc = h % 2, h // 2
            mm(PE_[:, h * 65:(h + 1) * 65], PT[:, b, h, :], vext[:, b, h, :], True, False, [(PT, b), (vext, b)], [PE_])
            mm(PE_[:, h * 65:(h + 1) * 65], q[hp][:, hc, tok], Cdbf[:, b, hc, :], False, True, [q[hp], (Cdbf, b)], [PE_])
        for h in range(4):
            hp, hc = h % 2, h // 2
            rws = slice(hp * 64, (hp + 1) * 64)
            tt(Cn[rws, hc, :], Cd[rws, hc, :], PD[rws, h * 65:(h + 1) * 65], ALU.add, [(Cd, h), PD], [(Cn, h)])
        cp(nd[:, b, :, :], PE_[:, 0:260].rearrange("p (h e) -> p h e", h=4), [PE_], [(nd, b)], eng="scalar")
        stt(small[:, b, 0:4], nd[:, b, :, 64], -1.0, nd[:, b, :, 64], ALU.mult, ALU.max, [(nd, b)], [(small, b)])
        tt(small[:, b, 0:4], small[:, b, 0:4], clamp[:, g4], ALU.max, [(small, b), clamp], [(small, b)])
        P.op("vector", lambda e, b=b: e.reciprocal(small[:, b, 4:8], small[:, b, 0:4]), [(small, b)], [(small, b)])
        tt(hsb[:, b, :].rearrange("p (h e) -> p h e", h=4), nd[:, b, :, 0:64], bcast(small[:, b, 4:8], [128, 4, 64], 2),
           ALU.mult, [(nd, b), (small, b)], [(hsb, b)])
        head_norm_gate(c, sbf, "m", hsb[:, b, :], gso[:, b, :], obf[:, b, :], b, (hsb, b))
        pbx = ps_bf[7]
        for ch in range(2):
            tr(pbx[:, ch * 128:(ch + 1) * 128], obf[:, b, ch * 128:(ch + 1) * 128], cmat_bf[:, IDENT, :],
               [(obf, b), cmat_bf], [PX])
        cp(ohT[:, b, :, :], pbx[:, 0:256].rearrange("p (c n) -> p c n", c=2), [PX], [(ohT, b)], eng="scalar")
        for hf in range(2):
            po = PO0 if hf == 0 else PO1
            for ch in range(2):
                mm(po[:, :], ohT[:, b, ch, :], wout[:, ch, hf * 512:(hf + 1) * 512], ch == 0, ch == 1, [(ohT, b), wout], [po])
            xs = x_tm[:, t, hf * 512:(hf + 1) * 512]
            if c["first_mixer"]:
                stt(xs, xs, ALPHA, po[:, :], ALU.mult, ALU.add, [(x_tm, t), po], [(x_tm, t)])
            else:
                tt(xs, xs, po[:, :], ALU.add, [(x_tm, t), po], [(x_tm, t)])
    P.soft_barrier()
    ph.close()


def mixer_mla(c, l):
    from contextlib import ExitStack
    import os
    nc, P, w = c["nc"], c["P"], c["w"]
    x_tm, xT, ps, cmat, cmat_bf, cs = c["x_tm"], c["xT"], c["ps"], c["cmat"], c["cmat_bf"], c["cs"]
    mm, act, tt, ts, stt, cp = c["mm"], c["act"], c["tt"], c["ts"], c["stt"], c["cp"]
    load, load_cast = c["load"], c["load_cast"]
    MMLA, ONES = 3, 4
    R_ = slice(64, 96)
    ph = ExitStack()

    def sb(name, shape, dt=F32, st=None):
        return P.tile(name, (st or ph).enter_context(nc.sbuf_tensor("%s_%d" % (name, l), list(shape), dt)))

    cqnT = sb("a_cqnT", [128, 2, S], BF16)
    ckvnT = sb("a_ckvnT", [128, S], BF16)
    krope = sb("a_krope", [96, S], BF16)
    v_all = sb("a_vall", [128, NT, 512], BF16)
    gq = sb("a_gq", [128, 2])
    gkv = sb("a_gkv", [128, 1])
    load(gq, gq[:], w["mla_q_norm_g"][l].rearrange("(rc p) -> p rc", p=128))
    load(gkv, gkv[:], w["mla_kv_norm_g"][l].rearrange("(o p) -> p o", o=1))

    p1 = ExitStack()
    win = sb("a_win", [128, 8, 416], BF16, p1)
    for kc in range(8):
        load_cast(win, win[:, kc, :], w["w_in"][l, kc * 128:(kc + 1) * 128, 2072:2488], sub=kc)
    wkr = sb("a_wkr", [128, 8, 2, 96], BF16, p1)
    P.op("vector", lambda e: e.memset(wkr[:], 0.0), [], [wkr])
    cp(wkr[:, :, 0, 64:96], win[:, :, 384:416], [win], [wkr])
    ts(wkr[:, :, 1, 64:80], win[:, :, 400:416], -1.0, ALU.mult, [win], [wkr])
    cp(wkr[:, :, 1, 80:96], win[:, :, 384:400], [win], [wkr])
    sq = sb("a_sq", [128, 2, 512], BF16, p1)
    rstd = sb("a_rstd", [128, 2, 512], F32, p1)
    tA = sb("a_tA", [96, 512], F32, p1)
    tB = sb("a_tB", [96, 512], F32, p1)
    for g in range(4):
        tok = slice(g * 512, (g + 1) * 512)
        for rc in range(2):
            for kc in range(8):
                mm(ps[rc][:, :], win[:, kc, rc * 128:(rc + 1) * 128], xT[:, kc, tok], kc == 0, kc == 7, [win, xT], [ps[rc]])
        for rc in range(2):
            act(sq[:, rc, :], ps[rc][:, :], AF.Square, [ps[rc]], [(sq, rc)])
        for rc in range(2):
            mm(ps[2][:, :], cmat_bf[:, ONES, :], sq[:, rc, :], rc == 0, rc == 1, [cmat_bf, (sq, rc)], [ps[2]])
        ts(rstd[:, 0, :], ps[2][:, :], 1.0 / 256, ALU.mult, [ps[2]], [(rstd, 0)], s2=LN_EPS, op1=ALU.add)
        act(rstd[:, 0, :], rstd[:, 0, :], AF.Ln, [(rstd, 0)], [(rstd, 0)])
        act(rstd[:, 0, :], rstd[:, 0, :], AF.Exp, [(rstd, 0)], [(rstd, 0)], scale=-0.5)
        for rc in range(2):
            stt(cqnT[:, rc, tok], ps[rc][:, :], gq[:, rc:rc + 1], rstd[:, 0, :], ALU.mult, ALU.mult,
                [ps[rc], gq, (rstd, 0)], [(cqnT, (rc, g))])
        for kc in range(8):
            mm(ps[3][:, :], win[:, kc, 256:384], xT[:, kc, tok], kc == 0, kc == 7, [win, xT], [ps[3]])
        act(sq[:, 0, :], ps[3][:, :], AF.Square, [ps[3]], [(sq, 0)])
        mm(ps[4][:, :], cmat_bf[:, ONES, :], sq[:, 0, :], True, True, [cmat_bf, (sq, 0)], [ps[4]])
        ts(rstd[:, 1, :], ps[4][:, :], 1.0 / 128, ALU.mult, [ps[4]], [(rstd, 1)], s2=LN_EPS, op1=ALU.add)
        act(rstd[:, 1, :], rstd[:, 1, :], AF.Ln, [(rstd, 1)], [(rstd, 1)])
        act(rstd[:, 1, :], rstd[:, 1, :], AF.Exp, [(rstd, 1)], [(rstd, 1)], scale=-0.5)
        stt(ckvnT[:, tok], ps[3][:, :], gkv[:, 0:1], rstd[:, 1, :], ALU.mult, ALU.mult, [ps[3], gkv, (rstd, 1)], [(ckvnT, g)])
        for r2 in range(2):
            for kc in range(8):
                mm(ps[5 + r2][0:96, :], wkr[:, kc, r2, :], xT[:, kc, tok], kc == 0, kc == 7, [wkr, xT], [ps[5 + r2]])
        tt(tA[R_, :], ps[5][R_, :], cs[R_, 0, tok], ALU.mult, [ps[5], cs], [tA])
        tt(tB[R_, :], ps[6][R_, :], cs[R_, 1, tok], ALU.mult, [ps[6], cs], [tB])
        tt(krope[R_, tok], tA[R_, :], tB[R_, :], ALU.add, [tA, tB], [(krope, g)])
    P.soft_barrier()
    p1.close()

    wuk = sb("a_wuk", [128, 8, 64], BF16)
    wuv = sb("a_wuv", [128, 8, 64], BF16)
    ukv = w["mla_w_ukv"][l].rearrange("p (h two d) -> p h two d", h=8, two=2)
    load_cast(wuk, wuk[:], ukv[:, :, 0, :])
    load_cast(wuv, wuv[:], ukv[:, :, 1, :])
    wuq = sb("a_wuq", [128, 2, 768], BF16)
    load_cast(wuq, wuq[:], w["mla_w_uq"][l].rearrange("(rc p) n -> p rc n", p=128))
    wuqr = sb("a_wuqr", [128, 2, 8, 96], BF16)
    P.op("vector", lambda e: e.memset(wuqr[:], 0.0), [], [wuqr])
    wq4 = wuq[:].rearrange("p r (h c) -> p r h c", h=8)
    ts(wuqr[:, :, :, 64:80], wq4[:, :, :, 80:96], -1.0, ALU.mult, [wuq], [wuqr])
    cp(wuqr[:, :, :, 80:96], wq4[:, :, :, 64:80], [wuq], [wuqr])
    wout = sb("a_wout", [128, 4, D], BF16)
    load_cast(wout, wout[:], w["w_out"][l, 512:1024, :].rearrange("(k p) n -> p k n", p=128))
    qTh = sb("a_qTh", [96, 2, 512], BF16)
    PTb = sb("a_PT", [128, 3, 512], BF16)
    mlaT = sb("a_mlaT", [128, 4, 512], BF16)
    rden = sb("a_rden", [128, 2, 512])
    tA2 = sb("a_tA2", [96, 2, 512])
    tB2 = sb("a_tB2", [96, 2, 512])
    KH = [("k", h) for h in range(8)]
    P.merge(xT)
    for g in range(4):
        tok = slice(g * 512, (g + 1) * 512)
        for h in range(8):
            pk = ps[h % 2]
            mm(pk[0:64, :], wuk[:, h, :], ckvnT[:, tok], True, True, [wuk, ckvnT], [pk])
            cp(xT[0:64, h, tok], pk[0:64, :], [pk], [(xT, KH[h])], eng="scalar" if h % 2 else "vector")
        cp(xT[R_, :, tok], bcast(krope[R_, tok], [32, 8, 512], 1), [krope], [(xT, k) for k in KH])
    for t in range(NT):
        pv = ps[2 + t % 2]
        mm(pv[:, :], ckvnT[:, t * 128:(t + 1) * 128], wuv[:].rearrange("p h d -> p (h d)"), True, True, [ckvnT, wuv], [pv])
        cp(v_all[:, t, :], pv[:, :], [pv], [(v_all, t)], eng="scalar" if t % 2 else "vector")
    scale = 96.0 ** -0.5
    NG = int(os.environ.get("DBG_GROUPS", 4))

    def qproj(g, h):
        tok = slice(g * 512, (g + 1) * 512)
        qb = h % 2
        pq, pr = ps[0], ps[1]
        for rc in range(2):
            mm(pq[0:96, :], wuq[:, rc, h * 96:(h + 1) * 96], cqnT[:, rc, tok], rc == 0, rc == 1, [wuq, cqnT], [pq])
        for rc in range(2):
            mm(pr[0:96, :], wuqr[:, rc, h, :], cqnT[:, rc, tok], rc == 0, rc == 1, [wuqr, cqnT], [pr])
        cp(qTh[0:64, qb, :], pq[0:64, :], [pq], [(qTh, qb)], eng="scalar")
        tt(tA2[R_, qb, :], pq[R_, :], cs[R_, 0, tok], ALU.mult, [pq, cs], [(tA2, qb)])
        tt(tB2[R_, qb, :], pr[R_, :], cs[R_, 1, tok], ALU.mult, [pr, cs], [(tB2, qb)])
        tt(qTh[R_, qb, :], tA2[R_, qb, :], tB2[R_, qb, :], ALU.add, [(tA2, qb), (tB2, qb)], [(qTh, qb)])

    def attend(g, h, nxt):
        nkt = 4 * g + 4
        hp, pair, qb = h % 2, h // 2, h % 2
        po, pd = ps[4 + h % 2], ps[6 + h % 2]

        def qk(kt):
            qlo = max(kt - 4 * g, 0) * 128
            N = 512 - qlo
            pst = ps[2 + kt % 2]
            mm(pst[:, 0:N], xT[0:96, h, kt * 128:(kt + 1) * 128], qTh[0:96, qb, qlo:512], True, True,
               [(xT, KH[h]), (qTh, qb)], [pst])

        qk(0)
        for kt in range(nkt):
            qlo = max(kt - 4 * g, 0) * 128
            N = 512 - qlo
            pst = ps[2 + kt % 2]
            pb = kt % 3
            if kt + 1 < nkt:
                qk(kt + 1)
            elif nxt is not None:
                qproj(*nxt)
            act(PTb[:, pb, 0:N], pst[:, 0:N], AF.Exp, [pst], [(PTb, pb)], scale=scale)
            if kt >= 4 * g:
                tt(PTb[:, pb, 0:128], PTb[:, pb, 0:128], cmat_bf[:, MMLA, :], ALU.mult, [(PTb, pb), cmat_bf], [(PTb, pb)])
            mm(po[:, qlo:512], v_all[:, kt, pair * 128:(pair + 1) * 128], PTb[:, pb, 0:N], kt == 0, kt == nkt - 1,
               [(v_all, kt), (PTb, pb)], [po])
            mm(pd[:, qlo:512], cmat_bf[:, ONES, :], PTb[:, pb, 0:N], kt == 0, kt == nkt - 1, [cmat_bf, (PTb, pb)], [pd])
        rws = slice(hp * 64, (hp + 1) * 64)
        act(rden[rws, qb, :], pd[rws, :], AF.Ln, [pd], [(rden, qb)])
        act(rden[rws, qb, :], rden[rws, qb, :], AF.Exp, [(rden, qb)], [(rden, qb)], scale=-1.0)
        tt(mlaT[rws, pair, :], po[rws, :], rden[rws, qb, :], ALU.mult, [po, (rden, qb)], [(mlaT, h)])

    work = [(g, h) for g in range(NG) for h in range(8)]
    qproj(*work[0])
    for i, (g, h) in enumerate(work):
        nxt = work[i + 1] if i + 1 < len(work) else None
        if nxt is not None and nxt[0] != g:
            attend(g, h, None)
        else:
            attend(g, h, nxt)
        if h != 7:
            continue
        if nxt is not None:
            qproj(*nxt)
        for tl in range(4):
            t = g * 4 + tl
            for hf in range(2):
                pp = ps[5 + 2 * hf]
                for pr_ in range(4):
                    mm(pp[:, :], mlaT[:, pr_, tl * 128:(tl + 1) * 128], wout[:, pr_, hf * 512:(hf + 1) * 512], pr_ == 0, pr_ == 3,
                       [mlaT, wout], [pp])
                xs = x_tm[:, t, hf * 512:(hf + 1) * 512]
                if c["first_mixer"]:
                    stt(xs, xs, ALPHA, pp[:, :], ALU.mult, ALU.add, [(x_tm, t), pp], [(x_tm, t)])
                else:
                    tt(xs, xs, pp[:, :], ALU.add, [(x_tm, t), pp], [(x_tm, t)])
    P.merge(xT)
    P.soft_barrier()
    ph.close()
```
